# Optimizing a Trainium2 kernel written in Bass

```python
import math
import jax, jax.numpy as jnp
from jax import lax
import numpy as np

D_MODEL = 1024
BATCH = 8
SEQ = 4096
DEPTH = 1

GRID_W = 64
CTX_LEN = 256
DA_HEADS = 4
DA_QK_DIM = 64
DA_V_DIM = 2 * DA_QK_DIM
DA_WIDTH = DA_HEADS * DA_V_DIM
Q_BLOCK = 128
ROPE_THETA = 10000.0
ROPE_FREQS = DA_QK_DIM // 4
HY_WIDTH = D_MODEL // 2
HY_EMB_DIM = 33
HY_BANDS = (HY_EMB_DIM - 1) // 2
HY_FILTER_WIDTH = 64
HY_DECAY_TARGET = 1e-2
HY_FAST_DECAY = 0.3
HY_SLOW_DECAY = 1.5
N_EXPERTS = 32
TOP_K = 4
D_FF_EXPERT = D_MODEL
SWIGLU_LIMIT = 7.0
SWIGLU_ALPHA = 1.702
EXPERT_BLOCK = 256
EPS = 1e-6
Q_COLS = DA_HEADS * 2 * DA_QK_DIM
K_COLS = DA_HEADS * 2 * DA_QK_DIM
V_COLS = DA_WIDTH
HY_COLS = 3 * HY_WIDTH
GATE_COLS = 2 * D_MODEL
IN_COLS = Q_COLS + K_COLS + V_COLS + HY_COLS + GATE_COLS
IN_SPLITS = (Q_COLS, Q_COLS + K_COLS, Q_COLS + K_COLS + V_COLS, Q_COLS + K_COLS + V_COLS + HY_COLS)

kernel_name = "hybrid_diffattn_hyena_moe_dit_block"


def rms_norm(x, g):
    xf = x.astype(jnp.float32)
    y = xf * lax.rsqrt(jnp.mean(xf * xf, axis=-1, keepdims=True) + EPS)
    return (y * g.astype(jnp.float32)).astype(x.dtype)


def modulate(x, g, shift, scale):
    return rms_norm(x, g) * (1 + scale) + shift


def axial_rope_tables(rows):
    row = jnp.repeat(jnp.arange(rows), GRID_W)
    col = jnp.tile(jnp.arange(GRID_W), rows)
    pos = jnp.stack([row, col], axis=-1).astype(jnp.float32)
    freqs = ROPE_THETA ** (-jnp.arange(ROPE_FREQS, dtype=jnp.float32) / ROPE_FREQS)
    ang = pos[:, :, None] * freqs
    return jnp.cos(ang), jnp.sin(ang)


def rope_2d(x, cos, sin):
    xr = x.reshape(*x.shape[:-1], 2, 2, ROPE_FREQS)
    x1, x2 = xr[..., 0, :], xr[..., 1, :]
    c = cos[None, :, None, None]
    s = sin[None, :, None, None]
    out = jnp.stack([x1 * c - x2 * s, x2 * c + x1 * s], axis=-2)
    return out.reshape(x.shape).astype(x.dtype)


def split_proj(p):
    b, L = p.shape[:2]
    q, k, v, hy, gates = jnp.split(p, IN_SPLITS, axis=-1)
    return (q.reshape(b, L, DA_HEADS, 2, DA_QK_DIM),
            k.reshape(b, L, DA_HEADS, 2, DA_QK_DIM),
            v.reshape(b, L, DA_HEADS, DA_V_DIM),
            hy, gates)


def diff_attention(q, k, v, lam):
    b, sq, h, m, dh = q.shape
    nb = sq // Q_BLOCK
    qb = jnp.moveaxis(q.reshape(b, nb, Q_BLOCK, h, m, dh), 1, 0)
    scale = DA_QK_DIM ** -0.5

    def one_block(qblk):
        s = jnp.einsum("bqhmd,bkhmd->bhmqk", qblk, k).astype(jnp.float32) * scale
        p = jax.nn.softmax(s, axis=-1)
        p = p[:, :, 0] - lam * p[:, :, 1]
        return jnp.einsum("bhqk,bkhe->bqhe", p.astype(v.dtype), v)

    o = lax.map(one_block, qb)
    return jnp.moveaxis(o, 0, 1).reshape(b, sq, h, v.shape[-1])


def diff_out(o, subln_g, lambda_init):
    b, L = o.shape[:2]
    return (rms_norm(o, subln_g) * (1.0 - lambda_init)).reshape(b, L, DA_WIDTH)


def short_conv3(u, w, bias):
    up = jnp.pad(u, ((0, 0), (1, 1), (0, 0)))
    return up[:, :-2] * w[0] + up[:, 1:-1] * w[1] + up[:, 2:] * w[2] + bias


def implicit_filter(L, w1, b1, w2, b2, w3, b3, w4, freq):
    t = jnp.linspace(0.0, 1.0, L, dtype=jnp.float32)[:, None]
    w = 2.0 * math.pi * jnp.arange(L, dtype=jnp.float32)[:, None] / L
    f = jnp.linspace(1e-4, HY_BANDS - 1, HY_BANDS, dtype=jnp.float32)
    z = jnp.concatenate([t, jnp.cos(f * w), -jnp.sin(f * w)], axis=-1)
    freq = freq.astype(jnp.float32)
    hdn = jnp.sin(freq * (z @ w1 + b1))
    hdn = jnp.sin(freq * (hdn @ w2 + b2))
    hdn = jnp.sin(freq * (hdn @ w3 + b3))
    k = (hdn @ w4).astype(jnp.float32).reshape(L, 2, HY_WIDTH)
    deltas = jnp.abs(jnp.linspace(math.log(HY_DECAY_TARGET) / HY_SLOW_DECAY,
                                  math.log(HY_DECAY_TARGET) / HY_FAST_DECAY,
                                  HY_WIDTH, dtype=jnp.float32))
    decay = jnp.exp(-t * deltas)
    return k * decay[:, None, :]


def long_conv_bidir(u, k_fwd, k_bwd, bias):
    L, W = k_fwd.shape
    k_full = jnp.concatenate([k_fwd, jnp.zeros((1, W), jnp.float32), k_bwd[:0:-1]], axis=0)
    kf = jnp.fft.rfft(k_full, n=2 * L, axis=0)
    uf = jnp.fft.rfft(u.astype(jnp.float32), n=2 * L, axis=1)
    y = jnp.fft.irfft(uf * kf[None], n=2 * L, axis=1)[:, :L]
    return (y + u.astype(jnp.float32) * bias.astype(jnp.float32)).astype(u.dtype)


def hyena_branch(hy, conv_w, conv_b, fw1, fb1, fw2, fb2, fw3, fb3, fw4, ffreq, hbias):
    L = hy.shape[1]
    z = short_conv3(hy, conv_w, conv_b)
    x0, x1, v = jnp.split(z, 3, axis=-1)
    k = implicit_filter(L, fw1, fb1, fw2, fb2, fw3, fb3, fw4, ffreq)
    return x0 * long_conv_bidir(x1 * v, k[:, 0], k[:, 1], hbias)


def merge_branches(oa, ob, gates, w_up_a, w_up_b, w_out):
    ga, gb = jnp.split(jax.nn.sigmoid(gates), 2, axis=-1)
    return (ga * (oa @ w_up_a) + gb * (ob @ w_up_b)) @ w_out


def moe_ffn(h, router_w, router_b, w1, b1, w2, b2):
    b, L, d = h.shape
    xt = h.reshape(-1, d)
    T = xt.shape[0]
    logits = (xt @ router_w + router_b).astype(jnp.float32)
    top_v, top_i = lax.top_k(logits, TOP_K)
    wts = jax.nn.softmax(top_v, axis=-1)
    A = T * TOP_K
    e_flat = top_i.reshape(-1)
    tok_flat = jnp.arange(A, dtype=jnp.int32) // TOP_K
    w_flat = wts.reshape(-1)
    order = jnp.argsort(e_flat)
    e_s, tok_s, w_s = e_flat[order], tok_flat[order], w_flat[order]
    counts = jnp.bincount(e_flat, length=N_EXPERTS)
    padded = (counts + EXPERT_BLOCK - 1) // EXPERT_BLOCK * EXPERT_BLOCK
    pend = jnp.cumsum(padded)
    pstart = pend - padded
    start = jnp.cumsum(counts) - counts
    dest = pstart[e_s] + jnp.arange(A, dtype=jnp.int32) - start[e_s]
    n_rows = A + N_EXPERTS * EXPERT_BLOCK
    n_blocks = n_rows // EXPERT_BLOCK
    row_tok = jnp.full((n_rows,), T, jnp.int32).at[dest].set(tok_s)
    row_w = jnp.zeros((n_rows,), jnp.float32).at[dest].set(w_s)
    block_e = jnp.minimum(jnp.searchsorted(pend, jnp.arange(n_blocks) * EXPERT_BLOCK, side="right"),
                          N_EXPERTS - 1)
    x_pad = jnp.concatenate([xt, jnp.zeros((1, d), xt.dtype)], axis=0)
    xb = x_pad[row_tok].reshape(n_blocks, EXPERT_BLOCK, d)

    def expert_block(args):
        xblk, e = args
        gu = xblk @ w1[e] + b1[e]
        gate, up = jnp.split(gu, 2, axis=-1)
        gate = jnp.minimum(gate, SWIGLU_LIMIT)
        up = jnp.clip(up, -SWIGLU_LIMIT, SWIGLU_LIMIT)
        act = (up + 1) * gate * jax.nn.sigmoid(SWIGLU_ALPHA * gate)
        return act @ w2[e] + b2[e]

    yb = lax.map(expert_block, (xb, block_e)).reshape(n_rows, d)
    y = jnp.zeros((T + 1, d), jnp.float32).at[row_tok].add(yb.astype(jnp.float32) * row_w[:, None])
    return y[:T].reshape(b, L, d).astype(h.dtype)


def setup_inputs(seed: int = 0) -> dict:
    key = jax.random.key(seed)
    ks = iter(jax.random.split(key, 40))

    def nrm(shape, std):
        return std * jax.random.normal(next(ks), shape, jnp.float32)

    L, D, F = DEPTH, D_MODEL, D_FF_EXPERT
    return {
        "x": nrm((BATCH, SEQ, D), 1.0),
        "c": nrm((BATCH, D), 1.0),
        "ctx": nrm((BATCH, CTX_LEN, D), 1.0),
        "c_ctx": nrm((D,), 1.0),
        "ada_w": nrm((L, D, 6 * D), 0.5 * D ** -0.5),
        "ada_b": nrm((L, 6 * D), 0.02),
        "norm1_g": 1.0 + nrm((L, D), 0.05),
        "norm2_g": 1.0 + nrm((L, D), 0.05),
        "w_in": nrm((L, D, IN_COLS), D ** -0.5),
        "b_in": nrm((L, IN_COLS), 0.02),
        "q_norm_g": 1.0 + nrm((L, DA_QK_DIM), 0.05),
        "k_norm_g": 1.0 + nrm((L, DA_QK_DIM), 0.05),
        "lambda_q1": nrm((L, DA_QK_DIM), 0.1),
        "lambda_k1": nrm((L, DA_QK_DIM), 0.1),
        "lambda_q2": nrm((L, DA_QK_DIM), 0.1),
        "lambda_k2": nrm((L, DA_QK_DIM), 0.1),
        "subln_g": 1.0 + nrm((L, DA_V_DIM), 0.05),
        "conv_w": nrm((L, 3, HY_COLS), 3 ** -0.5),
        "conv_b": nrm((L, HY_COLS), 0.02),
        "filt_w1": nrm((L, HY_EMB_DIM, HY_FILTER_WIDTH), HY_EMB_DIM ** -0.5),
        "filt_b1": nrm((L, HY_FILTER_WIDTH), 0.1),
        "filt_w2": nrm((L, HY_FILTER_WIDTH, HY_FILTER_WIDTH), HY_FILTER_WIDTH ** -0.5),
        "filt_b2": nrm((L, HY_FILTER_WIDTH), 0.1),
        "filt_w3": nrm((L, HY_FILTER_WIDTH, HY_FILTER_WIDTH), HY_FILTER_WIDTH ** -0.5),
        "filt_b3": nrm((L, HY_FILTER_WIDTH), 0.1),
        "filt_w4": nrm((L, HY_FILTER_WIDTH, 2 * HY_WIDTH), 0.1 * HY_FILTER_WIDTH ** -0.5),
        "filt_freq": 1.0 + nrm((L, HY_FILTER_WIDTH), 0.05),
        "hyena_bias": nrm((L, HY_WIDTH), 0.1),
        "w_up_a": nrm((L, DA_WIDTH, D), DA_WIDTH ** -0.5),
        "w_up_b": nrm((L, HY_WIDTH, D), HY_WIDTH ** -0.5),
        "w_out": nrm((L, D, D), D ** -0.5),
        "router_w": nrm((L, D, N_EXPERTS), D ** -0.5),
        "router_b": nrm((L, N_EXPERTS), 0.01),
        "exp_w1": nrm((L, N_EXPERTS, D, 2 * F), D ** -0.5),
        "exp_b1": nrm((L, N_EXPERTS, 2 * F), 0.02),
        "exp_w2": nrm((L, N_EXPERTS, F, D), F ** -0.5),
        "exp_b2": nrm((L, N_EXPERTS, D), 0.02),
    }


def reference(x, c, ctx, c_ctx, ada_w, ada_b, norm1_g, norm2_g, w_in, b_in, q_norm_g, k_norm_g,
              lambda_q1, lambda_k1, lambda_q2, lambda_k2, subln_g, conv_w, conv_b,
              filt_w1, filt_b1, filt_w2, filt_b2, filt_w3, filt_b3, filt_w4, filt_freq, hyena_bias,
              w_up_a, w_up_b, w_out, router_w, router_b, exp_w1, exp_b1, exp_w2, exp_b2):
    ROWS = x.shape[1] // GRID_W
    cos, sin = axial_rope_tables(ROWS)
    xc = ctx
    for l in range(DEPTH):
        lambda_init = 0.8 - 0.6 * math.exp(-0.3 * l)
        lam = (jnp.exp(jnp.sum(lambda_q1[l].astype(jnp.float32) * lambda_k1[l].astype(jnp.float32)))
               - jnp.exp(jnp.sum(lambda_q2[l].astype(jnp.float32) * lambda_k2[l].astype(jnp.float32)))
               + lambda_init)
        sh1, sc1, g1, sh2, sc2, g2 = [m[:, None, :] for m in
                                      jnp.split(jax.nn.silu(c) @ ada_w[l] + ada_b[l], 6, axis=-1)]
        sh1c, sc1c, g1c, sh2c, sc2c, g2c = jnp.split(jax.nn.silu(c_ctx) @ ada_w[l] + ada_b[l], 6, axis=-1)
        hy_params = (conv_w[l], conv_b[l], filt_w1[l], filt_b1[l], filt_w2[l], filt_b2[l],
                     filt_w3[l], filt_b3[l], filt_w4[l], filt_freq[l], hyena_bias[l])
        merge_params = (w_up_a[l], w_up_b[l], w_out[l])
        moe_params = (router_w[l], router_b[l], exp_w1[l], exp_b1[l], exp_w2[l], exp_b2[l])

        px = modulate(x, norm1_g[l], sh1, sc1) @ w_in[l] + b_in[l]
        pc = modulate(xc, norm1_g[l], sh1c, sc1c) @ w_in[l] + b_in[l]
        qx, kx, vx, hyx, gx = split_proj(px)
        qc, kc, vc, hyc, gc = split_proj(pc)
        qx = rope_2d(rms_norm(qx, q_norm_g[l]), cos, sin)
        kx = rope_2d(rms_norm(kx, k_norm_g[l]), cos, sin)
        qc = rms_norm(qc, q_norm_g[l])
        kc = rms_norm(kc, k_norm_g[l])
        k_all = jnp.concatenate([kc, kx], axis=1)
        v_all = jnp.concatenate([vc, vx], axis=1)
        oa = diff_out(diff_attention(qx, k_all, v_all, lam), subln_g[l], lambda_init)
        ob = hyena_branch(hyx, *hy_params)
        x_new = x + g1 * merge_branches(oa, ob, gx, *merge_params)
        x_new = x_new + g2 * moe_ffn(modulate(x_new, norm2_g[l], sh2, sc2), *moe_params)

        if l < DEPTH - 1:
            oac = diff_out(diff_attention(qc, kc, vc, lam), subln_g[l], lambda_init)
            obc = hyena_branch(hyc, *hy_params)
            xc = xc + g1c * merge_branches(oac, obc, gc, *merge_params)
            xc = xc + g2c * moe_ffn(modulate(xc, norm2_g[l], sh2c, sc2c), *moe_params)
        x = x_new
    return x
```

```python
import contextlib
import math
import numpy as np
import ml_dtypes
import concourse.bass as bass
import concourse.mybir as mybir
from concourse.bass_utils import run_bass_kernel_spmd

F32 = mybir.dt.float32
BF16 = mybir.dt.bfloat16
I32 = mybir.dt.int32
AF = mybir.ActivationFunctionType
ALU = mybir.AluOpType
AX = mybir.AxisListType
D = 1024
EPS = 1e-6
RB = 256
PI = math.pi


class Buf:
    __slots__ = ("w", "r")

    def __init__(self):
        self.w = None
        self.r = {}


ENGS = ("pe", "act", "dve", "pool", "sp")
DMA_RING = {"sp": 8, "pool": 8}


class Sched:
    def __init__(self, nc, same_engine_sync=True):
        self.nc = nc
        self.prog = {e: [] for e in ENGS}
        self.nops = {e: 0 for e in ENGS}
        self.waited = {e: {} for e in ENGS}
        self.same = same_engine_sync
        self.dma_n = {q: 0 for q in DMA_RING}
        self.dma_val = {}
        self.semkeys = list(ENGS)
        for q, n in DMA_RING.items():
            for i in range(n):
                self.semkeys.append(("dma", q, i))
                self.dma_val[("dma", q, i)] = 0

    def _wait(self, eng, semkey, val):
        if self.waited[eng].get(semkey, -1) >= val:
            return
        self.waited[eng][semkey] = val
        if isinstance(semkey, str):
            self.prog[semkey][val][3] = True
        self.prog[eng].append(["w", semkey, val])

    def _deps(self, eng, reads, writes, is_dma):
        deps = {}

        def add(k, v, e):
            if (not is_dma) and e == eng and k == eng:
                if eng == "pe" or not self.same:
                    return
            if deps.get(k, -1) < v:
                deps[k] = v
        for b in reads:
            if b.w is not None:
                add(*b.w)
        for b in writes:
            if b.w is not None:
                add(*b.w)
            for k, (v, e) in b.r.items():
                add(k, v, e)
        for k, v in deps.items():
            self._wait(eng, k, v)

    def _update(self, tok, reads, writes):
        k, v, e = tok
        for b in reads:
            b.r[k] = (v, e)
        for b in writes:
            b.w = tok
            b.r = {}

    def op(self, eng, fn, reads=(), writes=()):
        self._deps(eng, reads, writes, False)
        pos = len(self.prog[eng])
        self.prog[eng].append(["op", fn, eng, False])
        self.nops[eng] += 1
        self._update((eng, pos, eng), reads, writes)

    def dma(self, q, fn, reads=(), writes=()):
        n = self.dma_n[q]
        self.dma_n[q] += 1
        key = ("dma", q, n % DMA_RING[q])
        if self.dma_val[key] > 0:
            self._wait(q, key, self.dma_val[key])
        self._deps(q, reads, writes, True)
        self.dma_val[key] += 16
        tok = (key, self.dma_val[key], q)
        self.prog[q].append(["dma", fn, key, 16])
        self._update(tok, reads, writes)

    def _last_op(self, eng):
        for i in range(len(self.prog[eng]) - 1, -1, -1):
            if self.prog[eng][i][0] == "op":
                return i
        return None

    def barrier(self):
        for e in ENGS:
            for e2 in ENGS:
                if e2 != e:
                    lp = self._last_op(e2)
                    if lp is not None:
                        self._wait(e, e2, lp)
            for k, v in self.dma_val.items():
                if v > 0:
                    self._wait(e, k, v)

    def emit(self, sems):
        nc = self.nc
        value_at = {}
        for e in ENGS:
            c = 0
            va = {}
            for pos, item in enumerate(self.prog[e]):
                if item[0] == "op" and item[3]:
                    c += 1
                    va[pos] = c
            value_at[e] = va
        with nc.Block() as block:
            def run(engname):
                def body(eng):
                    for item in self.prog[engname]:
                        if item[0] == "w":
                            k, v = item[1], item[2]
                            eng.wait_ge(sems[k], value_at[k][v] if isinstance(k, str) else v)
                        elif item[0] == "dma":
                            item[1](eng).then_inc(sems[item[2]], 16)
                        elif item[3]:
                            item[1](eng).then_inc(sems[item[2]], 1)
                        else:
                            item[1](eng)
                return body
            block.tensor(run("pe"))
            block.scalar(run("act"))
            block.vector(run("dve"))
            block.gpsimd(run("pool"))
            block.sync(run("sp"))


class T:
    def __init__(self, t, nb=1):
        self.t = t
        self.b = Buf()
        self.bs = [Buf() for _ in range(nb)] if nb > 1 else [self.b]


def build_program(S, CTXL, NE, debug=False):
    NT = S // 128
    NC = CTXL // 128
    NKT = NT + NC
    TK = S + CTXL
    BLK = min(512, S)
    NB = S // BLK
    TPB = BLK // 128
    NKC = S // 128
    KG = min(8, NKC)
    NKG = NKC // KG
    QS = min(1024, S)
    NQ = S // QS
    NQT = QS // 128
    NQB = QS // BLK
    N2 = 2 * S
    NR = 4 * S + NE * RB
    NBLK = NR // RB
    NZ = NR // 256
    NST = RB // 128

    nc = bass.Bass("TRN2", target_bir_lowering=False)
    S_ = Sched(nc)

    def din(name, shape, dt=F32):
        return nc.dram_tensor(name, list(shape), dt, kind="ExternalInput").ap()

    def dscr(name, shape, dt=BF16):
        return nc.dram_tensor(name, list(shape), dt, kind="Internal").ap()

    x_d = din("x", [S, D]); ctx_d = din("ctx", [CTXL, D]); cc_d = din("cc", [128, 8, 2])
    adaw_d = din("ada_w", [D, 6 * D]); adab_row_d = din("ada_b_row", [1, 6 * D]); adab_fm_d = din("ada_b_fm", [128, 48])
    n1g_d = din("n1g", [128, 8]); n2g_d = din("n2g", [128, 8])
    win_d = din("w_in", [D, 5120]); bin_row_d = din("b_in_row", [1, 5120]); bin_fm_d = din("b_in_fm", [128, 40])
    qg_d = din("qg", [1, 64]); kg_d = din("kg", [1, 64]); lam4_d = din("lam4", [1, 256]); subg_d = din("subg", [128, 1])
    convw_d = din("convw", [128, 12, 3]); convb_d = din("convb", [128, 12])
    fw1_d = din("fw1", [33, 64]); fw2_d = din("fw2", [64, 64]); fw3_d = din("fw3", [64, 64]); fw4_d = din("fw4", [64, 1024])
    fvec_d = din("fvec", [64, 4])
    hb_d = din("hbias", [128, 4])
    wua_d = din("w_up_a", [512, D]); wub_d = din("w_up_b", [512, D]); wout_d = din("w_out", [D, D])
    rw_d = din("router_w", [D, 32]); rb_d = din("router_b", [1, 32])
    ew1_d = din("ew1", [NE, D, 2048]); eb1_d = din("eb1", [NE * 128, 16]); ew2_d = din("ew2", [NE, D, D]); eb2_d = din("eb2", [NE, D])
    ustr_d = din("ustrict", [128, 128], BF16); kp_d = din("kp", [128, 8]); bstart_d = din("bstart", [128, NBLK]); n2grow_d = din("n2g_row", [1, D])
    ident_d = din("ident", [128, 128], BF16)
    ropec_d = din("ropec", [S, 64]); ropes_d = din("ropes", [S, 64])
    zT_d = din("zT", [33, S]); trow_d = din("trow", [1, S]); drow_d = din("drow", [1, 512])
    Cf_d = din("Cf", [NKC, 128, NT, 128], BF16); Sf_d = din("Sf", [NKC, 128, NT, 128], BF16)
    Ci_d = din("Ci", [NB, 128, NKC, BLK], BF16); Si_d = din("nSi", [NB, 128, NKC, BLK], BF16)
    y_d = nc.dram_tensor("y", [S, D], F32, kind="ExternalOutput").ap()

    hTc_d = dscr("hTc_d", [128, 8, CTXL]); hT_d = dscr("hT_d", [128, 8, S])
    oaT_d = dscr("oaT_d", [128, 4, S]); obT_d = dscr("obT_d", [128, 4, S])
    x0c_d = dscr("x0c_d", [128, 4, S]); uT_d = dscr("uT_d", [128, 4, S])
    Y_d = dscr("Y_d", [128, 2, NKC, 512])
    H2tm_d = dscr("H2tm_d", [S, D]); Xs_d = dscr("Xs_d", [NR, D]); Ys_d = dscr("Ys_d", [NR, D], F32)
    W1b_d = dscr("W1b_d", [NE * 128, 8 * 2048]); W2b_d = dscr("W2b_d", [NE * 128, 8 * D])
    dbg = {}
    if debug:
        for n, shp in [("dbg_oaT", [128, 4, S]), ("dbg_obT", [128, 4, S]), ("dbg_hT", [128, 8, S]), ("dbg_G", [128, NT, 32])]:
            dbg[n] = nc.dram_tensor(n, shp, F32, kind="ExternalOutput").ap()
    B_hTc = Buf(); B_hT = [Buf() for _ in range(NB)]
    B_oaT = [Buf() for _ in range(NB)]; B_obT = [Buf() for _ in range(NB)]
    B_x0c = [Buf() for _ in range(4)]; B_uT = [Buf() for _ in range(4)]
    B_Y = [Buf() for _ in range(NKC)]; B_h2tm = [Buf() for _ in range(NT)]
    B_Xs = [Buf() for _ in range(NZ)]; B_Ys = [Buf() for _ in range(NBLK)]
    B_y = [Buf() for _ in range(NT)]

    w_in_v = win_d.rearrange("(k p) n -> p k n", p=128)

    def bl(xs):
        return [x.b if isinstance(x, T) else x for x in xs]

    def mm(out, lhsT, rhs, start, stop, r, w, tp=None):
        if tp is None:
            S_.op("pe", lambda e: e.matmul(out, lhsT, rhs, start=start, stop=stop), bl(r), bl(w))
        else:
            S_.op("pe", lambda e: e.matmul(out, lhsT, rhs, start=start, stop=stop, tile_position=tp), bl(r), bl(w))

    def tr(out, in_, ident, r, w):
        S_.op("pe", lambda e: e.transpose(out, in_, ident), bl(r), bl(w))

    def act(out, in_, func, r, w, **kw):
        S_.op("act", lambda e: e.activation(out=out, in_=in_, func=func, **kw), bl(r), bl(w))

    def tt(eng, out, in0, in1, op, r, w):
        S_.op(eng, lambda e: e.tensor_tensor(out, in0, in1, op), bl(r), bl(w))

    def ts(eng, out, in0, s1, s2, op0, op1, r, w):
        if op1 is None:
            S_.op(eng, lambda e: e.tensor_scalar(out, in0, s1, None, op0), bl(r), bl(w))
        else:
            S_.op(eng, lambda e: e.tensor_scalar(out, in0, s1, s2, op0, op1), bl(r), bl(w))

    def stt(eng, out, in0, sc, in1, op0, op1, r, w):
        S_.op(eng, lambda e: e.scalar_tensor_tensor(out, in0, sc, in1, op0, op1), bl(r), bl(w))

    def cp(eng, out, in_, r, w):
        S_.op(eng, lambda e: e.tensor_copy(out, in_), bl(r), bl(w))

    def recip(out, in_, r, w):
        S_.op("dve", lambda e: e.reciprocal(out, in_), bl(r), bl(w))

    def mset(eng, ap, val, w):
        S_.op(eng, lambda e: e.memset(ap, val), [], bl(w))

    def dma(q, out, in_, r, w):
        S_.dma(q, lambda e: e.dma_start(out=out, in_=in_), bl(r), bl(w))

    def vmax8(out, in_, r, w):
        S_.op("dve", lambda e: e.max(out, in_), bl(r), bl(w))

    def red(out, in_, r, w):
        S_.op("dve", lambda e: e.tensor_reduce(out, in_, AX.X, ALU.add), bl(r), bl(w))

    def gather(out, in_, idx_ap, r, w):
        S_.dma("pool", lambda e: e.indirect_dma_start(out=out, out_offset=None, in_=in_,
                                                      in_offset=bass.IndirectOffsetOnAxis(ap=idx_ap, axis=0), oob_is_err=False), bl(r), bl(w))

    def scatter(out, in_, idx_ap, r, w):
        S_.dma("pool", lambda e: e.indirect_dma_start(out=out, out_offset=bass.IndirectOffsetOnAxis(ap=idx_ap, axis=0), in_=in_,
                                                      in_offset=None, oob_is_err=False), bl(r), bl(w))

    with contextlib.ExitStack() as ges:
        def gsb(name, shape, dt=F32, nb=1):
            return T(ges.enter_context(nc.sbuf_tensor("s_" + name, list(shape), dt)), nb)
        psall = ges.enter_context(nc.psum_tensor("psall", [128, 4096], F32))
        PS = [T(psall[:, i * 512:(i + 1) * 512]) for i in range(8)]

        def psbf(i):
            return PS[i].t[:].bitcast(BF16)

        ident = gsb("ident", [128, 128], BF16); onesb = gsb("onesb", [128, 128], BF16); onesf = gsb("onesf", [128, 128])
        epst = gsb("epst", [128, 1])
        A1 = gsb("A1", [128, 8]); B1 = gsb("B1", [128, 8]); A1c = gsb("A1c", [128, 8]); B1c = gsb("B1c", [128, 8])
        A2 = gsb("A2", [128, 8]); B2 = gsb("B2", [128, 8])
        g1row = gsb("g1row", [128, D]); g2row = gsb("g2row", [128, D])
        neglam = gsb("neglam", [128, 1]); gsub = gsb("gsub", [128, 1])
        G = gsb("G", [128, NT, 32], F32, nb=NT)
        A2row = gsb("A2row", [128, D]); B2row = gsb("B2row", [128, D])
        LG = gsb("LG", [128, NT, 32], F32, nb=NT); MX = gsb("MX", [128, NT, 8], F32, nb=NT); POS = gsb("POS", [128, NT, 32], F32, nb=NT)
        IDX = gsb("IDX", [128, NT, 4], I32, nb=NT); W4 = gsb("W4", [128, NT, 4], F32, nb=NT)
        BIDX = gsb("BIDX", [128, NBLK, 2], I32)
        ustr = gsb("ustr", [128, 128], BF16); cumf = gsb("cumf", [128, 32]); cumb = gsb("cumb", [128, 32], BF16)

        dma("sp", ident.t[:], ident_d, [], [ident]); dma("sp", ustr.t[:], ustr_d, [], [ustr])
        mset("pool", cumf.t[:], 0.0, [cumf]); mset("pool", cumb.t[:], 0.0, [cumb])
        mset("pool", onesb.t[:], 1.0, [onesb]); mset("pool", onesf.t[:], 1.0, [onesf]); mset("pool", epst.t[:], EPS, [epst])

        with contextlib.ExitStack() as es:
            def sb(name, shape, dt=F32, nb=1):
                return T(es.enter_context(nc.sbuf_tensor("s_" + name, list(shape), dt)), nb)
            cc = sb("cc", [128, 8, 2]); scv = sb("scv", [128, 8, 2]); screp = sb("screp", [128, 8, 128])
            aw = [sb("aw%d" % i, [128, 8, 512]) for i in range(2)]
            adab_fm = sb("adab_fm", [128, 48]); adab_row = sb("adab_row", [1, 6 * D])
            modF = sb("modF", [128, 48, 2]); n1g = sb("n1g", [128, 8]); n2g = sb("n2g", [128, 8])
            lam4 = sb("lam4", [128, 256]); lt1 = sb("lt1", [128, 64]); lt2 = sb("lt2", [128, 64])
            ls1 = sb("ls1", [128, 1]); ls2 = sb("ls2", [128, 1]); subg = sb("subg", [128, 1])
            dma("sp", cc.t[:], cc_d, [], [cc]); dma("sp", adab_fm.t[:], adab_fm_d, [], [adab_fm])
            dma("sp", adab_row.t[:], adab_row_d, [], [adab_row])
            dma("sp", n1g.t[:], n1g_d, [], [n1g]); dma("sp", n2g.t[:], n2g_d, [], [n2g])
            dma("sp", lam4.t[:], lam4_d.partition_broadcast(128), [], [lam4]); dma("sp", subg.t[:], subg_d, [], [subg])
            act(scv.t[:], cc.t[:], AF.Silu, [cc], [scv])
            for k in range(8):
                cp("dve", screp.t[:, k, :], scv.t[:, k, 0:1].broadcast_to([128, 128]), [scv], [screp])
            adaw_v = adaw_d.rearrange("(k p) n -> p k n", p=128)
            pmod = PS[0]
            for g in range(12):
                a = aw[g % 2]
                dma("sp", a.t[:], adaw_v[:, :, g * 512:(g + 1) * 512], [], [a])
                for c in range(4):
                    ch = g * 4 + c
                    for k in range(8):
                        mm(pmod.t[:, 2 * ch:2 * ch + 2], a.t[:, k, c * 128:(c + 1) * 128], scv.t[:, k, :], k == 0, k == 7, [a, scv], [pmod])
                if g in (4, 5, 6, 7, 8, 9, 10, 11):
                    pr = PS[1 + (g % 2)]
                    for k in range(8):
                        mm(pr.t[:], screp.t[:, k, :], a.t[:, k, :], k == 0, False, [a, screp], [pr])
                    mm(pr.t[:], onesf.t[0:1, :], adab_row.t[0:1, g * 512:(g + 1) * 512], False, True, [onesf, adab_row], [pr])
                    dst = {2: g1row, 3: B2row, 4: A2row, 5: g2row}[g // 2]
                    half = g % 2
                    cp("dve", dst.t[:, half * 512:(half + 1) * 512], pr.t[:], [pr], [dst])
            tt("dve", modF.t[:], pmod.t[:, 0:96].rearrange("p (c j) -> p c j", j=2),
               adab_fm.t[:].unsqueeze(2).broadcast_to([128, 48, 2]), ALU.add, [pmod, adab_fm], [modF])
            stt("dve", A1.t[:], modF.t[:, 8:16, 0], 1.0, n1g.t[:], ALU.add, ALU.mult, [modF, n1g], [A1])
            stt("dve", A1c.t[:], modF.t[:, 8:16, 1], 1.0, n1g.t[:], ALU.add, ALU.mult, [modF, n1g], [A1c])
            stt("dve", A2.t[:], modF.t[:, 32:40, 0], 1.0, n2g.t[:], ALU.add, ALU.mult, [modF, n2g], [A2])
            cp("dve", B1.t[:], modF.t[:, 0:8, 0], [modF], [B1]); cp("dve", B1c.t[:], modF.t[:, 0:8, 1], [modF], [B1c])
            cp("dve", B2.t[:], modF.t[:, 24:32, 0], [modF], [B2])
            n2grow = sb("n2grow", [128, D])
            dma("sp", n2grow.t[:], n2grow_d.partition_broadcast(128), [], [n2grow])
            stt("dve", A2row.t[:], A2row.t[:], 1.0, n2grow.t[:], ALU.add, ALU.mult, [A2row, n2grow], [A2row])
            tt("dve", lt1.t[:], lam4.t[:, 0:64], lam4.t[:, 64:128], ALU.mult, [lam4], [lt1])
            tt("dve", lt2.t[:], lam4.t[:, 128:192], lam4.t[:, 192:256], ALU.mult, [lam4], [lt2])
            S_.op("dve", lambda e: e.tensor_reduce(ls1.t[:], lt1.t[:], AX.X, ALU.add), [lt1.b], [ls1.b])
            S_.op("dve", lambda e: e.tensor_reduce(ls2.t[:], lt2.t[:], AX.X, ALU.add), [lt2.b], [ls2.b])
            act(ls1.t[:], ls1.t[:], AF.Exp, [ls1], [ls1]); act(ls2.t[:], ls2.t[:], AF.Exp, [ls2], [ls2])
            tt("dve", neglam.t[:], ls2.t[:], ls1.t[:], ALU.subtract, [ls1, ls2], [neglam])
            ts("dve", neglam.t[:], neglam.t[:], -0.2, None, ALU.add, None, [neglam], [neglam])
            ts("dve", gsub.t[:], subg.t[:], 0.8, None, ALU.mult, None, [subg], [gsub])
            S_.barrier()

        def norm_transpose(es, tag):
            def sb(name, shape, dt=F32, nb=1):
                return T(es.enter_context(nc.sbuf_tensor("s_" + tag + name, list(shape), dt)), nb)
            st = dict(junk=sb("junk", [128, D], BF16), ss=[sb("ss%d" % i, [128, 1]) for i in range(2)],
                      xs=[sb("xs%d" % i, [128, D], BF16) for i in range(2)], n=0)
            return st

        def do_norm_transpose(st, xin, A, Bv, psi, dst_fn, dst_bufs, defer=False):
            i = st["n"]; st["n"] += 1
            ss = st["ss"][i % 2]; xs = st["xs"][i % 2]
            mset("pool", ss.t[:], 0.0, [ss])
            act(st["junk"].t[:], xin.t[:], AF.Square, [xin], [st["junk"], ss], accum_out=ss.t[:])
            act(ss.t[:], ss.t[:], AF.Sqrt, [ss, epst], [ss], scale=1.0 / D, bias=epst.t[:])
            recip(ss.t[:], ss.t[:], [ss], [ss])
            ts("dve", xs.t[:], xin.t[:], ss.t[:, 0:1], None, ALU.mult, None, [xin, ss], [xs])
            pb = psbf(psi)
            for k in range(8):
                tr(pb[:, k * 128:(k + 1) * 128], xs.t[:, k * 128:(k + 1) * 128], ident.t[:], [xs, ident], [PS[psi]])
            def evac():
                for k in range(8):
                    act(dst_fn(k), pb[:, k * 128:(k + 1) * 128], AF.Identity, [PS[psi], A, Bv], dst_bufs,
                        scale=A.t[:, k:k + 1], bias=Bv.t[:, k:k + 1])
            if defer:
                return ss, evac
            evac()
            return ss

        with contextlib.ExitStack() as es:
            def sb(name, shape, dt=F32, nb=1):
                return T(es.enter_context(nc.sbuf_tensor("s_" + name, list(shape), dt)), nb)
            st = norm_transpose(es, "p1")
            xin = [sb("p1xin%d" % i, [128, D]) for i in range(3)]
            hblk = [sb("p1hb%d" % i, [128, 8, BLK], BF16) for i in range(2)]
            n = 0
            pend_ev = None
            hb = hblk[0]
            for i in range(NC):
                xi = xin[n % 3]
                dma("sp", xi.t[:], ctx_d[i * 128:(i + 1) * 128, :], [], [xi])
                _, ev_ = do_norm_transpose(st, xi, A1c, B1c, n % 2, lambda k, i=i, hb=hb: hb.t[:, k, i * 128:(i + 1) * 128], [hb], defer=True)
                if pend_ev is not None:
                    pend_ev()
                pend_ev = ev_
                n += 1
            pend_ev(); pend_ev = None
            dma("sp", hTc_d, hblk[0].t[:, :, 0:CTXL], [hblk[0]], [B_hTc])
            for b in range(NB):
                hb = hblk[(b + 1) % 2]
                for s in range(TPB):
                    i = b * TPB + s
                    xi = xin[n % 3]
                    dma("sp", xi.t[:], x_d[i * 128:(i + 1) * 128, :], [], [xi])
                    _, ev_ = do_norm_transpose(st, xi, A1, B1, n % 2, lambda k, s=s, hb=hb: hb.t[:, k, s * 128:(s + 1) * 128], [hb], defer=True)
                    if pend_ev is not None:
                        pend_ev()
                    pend_ev = ev_
                    n += 1
                pend_ev(); pend_ev = None
                dma("sp", hT_d[:, :, b * BLK:(b + 1) * BLK], hb.t[:], [hb], [B_hT[b]])
            S_.barrier()

        with contextlib.ExitStack() as es2:
            def sb2(name, shape, dt=F32, nb=1):
                return T(es2.enter_context(nc.sbuf_tensor("s_" + name, list(shape), dt)), nb)
            QT = sb2("QT", [128, 4, S], BF16, nb=NT); KT = sb2("KT", [128, 4, TK], BF16, nb=NKT); V = sb2("V", [128, NKT, 512], BF16, nb=NKT)
            with contextlib.ExitStack() as es:
                def sb(name, shape, dt=F32, nb=1):
                    return T(es.enter_context(nc.sbuf_tensor("s_" + name, list(shape), dt)), nb)
                wqkv = sb("wqkv", [128, 8, 1536], BF16); brow = sb("brow", [1, 1536], BF16)
                gq = sb("gq", [128, 64]); gk = sb("gk", [128, 64])
                rcs = [sb("rc%d" % i, [128, 64]) for i in range(3)]; rss = [sb("rs%d" % i, [128, 64]) for i in range(3)]
                gtab = [[sb("gtab%d_%d" % (i, j), [128, 64]) for j in range(4)] for i in range(3)]
                hblk = [sb("p2hb%d" % i, [128, 8, BLK], BF16) for i in range(2)]
                sqt = [sb("sqt%d" % i, [128, 512]) for i in range(2)]
                ssq = [sb("ssq%d" % i, [128, 8]) for i in range(2)]
                qn = [sb("qn%d" % i, [128, 512]) for i in range(2)]
                qg2 = [sb("qg2%d" % i, [128, 512]) for i in range(2)]
                ru = [sb("ru%d" % i, [128, 512]) for i in range(2)]
                rw_ = [sb("rw%d" % i, [128, 512]) for i in range(2)]
                qr = [sb("qr%d" % i, [128, 512], BF16) for i in range(4)]
                for c in range(3):
                    dma("pool", wqkv.t[:, :, c * 512:(c + 1) * 512], w_in_v[:, :, c * 512:(c + 1) * 512], [], [wqkv])
                dma("pool", brow.t[:], bin_row_d[0:1, 0:1536], [], [brow])
                dma("sp", gq.t[:], qg_d.partition_broadcast(128), [], [gq]); dma("sp", gk.t[:], kg_d.partition_broadcast(128), [], [gk])
                cnt = {"n": 0}

                def qknorm(ps, gt, xt, dstT, dbuf, dcol, psT, pcol, rc=None, rs_=None):
                    i = cnt["n"]; cnt["n"] += 1
                    sq = sqt[i % 2]; sm = ssq[i % 2]; q1 = qn[i % 2]; q2 = qg2[i % 2]; u_ = ru[i % 2]; w_ = rw_[i % 2]; o_ = qr[i % 4]
                    def part_a():
                        act(sq.t[:], ps.t[:], AF.Square, [ps], [sq])
                        yield
                        S_.op("dve", lambda e: e.tensor_reduce(sm.t[:], sq.t[:].rearrange("p (g d) -> p g d", d=64), AX.X, ALU.add), [sq.b], [sm.b])
                        yield
                        act(sm.t[:], sm.t[:], AF.Sqrt, [sm, epst], [sm], scale=1.0 / 64, bias=epst.t[:])
                        yield
                        recip(sm.t[:], sm.t[:], [sm], [sm])
                        yield
                        tt("dve", q1.t[:].rearrange("p (g d) -> p g d", d=64), ps.t[:].rearrange("p (g d) -> p g d", d=64),
                           sm.t[:].unsqueeze(2).broadcast_to([128, 8, 64]), ALU.mult, [ps, sm], [q1])
                        yield
                        if xt is None:
                            tt("pool", o_.t[:].rearrange("p (g d) -> p g d", d=64), q1.t[:].rearrange("p (g d) -> p g d", d=64),
                               gt.t[:].unsqueeze(1).broadcast_to([128, 8, 64]), ALU.mult, [q1, gt], [o_])
                            yield
                        else:
                            tt("pool", u_.t[:].rearrange("p (g d) -> p g d", d=64), q1.t[:].rearrange("p (g d) -> p g d", d=64),
                               rc.t[:].unsqueeze(1).broadcast_to([128, 8, 64]), ALU.mult, [q1, rc], [u_])
                            yield
                            tt("dve", w_.t[:].rearrange("p (g d) -> p g d", d=64), q1.t[:].rearrange("p (g d) -> p g d", d=64),
                               rs_.t[:].unsqueeze(1).broadcast_to([128, 8, 64]), ALU.mult, [q1, rs_], [w_])
                            yield
                            u4 = u_.t[:].rearrange("p (a h f) -> p a h f", h=2, f=16)
                            w4 = w_.t[:].rearrange("p (a h f) -> p a h f", h=2, f=16)
                            o4 = o_.t[:].rearrange("p (a h f) -> p a h f", h=2, f=16)
                            tt("dve", o4[:, :, 0, :], u4[:, :, 0, :], w4[:, :, 1, :], ALU.add, [u_, w_], [o_])
                            yield
                            tt("dve", o4[:, :, 1, :], u4[:, :, 1, :], w4[:, :, 0, :], ALU.add, [u_, w_], [o_])
                            yield
                    def part_b():
                        pb = psbf(psT)
                        for h in range(4):
                            tr(pb[:, pcol + h * 128: pcol + (h + 1) * 128], o_.t[:, h * 128:(h + 1) * 128], ident.t[:], [o_, ident], [PS[psT]])
                        cp("dve", dstT.t[:, :, dcol:dcol + 128], pb[:, pcol:pcol + 512].rearrange("p (h t) -> p h t", t=128), [PS[psT]], [dbuf])
                    return part_a(), part_b

                tno = 0
                pend_b = []
                blocks = [("c", 0, NC)] + [("x", b, TPB) for b in range(NB)]
                for bi, (kind, b, ntl) in enumerate(blocks):
                    hb = hblk[bi % 2]
                    if kind == "c":
                        dma("sp", hb.t[:, :, 0:CTXL], hTc_d, [B_hTc], [hb])
                    else:
                        dma("sp", hb.t[:], hT_d[:, :, b * BLK:(b + 1) * BLK], [B_hT[b]], [hb])
                    for s in range(ntl):
                        kt = s if kind == "c" else NC + b * TPB + s
                        xt = None if kind == "c" else b * TPB + s
                        st3 = (tno % 2) * 3
                        psT = 6 + (tno % 2)
                        tno += 1
                        lh = lambda k: hb.t[:, k, s * 128:(s + 1) * 128]
                        for c in range(3):
                            if c == 0 and kind == "c":
                                continue
                            pp = PS[st3 + c]
                            for k in range(8):
                                mm(pp.t[:], lh(k), wqkv.t[:, k, c * 512:(c + 1) * 512], k == 0, False, [hb, wqkv], [pp])
                            mm(pp.t[:], onesb.t[0:1, :], brow.t[0:1, c * 512:(c + 1) * 512], False, True, [onesb, brow], [pp])
                        act(V.t[:, kt, :], PS[st3 + 2].t[:], AF.Copy, [PS[st3 + 2]], [V.bs[kt]])
                        rc = rs_ = None
                        if kind == "x":
                            rc = rcs[xt % 3]; rs_ = rss[xt % 3]
                            dma("sp", rc.t[:], ropec_d[xt * 128:(xt + 1) * 128, :], [], [rc])
                            dma("sp", rs_.t[:], ropes_d[xt * 128:(xt + 1) * 128, :], [], [rs_])
                            gt4 = gtab[xt % 3]
                            tt("pool", gt4[0].t[:], rc.t[:], gq.t[:], ALU.mult, [rc, gq], [gt4[0]]); tt("pool", gt4[1].t[:], rs_.t[:], gq.t[:], ALU.mult, [rs_, gq], [gt4[1]])
                            tt("pool", gt4[2].t[:], rc.t[:], gk.t[:], ALU.mult, [rc, gk], [gt4[2]]); tt("pool", gt4[3].t[:], rs_.t[:], gk.t[:], ALU.mult, [rs_, gk], [gt4[3]])
                            pairs = [qknorm(PS[st3 + 0], gq, xt, QT, QT.bs[xt], xt * 128, psT, 0, gt4[0], gt4[1]),
                                     qknorm(PS[st3 + 1], gk, xt, KT, KT.bs[kt], kt * 128, psT, 512, gt4[2], gt4[3])]
                        else:
                            pairs = [qknorm(PS[st3 + 1], gk, xt, KT, KT.bs[kt], kt * 128, psT, 512, None, None)]
                        gens = [p_[0] for p_ in pairs]
                        while gens:
                            gens = [g_ for g_ in gens if next(g_, "done") != "done"]
                        newb = [p_[1] for p_ in pairs]
                        for fb_ in pend_b:
                            fb_()
                        pend_b = newb
                for fb_ in pend_b:
                    fb_()
                S_.barrier()

            with contextlib.ExitStack() as es:
                def sb(name, shape, dt=F32, nb=1):
                    return T(es.enter_context(nc.sbuf_tensor("s_" + name, list(shape), dt)), nb)
                pt2 = [sb("pt2_%d" % i, [128, 2, 512], BF16) for i in range(3)]
                r0 = sb("r0", [128, BLK]); r1 = sb("r1", [128, BLK]); t0 = sb("t0", [128, BLK]); t1 = sb("t1", [128, BLK])
                dd = sb("dd", [128, BLK]); dsq = sb("dsq", [128, BLK]); rsd = sb("rsd", [128, BLK])
                oat = [sb("oat%d" % i, [128, BLK], BF16) for i in range(2)]
                o_acc = [PS[0], PS[1]]; s_acc = [PS[2], PS[3]]
                scp = [[PS[4], PS[5]], [PS[6], PS[7]]]
                zt = sb("zt", [128, 2 * D], BF16)
                mset("pool", zt.t[:], 0.0, [zt])
                Xs_z = Xs_d.rearrange("(c p r) d -> c p (r d)", p=128, r=2)
                for c_ in range(NZ):
                    dma("pool", Xs_z[c_], zt.t[:], [zt], [B_Xs[c_]])
                for e_ in range(NE):
                    w1src = ew1_d[e_].rearrange("(k p) n -> p k n", p=128)
                    w1dst = W1b_d[e_ * 128:(e_ + 1) * 128, :].rearrange("p (k n) -> p k n", k=8)
                    for k0 in (0, 4):
                        dma("pool", w1dst[:, k0:k0 + 4, :], w1src[:, k0:k0 + 4, :], [], [])
                    w2src = ew2_d[e_].rearrange("(k p) n -> p k n", p=128)
                    w2dst = W2b_d[e_ * 128:(e_ + 1) * 128, :].rearrange("p (k n) -> p k n", k=8)
                    dma("pool", w2dst, w2src, [], [])
                its = [(h, qb, kt) for h in range(4) for qb in range(NB) for kt in range(NKT)]

                def scores(n_):
                    h, qb, kt = its[n_]
                    sp_ = scp[n_ % 2]
                    for m in range(2):
                        mm(sp_[m].t[:, 0:BLK], KT.t[m * 64:(m + 1) * 64, h, kt * 128:(kt + 1) * 128],
                           QT.t[m * 64:(m + 1) * 64, h, qb * BLK:(qb + 1) * BLK], True, True,
                           [KT.bs[kt]] + [QT.bs[qb * TPB + j] for j in range(TPB)], [sp_[m]])
                o0s = sb("o0s", [128, BLK]); o1s = sb("o1s", [128, BLK]); ssb = sb("ssb", [128, BLK]); w32 = sb("w32", [128, 128])
                mset("pool", w32.t[:], 1.0 / 32, [w32])
                sbank = PS[2]; fbank = PS[3]

                def finalize_gen(h, qb):
                    recip(ssb.t[0:64, :], ssb.t[0:64, :], [ssb], [ssb])
                    mm(fbank.t[:, 0:BLK], w32.t[0:32, :], ssb.t[0:32, :], True, True, [w32, ssb], [fbank])
                    yield
                    tt("dve", t0.t[:], o0s.t[:], fbank.t[:, 0:BLK], ALU.mult, [o0s, fbank], [t0])
                    mm(fbank.t[:, 0:BLK], w32.t[32:64, :], ssb.t[32:64, :], True, True, [w32, ssb], [fbank])
                    yield
                    tt("dve", t1.t[:], o1s.t[:], fbank.t[:, 0:BLK], ALU.mult, [o1s, fbank], [t1])
                    stt("dve", dd.t[:], t1.t[:], neglam.t[:, 0:1], t0.t[:], ALU.mult, ALU.add, [t0, t1, neglam], [dd])
                    tt("dve", dsq.t[:], dd.t[:], dd.t[:], ALU.mult, [dd], [dsq])
                    yield
                    mm(fbank.t[:, 0:BLK], onesf.t[:], dsq.t[:], True, True, [onesf, dsq], [fbank])
                    yield
                    act(rsd.t[:], fbank.t[:, 0:BLK], AF.Sqrt, [fbank, epst], [rsd], scale=1.0 / 128, bias=epst.t[:])
                    recip(rsd.t[:], rsd.t[:], [rsd], [rsd])
                    oo = oat[(h * NB + qb) % 2]
                    stt("dve", oo.t[:], dd.t[:], gsub.t[:, 0:1], rsd.t[:], ALU.mult, ALU.mult, [dd, gsub, rsd], [oo])
                    dma("sp", oaT_d[:, h, qb * BLK:(qb + 1) * BLK], oo.t[:], [oo], [B_oaT[qb]])

                pending = []
                scores(0)
                for it in range(len(its)):
                    h, qb, kt = its[it]
                    if it + 1 < len(its):
                        scores(it + 1)
                    sp_ = scp[it % 2]
                    p2 = pt2[it % 3]
                    bank0 = 4 + 2 * (it % 2)
                    act(p2.t[:, :, 0:BLK], psall[:, bank0 * 512:(bank0 + 2) * 512].rearrange("p (m q) -> p m q", m=2)[:, :, 0:BLK], AF.Exp,
                        [sp_[0], sp_[1]], [p2], scale=0.125)
                    for m in range(2):
                        mm(o_acc[m].t[:, 0:BLK], V.t[:, kt, h * 128:(h + 1) * 128], p2.t[:, m, 0:BLK], kt == 0, kt == NKT - 1, [V.bs[kt], p2], [o_acc[m]])
                    for m in range(2):
                        mm(sbank.t[32 * m:32 * (m + 1), 0:BLK], onesb.t[:, 32 * m:32 * (m + 1)], p2.t[:, m, 0:BLK], kt == 0, kt == NKT - 1,
                           [onesb, p2], [sbank], tp=(0, 32 * m))
                    if pending and kt >= 1:
                        if next(pending[0], "done") == "done":
                            pending.pop(0)
                    if kt == NKT - 1:
                        S_.op("act", lambda e: e.copy(o0s.t[:], o_acc[0].t[:, 0:BLK]), [o_acc[0].b], [o0s.b])
                        cp("dve", o1s.t[:], o_acc[1].t[:, 0:BLK], [o_acc[1]], [o1s])
                        cp("dve", ssb.t[0:64, :], sbank.t[0:64, 0:BLK], [sbank], [ssb])
                        pending.append(finalize_gen(h, qb))
                for g_ in pending:
                    for _ in g_:
                        pass
                S_.barrier()

        with contextlib.ExitStack() as esh:
            def sbh(name, shape, dt=F32, nb=1):
                return T(esh.enter_context(nc.sbuf_tensor("s_" + name, list(shape), dt)), nb)
            u_tm = sbh("u_tm", [128, NT, 512], BF16, nb=NT)
            with contextlib.ExitStack() as es:
                def sb(name, shape, dt=F32, nb=1):
                    return T(es.enter_context(nc.sbuf_tensor("s_" + name, list(shape), dt)), nb)
                hring = [sb("h0hb%d" % i, [128, 8, BLK], BF16) for i in range(2)]; whY = sb("whY", [128, 8, 1536], BF16)
                bhy = sb("bhy", [128, 40]); cw = sb("cw", [128, 12, 3]); cb = sb("cb", [128, 12])
                ppads = [[sb("ppad%d_%d" % (i, c), [128, S + 2], BF16) for c in range(3)] for i in range(2)]
                zf = sb("zf", [128, S])
                zX = sb("zX", [128, S], BF16); zA = sb("zA", [128, S], BF16); zB = sb("zB", [128, S], BF16); uTb = sb("uTb", [128, S], BF16)
                for c in range(3):
                    dma("pool", whY.t[:, :, c * 512:(c + 1) * 512], w_in_v[:, :, 1536 + c * 512:1536 + (c + 1) * 512], [], [whY])
                dma("sp", bhy.t[:], bin_fm_d, [], [bhy]); dma("sp", cw.t[:], convw_d, [], [cw]); dma("sp", cb.t[:], convb_d, [], [cb])
                for i in range(2):
                    for c in range(3):
                        mset("pool", ppads[i][c].t[:, 0:1], 0.0, [ppads[i][c]]); mset("pool", ppads[i][c].t[:, S + 1:S + 2], 0.0, [ppads[i][c]])
                zn = 0
                for j in range(4):
                    pset = ppads[j % 2]
                    for b in range(NB):
                        hb = hring[b % 2]
                        dma("sp", hb.t[:], hT_d[:, :, b * BLK:(b + 1) * BLK], [B_hT[b]], [hb])
                        for c3 in range(3):
                            ch = c3 * 4 + j
                            pp = PS[zn % 4]; zn += 1
                            for k in range(8):
                                mm(pp.t[:, 0:BLK], whY.t[:, k, ch * 128:(ch + 1) * 128], hb.t[:, k, :], k == 0, k == 7, [whY, hb], [pp])
                            act(pset[c3].t[:, 1 + b * BLK:1 + (b + 1) * BLK], pp.t[:, 0:BLK], AF.Identity, [pp, bhy], [pset[c3]], bias=bhy.t[:, 12 + ch:13 + ch])
                    for c3, out in ((0, zX), (1, zA), (2, zB)):
                        ch = c3 * 4 + j
                        pp_ = pset[c3]
                        ts("dve", zf.t[:], pp_.t[:, 0:S], cw.t[:, ch, 0:1], cb.t[:, ch:ch + 1], ALU.mult, ALU.add, [pp_, cw, cb], [zf])
                        stt("dve", zf.t[:], pp_.t[:, 1:S + 1], cw.t[:, ch, 1:2], zf.t[:], ALU.mult, ALU.add, [pp_, cw, zf], [zf])
                        stt("dve", out.t[:], pp_.t[:, 2:S + 2], cw.t[:, ch, 2:3], zf.t[:], ALU.mult, ALU.add, [pp_, cw, zf], [out])
                    dma("sp", x0c_d[:, j, :], zX.t[:], [zX], [B_x0c[j]])
                    tt("pool", uTb.t[:], zA.t[:], zB.t[:], ALU.mult, [zA, zB], [uTb])
                    dma("sp", uT_d[:, j, :], uTb.t[:], [uTb], [B_uT[j]])
                    for t0_ in range(0, NT, 8):
                        nt_ = min(8, NT - t0_)
                        psi = 4 + ((j * NT + t0_) // 8) % 2
                        pb = psbf(psi)
                        for t_ in range(nt_):
                            tr(pb[:, t_ * 128:(t_ + 1) * 128], uTb.t[:, (t0_ + t_) * 128:(t0_ + t_ + 1) * 128], ident.t[:], [uTb, ident], [PS[psi]])
                        cp("dve", u_tm.t[:, t0_:t0_ + nt_, j * 128:(j + 1) * 128], pb[:, 0:nt_ * 128].rearrange("p (t c) -> p t c", c=128),
                           [PS[psi]], [u_tm.bs[t] for t in range(t0_, t0_ + nt_)])
                S_.barrier()

            with contextlib.ExitStack() as esk:
                ksum = T(esk.enter_context(nc.sbuf_tensor("s_ksum", [128, NT, 512], BF16)), NT)
                kdiff = T(esk.enter_context(nc.sbuf_tensor("s_kdiff", [128, NT, 512], BF16)), NT)
                with contextlib.ExitStack() as es:
                    def sb(name, shape, dt=F32, nb=1):
                        return T(es.enter_context(nc.sbuf_tensor("s_" + name, list(shape), dt)), nb)
                    zT = sb("zT", [33, S]); fw1 = sb("fw1", [33, 64]); fw2 = sb("fw2", [64, 64]); fw3 = sb("fw3", [64, 64]); fw4 = sb("fw4", [64, 1024])
                    fvec = sb("fvec", [64, 4]); trow = sb("trow", [1, S]); drow = sb("drow", [1, 512])
                    H3 = sb("H3", [64, S])
                    arg = sb("arg", [64, BLK]); ai = sb("ai", [64, BLK], I32); af = sb("af", [64, BLK]); hh = [sb("hh%d" % i, [64, BLK]) for i in range(2)]
                    dec = sb("dec", [128, 512]); kf = sb("kf", [128, 512]); kb = sb("kb", [128, 512])
                    for t_, d_ in [(zT, zT_d), (fw1, fw1_d), (fw2, fw2_d), (fw3, fw3_d), (fw4, fw4_d), (fvec, fvec_d), (trow, trow_d), (drow, drow_d)]:
                        dma("sp", t_.t[:], d_, [], [t_])

                    def sin_layer(ps, li, out_ap, out_t):
                        ts("dve", arg.t[:], ps.t[0:64, 0:BLK], fvec.t[:, li:li + 1], fvec.t[:, 3:4], ALU.add, ALU.mult, [ps, fvec], [arg])
                        ts("dve", arg.t[:], arg.t[:], 1.0 / (2 * PI), 16.0, ALU.mult, ALU.add, [arg], [arg])
                        cp("dve", ai.t[:], arg.t[:], [arg], [ai])
                        cp("dve", af.t[:], ai.t[:], [ai], [af])
                        tt("dve", arg.t[:], arg.t[:], af.t[:], ALU.subtract, [arg, af], [arg])
                        ts("dve", af.t[:], arg.t[:], 0.5, None, ALU.is_gt, None, [arg], [af])
                        tt("dve", arg.t[:], arg.t[:], af.t[:], ALU.subtract, [arg, af], [arg])
                        act(out_ap, arg.t[:], AF.Sin, [arg], [out_t], scale=2 * PI)

                    for b in range(NB):
                        sl = slice(b * BLK, (b + 1) * BLK)
                        mm(PS[0].t[0:64, 0:BLK], fw1.t[:], zT.t[:, sl], True, True, [fw1, zT], [PS[0]])
                        sin_layer(PS[0], 0, hh[0].t[:], hh[0])
                        mm(PS[1].t[0:64, 0:BLK], fw2.t[:], hh[0].t[:], True, True, [fw2, hh[0]], [PS[1]])
                        sin_layer(PS[1], 1, hh[1].t[:], hh[1])
                        mm(PS[2].t[0:64, 0:BLK], fw3.t[:], hh[1].t[:], True, True, [fw3, hh[1]], [PS[2]])
                        sin_layer(PS[2], 2, H3.t[:, sl], H3)
                    for lt in range(NT):
                        pf = PS[(lt % 2) * 3]; pb_ = PS[(lt % 2) * 3 + 1]; pd = PS[(lt % 2) * 3 + 2]
                        mm(pf.t[:], H3.t[:, lt * 128:(lt + 1) * 128], fw4.t[:, 0:512], True, True, [H3, fw4], [pf])
                        mm(pb_.t[:], H3.t[:, lt * 128:(lt + 1) * 128], fw4.t[:, 512:1024], True, True, [H3, fw4], [pb_])
                        mm(pd.t[:], trow.t[0:1, lt * 128:(lt + 1) * 128], drow.t[0:1, :], True, True, [trow, drow], [pd])
                        act(dec.t[:], pd.t[:], AF.Exp, [pd], [dec], scale=-1.0)
                        stt("dve", kf.t[:], pf.t[:], 2.0 / N2, dec.t[:], ALU.mult, ALU.mult, [pf, dec], [kf])
                        stt("dve", kb.t[:], pb_.t[:], 2.0 / N2, dec.t[:], ALU.mult, ALU.mult, [pb_, dec], [kb])
                        if lt == 0:
                            mset("dve", kb.t[0:1, :], 0.0, [kb])
                        tt("pool", ksum.t[:, lt, :], kf.t[:], kb.t[:], ALU.add, [kf, kb], [ksum.bs[lt]])
                        tt("pool", kdiff.t[:, lt, :], kb.t[:], kf.t[:], ALU.subtract, [kf, kb], [kdiff.bs[lt]])
                    S_.barrier()

                with contextlib.ExitStack() as es:
                    def sb(name, shape, dt=F32, nb=1):
                        return T(es.enter_context(nc.sbuf_tensor("s_" + name, list(shape), dt)), nb)
                    cf = [sb("cf%d" % i, [128, NT, 128], BF16) for i in range(2)]; sf = [sb("sf%d" % i, [128, NT, 128], BF16) for i in range(2)]
                    kre = sb("kre", [128, 512]); kim = sb("kim", [128, 512]); m1 = sb("m1", [128, 512]); m2 = sb("m2", [128, 512])
                    yt = [sb("yt%d" % i, [128, 2, 512], BF16) for i in range(2)]
                    for kc in range(NKC):
                        c_ = cf[kc % 2]; s_ = sf[kc % 2]
                        if kc == 0:
                            dma("sp", c_.t[:], Cf_d[0], [], [c_]); dma("sp", s_.t[:], Sf_d[0], [], [s_])
                        if kc + 1 < NKC:
                            cn_ = cf[(kc + 1) % 2]; sn_ = sf[(kc + 1) % 2]
                            dma("sp", cn_.t[:], Cf_d[kc + 1], [], [cn_]); dma("sp", sn_.t[:], Sf_d[kc + 1], [], [sn_])
                        pa = PS[(kc % 2) * 4: (kc % 2) * 4 + 4]
                        for t_ in range(NT):
                            st_, sp2 = (t_ == 0), (t_ == NT - 1)
                            mm(pa[0].t[:], c_.t[:, t_, :], u_tm.t[:, t_, :], st_, sp2, [c_, u_tm.bs[t_]], [pa[0]])
                            mm(pa[1].t[:], s_.t[:, t_, :], u_tm.t[:, t_, :], st_, sp2, [s_, u_tm.bs[t_]], [pa[1]])
                            mm(pa[2].t[:], c_.t[:, t_, :], ksum.t[:, t_, :], st_, sp2, [c_, ksum.bs[t_]], [pa[2]])
                            mm(pa[3].t[:], s_.t[:, t_, :], kdiff.t[:, t_, :], st_, sp2, [s_, kdiff.bs[t_]], [pa[3]])
                        y_ = yt[kc % 2]
                        act(kre.t[:], pa[2].t[:], AF.Copy, [pa[2]], [kre]); act(kim.t[:], pa[3].t[:], AF.Copy, [pa[3]], [kim])
                        tt("dve", m1.t[:], pa[0].t[:], kre.t[:], ALU.mult, [pa[0], kre], [m1])
                        tt("dve", m2.t[:], pa[1].t[:], kim.t[:], ALU.mult, [pa[1], kim], [m2])
                        tt("pool", y_.t[:, 0, :], m1.t[:], m2.t[:], ALU.add, [m1, m2], [y_])
                        tt("dve", m1.t[:], pa[0].t[:], kim.t[:], ALU.mult, [pa[0], kim], [m1])
                        tt("dve", m2.t[:], pa[1].t[:], kre.t[:], ALU.mult, [pa[1], kre], [m2])
                        tt("pool", y_.t[:, 1, :], m1.t[:], m2.t[:], ALU.subtract, [m1, m2], [y_])
                        dma("sp", Y_d[:, :, kc, :], y_.t[:], [y_], [B_Y[kc]])
                    S_.barrier()

        with contextlib.ExitStack() as es:
            def sb(name, shape, dt=F32, nb=1):
                return T(es.enter_context(nc.sbuf_tensor("s_" + name, list(shape), dt)), nb)
            Yall = sb("Yall", [128, 2, NKC, 512], BF16, nb=NKC)
            ci = [sb("ci%d" % i, [128, KG, BLK], BF16) for i in range(3)]; si = [sb("si%d" % i, [128, KG, BLK], BF16) for i in range(3)]
            x0b = [sb("x0b%d" % i, [128, 4, BLK], BF16) for i in range(2)]; uTk = [sb("uTk%d" % i, [128, 4, BLK], BF16) for i in range(2)]
            obb = [sb("obb%d" % i, [128, 4, BLK], BF16) for i in range(2)]
            tmp = [sb("h2tmp%d" % i, [128, BLK]) for i in range(2)]; hbv = sb("hbv", [128, 4])
            dma("sp", hbv.t[:], hb_d, [], [hbv])
            def load_y(kg_):
                for kc_ in range(kg_ * KG, (kg_ + 1) * KG):
                    dma("sp", Yall.t[:, :, kc_, :], Y_d[:, :, kc_, :], [B_Y[kc_]], [Yall.bs[kc_]])
            load_y(0)
            n = 0
            for tb in range(NB):
                acc = PS[(tb % 2) * 4:(tb % 2) * 4 + 4]
                xb = x0b[tb % 2]; ub = uTk[tb % 2]; ob = obb[tb % 2]
                dma("sp", xb.t[:], x0c_d[:, :, tb * BLK:(tb + 1) * BLK], B_x0c, [xb])
                dma("sp", ub.t[:], uT_d[:, :, tb * BLK:(tb + 1) * BLK], B_uT, [ub])
                for kg in range(NKG):
                    c_ = ci[n % 3]; s_ = si[n % 3]; n += 1
                    dma("sp", c_.t[:], Ci_d[tb, :, kg * KG:(kg + 1) * KG, :], [], [c_])
                    dma("sp", s_.t[:], Si_d[tb, :, kg * KG:(kg + 1) * KG, :], [], [s_])
                    if tb == 0 and kg + 1 < NKG:
                        load_y(kg + 1)
                    for kl in range(KG):
                        kc = kg * KG + kl
                        for c4 in range(4):
                            mm(acc[c4].t[:, 0:BLK], Yall.t[:, 0, kc, c4 * 128:(c4 + 1) * 128], c_.t[:, kl, :], kc == 0, False, [Yall.bs[kc], c_], [acc[c4]])
                            mm(acc[c4].t[:, 0:BLK], Yall.t[:, 1, kc, c4 * 128:(c4 + 1) * 128], s_.t[:, kl, :], False, kc == NKC - 1, [Yall.bs[kc], s_], [acc[c4]])
                for c4 in range(4):
                    tm = tmp[c4 % 2]
                    stt("dve", tm.t[:], ub.t[:, c4, :], hbv.t[:, c4:c4 + 1], acc[c4].t[:, 0:BLK], ALU.mult, ALU.add, [ub, hbv, acc[c4]], [tm])
                    tt("pool", ob.t[:, c4, :], tm.t[:], xb.t[:, c4, :], ALU.mult, [tm, xb], [ob])
                dma("sp", obT_d[:, :, tb * BLK:(tb + 1) * BLK], ob.t[:], [ob], [B_obT[tb]])
            S_.barrier()

        with contextlib.ExitStack() as es:
            def sb(name, shape, dt=F32, nb=1):
                return T(es.enter_context(nc.sbuf_tensor("s_" + name, list(shape), dt)), nb)
            wg = sb("wg", [128, 8, 2048], BF16); wua = sb("wua", [128, 4, D], BF16); wub = sb("wub", [128, 4, D], BF16)
            wout = sb("wout", [128, 8, D], BF16); rwt = sb("rwt", [128, 8, 32], BF16); rbt = sb("rbt", [1, 32], BF16)
            bgf = sb("bgf", [128, 40])
            hblk = [sb("mhb%d" % i, [128, 8, BLK], BF16) for i in range(2)]
            oab = [sb("moa%d" % i, [128, 4, BLK], BF16) for i in range(2)]; obk = [sb("mob%d" % i, [128, 4, BLK], BF16) for i in range(2)]
            gas = [sb("gas%d" % i, [128, BLK]) for i in range(2)]; gbs = [sb("gbs%d" % i, [128, BLK]) for i in range(2)]
            mm1 = [sb("mm1%d" % i, [128, BLK]) for i in range(1)]; mm2 = [sb("mm2%d" % i, [128, BLK]) for i in range(1)]
            mTs = [sb("mT%d" % i, [128, 8, BLK], BF16, nb=8) for i in range(2)]
            xin = [sb("mxin%d" % i, [128, D]) for i in range(2)]; xn = [sb("mxn%d" % i, [128, D]) for i in range(2)]
            tg = sb("mtg", [128, 512])
            h2b = [sb("h2b%d" % i, [128, 8, BLK], BF16) for i in range(2)]
            msk = sb("msk", [128, 32]); mskb = sb("mskb", [128, 32], BF16); nmx = sb("nmx", [128, 1])
            htmp = sb("htmp", [128, D]); h2tm = [sb("h2tm%d" % i, [128, D], BF16) for i in range(1)]
            ex = sb("ex", [128, 32]); sm_ = sb("sm", [128, 1])
            st = norm_transpose(es, "m")
            for c in range(4):
                dma("pool", wg.t[:, :, c * 512:(c + 1) * 512], w_in_v[:, :, 3072 + c * 512:3072 + (c + 1) * 512], [], [wg])
            dma("pool", wua.t[:], wua_d.rearrange("(k p) n -> p k n", p=128), [], [wua])
            dma("pool", wub.t[:], wub_d.rearrange("(k p) n -> p k n", p=128), [], [wub])
            for c in range(2):
                dma("pool", wout.t[:, :, c * 512:(c + 1) * 512], wout_d.rearrange("(k p) n -> p k n", p=128)[:, :, c * 512:(c + 1) * 512], [], [wout])
            dma("pool", rwt.t[:], rw_d.rearrange("(k p) n -> p k n", p=128), [], [rwt]); dma("pool", rbt.t[:], rb_d, [], [rbt])
            dma("sp", bgf.t[:], bin_fm_d, [], [bgf])
            tno = 0

            def gates(tb):
                hb = hblk[tb % 2]; oa = oab[tb % 2]; ob = obk[tb % 2]; mT = mTs[tb % 2]
                dma("sp", hb.t[:], hT_d[:, :, tb * BLK:(tb + 1) * BLK], [B_hT[tb]], [hb])
                dma("sp", oa.t[:], oaT_d[:, :, tb * BLK:(tb + 1) * BLK], [B_oaT[tb]], [oa])
                dma("sp", ob.t[:], obT_d[:, :, tb * BLK:(tb + 1) * BLK], [B_obT[tb]], [ob])
                for j in range(8):
                    pga, pgb, pA, pB = PS[0], PS[1], PS[2], PS[3]
                    for k in range(8):
                        mm(pga.t[:, 0:BLK], wg.t[:, k, j * 128:(j + 1) * 128], hb.t[:, k, :], k == 0, k == 7, [wg, hb], [pga])
                    for k in range(8):
                        mm(pgb.t[:, 0:BLK], wg.t[:, k, 1024 + j * 128:1024 + (j + 1) * 128], hb.t[:, k, :], k == 0, k == 7, [wg, hb], [pgb])
                    for k in range(4):
                        mm(pA.t[:, 0:BLK], wua.t[:, k, j * 128:(j + 1) * 128], oa.t[:, k, :], k == 0, k == 3, [wua, oa], [pA])
                    for k in range(4):
                        mm(pB.t[:, 0:BLK], wub.t[:, k, j * 128:(j + 1) * 128], ob.t[:, k, :], k == 0, k == 3, [wub, ob], [pB])
                    ga_ = gas[j % 2]; gb_ = gbs[j % 2]; a1 = mm1[0]; a2 = mm2[0]
                    act(ga_.t[:], pga.t[:, 0:BLK], AF.Sigmoid, [pga, bgf], [ga_], bias=bgf.t[:, 24 + j:25 + j])
                    act(gb_.t[:], pgb.t[:, 0:BLK], AF.Sigmoid, [pgb, bgf], [gb_], bias=bgf.t[:, 32 + j:33 + j])
                    tt("dve", a1.t[:], pA.t[:, 0:BLK], ga_.t[:], ALU.mult, [pA, ga_], [a1])
                    tt("dve", a2.t[:], pB.t[:, 0:BLK], gb_.t[:], ALU.mult, [pB, gb_], [a2])
                    tt("pool", mT.t[:, j, :], a1.t[:], a2.t[:], ALU.add, [a1, a2], [mT.bs[j]])

            def tiles(tb):
                nonlocal tno
                h2 = h2b[tb % 2]; mT = mTs[tb % 2]
                for s in range(TPB):
                    i = tb * TPB + s
                    xi = xin[tno % 2]; xo = xn[tno % 2]; tno += 1
                    dma("sp", xi.t[:], x_d[i * 128:(i + 1) * 128, :], [], [xi])
                    for half in range(2):
                        po = PS[4 + half]
                        for j in range(8):
                            mm(po.t[:], mT.t[:, j, s * 128:(s + 1) * 128], wout.t[:, j, half * 512:(half + 1) * 512], j == 0, j == 7, [mT.bs[j], wout], [po])
                        tt("dve", tg.t[:], po.t[:], g1row.t[:, half * 512:(half + 1) * 512], ALU.mult, [po, g1row], [tg])
                        tt("pool", xo.t[:, half * 512:(half + 1) * 512], tg.t[:], xi.t[:, half * 512:(half + 1) * 512], ALU.add, [tg, xi], [xo])
                    dma("sp", y_d[i * 128:(i + 1) * 128, :], xo.t[:], [xo], [B_y[i]])
                    ss_t = do_norm_transpose(st, xo, A2, B2, 6, lambda k, s=s, h2=h2: h2.t[:, k, s * 128:(s + 1) * 128], [h2])
                    hm = h2tm[0]
                    stt("dve", htmp.t[:], xo.t[:], ss_t.t[:, 0:1], A2row.t[:], ALU.mult, ALU.mult, [xo, ss_t, A2row], [htmp])
                    tt("pool", hm.t[:], htmp.t[:], B2row.t[:], ALU.add, [htmp, B2row], [hm])
                    dma("sp", H2tm_d[i * 128:(i + 1) * 128, :], hm.t[:], [hm], [B_h2tm[i]])
                    pr = PS[7]
                    for k in range(8):
                        mm(pr.t[:, 0:32], h2.t[:, k, s * 128:(s + 1) * 128], rwt.t[:, k, :], k == 0, False, [h2, rwt], [pr])
                    mm(pr.t[:, 0:32], onesb.t[0:1, :], rbt.t[0:1, :], False, True, [onesb, rbt], [pr])
                    lgi = LG.t[:, i, :]
                    act(lgi, pr.t[:, 0:32], AF.Copy, [pr], [LG.bs[i]])
                    vmax8(MX.t[:, i, :], lgi, [LG.bs[i]], [MX.bs[i]])
                    ts("dve", msk.t[:], lgi, MX.t[:, i, 3:4], None, ALU.is_ge, None, [LG.bs[i], MX.bs[i]], [msk])
                    cp("dve", mskb.t[:], msk.t[:], [msk], [mskb])
                    ts("dve", nmx.t[:], MX.t[:, i, 0:1], -1.0, None, ALU.mult, None, [MX.bs[i]], [nmx])
                    act(ex.t[:], lgi, AF.Exp, [LG.bs[i], nmx], [ex], bias=nmx.t[:, 0:1])
                    tt("dve", ex.t[:], ex.t[:], msk.t[:], ALU.mult, [ex, msk], [ex])
                    red(sm_.t[:], ex.t[:], [ex], [sm_])
                    recip(sm_.t[:], sm_.t[:], [sm_], [sm_])
                    ts("dve", G.t[:, i, :], ex.t[:], sm_.t[:, 0:1], None, ALU.mult, None, [ex, sm_], [G.bs[i]])
                    mm(pr.t[:, 32:64], ustr.t[:], mskb.t[:], True, False, [ustr, mskb], [pr])
                    mm(pr.t[:, 32:64], onesb.t[:], cumb.t[:], False, True, [onesb, cumb], [pr])
                    cp("dve", POS.t[:, i, :], pr.t[:, 32:64], [pr], [POS.bs[i]])
                    tt("dve", cumf.t[:], cumf.t[:], msk.t[:], ALU.add, [cumf, msk], [cumf])
                    cp("dve", cumb.t[:], cumf.t[:], [cumf], [cumb])

            gates(0)
            for tb in range(NB):
                if tb + 1 < NB:
                    gates(tb + 1)
                tiles(tb)
            S_.barrier()
        if debug:
            with contextlib.ExitStack() as es3:
                dt_ = T(es3.enter_context(nc.sbuf_tensor("s_dbgt", [128, 8, S], F32)))
                db_ = T(es3.enter_context(nc.sbuf_tensor("s_dbgb", [128, 8, S], BF16)))
                for nm, src, nch in [("dbg_oaT", oaT_d, 4), ("dbg_obT", obT_d, 4), ("dbg_hT", hT_d, 8)]:
                    dma("sp", db_.t[:, 0:nch, :], src, [], [db_])
                    cp("dve", dt_.t[:, 0:nch, :], db_.t[:, 0:nch, :], [db_], [dt_])
                    dma("sp", dbg[nm], dt_.t[:, 0:nch, :], [dt_], [Buf()])
                    S_.barrier()
                dma("sp", dbg["dbg_G"], G.t[:], G.bs, [Buf()])
                S_.barrier()

        with contextlib.ExitStack() as es:
            def sb(name, shape, dt=F32, nb=1):
                return T(es.enter_context(nc.sbuf_tensor("s_" + name, list(shape), dt)), nb)
            cnt = sb("cnt", [128, 32]); yv = sb("yv", [128, 32]); yi_ = sb("yi", [128, 32], I32); yf = sb("yf", [128, 32]); ygt = sb("ygt", [128, 32])
            padded = sb("padded", [128, 32]); ca = sb("csuma", [128, 32]); cb_ = sb("csumb", [128, 32]); pstart = sb("pstart", [128, 32])
            kp = sb("kp", [128, 8]); bstart = sb("bstart", [128, NBLK]); cmp_ = sb("cmp", [128, NBLK, 32]); be = sb("be", [128, NBLK])
            bf_ = sb("bf", [128, NBLK, 2])
            slotv = [sb("slotv%d" % i, [128, 32]) for i in range(2)]; oh4 = [sb("oh4%d" % i, [128, 4, 32]) for i in range(2)]
            pr4 = [sb("pr4%d" % i, [128, 4, 32]) for i in range(2)]; i4f = [sb("i4f%d" % i, [128, 4]) for i in range(2)]
            hrow = [sb("hrow%d" % i, [128, D], BF16) for i in range(3)]
            dma("sp", kp.t[:], kp_d, [], [kp]); dma("sp", bstart.t[:], bstart_d, [], [bstart])
            mm(PS[0].t[:, 0:32], onesb.t[:], cumb.t[:], True, True, [onesb, cumb], [PS[0]])
            cp("dve", cnt.t[:], PS[0].t[:, 0:32], [PS[0]], [cnt])
            ts("dve", yv.t[:], cnt.t[:], float(RB - 1), 1.0 / RB, ALU.add, ALU.mult, [cnt], [yv])
            cp("dve", yi_.t[:], yv.t[:], [yv], [yi_]); cp("dve", yf.t[:], yi_.t[:], [yi_], [yf])
            tt("dve", ygt.t[:], yf.t[:], yv.t[:], ALU.is_gt, [yf, yv], [ygt])
            tt("dve", yf.t[:], yf.t[:], ygt.t[:], ALU.subtract, [yf, ygt], [yf])
            ts("dve", padded.t[:], yf.t[:], float(RB), None, ALU.mult, None, [yf], [padded])
            cp("dve", ca.t[:], padded.t[:], [padded], [ca])
            src_, dst_ = ca, cb_
            for sh in (1, 2, 4, 8, 16):
                cp("dve", dst_.t[:, 0:sh], src_.t[:, 0:sh], [src_], [dst_])
                tt("dve", dst_.t[:, sh:32], src_.t[:, sh:32], src_.t[:, 0:32 - sh], ALU.add, [src_], [dst_])
                src_, dst_ = dst_, src_
            pend = src_
            tt("dve", pstart.t[:], pend.t[:], padded.t[:], ALU.subtract, [pend, padded], [pstart])
            tt("dve", cmp_.t[:], pend.t[:].unsqueeze(1).broadcast_to([128, NBLK, 32]), bstart.t[:].unsqueeze(2).broadcast_to([128, NBLK, 32]),
               ALU.is_le, [pend, bstart], [cmp_])
            red(be.t[:], cmp_.t[:], [cmp_], [be])
            ts("dve", be.t[:], be.t[:], float(NE - 1), None, ALU.min, None, [be], [be])
            stt("dve", bf_.t[:, :, 0], be.t[:], 128.0, kp.t[:, 0:1].broadcast_to([128, NBLK]), ALU.mult, ALU.add, [be, kp], [bf_])
            cp("dve", bf_.t[:, :, 1], be.t[:], [be], [bf_])
            cp("dve", BIDX.t[:], bf_.t[:], [bf_], [BIDX])
            for i in range(NT):
                sv = slotv[i % 2]; oh = oh4[i % 2]; p4 = pr4[i % 2]; f4 = i4f[i % 2]; hr = hrow[i % 3]
                tt("dve", sv.t[:], POS.t[:, i, :], pstart.t[:], ALU.add, [POS.bs[i], pstart], [sv])
                tt("dve", oh.t[:], LG.t[:, i, :].unsqueeze(1).broadcast_to([128, 4, 32]), MX.t[:, i, 0:4].unsqueeze(2).broadcast_to([128, 4, 32]),
                   ALU.is_equal, [LG.bs[i], MX.bs[i]], [oh])
                tt("dve", p4.t[:], oh.t[:], sv.t[:].unsqueeze(1).broadcast_to([128, 4, 32]), ALU.mult, [oh, sv], [p4])
                red(f4.t[:], p4.t[:], [p4], [f4])
                cp("dve", IDX.t[:, i, :], f4.t[:], [f4], [IDX.bs[i]])
                tt("dve", p4.t[:], oh.t[:], G.t[:, i, :].unsqueeze(1).broadcast_to([128, 4, 32]), ALU.mult, [oh, G.bs[i]], [p4])
                red(W4.t[:, i, :], p4.t[:], [p4], [W4.bs[i]])
                dma("sp", hr.t[:], H2tm_d[i * 128:(i + 1) * 128, :], [B_h2tm[i]], [hr])
                for j in range(4):
                    scatter(Xs_d, hr.t[:], IDX.t[:, i, j:j + 1], [hr, IDX.bs[i]], [])
            S_.barrier()

        with contextlib.ExitStack() as es:
            def sb(name, shape, dt=F32, nb=1):
                return T(es.enter_context(nc.sbuf_tensor("s_" + name, list(shape), dt)), nb)
            w1t = [sb("w1t%d" % i, [128, 8, 2048], BF16) for i in range(2)]; w2t = [sb("w2t%d" % i, [128, 8, D], BF16) for i in range(2)]
            b1t = [sb("b1t%d" % i, [128, 16]) for i in range(2)]; b2rep = [sb("b2rep%d" % i, [128, D], BF16) for i in range(2)]
            xsb = [sb("xsb%d" % i, [128, NST, D], BF16) for i in range(2)]; XsT = [sb("XsT%d" % i, [128, 8, RB], BF16) for i in range(2)]
            actT = [sb("actT%d" % i, [128, 8, RB], BF16, nb=8) for i in range(2)]
            gcl = [sb("gcl%d" % i, [128, RB]) for i in range(2)]; sg = [sb("sg%d" % i, [128, RB]) for i in range(2)]
            ucl = [sb("ucl%d" % i, [128, RB]) for i in range(2)]
            ysb = [sb("ysb%d" % i, [128, D]) for i in range(2)]
            ew1f = ew1_d.rearrange("e d n -> (e d) n"); ew2f = ew2_d.rearrange("e d n -> (e d) n")
            en = 0; yn = 0
            for b in range(NBLK):
                w1 = w1t[b % 2]; w2 = w2t[b % 2]; b1 = b1t[b % 2]; b2 = b2rep[b % 2]; xs_ = xsb[b % 2]; xT = XsT[b % 2]; aT = actT[b % 2]
                gather(w1.t[:].rearrange("p k n -> p (k n)"), W1b_d, BIDX.t[:, b, 0:1], [BIDX], [w1])
                gather(b1.t[:], eb1_d, BIDX.t[:, b, 0:1], [BIDX], [b1])
                gather(w2.t[:].rearrange("p k n -> p (k n)"), W2b_d, BIDX.t[:, b, 0:1], [BIDX], [w2])
                gather(b2.t[:], eb2_d, BIDX.t[:, b, 1:2], [BIDX], [b2])
                ts("dve", b1.t[:, 8:16], b1.t[:, 8:16], 1.0, None, ALU.add, None, [b1], [b1])
                if b == 0:
                    dma("sp", xs_.t[:], Xs_d[0:RB, :].rearrange("(s p) d -> p s d", p=128), [], [xs_])
                if b + 1 < NBLK:
                    xn_ = xsb[(b + 1) % 2]
                    dma("sp", xn_.t[:], Xs_d[(b + 1) * RB:(b + 2) * RB, :].rearrange("(s p) d -> p s d", p=128), [], [xn_])
                for s2 in range(NST):
                    pb = psbf(6 + s2 % 2)
                    for k in range(8):
                        tr(pb[:, k * 128:(k + 1) * 128], xs_.t[:, s2, k * 128:(k + 1) * 128], ident.t[:], [xs_, ident], [PS[6 + s2 % 2]])
                    S_.op("act", (lambda o, i_: lambda e: e.copy(o, i_))(xT.t[:, :, s2 * 128:(s2 + 1) * 128], pb[:, :].rearrange("p (k t) -> p k t", t=128)),
                          [PS[6 + s2 % 2].b], [xT.b])
                for Fi in range(8):
                    pg = PS[(en % 2) * 2]; pu = PS[(en % 2) * 2 + 1]
                    gc = gcl[en % 2]; sgt = sg[en % 2]; uc = ucl[en % 2]; gs_ = sgt; en += 1
                    for k in range(8):
                        mm(pg.t[:, 0:RB], w1.t[:, k, Fi * 128:(Fi + 1) * 128], xT.t[:, k, :], k == 0, k == 7, [w1, xT], [pg])
                    for k in range(8):
                        mm(pu.t[:, 0:RB], w1.t[:, k, 1024 + Fi * 128:1024 + (Fi + 1) * 128], xT.t[:, k, :], k == 0, k == 7, [w1, xT], [pu])
                    ts("dve", gc.t[:], pg.t[:, 0:RB], b1.t[:, Fi:Fi + 1], 7.0, ALU.add, ALU.min, [pg, b1], [gc])
                    act(sgt.t[:], gc.t[:], AF.Sigmoid, [gc], [sgt], scale=1.702)
                    ts("dve", uc.t[:], pu.t[:, 0:RB], b1.t[:, 8 + Fi:9 + Fi], 8.0, ALU.add, ALU.min, [pu, b1], [uc])
                    tt("dve", gs_.t[:], gc.t[:], sgt.t[:], ALU.mult, [gc, sgt], [gs_])
                    stt("dve", aT.t[:, Fi, :], uc.t[:], -6.0, gs_.t[:], ALU.max, ALU.mult, [gs_, uc], [aT.bs[Fi]])
                for s2 in range(NST):
                    yt_ = ysb[yn % 2]; yn += 1
                    for half in range(2):
                        py = PS[4 + half]
                        for k in range(8):
                            mm(py.t[:], aT.t[:, k, s2 * 128:(s2 + 1) * 128], w2.t[:, k, half * 512:(half + 1) * 512], k == 0, k == 7, [aT.bs[k], w2], [py])
                        tt("dve", yt_.t[:, half * 512:(half + 1) * 512], py.t[:], b2.t[:, half * 512:(half + 1) * 512], ALU.add, [py, b2], [yt_])
                    dma("sp", Ys_d[b * RB + s2 * 128:b * RB + (s2 + 1) * 128, :], yt_.t[:], [yt_], [B_Ys[b]])
            S_.barrier()

        with contextlib.ExitStack() as es:
            def sb(name, shape, dt=F32, nb=1):
                return T(es.enter_context(nc.sbuf_tensor("s_" + name, list(shape), dt)), nb)
            gat = [[sb("gat%d_%d" % (i, j), [128, D]) for j in range(4)] for i in range(2)]
            xq = [sb("cxq%d" % i, [128, D]) for i in range(2)]; acc_ = [sb("cacc%d" % i, [128, D]) for i in range(2)]
            for i in range(NT):
                g4 = gat[i % 2]; xq_ = xq[i % 2]; ac = acc_[i % 2]
                for j in range(4):
                    gather(g4[j].t[:], Ys_d, IDX.t[:, i, j:j + 1], [IDX.bs[i]] + B_Ys, [g4[j]])
                if i == 0:
                    dma("sp", xq_.t[:], y_d[0:128, :], [B_y[0]], [xq_])
                if i + 1 < NT:
                    xqn = xq[(i + 1) % 2]
                    dma("sp", xqn.t[:], y_d[(i + 1) * 128:(i + 2) * 128, :], [B_y[i + 1]], [xqn])
                ts("dve", ac.t[:], g4[0].t[:], W4.t[:, i, 0:1], None, ALU.mult, None, [g4[0], W4.bs[i]], [ac])
                for j in range(1, 4):
                    stt("dve", ac.t[:], g4[j].t[:], W4.t[:, i, j:j + 1], ac.t[:], ALU.mult, ALU.add, [g4[j], W4.bs[i], ac], [ac])
                tt("dve", ac.t[:], ac.t[:], g2row.t[:], ALU.mult, [ac, g2row], [ac])
                tt("dve", ac.t[:], ac.t[:], xq_.t[:], ALU.add, [ac, xq_], [ac])
                dma("sp", y_d[i * 128:(i + 1) * 128, :], ac.t[:], [ac], [B_y[i]])
            S_.barrier()

        sems = {k: ges.enter_context(nc.semaphore("s%d" % i)) for i, k in enumerate(S_.semkeys)}
        S_.emit(sems)
    return nc


_CONST_CACHE = {}


def make_constants(S):
    if S in _CONST_CACHE:
        return _CONST_CACHE[S]
    bf = ml_dtypes.bfloat16
    NT = S // 128; NKC = S // 128; BLK = min(512, S); NB = S // BLK
    N2 = 2 * S
    n = np.arange(S, dtype=np.int64)[:, None]; k = np.arange(S, dtype=np.int64)[None, :]
    ph = ((2 * k + 1) * n) % (2 * N2)
    ang = ph.astype(np.float64) * (np.pi / N2)
    C = np.cos(ang); Sm = np.sin(ang)
    del ang, ph
    def fwd(M):
        return np.ascontiguousarray(M.reshape(NT, 128, NKC, 128).transpose(2, 1, 0, 3)).astype(bf)
    def inv(M):
        return np.ascontiguousarray(M.reshape(NB, BLK, NKC, 128).transpose(0, 3, 2, 1)).astype(bf)
    consts = {"Cf": fwd(C), "Sf": fwd(Sm), "Ci": inv(C), "nSi": inv(-Sm)}
    del C, Sm
    consts["ident"] = np.eye(128, dtype=np.float32).astype(bf)
    GRID_W = 64; RF = 16
    rows = S // GRID_W
    row = np.repeat(np.arange(rows), GRID_W); col = np.tile(np.arange(GRID_W), rows)
    pos = np.stack([row, col], -1).astype(np.float32)
    freqs = (np.float32(10000.0) ** (-np.arange(RF, dtype=np.float32) / np.float32(RF))).astype(np.float32)
    ang = (pos[:, :, None] * freqs).astype(np.float32)
    cos = np.cos(ang).astype(np.float32); sin = np.sin(ang).astype(np.float32)
    ropec = np.stack([cos, cos], 2).reshape(S, 64)
    ropes = np.stack([sin, -sin], 2).reshape(S, 64)
    consts["ropec"] = np.ascontiguousarray(ropec, dtype=np.float32); consts["ropes"] = np.ascontiguousarray(ropes, dtype=np.float32)
    t = np.linspace(0.0, 1.0, S, dtype=np.float32)[:, None]
    w = (np.float32(2.0 * math.pi) * np.arange(S, dtype=np.float32)[:, None] / np.float32(S)).astype(np.float32)
    f = np.linspace(1e-4, 15, 16, dtype=np.float32)
    z = np.concatenate([t, np.cos(f * w), -np.sin(f * w)], -1).astype(np.float32)
    consts["zT"] = np.ascontiguousarray(z.T)
    consts["trow"] = np.ascontiguousarray(t.T)
    deltas = np.abs(np.linspace(math.log(1e-2) / 1.5, math.log(1e-2) / 0.3, 512, dtype=np.float32))
    consts["drow"] = deltas.reshape(1, 512).astype(np.float32)
    _CONST_CACHE[S] = consts
    return consts


def fm(v, nchunk):
    return np.ascontiguousarray(np.asarray(v, np.float32).reshape(nchunk, 128).T)


def make_in_maps(inp, S, CTXL, NE, B):
    consts = make_constants(S)
    f32 = lambda a: np.ascontiguousarray(np.asarray(a, np.float32))
    shared = dict(consts)
    shared["ada_w"] = f32(inp["ada_w"][0]); shared["ada_b_row"] = f32(inp["ada_b"][0]).reshape(1, -1); shared["ada_b_fm"] = fm(inp["ada_b"][0], 48)
    shared["n1g"] = fm(inp["norm1_g"][0], 8); shared["n2g"] = fm(inp["norm2_g"][0], 8)
    shared["w_in"] = f32(inp["w_in"][0]); shared["b_in_row"] = f32(inp["b_in"][0]).reshape(1, -1); shared["b_in_fm"] = fm(inp["b_in"][0], 40)
    shared["qg"] = f32(inp["q_norm_g"][0]).reshape(1, 64); shared["kg"] = f32(inp["k_norm_g"][0]).reshape(1, 64)
    shared["lam4"] = np.concatenate([f32(inp[n][0]) for n in ("lambda_q1", "lambda_k1", "lambda_q2", "lambda_k2")]).reshape(1, 256)
    shared["subg"] = f32(inp["subln_g"][0]).reshape(128, 1)
    cw = f32(inp["conv_w"][0])
    shared["convw"] = np.ascontiguousarray(cw.reshape(3, 12, 128).transpose(2, 1, 0)); shared["convb"] = fm(inp["conv_b"][0], 12)
    shared["fw1"] = f32(inp["filt_w1"][0]); shared["fw2"] = f32(inp["filt_w2"][0]); shared["fw3"] = f32(inp["filt_w3"][0]); shared["fw4"] = f32(inp["filt_w4"][0])
    shared["fvec"] = np.ascontiguousarray(np.stack([f32(inp["filt_b1"][0]), f32(inp["filt_b2"][0]), f32(inp["filt_b3"][0]), f32(inp["filt_freq"][0])], -1))
    shared["hbias"] = fm(inp["hyena_bias"][0], 4)
    shared["w_up_a"] = f32(inp["w_up_a"][0]); shared["w_up_b"] = f32(inp["w_up_b"][0]); shared["w_out"] = f32(inp["w_out"][0])
    shared["router_w"] = f32(inp["router_w"][0]); shared["router_b"] = f32(inp["router_b"][0]).reshape(1, 32)
    shared["ew1"] = f32(inp["exp_w1"][0]); shared["ew2"] = f32(inp["exp_w2"][0])
    shared["eb1"] = np.ascontiguousarray(f32(inp["exp_b1"][0]).reshape(NE, 16, 128).transpose(0, 2, 1).reshape(NE * 128, 16))
    shared["eb2"] = f32(inp["exp_b2"][0]).reshape(NE, D)
    shared["n2g_row"] = f32(inp["norm2_g"][0]).reshape(1, D)
    NBLK = (4 * S + NE * RB) // RB
    shared["ustrict"] = np.triu(np.ones((128, 128), np.float32), 1).astype(ml_dtypes.bfloat16)
    shared["kp"] = np.ascontiguousarray((np.arange(8)[None, :] * 128 + np.arange(128)[:, None]).astype(np.float32))
    shared["bstart"] = np.ascontiguousarray(np.broadcast_to((np.arange(NBLK) * RB).astype(np.float32)[None, :], (128, NBLK)))
    maps = []
    for b in range(B):
        m = dict(shared)
        m["x"] = f32(inp["x"][b]); m["ctx"] = f32(inp["ctx"][b])
        m["cc"] = np.ascontiguousarray(np.stack([f32(inp["c"][b]), f32(inp["c_ctx"])], -1).reshape(8, 128, 2).transpose(1, 0, 2))
        maps.append(m)
    return maps


_PROG_CACHE = {}


def kernel(**inputs):
    x = np.asarray(inputs["x"])
    B, S, _ = x.shape
    CTXL = np.asarray(inputs["ctx"]).shape[1]
    NE = np.asarray(inputs["exp_w1"]).shape[1]
    key = (S, CTXL, NE)
    if key not in _PROG_CACHE:
        _PROG_CACHE[key] = build_program(S, CTXL, NE)
    nc = _PROG_CACHE[key]
    maps = make_in_maps(inputs, S, CTXL, NE, B)
    res = run_bass_kernel_spmd(nc, maps, core_ids=list(range(B)))
    return np.stack([np.asarray(r["y"], dtype=np.float32) for r in res.results], 0)
```

```python
import contextlib
import math
import numpy as np
import ml_dtypes
import concourse.bass as bass
import concourse.mybir as mybir
from concourse.bass_utils import run_bass_kernel_spmd

F32 = mybir.dt.float32
BF16 = mybir.dt.bfloat16
I32 = mybir.dt.int32
AF = mybir.ActivationFunctionType
ALU = mybir.AluOpType
AX = mybir.AxisListType
D = 1024
EPS = 1e-6
RB = 256
PI = math.pi


class Buf:
    __slots__ = ("w", "r")

    def __init__(self):
        self.w = None
        self.r = {}


ENGS = ("pe", "act", "dve", "pool", "sp")
DMA_RING = {"sp": 8, "pool": 8}


class Sched:
    def __init__(self, nc, same_engine_sync=True):
        self.nc = nc
        self.prog = {e: [] for e in ENGS}
        self.nops = {e: 0 for e in ENGS}
        self.waited = {e: {} for e in ENGS}
        self.same = same_engine_sync
        self.dma_n = {q: 0 for q in DMA_RING}
        self.dma_val = {}
        self.semkeys = list(ENGS)
        for q, n in DMA_RING.items():
            for i in range(n):
                self.semkeys.append(("dma", q, i))
                self.dma_val[("dma", q, i)] = 0

    def _wait(self, eng, semkey, val):
        if self.waited[eng].get(semkey, -1) >= val:
            return
        self.waited[eng][semkey] = val
        if isinstance(semkey, str):
            self.prog[semkey][val][3] = True
        self.prog[eng].append(["w", semkey, val])

    def _deps(self, eng, reads, writes, is_dma):
        deps = {}

        def add(k, v, e):
            if (not is_dma) and e == eng and k == eng:
                if eng == "pe" or not self.same:
                    return
            if deps.get(k, -1) < v:
                deps[k] = v
        for b in reads:
            if b.w is not None:
                add(*b.w)
        for b in writes:
            if b.w is not None:
                add(*b.w)
            for k, (v, e) in b.r.items():
                add(k, v, e)
        for k, v in deps.items():
            self._wait(eng, k, v)

    def _update(self, tok, reads, writes):
        k, v, e = tok
        for b in reads:
            b.r[k] = (v, e)
        for b in writes:
            b.w = tok
            b.r = {}

    def op(self, eng, fn, reads=(), writes=()):
        self._deps(eng, reads, writes, False)
        pos = len(self.prog[eng])
        self.prog[eng].append(["op", fn, eng, False])
        self.nops[eng] += 1
        self._update((eng, pos, eng), reads, writes)

    def dma(self, q, fn, reads=(), writes=()):
        n = self.dma_n[q]
        self.dma_n[q] += 1
        key = ("dma", q, n % DMA_RING[q])
        if self.dma_val[key] > 0:
            self._wait(q, key, self.dma_val[key])
        self._deps(q, reads, writes, True)
        self.dma_val[key] += 16
        tok = (key, self.dma_val[key], q)
        self.prog[q].append(["dma", fn, key, 16])
        self._update(tok, reads, writes)

    def _last_op(self, eng):
        for i in range(len(self.prog[eng]) - 1, -1, -1):
            if self.prog[eng][i][0] == "op":
                return i
        return None

    def barrier(self):
        for e in ENGS:
            for e2 in ENGS:
                if e2 != e:
                    lp = self._last_op(e2)
                    if lp is not None:
                        self._wait(e, e2, lp)
            for k, v in self.dma_val.items():
                if v > 0:
                    self._wait(e, k, v)

    def emit(self, sems):
        nc = self.nc
        value_at = {}
        for e in ENGS:
            c = 0
            va = {}
            for pos, item in enumerate(self.prog[e]):
                if item[0] == "op" and item[3]:
                    c += 1
                    va[pos] = c
            value_at[e] = va
        with nc.Block() as block:
            def run(engname):
                def body(eng):
                    for item in self.prog[engname]:
                        if item[0] == "w":
                            k, v = item[1], item[2]
                            eng.wait_ge(sems[k], value_at[k][v] if isinstance(k, str) else v)
                        elif item[0] == "dma":
                            item[1](eng).then_inc(sems[item[2]], 16)
                        elif item[3]:
                            item[1](eng).then_inc(sems[item[2]], 1)
                        else:
                            item[1](eng)
                return body
            block.tensor(run("pe"))
            block.scalar(run("act"))
            block.vector(run("dve"))
            block.gpsimd(run("pool"))
            block.sync(run("sp"))


class T:
    def __init__(self, t, nb=1):
        self.t = t
        self.b = Buf()
        self.bs = [Buf() for _ in range(nb)] if nb > 1 else [self.b]


def build_program(S, CTXL, NE, debug=False):
    NT = S // 128
    NC = CTXL // 128
    NKT = NT + NC
    TK = S + CTXL
    BLK = min(512, S)
    NB = S // BLK
    TPB = BLK // 128
    NKC = S // 128
    KG = min(8, NKC)
    NKG = NKC // KG
    QS = min(1024, S)
    NQ = S // QS
    NQT = QS // 128
    NQB = QS // BLK
    N2 = 2 * S
    NR = 4 * S + NE * RB
    NBLK = NR // RB
    NZ = NR // 256
    NST = RB // 128

    nc = bass.Bass("TRN2", target_bir_lowering=False)
    S_ = Sched(nc)

    def din(name, shape, dt=F32):
        return nc.dram_tensor(name, list(shape), dt, kind="ExternalInput").ap()

    def dscr(name, shape, dt=BF16):
        return nc.dram_tensor(name, list(shape), dt, kind="Internal").ap()

    x_d = din("x", [S, D]); ctx_d = din("ctx", [CTXL, D]); cc_d = din("cc", [128, 8, 2])
    adaw_d = din("ada_w", [D, 6 * D]); adab_row_d = din("ada_b_row", [1, 6 * D]); adab_fm_d = din("ada_b_fm", [128, 48])
    n1g_d = din("n1g", [128, 8]); n2g_d = din("n2g", [128, 8])
    win_d = din("w_in", [D, 5120]); bin_row_d = din("b_in_row", [1, 5120]); bin_fm_d = din("b_in_fm", [128, 40])
    qg_d = din("qg", [1, 64]); kg_d = din("kg", [1, 64]); lam4_d = din("lam4", [1, 256]); subg_d = din("subg", [128, 1])
    convw_d = din("convw", [128, 12, 3]); convb_d = din("convb", [128, 12])
    fw1_d = din("fw1", [33, 64]); fw2_d = din("fw2", [64, 64]); fw3_d = din("fw3", [64, 64]); fw4_d = din("fw4", [64, 1024])
    fvec_d = din("fvec", [64, 4])
    hb_d = din("hbias", [128, 4])
    wua_d = din("w_up_a", [512, D]); wub_d = din("w_up_b", [512, D]); wout_d = din("w_out", [D, D])
    rw_d = din("router_w", [D, 32]); rb_d = din("router_b", [1, 32])
    ew1_d = din("ew1", [NE, D, 2048]); eb1_d = din("eb1", [NE * 128, 16]); ew2_d = din("ew2", [NE, D, D]); eb2_d = din("eb2", [NE, D])
    ustr_d = din("ustrict", [128, 128], BF16); kp_d = din("kp", [128, 8]); bstart_d = din("bstart", [128, NBLK]); n2grow_d = din("n2g_row", [1, D])
    ident_d = din("ident", [128, 128], BF16)
    ropec_d = din("ropec", [S, 64]); ropes_d = din("ropes", [S, 64])
    zT_d = din("zT", [33, S]); trow_d = din("trow", [1, S]); drow_d = din("drow", [1, 512])
    Cf_d = din("Cf", [NKC, 128, NT, 128], BF16); Sf_d = din("Sf", [NKC, 128, NT, 128], BF16)
    Ci_d = din("Ci", [NB, 128, NKC, BLK], BF16); Si_d = din("nSi", [NB, 128, NKC, BLK], BF16)
    y_d = nc.dram_tensor("y", [S, D], F32, kind="ExternalOutput").ap()

    hTc_d = dscr("hTc_d", [128, 8, CTXL]); hT_d = dscr("hT_d", [128, 8, S])
    oaT_d = dscr("oaT_d", [128, 4, S]); obT_d = dscr("obT_d", [128, 4, S])
    x0c_d = dscr("x0c_d", [128, 4, S]); uT_d = dscr("uT_d", [128, 4, S])
    Y_d = dscr("Y_d", [128, 2, NKC, 512])
    H2tm_d = dscr("H2tm_d", [S, D]); Xs_d = dscr("Xs_d", [NR, D]); Ys_d = dscr("Ys_d", [NR, D], F32)
    W1b_d = dscr("W1b_d", [NE * 128, 8 * 2048]); W2b_d = dscr("W2b_d", [NE * 128, 8 * D])
    dbg = {}
    if debug:
        for n, shp in [("dbg_oaT", [128, 4, S]), ("dbg_obT", [128, 4, S]), ("dbg_hT", [128, 8, S]), ("dbg_G", [128, NT, 32])]:
            dbg[n] = nc.dram_tensor(n, shp, F32, kind="ExternalOutput").ap()
    B_hTc = Buf(); B_hT = [Buf() for _ in range(NB)]
    B_oaT = [Buf() for _ in range(NB)]; B_obT = [Buf() for _ in range(NB)]
    B_x0c = [Buf() for _ in range(4)]; B_uT = [Buf() for _ in range(4)]
    B_Y = [Buf() for _ in range(NKC)]; B_h2tm = [Buf() for _ in range(NT)]
    B_Xs = [Buf() for _ in range(NZ)]; B_Ys = [Buf() for _ in range(NBLK)]
    B_y = [Buf() for _ in range(NT)]

    w_in_v = win_d.rearrange("(k p) n -> p k n", p=128)

    def bl(xs):
        return [x.b if isinstance(x, T) else x for x in xs]

    def mm(out, lhsT, rhs, start, stop, r, w, tp=None):
        if tp is None:
            S_.op("pe", lambda e: e.matmul(out, lhsT, rhs, start=start, stop=stop), bl(r), bl(w))
        else:
            S_.op("pe", lambda e: e.matmul(out, lhsT, rhs, start=start, stop=stop, tile_position=tp), bl(r), bl(w))

    def tr(out, in_, ident, r, w):
        S_.op("pe", lambda e: e.transpose(out, in_, ident), bl(r), bl(w))

    def act(out, in_, func, r, w, **kw):
        S_.op("act", lambda e: e.activation(out=out, in_=in_, func=func, **kw), bl(r), bl(w))

    def tt(eng, out, in0, in1, op, r, w):
        S_.op(eng, lambda e: e.tensor_tensor(out, in0, in1, op), bl(r), bl(w))

    def ts(eng, out, in0, s1, s2, op0, op1, r, w):
        if op1 is None:
            S_.op(eng, lambda e: e.tensor_scalar(out, in0, s1, None, op0), bl(r), bl(w))
        else:
            S_.op(eng, lambda e: e.tensor_scalar(out, in0, s1, s2, op0, op1), bl(r), bl(w))

    def stt(eng, out, in0, sc, in1, op0, op1, r, w):
        S_.op(eng, lambda e: e.scalar_tensor_tensor(out, in0, sc, in1, op0, op1), bl(r), bl(w))

    def cp(eng, out, in_, r, w):
        S_.op(eng, lambda e: e.tensor_copy(out, in_), bl(r), bl(w))

    def recip(out, in_, r, w):
        S_.op("dve", lambda e: e.reciprocal(out, in_), bl(r), bl(w))

    def mset(eng, ap, val, w):
        S_.op(eng, lambda e: e.memset(ap, val), [], bl(w))

    def dma(q, out, in_, r, w):
        S_.dma(q, lambda e: e.dma_start(out=out, in_=in_), bl(r), bl(w))

    def vmax8(out, in_, r, w):
        S_.op("dve", lambda e: e.max(out, in_), bl(r), bl(w))

    def red(out, in_, r, w):
        S_.op("dve", lambda e: e.tensor_reduce(out, in_, AX.X, ALU.add), bl(r), bl(w))

    def gather(out, in_, idx_ap, r, w):
        S_.dma("pool", lambda e: e.indirect_dma_start(out=out, out_offset=None, in_=in_,
                                                      in_offset=bass.IndirectOffsetOnAxis(ap=idx_ap, axis=0), oob_is_err=False), bl(r), bl(w))

    def scatter(out, in_, idx_ap, r, w):
        S_.dma("pool", lambda e: e.indirect_dma_start(out=out, out_offset=bass.IndirectOffsetOnAxis(ap=idx_ap, axis=0), in_=in_,
                                                      in_offset=None, oob_is_err=False), bl(r), bl(w))

    with contextlib.ExitStack() as ges:
        def gsb(name, shape, dt=F32, nb=1):
            return T(ges.enter_context(nc.sbuf_tensor("s_" + name, list(shape), dt)), nb)
        psall = ges.enter_context(nc.psum_tensor("psall", [128, 4096], F32))
        PS = [T(psall[:, i * 512:(i + 1) * 512]) for i in range(8)]

        def psbf(i):
            return PS[i].t[:].bitcast(BF16)

        ident = gsb("ident", [128, 128], BF16); onesb = gsb("onesb", [128, 128], BF16); onesf = gsb("onesf", [128, 128])
        epst = gsb("epst", [128, 1])
        A1 = gsb("A1", [128, 8]); B1 = gsb("B1", [128, 8]); A1c = gsb("A1c", [128, 8]); B1c = gsb("B1c", [128, 8])
        A2 = gsb("A2", [128, 8]); B2 = gsb("B2", [128, 8])
        g1row = gsb("g1row", [128, D]); g2row = gsb("g2row", [128, D])
        neglam = gsb("neglam", [128, 1]); gsub = gsb("gsub", [128, 1])
        G = gsb("G", [128, NT, 32], F32, nb=NT)
        A2row = gsb("A2row", [128, D]); B2row = gsb("B2row", [128, D])
        LG = gsb("LG", [128, NT, 32], F32, nb=NT); MX = gsb("MX", [128, NT, 8], F32, nb=NT); POS = gsb("POS", [128, NT, 32], F32, nb=NT)
        IDX = gsb("IDX", [128, NT, 4], I32, nb=NT); W4 = gsb("W4", [128, NT, 4], F32, nb=NT)
        BIDX = gsb("BIDX", [128, NBLK, 2], I32)
        ustr = gsb("ustr", [128, 128], BF16); cumf = gsb("cumf", [128, 32]); cumb = gsb("cumb", [128, 32], BF16)

        dma("sp", ident.t[:], ident_d, [], [ident]); dma("sp", ustr.t[:], ustr_d, [], [ustr])
        mset("pool", cumf.t[:], 0.0, [cumf]); mset("pool", cumb.t[:], 0.0, [cumb])
        mset("pool", onesb.t[:], 1.0, [onesb]); mset("pool", onesf.t[:], 1.0, [onesf]); mset("pool", epst.t[:], EPS, [epst])

        with contextlib.ExitStack() as es:
            def sb(name, shape, dt=F32, nb=1):
                return T(es.enter_context(nc.sbuf_tensor("s_" + name, list(shape), dt)), nb)
            cc = sb("cc", [128, 8, 2]); scv = sb("scv", [128, 8, 2]); screp = sb("screp", [128, 8, 128])
            aw = [sb("aw%d" % i, [128, 8, 512]) for i in range(2)]
            adab_fm = sb("adab_fm", [128, 48]); adab_row = sb("adab_row", [1, 6 * D])
            modF = sb("modF", [128, 48, 2]); n1g = sb("n1g", [128, 8]); n2g = sb("n2g", [128, 8])
            lam4 = sb("lam4", [128, 256]); lt1 = sb("lt1", [128, 64]); lt2 = sb("lt2", [128, 64])
            ls1 = sb("ls1", [128, 1]); ls2 = sb("ls2", [128, 1]); subg = sb("subg", [128, 1])
            dma("sp", cc.t[:], cc_d, [], [cc]); dma("sp", adab_fm.t[:], adab_fm_d, [], [adab_fm])
            dma("sp", adab_row.t[:], adab_row_d, [], [adab_row])
            dma("sp", n1g.t[:], n1g_d, [], [n1g]); dma("sp", n2g.t[:], n2g_d, [], [n2g])
            dma("sp", lam4.t[:], lam4_d.partition_broadcast(128), [], [lam4]); dma("sp", subg.t[:], subg_d, [], [subg])
            act(scv.t[:], cc.t[:], AF.Silu, [cc], [scv])
            for k in range(8):
                cp("dve", screp.t[:, k, :], scv.t[:, k, 0:1].broadcast_to([128, 128]), [scv], [screp])
            adaw_v = adaw_d.rearrange("(k p) n -> p k n", p=128)
            pmod = PS[0]
            for g in range(12):
                a = aw[g % 2]
                dma("sp", a.t[:], adaw_v[:, :, g * 512:(g + 1) * 512], [], [a])
                for c in range(4):
                    ch = g * 4 + c
                    for k in range(8):
                        mm(pmod.t[:, 2 * ch:2 * ch + 2], a.t[:, k, c * 128:(c + 1) * 128], scv.t[:, k, :], k == 0, k == 7, [a, scv], [pmod])
                if g in (4, 5, 6, 7, 8, 9, 10, 11):
                    pr = PS[1 + (g % 2)]
                    for k in range(8):
                        mm(pr.t[:], screp.t[:, k, :], a.t[:, k, :], k == 0, False, [a, screp], [pr])
                    mm(pr.t[:], onesf.t[0:1, :], adab_row.t[0:1, g * 512:(g + 1) * 512], False, True, [onesf, adab_row], [pr])
                    dst = {2: g1row, 3: B2row, 4: A2row, 5: g2row}[g // 2]
                    half = g % 2
                    cp("dve", dst.t[:, half * 512:(half + 1) * 512], pr.t[:], [pr], [dst])
            tt("dve", modF.t[:], pmod.t[:, 0:96].rearrange("p (c j) -> p c j", j=2),
               adab_fm.t[:].unsqueeze(2).broadcast_to([128, 48, 2]), ALU.add, [pmod, adab_fm], [modF])
            stt("dve", A1.t[:], modF.t[:, 8:16, 0], 1.0, n1g.t[:], ALU.add, ALU.mult, [modF, n1g], [A1])
            stt("dve", A1c.t[:], modF.t[:, 8:16, 1], 1.0, n1g.t[:], ALU.add, ALU.mult, [modF, n1g], [A1c])
            stt("dve", A2.t[:], modF.t[:, 32:40, 0], 1.0, n2g.t[:], ALU.add, ALU.mult, [modF, n2g], [A2])
            cp("dve", B1.t[:], modF.t[:, 0:8, 0], [modF], [B1]); cp("dve", B1c.t[:], modF.t[:, 0:8, 1], [modF], [B1c])
            cp("dve", B2.t[:], modF.t[:, 24:32, 0], [modF], [B2])
            n2grow = sb("n2grow", [128, D])
            dma("sp", n2grow.t[:], n2grow_d.partition_broadcast(128), [], [n2grow])
            stt("dve", A2row.t[:], A2row.t[:], 1.0, n2grow.t[:], ALU.add, ALU.mult, [A2row, n2grow], [A2row])
            tt("dve", lt1.t[:], lam4.t[:, 0:64], lam4.t[:, 64:128], ALU.mult, [lam4], [lt1])
            tt("dve", lt2.t[:], lam4.t[:, 128:192], lam4.t[:, 192:256], ALU.mult, [lam4], [lt2])
            S_.op("dve", lambda e: e.tensor_reduce(ls1.t[:], lt1.t[:], AX.X, ALU.add), [lt1.b], [ls1.b])
            S_.op("dve", lambda e: e.tensor_reduce(ls2.t[:], lt2.t[:], AX.X, ALU.add), [lt2.b], [ls2.b])
            act(ls1.t[:], ls1.t[:], AF.Exp, [ls1], [ls1]); act(ls2.t[:], ls2.t[:], AF.Exp, [ls2], [ls2])
            tt("dve", neglam.t[:], ls2.t[:], ls1.t[:], ALU.subtract, [ls1, ls2], [neglam])
            ts("dve", neglam.t[:], neglam.t[:], -0.2, None, ALU.add, None, [neglam], [neglam])
            ts("dve", gsub.t[:], subg.t[:], 0.8, None, ALU.mult, None, [subg], [gsub])
            S_.barrier()

        def norm_transpose(es, tag):
            def sb(name, shape, dt=F32, nb=1):
                return T(es.enter_context(nc.sbuf_tensor("s_" + tag + name, list(shape), dt)), nb)
            st = dict(junk=sb("junk", [128, D], BF16), ss=[sb("ss%d" % i, [128, 1]) for i in range(2)],
                      xs=[sb("xs%d" % i, [128, D], BF16) for i in range(2)], n=0)
            return st

        def do_norm_transpose(st, xin, A, Bv, psi, dst_fn, dst_bufs, defer=False):
            i = st["n"]; st["n"] += 1
            ss = st["ss"][i % 2]; xs = st["xs"][i % 2]
            mset("pool", ss.t[:], 0.0, [ss])
            act(st["junk"].t[:], xin.t[:], AF.Square, [xin], [st["junk"], ss], accum_out=ss.t[:])
            act(ss.t[:], ss.t[:], AF.Sqrt, [ss, epst], [ss], scale=1.0 / D, bias=epst.t[:])
            recip(ss.t[:], ss.t[:], [ss], [ss])
            ts("dve", xs.t[:], xin.t[:], ss.t[:, 0:1], None, ALU.mult, None, [xin, ss], [xs])
            pb = psbf(psi)
            for k in range(8):
                tr(pb[:, k * 128:(k + 1) * 128], xs.t[:, k * 128:(k + 1) * 128], ident.t[:], [xs, ident], [PS[psi]])
            def evac():
                for k in range(8):
                    act(dst_fn(k), pb[:, k * 128:(k + 1) * 128], AF.Identity, [PS[psi], A, Bv], dst_bufs,
                        scale=A.t[:, k:k + 1], bias=Bv.t[:, k:k + 1])
            if defer:
                return ss, evac
            evac()
            return ss

        with contextlib.ExitStack() as es:
            def sb(name, shape, dt=F32, nb=1):
                return T(es.enter_context(nc.sbuf_tensor("s_" + name, list(shape), dt)), nb)
            st = norm_transpose(es, "p1")
            xin = [sb("p1xin%d" % i, [128, D]) for i in range(3)]
            hblk = [sb("p1hb%d" % i, [128, 8, BLK], BF16) for i in range(2)]
            n = 0
            pend_ev = None
            hb = hblk[0]
            for i in range(NC):
                xi = xin[n % 3]
                dma("sp", xi.t[:], ctx_d[i * 128:(i + 1) * 128, :], [], [xi])
                _, ev_ = do_norm_transpose(st, xi, A1c, B1c, n % 2, lambda k, i=i, hb=hb: hb.t[:, k, i * 128:(i + 1) * 128], [hb], defer=True)
                if pend_ev is not None:
                    pend_ev()
                pend_ev = ev_
                n += 1
            pend_ev(); pend_ev = None
            dma("sp", hTc_d, hblk[0].t[:, :, 0:CTXL], [hblk[0]], [B_hTc])
            for b in range(NB):
                hb = hblk[(b + 1) % 2]
                for s in range(TPB):
                    i = b * TPB + s
                    xi = xin[n % 3]
                    dma("sp", xi.t[:], x_d[i * 128:(i + 1) * 128, :], [], [xi])
                    _, ev_ = do_norm_transpose(st, xi, A1, B1, n % 2, lambda k, s=s, hb=hb: hb.t[:, k, s * 128:(s + 1) * 128], [hb], defer=True)
                    if pend_ev is not None:
                        pend_ev()
                    pend_ev = ev_
                    n += 1
                pend_ev(); pend_ev = None
                dma("sp", hT_d[:, :, b * BLK:(b + 1) * BLK], hb.t[:], [hb], [B_hT[b]])
            S_.barrier()

        with contextlib.ExitStack() as es2:
            def sb2(name, shape, dt=F32, nb=1):
                return T(es2.enter_context(nc.sbuf_tensor("s_" + name, list(shape), dt)), nb)
            QT = sb2("QT", [128, 4, S], BF16, nb=NT); KT = sb2("KT", [128, 4, TK], BF16, nb=NKT); V = sb2("V", [128, NKT, 512], BF16, nb=NKT)
            with contextlib.ExitStack() as es:
                def sb(name, shape, dt=F32, nb=1):
                    return T(es.enter_context(nc.sbuf_tensor("s_" + name, list(shape), dt)), nb)
                wqkv = sb("wqkv", [128, 8, 1536], BF16); brow = sb("brow", [1, 1536], BF16)
                gq = sb("gq", [128, 64]); gk = sb("gk", [128, 64])
                rcs = [sb("rc%d" % i, [128, 64]) for i in range(3)]; rss = [sb("rs%d" % i, [128, 64]) for i in range(3)]
                gtab = [[sb("gtab%d_%d" % (i, j), [128, 64]) for j in range(4)] for i in range(3)]
                hblk = [sb("p2hb%d" % i, [128, 8, BLK], BF16) for i in range(2)]
                sqt = [sb("sqt%d" % i, [128, 512]) for i in range(2)]
                ssq = [sb("ssq%d" % i, [128, 8]) for i in range(2)]
                qn = [sb("qn%d" % i, [128, 512]) for i in range(2)]
                qg2 = [sb("qg2%d" % i, [128, 512]) for i in range(2)]
                ru = [sb("ru%d" % i, [128, 512]) for i in range(2)]
                rw_ = [sb("rw%d" % i, [128, 512]) for i in range(2)]
                qr = [sb("qr%d" % i, [128, 512], BF16) for i in range(4)]
                for c in range(3):
                    dma("pool", wqkv.t[:, :, c * 512:(c + 1) * 512], w_in_v[:, :, c * 512:(c + 1) * 512], [], [wqkv])
                dma("pool", brow.t[:], bin_row_d[0:1, 0:1536], [], [brow])
                dma("sp", gq.t[:], qg_d.partition_broadcast(128), [], [gq]); dma("sp", gk.t[:], kg_d.partition_broadcast(128), [], [gk])
                cnt = {"n": 0}

                def qknorm(ps, gt, xt, dstT, dbuf, dcol, psT, pcol, rc=None, rs_=None):
                    i = cnt["n"]; cnt["n"] += 1
                    sq = sqt[i % 2]; sm = ssq[i % 2]; q1 = qn[i % 2]; q2 = qg2[i % 2]; u_ = ru[i % 2]; w_ = rw_[i % 2]; o_ = qr[i % 4]
                    def part_a():
                        act(sq.t[:], ps.t[:], AF.Square, [ps], [sq])
                        yield
                        S_.op("dve", lambda e: e.tensor_reduce(sm.t[:], sq.t[:].rearrange("p (g d) -> p g d", d=64), AX.X, ALU.add), [sq.b], [sm.b])
                        yield
                        act(sm.t[:], sm.t[:], AF.Sqrt, [sm, epst], [sm], scale=1.0 / 64, bias=epst.t[:])
                        yield
                        recip(sm.t[:], sm.t[:], [sm], [sm])
                        yield
                        tt("dve", q1.t[:].rearrange("p (g d) -> p g d", d=64), ps.t[:].rearrange("p (g d) -> p g d", d=64),
                           sm.t[:].unsqueeze(2).broadcast_to([128, 8, 64]), ALU.mult, [ps, sm], [q1])
                        yield
                        if xt is None:
                            tt("pool", o_.t[:].rearrange("p (g d) -> p g d", d=64), q1.t[:].rearrange("p (g d) -> p g d", d=64),
                               gt.t[:].unsqueeze(1).broadcast_to([128, 8, 64]), ALU.mult, [q1, gt], [o_])
                            yield
                        else:
                            tt("pool", u_.t[:].rearrange("p (g d) -> p g d", d=64), q1.t[:].rearrange("p (g d) -> p g d", d=64),
                               rc.t[:].unsqueeze(1).broadcast_to([128, 8, 64]), ALU.mult, [q1, rc], [u_])
                            yield
                            tt("dve", w_.t[:].rearrange("p (g d) -> p g d", d=64), q1.t[:].rearrange("p (g d) -> p g d", d=64),
                               rs_.t[:].unsqueeze(1).broadcast_to([128, 8, 64]), ALU.mult, [q1, rs_], [w_])
                            yield
                            u4 = u_.t[:].rearrange("p (a h f) -> p a h f", h=2, f=16)
                            w4 = w_.t[:].rearrange("p (a h f) -> p a h f", h=2, f=16)
                            o4 = o_.t[:].rearrange("p (a h f) -> p a h f", h=2, f=16)
                            tt("dve", o4[:, :, 0, :], u4[:, :, 0, :], w4[:, :, 1, :], ALU.add, [u_, w_], [o_])
                            yield
                            tt("dve", o4[:, :, 1, :], u4[:, :, 1, :], w4[:, :, 0, :], ALU.add, [u_, w_], [o_])
                            yield
                    def part_b():
                        pb = psbf(psT)
                        for h in range(4):
                            tr(pb[:, pcol + h * 128: pcol + (h + 1) * 128], o_.t[:, h * 128:(h + 1) * 128], ident.t[:], [o_, ident], [PS[psT]])
                        cp("dve", dstT.t[:, :, dcol:dcol + 128], pb[:, pcol:pcol + 512].rearrange("p (h t) -> p h t", t=128), [PS[psT]], [dbuf])
                    return part_a(), part_b

                tno = 0
                pend_b = []
                blocks = [("c", 0, NC)] + [("x", b, TPB) for b in range(NB)]
                for bi, (kind, b, ntl) in enumerate(blocks):
                    hb = hblk[bi % 2]
                    if kind == "c":
                        dma("sp", hb.t[:, :, 0:CTXL], hTc_d, [B_hTc], [hb])
                    else:
                        dma("sp", hb.t[:], hT_d[:, :, b * BLK:(b + 1) * BLK], [B_hT[b]], [hb])
                    for s in range(ntl):
                        kt = s if kind == "c" else NC + b * TPB + s
                        xt = None if kind == "c" else b * TPB + s
                        st3 = (tno % 2) * 3
                        psT = 6 + (tno % 2)
                        tno += 1
                        lh = lambda k: hb.t[:, k, s * 128:(s + 1) * 128]
                        for c in range(3):
                            if c == 0 and kind == "c":
                                continue
                            pp = PS[st3 + c]
                            for k in range(8):
                                mm(pp.t[:], lh(k), wqkv.t[:, k, c * 512:(c + 1) * 512], k == 0, False, [hb, wqkv], [pp])
                            mm(pp.t[:], onesb.t[0:1, :], brow.t[0:1, c * 512:(c + 1) * 512], False, True, [onesb, brow], [pp])
                        act(V.t[:, kt, :], PS[st3 + 2].t[:], AF.Copy, [PS[st3 + 2]], [V.bs[kt]])
                        rc = rs_ = None
                        if kind == "x":
                            rc = rcs[xt % 3]; rs_ = rss[xt % 3]
                            dma("sp", rc.t[:], ropec_d[xt * 128:(xt + 1) * 128, :], [], [rc])
                            dma("sp", rs_.t[:], ropes_d[xt * 128:(xt + 1) * 128, :], [], [rs_])
                            gt4 = gtab[xt % 3]
                            tt("pool", gt4[0].t[:], rc.t[:], gq.t[:], ALU.mult, [rc, gq], [gt4[0]]); tt("pool", gt4[1].t[:], rs_.t[:], gq.t[:], ALU.mult, [rs_, gq], [gt4[1]])
                            tt("pool", gt4[2].t[:], rc.t[:], gk.t[:], ALU.mult, [rc, gk], [gt4[2]]); tt("pool", gt4[3].t[:], rs_.t[:], gk.t[:], ALU.mult, [rs_, gk], [gt4[3]])
                            pairs = [qknorm(PS[st3 + 0], gq, xt, QT, QT.bs[xt], xt * 128, psT, 0, gt4[0], gt4[1]),
                                     qknorm(PS[st3 + 1], gk, xt, KT, KT.bs[kt], kt * 128, psT, 512, gt4[2], gt4[3])]
                        else:
                            pairs = [qknorm(PS[st3 + 1], gk, xt, KT, KT.bs[kt], kt * 128, psT, 512, None, None)]
                        gens = [p_[0] for p_ in pairs]
                        while gens:
                            gens = [g_ for g_ in gens if next(g_, "done") != "done"]
                        newb = [p_[1] for p_ in pairs]
                        for fb_ in pend_b:
                            fb_()
                        pend_b = newb
                for fb_ in pend_b:
                    fb_()
                S_.barrier()

            with contextlib.ExitStack() as es:
                def sb(name, shape, dt=F32, nb=1):
                    return T(es.enter_context(nc.sbuf_tensor("s_" + name, list(shape), dt)), nb)
                pt2 = [sb("pt2_%d" % i, [128, 2, 512], BF16) for i in range(3)]
                r0 = sb("r0", [128, BLK]); r1 = sb("r1", [128, BLK]); t0 = sb("t0", [128, BLK]); t1 = sb("t1", [128, BLK])
                dd = sb("dd", [128, BLK]); dsq = sb("dsq", [128, BLK]); rsd = sb("rsd", [128, BLK])
                oat = [sb("oat%d" % i, [128, BLK], BF16) for i in range(2)]
                o_acc = [PS[0], PS[1]]; s_acc = [PS[2], PS[3]]
                scp = [[PS[4], PS[5]], [PS[6], PS[7]]]
                zt = sb("zt", [128, 2 * D], BF16)
                mset("pool", zt.t[:], 0.0, [zt])
                Xs_z = Xs_d.rearrange("(c p r) d -> c p (r d)", p=128, r=2)
                for c_ in range(NZ):
                    dma("pool", Xs_z[c_], zt.t[:], [zt], [B_Xs[c_]])
                for e_ in range(NE):
                    w1src = ew1_d[e_].rearrange("(k p) n -> p k n", p=128)
                    w1dst = W1b_d[e_ * 128:(e_ + 1) * 128, :].rearrange("p (k n) -> p k n", k=8)
                    for k0 in (0, 4):
                        dma("pool", w1dst[:, k0:k0 + 4, :], w1src[:, k0:k0 + 4, :], [], [])
                    w2src = ew2_d[e_].rearrange("(k p) n -> p k n", p=128)
                    w2dst = W2b_d[e_ * 128:(e_ + 1) * 128, :].rearrange("p (k n) -> p k n", k=8)
                    dma("pool", w2dst, w2src, [], [])
                its = [(h, qb, kt) for h in range(4) for qb in range(NB) for kt in range(NKT)]

                def scores(n_):
                    h, qb, kt = its[n_]
                    sp_ = scp[n_ % 2]
                    for m in range(2):
                        mm(sp_[m].t[:, 0:BLK], KT.t[m * 64:(m + 1) * 64, h, kt * 128:(kt + 1) * 128],
                           QT.t[m * 64:(m + 1) * 64, h, qb * BLK:(qb + 1) * BLK], True, True,
                           [KT.bs[kt]] + [QT.bs[qb * TPB + j] for j in range(TPB)], [sp_[m]])
                o0s = sb("o0s", [128, BLK]); o1s = sb("o1s", [128, BLK]); ssb = sb("ssb", [128, BLK]); w32 = sb("w32", [128, 128])
                mset("dve", w32.t[:], 1.0 / 32, [w32])
                sbank = PS[2]; fbank = PS[3]

                def finalize_gen(h, qb):
                    recip(ssb.t[0:64, :], ssb.t[0:64, :], [ssb], [ssb])
                    mm(fbank.t[:, 0:BLK], w32.t[0:32, :], ssb.t[0:32, :], True, True, [w32, ssb], [fbank])
                    yield
                    tt("dve", t0.t[:], o0s.t[:], fbank.t[:, 0:BLK], ALU.mult, [o0s, fbank], [t0])
                    mm(fbank.t[:, 0:BLK], w32.t[32:64, :], ssb.t[32:64, :], True, True, [w32, ssb], [fbank])
                    yield
                    tt("dve", t1.t[:], o1s.t[:], fbank.t[:, 0:BLK], ALU.mult, [o1s, fbank], [t1])
                    stt("dve", dd.t[:], t1.t[:], neglam.t[:, 0:1], t0.t[:], ALU.mult, ALU.add, [t0, t1, neglam], [dd])
                    tt("dve", dsq.t[:], dd.t[:], dd.t[:], ALU.mult, [dd], [dsq])
                    yield
                    mm(fbank.t[:, 0:BLK], onesf.t[:], dsq.t[:], True, True, [onesf, dsq], [fbank])
                    yield
                    act(rsd.t[:], fbank.t[:, 0:BLK], AF.Sqrt, [fbank, epst], [rsd], scale=1.0 / 128, bias=epst.t[:])
                    recip(rsd.t[:], rsd.t[:], [rsd], [rsd])
                    oo = oat[(h * NB + qb) % 2]
                    stt("dve", oo.t[:], dd.t[:], gsub.t[:, 0:1], rsd.t[:], ALU.mult, ALU.mult, [dd, gsub, rsd], [oo])
                    dma("sp", oaT_d[:, h, qb * BLK:(qb + 1) * BLK], oo.t[:], [oo], [B_oaT[qb]])

                pending = []
                scores(0)
                for it in range(len(its)):
                    h, qb, kt = its[it]
                    if it + 1 < len(its):
                        scores(it + 1)
                    sp_ = scp[it % 2]
                    p2 = pt2[it % 3]
                    bank0 = 4 + 2 * (it % 2)
                    act(p2.t[:, :, 0:BLK], psall[:, bank0 * 512:(bank0 + 2) * 512].rearrange("p (m q) -> p m q", m=2)[:, :, 0:BLK], AF.Exp,
                        [sp_[0], sp_[1]], [p2], scale=0.125)
                    for m in range(2):
                        mm(o_acc[m].t[:, 0:BLK], V.t[:, kt, h * 128:(h + 1) * 128], p2.t[:, m, 0:BLK], kt == 0, kt == NKT - 1, [V.bs[kt], p2], [o_acc[m]])
                    for m in range(2):
                        mm(sbank.t[32 * m:32 * (m + 1), 0:BLK], onesb.t[:, 32 * m:32 * (m + 1)], p2.t[:, m, 0:BLK], kt == 0, kt == NKT - 1,
                           [onesb, p2], [sbank], tp=(0, 32 * m))
                    if pending and kt >= 1:
                        if next(pending[0], "done") == "done":
                            pending.pop(0)
                    if kt == NKT - 1:
                        S_.op("act", lambda e: e.copy(o0s.t[:], o_acc[0].t[:, 0:BLK]), [o_acc[0].b], [o0s.b])
                        cp("dve", o1s.t[:], o_acc[1].t[:, 0:BLK], [o_acc[1]], [o1s])
                        cp("dve", ssb.t[0:64, :], sbank.t[0:64, 0:BLK], [sbank], [ssb])
                        pending.append(finalize_gen(h, qb))
                for g_ in pending:
                    for _ in g_:
                        pass
                S_.barrier()

        with contextlib.ExitStack() as esh:
            def sbh(name, shape, dt=F32, nb=1):
                return T(esh.enter_context(nc.sbuf_tensor("s_" + name, list(shape), dt)), nb)
            u_tm = sbh("u_tm", [128, NT, 512], BF16, nb=NT)
            with contextlib.ExitStack() as es:
                def sb(name, shape, dt=F32, nb=1):
                    return T(es.enter_context(nc.sbuf_tensor("s_" + name, list(shape), dt)), nb)
                hring = [sb("h0hb%d" % i, [128, 8, BLK], BF16) for i in range(2)]; whY = sb("whY", [128, 8, 1536], BF16)
                bhy = sb("bhy", [128, 40]); cw = sb("cw", [128, 12, 3]); cb = sb("cb", [128, 12])
                ppads = [[sb("ppad%d_%d" % (i, c), [128, S + 2], BF16) for c in range(3)] for i in range(2)]
                zf = sb("zf", [128, S])
                zX = sb("zX", [128, S], BF16); zA = sb("zA", [128, S], BF16); zB = sb("zB", [128, S], BF16); uTb = sb("uTb", [128, S], BF16)
                for c in range(3):
                    dma("pool", whY.t[:, :, c * 512:(c + 1) * 512], w_in_v[:, :, 1536 + c * 512:1536 + (c + 1) * 512], [], [whY])
                dma("sp", bhy.t[:], bin_fm_d, [], [bhy]); dma("sp", cw.t[:], convw_d, [], [cw]); dma("sp", cb.t[:], convb_d, [], [cb])
                for i in range(2):
                    for c in range(3):
                        mset("pool", ppads[i][c].t[:, 0:1], 0.0, [ppads[i][c]]); mset("pool", ppads[i][c].t[:, S + 1:S + 2], 0.0, [ppads[i][c]])
                zn = 0
                for j in range(4):
                    pset = ppads[j % 2]
                    for b in range(NB):
                        hb = hring[b % 2]
                        dma("sp", hb.t[:], hT_d[:, :, b * BLK:(b + 1) * BLK], [B_hT[b]], [hb])
                        for c3 in range(3):
                            ch = c3 * 4 + j
                            pp = PS[zn % 4]; zn += 1
                            for k in range(8):
                                mm(pp.t[:, 0:BLK], whY.t[:, k, ch * 128:(ch + 1) * 128], hb.t[:, k, :], k == 0, k == 7, [whY, hb], [pp])
                            act(pset[c3].t[:, 1 + b * BLK:1 + (b + 1) * BLK], pp.t[:, 0:BLK], AF.Identity, [pp, bhy], [pset[c3]], bias=bhy.t[:, 12 + ch:13 + ch])
                    for c3, out in ((0, zX), (1, zA), (2, zB)):
                        ch = c3 * 4 + j
                        pp_ = pset[c3]
                        ts("dve", zf.t[:], pp_.t[:, 0:S], cw.t[:, ch, 0:1], cb.t[:, ch:ch + 1], ALU.mult, ALU.add, [pp_, cw, cb], [zf])
                        stt("dve", zf.t[:], pp_.t[:, 1:S + 1], cw.t[:, ch, 1:2], zf.t[:], ALU.mult, ALU.add, [pp_, cw, zf], [zf])
                        stt("dve", out.t[:], pp_.t[:, 2:S + 2], cw.t[:, ch, 2:3], zf.t[:], ALU.mult, ALU.add, [pp_, cw, zf], [out])
                    dma("sp", x0c_d[:, j, :], zX.t[:], [zX], [B_x0c[j]])
                    tt("pool", uTb.t[:], zA.t[:], zB.t[:], ALU.mult, [zA, zB], [uTb])
                    dma("sp", uT_d[:, j, :], uTb.t[:], [uTb], [B_uT[j]])
                    for t0_ in range(0, NT, 8):
                        nt_ = min(8, NT - t0_)
                        psi = 4 + ((j * NT + t0_) // 8) % 2
                        pb = psbf(psi)
                        for t_ in range(nt_):
                            tr(pb[:, t_ * 128:(t_ + 1) * 128], uTb.t[:, (t0_ + t_) * 128:(t0_ + t_ + 1) * 128], ident.t[:], [uTb, ident], [PS[psi]])
                        cp("dve", u_tm.t[:, t0_:t0_ + nt_, j * 128:(j + 1) * 128], pb[:, 0:nt_ * 128].rearrange("p (t c) -> p t c", c=128),
                           [PS[psi]], [u_tm.bs[t] for t in range(t0_, t0_ + nt_)])
                S_.barrier()

            with contextlib.ExitStack() as esk:
                ksum = T(esk.enter_context(nc.sbuf_tensor("s_ksum", [128, NT, 512], BF16)), NT)
                kdiff = T(esk.enter_context(nc.sbuf_tensor("s_kdiff", [128, NT, 512], BF16)), NT)
                with contextlib.ExitStack() as es:
                    def sb(name, shape, dt=F32, nb=1):
                        return T(es.enter_context(nc.sbuf_tensor("s_" + name, list(shape), dt)), nb)
                    zT = sb("zT", [33, S]); fw1 = sb("fw1", [33, 64]); fw2 = sb("fw2", [64, 64]); fw3 = sb("fw3", [64, 64]); fw4 = sb("fw4", [64, 1024])
                    fvec = sb("fvec", [64, 4]); trow = sb("trow", [1, S]); drow = sb("drow", [1, 512])
                    H3 = sb("H3", [64, S])
                    arg = sb("arg", [64, BLK]); ai = sb("ai", [64, BLK], I32); af = sb("af", [64, BLK]); hh = [sb("hh%d" % i, [64, BLK]) for i in range(2)]
                    dec = sb("dec", [128, 512]); kf = sb("kf", [128, 512]); kb = sb("kb", [128, 512])
                    for t_, d_ in [(zT, zT_d), (fw1, fw1_d), (fw2, fw2_d), (fw3, fw3_d), (fw4, fw4_d), (fvec, fvec_d), (trow, trow_d), (drow, drow_d)]:
                        dma("sp", t_.t[:], d_, [], [t_])

                    def sin_layer(ps, li, out_ap, out_t):
                        ts("dve", arg.t[:], ps.t[0:64, 0:BLK], fvec.t[:, li:li + 1], fvec.t[:, 3:4], ALU.add, ALU.mult, [ps, fvec], [arg])
                        ts("dve", arg.t[:], arg.t[:], 1.0 / (2 * PI), 16.0, ALU.mult, ALU.add, [arg], [arg])
                        cp("dve", ai.t[:], arg.t[:], [arg], [ai])
                        cp("dve", af.t[:], ai.t[:], [ai], [af])
                        tt("dve", arg.t[:], arg.t[:], af.t[:], ALU.subtract, [arg, af], [arg])
                        ts("dve", af.t[:], arg.t[:], 0.5, None, ALU.is_gt, None, [arg], [af])
                        tt("dve", arg.t[:], arg.t[:], af.t[:], ALU.subtract, [arg, af], [arg])
                        act(out_ap, arg.t[:], AF.Sin, [arg], [out_t], scale=2 * PI)

                    for b in range(NB):
                        sl = slice(b * BLK, (b + 1) * BLK)
                        mm(PS[0].t[0:64, 0:BLK], fw1.t[:], zT.t[:, sl], True, True, [fw1, zT], [PS[0]])
                        sin_layer(PS[0], 0, hh[0].t[:], hh[0])
                        mm(PS[1].t[0:64, 0:BLK], fw2.t[:], hh[0].t[:], True, True, [fw2, hh[0]], [PS[1]])
                        sin_layer(PS[1], 1, hh[1].t[:], hh[1])
                        mm(PS[2].t[0:64, 0:BLK], fw3.t[:], hh[1].t[:], True, True, [fw3, hh[1]], [PS[2]])
                        sin_layer(PS[2], 2, H3.t[:, sl], H3)
                    for lt in range(NT):
                        pf = PS[(lt % 2) * 3]; pb_ = PS[(lt % 2) * 3 + 1]; pd = PS[(lt % 2) * 3 + 2]
                        mm(pf.t[:], H3.t[:, lt * 128:(lt + 1) * 128], fw4.t[:, 0:512], True, True, [H3, fw4], [pf])
                        mm(pb_.t[:], H3.t[:, lt * 128:(lt + 1) * 128], fw4.t[:, 512:1024], True, True, [H3, fw4], [pb_])
                        mm(pd.t[:], trow.t[0:1, lt * 128:(lt + 1) * 128], drow.t[0:1, :], True, True, [trow, drow], [pd])
                        act(dec.t[:], pd.t[:], AF.Exp, [pd], [dec], scale=-1.0)
                        stt("dve", kf.t[:], pf.t[:], 2.0 / N2, dec.t[:], ALU.mult, ALU.mult, [pf, dec], [kf])
                        stt("dve", kb.t[:], pb_.t[:], 2.0 / N2, dec.t[:], ALU.mult, ALU.mult, [pb_, dec], [kb])
                        if lt == 0:
                            mset("dve", kb.t[0:1, :], 0.0, [kb])
                        tt("pool", ksum.t[:, lt, :], kf.t[:], kb.t[:], ALU.add, [kf, kb], [ksum.bs[lt]])
                        tt("pool", kdiff.t[:, lt, :], kb.t[:], kf.t[:], ALU.subtract, [kf, kb], [kdiff.bs[lt]])
                    S_.barrier()

                with contextlib.ExitStack() as es:
                    def sb(name, shape, dt=F32, nb=1):
                        return T(es.enter_context(nc.sbuf_tensor("s_" + name, list(shape), dt)), nb)
                    cf = [sb("cf%d" % i, [128, NT, 128], BF16) for i in range(2)]; sf = [sb("sf%d" % i, [128, NT, 128], BF16) for i in range(2)]
                    kre = sb("kre", [128, 512]); kim = sb("kim", [128, 512]); m1 = sb("m1", [128, 512]); m2 = sb("m2", [128, 512])
                    yt = [sb("yt%d" % i, [128, 2, 512], BF16) for i in range(2)]
                    for kc in range(NKC):
                        c_ = cf[kc % 2]; s_ = sf[kc % 2]
                        if kc == 0:
                            dma("sp", c_.t[:], Cf_d[0], [], [c_]); dma("sp", s_.t[:], Sf_d[0], [], [s_])
                        if kc + 1 < NKC:
                            cn_ = cf[(kc + 1) % 2]; sn_ = sf[(kc + 1) % 2]
                            dma("sp", cn_.t[:], Cf_d[kc + 1], [], [cn_]); dma("sp", sn_.t[:], Sf_d[kc + 1], [], [sn_])
                        pa = PS[(kc % 2) * 4: (kc % 2) * 4 + 4]
                        for t_ in range(NT):
                            st_, sp2 = (t_ == 0), (t_ == NT - 1)
                            mm(pa[0].t[:], c_.t[:, t_, :], u_tm.t[:, t_, :], st_, sp2, [c_, u_tm.bs[t_]], [pa[0]])
                            mm(pa[1].t[:], s_.t[:, t_, :], u_tm.t[:, t_, :], st_, sp2, [s_, u_tm.bs[t_]], [pa[1]])
                            mm(pa[2].t[:], c_.t[:, t_, :], ksum.t[:, t_, :], st_, sp2, [c_, ksum.bs[t_]], [pa[2]])
                            mm(pa[3].t[:], s_.t[:, t_, :], kdiff.t[:, t_, :], st_, sp2, [s_, kdiff.bs[t_]], [pa[3]])
                        y_ = yt[kc % 2]
                        act(kre.t[:], pa[2].t[:], AF.Copy, [pa[2]], [kre]); act(kim.t[:], pa[3].t[:], AF.Copy, [pa[3]], [kim])
                        tt("dve", m1.t[:], pa[0].t[:], kre.t[:], ALU.mult, [pa[0], kre], [m1])
                        tt("dve", m2.t[:], pa[1].t[:], kim.t[:], ALU.mult, [pa[1], kim], [m2])
                        tt("pool", y_.t[:, 0, :], m1.t[:], m2.t[:], ALU.add, [m1, m2], [y_])
                        tt("dve", m1.t[:], pa[0].t[:], kim.t[:], ALU.mult, [pa[0], kim], [m1])
                        tt("dve", m2.t[:], pa[1].t[:], kre.t[:], ALU.mult, [pa[1], kre], [m2])
                        tt("pool", y_.t[:, 1, :], m1.t[:], m2.t[:], ALU.subtract, [m1, m2], [y_])
                        dma("sp", Y_d[:, :, kc, :], y_.t[:], [y_], [B_Y[kc]])
                    S_.barrier()

        with contextlib.ExitStack() as es:
            def sb(name, shape, dt=F32, nb=1):
                return T(es.enter_context(nc.sbuf_tensor("s_" + name, list(shape), dt)), nb)
            Yall = sb("Yall", [128, 2, NKC, 512], BF16, nb=NKC)
            ci = [sb("ci%d" % i, [128, KG, BLK], BF16) for i in range(3)]; si = [sb("si%d" % i, [128, KG, BLK], BF16) for i in range(3)]
            x0b = [sb("x0b%d" % i, [128, 4, BLK], BF16) for i in range(2)]; uTk = [sb("uTk%d" % i, [128, 4, BLK], BF16) for i in range(2)]
            obb = [sb("obb%d" % i, [128, 4, BLK], BF16) for i in range(2)]
            tmp = [sb("h2tmp%d" % i, [128, BLK]) for i in range(2)]; hbv = sb("hbv", [128, 4])
            dma("sp", hbv.t[:], hb_d, [], [hbv])
            def load_y(kg_):
                for kc_ in range(kg_ * KG, (kg_ + 1) * KG):
                    dma("sp", Yall.t[:, :, kc_, :], Y_d[:, :, kc_, :], [B_Y[kc_]], [Yall.bs[kc_]])
            load_y(0)
            n = 0
            for tb in range(NB):
                acc = PS[(tb % 2) * 4:(tb % 2) * 4 + 4]
                xb = x0b[tb % 2]; ub = uTk[tb % 2]; ob = obb[tb % 2]
                dma("sp", xb.t[:], x0c_d[:, :, tb * BLK:(tb + 1) * BLK], B_x0c, [xb])
                dma("sp", ub.t[:], uT_d[:, :, tb * BLK:(tb + 1) * BLK], B_uT, [ub])
                for kg in range(NKG):
                    c_ = ci[n % 3]; s_ = si[n % 3]; n += 1
                    dma("sp", c_.t[:], Ci_d[tb, :, kg * KG:(kg + 1) * KG, :], [], [c_])
                    dma("sp", s_.t[:], Si_d[tb, :, kg * KG:(kg + 1) * KG, :], [], [s_])
                    if tb == 0 and kg + 1 < NKG:
                        load_y(kg + 1)
                    for kl in range(KG):
                        kc = kg * KG + kl
                        for c4 in range(4):
                            mm(acc[c4].t[:, 0:BLK], Yall.t[:, 0, kc, c4 * 128:(c4 + 1) * 128], c_.t[:, kl, :], kc == 0, False, [Yall.bs[kc], c_], [acc[c4]])
                            mm(acc[c4].t[:, 0:BLK], Yall.t[:, 1, kc, c4 * 128:(c4 + 1) * 128], s_.t[:, kl, :], False, kc == NKC - 1, [Yall.bs[kc], s_], [acc[c4]])
                for c4 in range(4):
                    tm = tmp[c4 % 2]
                    stt("dve", tm.t[:], ub.t[:, c4, :], hbv.t[:, c4:c4 + 1], acc[c4].t[:, 0:BLK], ALU.mult, ALU.add, [ub, hbv, acc[c4]], [tm])
                    tt("pool", ob.t[:, c4, :], tm.t[:], xb.t[:, c4, :], ALU.mult, [tm, xb], [ob])
                dma("sp", obT_d[:, :, tb * BLK:(tb + 1) * BLK], ob.t[:], [ob], [B_obT[tb]])
            S_.barrier()

        with contextlib.ExitStack() as es:
            def sb(name, shape, dt=F32, nb=1):
                return T(es.enter_context(nc.sbuf_tensor("s_" + name, list(shape), dt)), nb)
            wg = sb("wg", [128, 8, 2048], BF16); wua = sb("wua", [128, 4, D], BF16); wub = sb("wub", [128, 4, D], BF16)
            wout = sb("wout", [128, 8, D], BF16); rwt = sb("rwt", [128, 8, 32], BF16); rbt = sb("rbt", [1, 32], BF16)
            bgf = sb("bgf", [128, 40])
            hblk = [sb("mhb%d" % i, [128, 8, BLK], BF16) for i in range(2)]
            oab = [sb("moa%d" % i, [128, 4, BLK], BF16) for i in range(2)]; obk = [sb("mob%d" % i, [128, 4, BLK], BF16) for i in range(2)]
            gas = [sb("gas%d" % i, [128, BLK]) for i in range(2)]; gbs = [sb("gbs%d" % i, [128, BLK]) for i in range(2)]
            mm1 = [sb("mm1%d" % i, [128, BLK]) for i in range(1)]; mm2 = [sb("mm2%d" % i, [128, BLK]) for i in range(1)]
            mTs = [sb("mT%d" % i, [128, 8, BLK], BF16, nb=8) for i in range(2)]
            xin = [sb("mxin%d" % i, [128, D]) for i in range(2)]; xn = [sb("mxn%d" % i, [128, D]) for i in range(2)]
            tg = sb("mtg", [128, 512])
            h2b = [sb("h2b%d" % i, [128, 8, BLK], BF16) for i in range(2)]
            msk = sb("msk", [128, 32]); mskb = sb("mskb", [128, 32], BF16); nmx = sb("nmx", [128, 1])
            htmp = sb("htmp", [128, D]); h2tm = [sb("h2tm%d" % i, [128, D], BF16) for i in range(1)]
            ex = sb("ex", [128, 32]); sm_ = sb("sm", [128, 1])
            st = norm_transpose(es, "m")
            for c in range(4):
                dma("pool", wg.t[:, :, c * 512:(c + 1) * 512], w_in_v[:, :, 3072 + c * 512:3072 + (c + 1) * 512], [], [wg])
            dma("pool", wua.t[:], wua_d.rearrange("(k p) n -> p k n", p=128), [], [wua])
            dma("pool", wub.t[:], wub_d.rearrange("(k p) n -> p k n", p=128), [], [wub])
            for c in range(2):
                dma("pool", wout.t[:, :, c * 512:(c + 1) * 512], wout_d.rearrange("(k p) n -> p k n", p=128)[:, :, c * 512:(c + 1) * 512], [], [wout])
            dma("pool", rwt.t[:], rw_d.rearrange("(k p) n -> p k n", p=128), [], [rwt]); dma("pool", rbt.t[:], rb_d, [], [rbt])
            dma("sp", bgf.t[:], bin_fm_d, [], [bgf])
            tno = 0

            def gates(tb):
                hb = hblk[tb % 2]; oa = oab[tb % 2]; ob = obk[tb % 2]; mT = mTs[tb % 2]
                dma("sp", hb.t[:], hT_d[:, :, tb * BLK:(tb + 1) * BLK], [B_hT[tb]], [hb])
                dma("sp", oa.t[:], oaT_d[:, :, tb * BLK:(tb + 1) * BLK], [B_oaT[tb]], [oa])
                dma("sp", ob.t[:], obT_d[:, :, tb * BLK:(tb + 1) * BLK], [B_obT[tb]], [ob])
                for j in range(8):
                    pga, pgb, pA, pB = PS[0], PS[1], PS[2], PS[3]
                    for k in range(8):
                        mm(pga.t[:, 0:BLK], wg.t[:, k, j * 128:(j + 1) * 128], hb.t[:, k, :], k == 0, k == 7, [wg, hb], [pga])
                    for k in range(8):
                        mm(pgb.t[:, 0:BLK], wg.t[:, k, 1024 + j * 128:1024 + (j + 1) * 128], hb.t[:, k, :], k == 0, k == 7, [wg, hb], [pgb])
                    for k in range(4):
                        mm(pA.t[:, 0:BLK], wua.t[:, k, j * 128:(j + 1) * 128], oa.t[:, k, :], k == 0, k == 3, [wua, oa], [pA])
                    for k in range(4):
                        mm(pB.t[:, 0:BLK], wub.t[:, k, j * 128:(j + 1) * 128], ob.t[:, k, :], k == 0, k == 3, [wub, ob], [pB])
                    ga_ = gas[j % 2]; gb_ = gbs[j % 2]; a1 = mm1[0]; a2 = mm2[0]
                    act(ga_.t[:], pga.t[:, 0:BLK], AF.Sigmoid, [pga, bgf], [ga_], bias=bgf.t[:, 24 + j:25 + j])
                    act(gb_.t[:], pgb.t[:, 0:BLK], AF.Sigmoid, [pgb, bgf], [gb_], bias=bgf.t[:, 32 + j:33 + j])
                    tt("dve", a1.t[:], pA.t[:, 0:BLK], ga_.t[:], ALU.mult, [pA, ga_], [a1])
                    tt("dve", a2.t[:], pB.t[:, 0:BLK], gb_.t[:], ALU.mult, [pB, gb_], [a2])
                    tt("pool", mT.t[:, j, :], a1.t[:], a2.t[:], ALU.add, [a1, a2], [mT.bs[j]])

            def tiles(tb):
                nonlocal tno
                h2 = h2b[tb % 2]; mT = mTs[tb % 2]
                for s in range(TPB):
                    i = tb * TPB + s
                    xi = xin[tno % 2]; xo = xn[tno % 2]; tno += 1
                    dma("sp", xi.t[:], x_d[i * 128:(i + 1) * 128, :], [], [xi])
                    for half in range(2):
                        po = PS[4 + half]
                        for j in range(8):
                            mm(po.t[:], mT.t[:, j, s * 128:(s + 1) * 128], wout.t[:, j, half * 512:(half + 1) * 512], j == 0, j == 7, [mT.bs[j], wout], [po])
                        tt("dve", tg.t[:], po.t[:], g1row.t[:, half * 512:(half + 1) * 512], ALU.mult, [po, g1row], [tg])
                        tt("pool", xo.t[:, half * 512:(half + 1) * 512], tg.t[:], xi.t[:, half * 512:(half + 1) * 512], ALU.add, [tg, xi], [xo])
                    dma("sp", y_d[i * 128:(i + 1) * 128, :], xo.t[:], [xo], [B_y[i]])
                    ss_t = do_norm_transpose(st, xo, A2, B2, 6, lambda k, s=s, h2=h2: h2.t[:, k, s * 128:(s + 1) * 128], [h2])
                    hm = h2tm[0]
                    stt("dve", htmp.t[:], xo.t[:], ss_t.t[:, 0:1], A2row.t[:], ALU.mult, ALU.mult, [xo, ss_t, A2row], [htmp])
                    tt("pool", hm.t[:], htmp.t[:], B2row.t[:], ALU.add, [htmp, B2row], [hm])
                    dma("sp", H2tm_d[i * 128:(i + 1) * 128, :], hm.t[:], [hm], [B_h2tm[i]])
                    pr = PS[7]
                    for k in range(8):
                        mm(pr.t[:, 0:32], h2.t[:, k, s * 128:(s + 1) * 128], rwt.t[:, k, :], k == 0, False, [h2, rwt], [pr])
                    mm(pr.t[:, 0:32], onesb.t[0:1, :], rbt.t[0:1, :], False, True, [onesb, rbt], [pr])
                    lgi = LG.t[:, i, :]
                    act(lgi, pr.t[:, 0:32], AF.Copy, [pr], [LG.bs[i]])
                    vmax8(MX.t[:, i, :], lgi, [LG.bs[i]], [MX.bs[i]])
                    ts("dve", msk.t[:], lgi, MX.t[:, i, 3:4], None, ALU.is_ge, None, [LG.bs[i], MX.bs[i]], [msk])
                    cp("dve", mskb.t[:], msk.t[:], [msk], [mskb])
                    ts("dve", nmx.t[:], MX.t[:, i, 0:1], -1.0, None, ALU.mult, None, [MX.bs[i]], [nmx])
                    act(ex.t[:], lgi, AF.Exp, [LG.bs[i], nmx], [ex], bias=nmx.t[:, 0:1])
                    tt("dve", ex.t[:], ex.t[:], msk.t[:], ALU.mult, [ex, msk], [ex])
                    red(sm_.t[:], ex.t[:], [ex], [sm_])
                    recip(sm_.t[:], sm_.t[:], [sm_], [sm_])
                    ts("dve", G.t[:, i, :], ex.t[:], sm_.t[:, 0:1], None, ALU.mult, None, [ex, sm_], [G.bs[i]])
                    mm(pr.t[:, 32:64], ustr.t[:], mskb.t[:], True, False, [ustr, mskb], [pr])
                    mm(pr.t[:, 32:64], onesb.t[:], cumb.t[:], False, True, [onesb, cumb], [pr])
                    cp("dve", POS.t[:, i, :], pr.t[:, 32:64], [pr], [POS.bs[i]])
                    tt("dve", cumf.t[:], cumf.t[:], msk.t[:], ALU.add, [cumf, msk], [cumf])
                    cp("dve", cumb.t[:], cumf.t[:], [cumf], [cumb])

            gates(0)
            for tb in range(NB):
                if tb + 1 < NB:
                    gates(tb + 1)
                tiles(tb)
            S_.barrier()
        if debug:
            with contextlib.ExitStack() as es3:
                dt_ = T(es3.enter_context(nc.sbuf_tensor("s_dbgt", [128, 8, S], F32)))
                db_ = T(es3.enter_context(nc.sbuf_tensor("s_dbgb", [128, 8, S], BF16)))
                for nm, src, nch in [("dbg_oaT", oaT_d, 4), ("dbg_obT", obT_d, 4), ("dbg_hT", hT_d, 8)]:
                    dma("sp", db_.t[:, 0:nch, :], src, [], [db_])
                    cp("dve", dt_.t[:, 0:nch, :], db_.t[:, 0:nch, :], [db_], [dt_])
                    dma("sp", dbg[nm], dt_.t[:, 0:nch, :], [dt_], [Buf()])
                    S_.barrier()
                dma("sp", dbg["dbg_G"], G.t[:], G.bs, [Buf()])
                S_.barrier()

        with contextlib.ExitStack() as es:
            def sb(name, shape, dt=F32, nb=1):
                return T(es.enter_context(nc.sbuf_tensor("s_" + name, list(shape), dt)), nb)
            cnt = sb("cnt", [128, 32]); yv = sb("yv", [128, 32]); yi_ = sb("yi", [128, 32], I32); yf = sb("yf", [128, 32]); ygt = sb("ygt", [128, 32])
            padded = sb("padded", [128, 32]); ca = sb("csuma", [128, 32]); cb_ = sb("csumb", [128, 32]); pstart = sb("pstart", [128, 32])
            kp = sb("kp", [128, 8]); bstart = sb("bstart", [128, NBLK]); cmp_ = sb("cmp", [128, NBLK, 32]); be = sb("be", [128, NBLK])
            bf_ = sb("bf", [128, NBLK, 2])
            slotv = [sb("slotv%d" % i, [128, 32]) for i in range(2)]; oh4 = [sb("oh4%d" % i, [128, 4, 32]) for i in range(2)]
            pr4 = [sb("pr4%d" % i, [128, 4, 32]) for i in range(2)]; i4f = [sb("i4f%d" % i, [128, 4]) for i in range(2)]
            hrow = [sb("hrow%d" % i, [128, D], BF16) for i in range(3)]
            dma("sp", kp.t[:], kp_d, [], [kp]); dma("sp", bstart.t[:], bstart_d, [], [bstart])
            mm(PS[0].t[:, 0:32], onesb.t[:], cumb.t[:], True, True, [onesb, cumb], [PS[0]])
            cp("dve", cnt.t[:], PS[0].t[:, 0:32], [PS[0]], [cnt])
            ts("dve", yv.t[:], cnt.t[:], float(RB - 1), 1.0 / RB, ALU.add, ALU.mult, [cnt], [yv])
            cp("dve", yi_.t[:], yv.t[:], [yv], [yi_]); cp("dve", yf.t[:], yi_.t[:], [yi_], [yf])
            tt("dve", ygt.t[:], yf.t[:], yv.t[:], ALU.is_gt, [yf, yv], [ygt])
            tt("dve", yf.t[:], yf.t[:], ygt.t[:], ALU.subtract, [yf, ygt], [yf])
            ts("dve", padded.t[:], yf.t[:], float(RB), None, ALU.mult, None, [yf], [padded])
            cp("dve", ca.t[:], padded.t[:], [padded], [ca])
            src_, dst_ = ca, cb_
            for sh in (1, 2, 4, 8, 16):
                cp("dve", dst_.t[:, 0:sh], src_.t[:, 0:sh], [src_], [dst_])
                tt("dve", dst_.t[:, sh:32], src_.t[:, sh:32], src_.t[:, 0:32 - sh], ALU.add, [src_], [dst_])
                src_, dst_ = dst_, src_
            pend = src_
            tt("dve", pstart.t[:], pend.t[:], padded.t[:], ALU.subtract, [pend, padded], [pstart])
            tt("dve", cmp_.t[:], pend.t[:].unsqueeze(1).broadcast_to([128, NBLK, 32]), bstart.t[:].unsqueeze(2).broadcast_to([128, NBLK, 32]),
               ALU.is_le, [pend, bstart], [cmp_])
            red(be.t[:], cmp_.t[:], [cmp_], [be])
            ts("dve", be.t[:], be.t[:], float(NE - 1), None, ALU.min, None, [be], [be])
            stt("dve", bf_.t[:, :, 0], be.t[:], 128.0, kp.t[:, 0:1].broadcast_to([128, NBLK]), ALU.mult, ALU.add, [be, kp], [bf_])
            cp("dve", bf_.t[:, :, 1], be.t[:], [be], [bf_])
            cp("dve", BIDX.t[:], bf_.t[:], [bf_], [BIDX])
            for i in range(NT):
                sv = slotv[i % 2]; oh = oh4[i % 2]; p4 = pr4[i % 2]; f4 = i4f[i % 2]; hr = hrow[i % 3]
                tt("dve", sv.t[:], POS.t[:, i, :], pstart.t[:], ALU.add, [POS.bs[i], pstart], [sv])
                tt("dve", oh.t[:], LG.t[:, i, :].unsqueeze(1).broadcast_to([128, 4, 32]), MX.t[:, i, 0:4].unsqueeze(2).broadcast_to([128, 4, 32]),
                   ALU.is_equal, [LG.bs[i], MX.bs[i]], [oh])
                tt("dve", p4.t[:], oh.t[:], sv.t[:].unsqueeze(1).broadcast_to([128, 4, 32]), ALU.mult, [oh, sv], [p4])
                red(f4.t[:], p4.t[:], [p4], [f4])
                cp("dve", IDX.t[:, i, :], f4.t[:], [f4], [IDX.bs[i]])
                tt("dve", p4.t[:], oh.t[:], G.t[:, i, :].unsqueeze(1).broadcast_to([128, 4, 32]), ALU.mult, [oh, G.bs[i]], [p4])
                red(W4.t[:, i, :], p4.t[:], [p4], [W4.bs[i]])
                dma("sp", hr.t[:], H2tm_d[i * 128:(i + 1) * 128, :], [B_h2tm[i]], [hr])
                for j in range(4):
                    scatter(Xs_d, hr.t[:], IDX.t[:, i, j:j + 1], [hr, IDX.bs[i]], [])
            S_.barrier()

        with contextlib.ExitStack() as es:
            def sb(name, shape, dt=F32, nb=1):
                return T(es.enter_context(nc.sbuf_tensor("s_" + name, list(shape), dt)), nb)
            w1t = [sb("w1t%d" % i, [128, 8, 2048], BF16) for i in range(2)]; w2t = [sb("w2t%d" % i, [128, 8, D], BF16) for i in range(2)]
            b1t = [sb("b1t%d" % i, [128, 16]) for i in range(2)]; b2rep = [sb("b2rep%d" % i, [128, D], BF16) for i in range(2)]
            xsb = [sb("xsb%d" % i, [128, NST, D], BF16) for i in range(2)]; XsT = [sb("XsT%d" % i, [128, 8, RB], BF16) for i in range(2)]
            actT = [sb("actT%d" % i, [128, 8, RB], BF16, nb=8) for i in range(2)]
            gcl = [sb("gcl%d" % i, [128, RB]) for i in range(2)]; sg = [sb("sg%d" % i, [128, RB]) for i in range(2)]
            ucl = [sb("ucl%d" % i, [128, RB]) for i in range(2)]
            ysb = [sb("ysb%d" % i, [128, D]) for i in range(2)]
            ew1f = ew1_d.rearrange("e d n -> (e d) n"); ew2f = ew2_d.rearrange("e d n -> (e d) n")
            en = 0; yn = 0
            for b in range(NBLK):
                w1 = w1t[b % 2]; w2 = w2t[b % 2]; b1 = b1t[b % 2]; b2 = b2rep[b % 2]; xs_ = xsb[b % 2]; xT = XsT[b % 2]; aT = actT[b % 2]
                gather(w1.t[:].rearrange("p k n -> p (k n)"), W1b_d, BIDX.t[:, b, 0:1], [BIDX], [w1])
                gather(b1.t[:], eb1_d, BIDX.t[:, b, 0:1], [BIDX], [b1])
                gather(w2.t[:].rearrange("p k n -> p (k n)"), W2b_d, BIDX.t[:, b, 0:1], [BIDX], [w2])
                gather(b2.t[:], eb2_d, BIDX.t[:, b, 1:2], [BIDX], [b2])
                ts("dve", b1.t[:, 8:16], b1.t[:, 8:16], 1.0, None, ALU.add, None, [b1], [b1])
                if b == 0:
                    dma("sp", xs_.t[:], Xs_d[0:RB, :].rearrange("(s p) d -> p s d", p=128), [], [xs_])
                if b + 1 < NBLK:
                    xn_ = xsb[(b + 1) % 2]
                    dma("sp", xn_.t[:], Xs_d[(b + 1) * RB:(b + 2) * RB, :].rearrange("(s p) d -> p s d", p=128), [], [xn_])
                for s2 in range(NST):
                    pb = psbf(6 + s2 % 2)
                    for k in range(8):
                        tr(pb[:, k * 128:(k + 1) * 128], xs_.t[:, s2, k * 128:(k + 1) * 128], ident.t[:], [xs_, ident], [PS[6 + s2 % 2]])
                    S_.op("act", (lambda o, i_: lambda e: e.copy(o, i_))(xT.t[:, :, s2 * 128:(s2 + 1) * 128], pb[:, :].rearrange("p (k t) -> p k t", t=128)),
                          [PS[6 + s2 % 2].b], [xT.b])
                for Fi in range(8):
                    pg = PS[(en % 2) * 2]; pu = PS[(en % 2) * 2 + 1]
                    gc = gcl[en % 2]; sgt = sg[en % 2]; uc = ucl[en % 2]; gs_ = sgt; en += 1
                    for k in range(8):
                        mm(pg.t[:, 0:RB], w1.t[:, k, Fi * 128:(Fi + 1) * 128], xT.t[:, k, :], k == 0, k == 7, [w1, xT], [pg])
                    for k in range(8):
                        mm(pu.t[:, 0:RB], w1.t[:, k, 1024 + Fi * 128:1024 + (Fi + 1) * 128], xT.t[:, k, :], k == 0, k == 7, [w1, xT], [pu])
                    ts("dve", gc.t[:], pg.t[:, 0:RB], b1.t[:, Fi:Fi + 1], 7.0, ALU.add, ALU.min, [pg, b1], [gc])
                    act(sgt.t[:], gc.t[:], AF.Sigmoid, [gc], [sgt], scale=1.702)
                    ts("dve", uc.t[:], pu.t[:, 0:RB], b1.t[:, 8 + Fi:9 + Fi], 8.0, ALU.add, ALU.min, [pu, b1], [uc])
                    tt("dve", gs_.t[:], gc.t[:], sgt.t[:], ALU.mult, [gc, sgt], [gs_])
                    stt("dve", aT.t[:, Fi, :], uc.t[:], -6.0, gs_.t[:], ALU.max, ALU.mult, [gs_, uc], [aT.bs[Fi]])
                for s2 in range(NST):
                    yt_ = ysb[yn % 2]; yn += 1
                    for half in range(2):
                        py = PS[4 + half]
                        for k in range(8):
                            mm(py.t[:], aT.t[:, k, s2 * 128:(s2 + 1) * 128], w2.t[:, k, half * 512:(half + 1) * 512], k == 0, k == 7, [aT.bs[k], w2], [py])
                        tt("dve", yt_.t[:, half * 512:(half + 1) * 512], py.t[:], b2.t[:, half * 512:(half + 1) * 512], ALU.add, [py, b2], [yt_])
                    dma("sp", Ys_d[b * RB + s2 * 128:b * RB + (s2 + 1) * 128, :], yt_.t[:], [yt_], [B_Ys[b]])
            S_.barrier()

        with contextlib.ExitStack() as es:
            def sb(name, shape, dt=F32, nb=1):
                return T(es.enter_context(nc.sbuf_tensor("s_" + name, list(shape), dt)), nb)
            gat = [[sb("gat%d_%d" % (i, j), [128, D]) for j in range(4)] for i in range(2)]
            xq = [sb("cxq%d" % i, [128, D]) for i in range(2)]; acc_ = [sb("cacc%d" % i, [128, D]) for i in range(2)]
            for i in range(NT):
                g4 = gat[i % 2]; xq_ = xq[i % 2]; ac = acc_[i % 2]
                for j in range(4):
                    gather(g4[j].t[:], Ys_d, IDX.t[:, i, j:j + 1], [IDX.bs[i]] + B_Ys, [g4[j]])
                if i == 0:
                    dma("sp", xq_.t[:], y_d[0:128, :], [B_y[0]], [xq_])
                if i + 1 < NT:
                    xqn = xq[(i + 1) % 2]
                    dma("sp", xqn.t[:], y_d[(i + 1) * 128:(i + 2) * 128, :], [B_y[i + 1]], [xqn])
                ts("dve", ac.t[:], g4[0].t[:], W4.t[:, i, 0:1], None, ALU.mult, None, [g4[0], W4.bs[i]], [ac])
                for j in range(1, 4):
                    stt("dve", ac.t[:], g4[j].t[:], W4.t[:, i, j:j + 1], ac.t[:], ALU.mult, ALU.add, [g4[j], W4.bs[i], ac], [ac])
                tt("dve", ac.t[:], ac.t[:], g2row.t[:], ALU.mult, [ac, g2row], [ac])
                tt("dve", ac.t[:], ac.t[:], xq_.t[:], ALU.add, [ac, xq_], [ac])
                dma("sp", y_d[i * 128:(i + 1) * 128, :], ac.t[:], [ac], [B_y[i]])
            S_.barrier()

        sems = {k: ges.enter_context(nc.semaphore("s%d" % i)) for i, k in enumerate(S_.semkeys)}
        S_.emit(sems)
    return nc


_CONST_CACHE = {}


def make_constants(S):
    if S in _CONST_CACHE:
        return _CONST_CACHE[S]
    bf = ml_dtypes.bfloat16
    NT = S // 128; NKC = S // 128; BLK = min(512, S); NB = S // BLK
    N2 = 2 * S
    n = np.arange(S, dtype=np.int64)[:, None]; k = np.arange(S, dtype=np.int64)[None, :]
    ph = ((2 * k + 1) * n) % (2 * N2)
    ang = ph.astype(np.float64) * (np.pi / N2)
    C = np.cos(ang); Sm = np.sin(ang)
    del ang, ph
    def fwd(M):
        return np.ascontiguousarray(M.reshape(NT, 128, NKC, 128).transpose(2, 1, 0, 3)).astype(bf)
    def inv(M):
        return np.ascontiguousarray(M.reshape(NB, BLK, NKC, 128).transpose(0, 3, 2, 1)).astype(bf)
    consts = {"Cf": fwd(C), "Sf": fwd(Sm), "Ci": inv(C), "nSi": inv(-Sm)}
    del C, Sm
    consts["ident"] = np.eye(128, dtype=np.float32).astype(bf)
    GRID_W = 64; RF = 16
    rows = S // GRID_W
    row = np.repeat(np.arange(rows), GRID_W); col = np.tile(np.arange(GRID_W), rows)
    pos = np.stack([row, col], -1).astype(np.float32)
    freqs = (np.float32(10000.0) ** (-np.arange(RF, dtype=np.float32) / np.float32(RF))).astype(np.float32)
    ang = (pos[:, :, None] * freqs).astype(np.float32)
    cos = np.cos(ang).astype(np.float32); sin = np.sin(ang).astype(np.float32)
    ropec = np.stack([cos, cos], 2).reshape(S, 64)
    ropes = np.stack([sin, -sin], 2).reshape(S, 64)
    consts["ropec"] = np.ascontiguousarray(ropec, dtype=np.float32); consts["ropes"] = np.ascontiguousarray(ropes, dtype=np.float32)
    t = np.linspace(0.0, 1.0, S, dtype=np.float32)[:, None]
    w = (np.float32(2.0 * math.pi) * np.arange(S, dtype=np.float32)[:, None] / np.float32(S)).astype(np.float32)
    f = np.linspace(1e-4, 15, 16, dtype=np.float32)
    z = np.concatenate([t, np.cos(f * w), -np.sin(f * w)], -1).astype(np.float32)
    consts["zT"] = np.ascontiguousarray(z.T)
    consts["trow"] = np.ascontiguousarray(t.T)
    deltas = np.abs(np.linspace(math.log(1e-2) / 1.5, math.log(1e-2) / 0.3, 512, dtype=np.float32))
    consts["drow"] = deltas.reshape(1, 512).astype(np.float32)
    _CONST_CACHE[S] = consts
    return consts


def fm(v, nchunk):
    return np.ascontiguousarray(np.asarray(v, np.float32).reshape(nchunk, 128).T)


def make_in_maps(inp, S, CTXL, NE, B):
    consts = make_constants(S)
    f32 = lambda a: np.ascontiguousarray(np.asarray(a, np.float32))
    shared = dict(consts)
    shared["ada_w"] = f32(inp["ada_w"][0]); shared["ada_b_row"] = f32(inp["ada_b"][0]).reshape(1, -1); shared["ada_b_fm"] = fm(inp["ada_b"][0], 48)
    shared["n1g"] = fm(inp["norm1_g"][0], 8); shared["n2g"] = fm(inp["norm2_g"][0], 8)
    shared["w_in"] = f32(inp["w_in"][0]); shared["b_in_row"] = f32(inp["b_in"][0]).reshape(1, -1); shared["b_in_fm"] = fm(inp["b_in"][0], 40)
    shared["qg"] = f32(inp["q_norm_g"][0]).reshape(1, 64); shared["kg"] = f32(inp["k_norm_g"][0]).reshape(1, 64)
    shared["lam4"] = np.concatenate([f32(inp[n][0]) for n in ("lambda_q1", "lambda_k1", "lambda_q2", "lambda_k2")]).reshape(1, 256)
    shared["subg"] = f32(inp["subln_g"][0]).reshape(128, 1)
    cw = f32(inp["conv_w"][0])
    shared["convw"] = np.ascontiguousarray(cw.reshape(3, 12, 128).transpose(2, 1, 0)); shared["convb"] = fm(inp["conv_b"][0], 12)
    shared["fw1"] = f32(inp["filt_w1"][0]); shared["fw2"] = f32(inp["filt_w2"][0]); shared["fw3"] = f32(inp["filt_w3"][0]); shared["fw4"] = f32(inp["filt_w4"][0])
    shared["fvec"] = np.ascontiguousarray(np.stack([f32(inp["filt_b1"][0]), f32(inp["filt_b2"][0]), f32(inp["filt_b3"][0]), f32(inp["filt_freq"][0])], -1))
    shared["hbias"] = fm(inp["hyena_bias"][0], 4)
    shared["w_up_a"] = f32(inp["w_up_a"][0]); shared["w_up_b"] = f32(inp["w_up_b"][0]); shared["w_out"] = f32(inp["w_out"][0])
    shared["router_w"] = f32(inp["router_w"][0]); shared["router_b"] = f32(inp["router_b"][0]).reshape(1, 32)
    shared["ew1"] = f32(inp["exp_w1"][0]); shared["ew2"] = f32(inp["exp_w2"][0])
    shared["eb1"] = np.ascontiguousarray(f32(inp["exp_b1"][0]).reshape(NE, 16, 128).transpose(0, 2, 1).reshape(NE * 128, 16))
    shared["eb2"] = f32(inp["exp_b2"][0]).reshape(NE, D)
    shared["n2g_row"] = f32(inp["norm2_g"][0]).reshape(1, D)
    NBLK = (4 * S + NE * RB) // RB
    shared["ustrict"] = np.triu(np.ones((128, 128), np.float32), 1).astype(ml_dtypes.bfloat16)
    shared["kp"] = np.ascontiguousarray((np.arange(8)[None, :] * 128 + np.arange(128)[:, None]).astype(np.float32))
    shared["bstart"] = np.ascontiguousarray(np.broadcast_to((np.arange(NBLK) * RB).astype(np.float32)[None, :], (128, NBLK)))
    maps = []
    for b in range(B):
        m = dict(shared)
        m["x"] = f32(inp["x"][b]); m["ctx"] = f32(inp["ctx"][b])
        m["cc"] = np.ascontiguousarray(np.stack([f32(inp["c"][b]), f32(inp["c_ctx"])], -1).reshape(8, 128, 2).transpose(1, 0, 2))
        maps.append(m)
    return maps


_PROG_CACHE = {}


def kernel(**inputs):
    x = np.asarray(inputs["x"])
    B, S, _ = x.shape
    CTXL = np.asarray(inputs["ctx"]).shape[1]
    NE = np.asarray(inputs["exp_w1"]).shape[1]
    key = (S, CTXL, NE)
    if key not in _PROG_CACHE:
        _PROG_CACHE[key] = build_program(S, CTXL, NE)
    nc = _PROG_CACHE[key]
    maps = make_in_maps(inputs, S, CTXL, NE, B)
    res = run_bass_kernel_spmd(nc, maps, core_ids=list(range(B)))
    return np.stack([np.asarray(r["y"], dtype=np.float32) for r in res.results], 0)
```

```python
import contextlib
import math
import numpy as np
import ml_dtypes
import concourse.bass as bass
import concourse.mybir as mybir
from concourse.bass_utils import run_bass_kernel_spmd

F32 = mybir.dt.float32
BF16 = mybir.dt.bfloat16
I32 = mybir.dt.int32
AF = mybir.ActivationFunctionType
ALU = mybir.AluOpType
AX = mybir.AxisListType
D = 1024
EPS = 1e-6
RB = 256
PI = math.pi


class Buf:
    __slots__ = ("w", "r")

    def __init__(self):
        self.w = None
        self.r = {}


ENGS = ("pe", "act", "dve", "pool", "sp")
DMA_RING = {"sp": 8, "pool": 8}


class Sched:
    def __init__(self, nc, same_engine_sync=True):
        self.nc = nc
        self.prog = {e: [] for e in ENGS}
        self.nops = {e: 0 for e in ENGS}
        self.waited = {e: {} for e in ENGS}
        self.same = same_engine_sync
        self.dma_n = {q: 0 for q in DMA_RING}
        self.dma_val = {}
        self.semkeys = list(ENGS)
        for q, n in DMA_RING.items():
            for i in range(n):
                self.semkeys.append(("dma", q, i))
                self.dma_val[("dma", q, i)] = 0

    def _wait(self, eng, semkey, val):
        if self.waited[eng].get(semkey, -1) >= val:
            return
        self.waited[eng][semkey] = val
        if isinstance(semkey, str):
            self.prog[semkey][val][3] = True
        self.prog[eng].append(["w", semkey, val])

    def _deps(self, eng, reads, writes, is_dma):
        deps = {}

        def add(k, v, e):
            if (not is_dma) and e == eng and k == eng:
                if eng == "pe" or not self.same:
                    return
            if deps.get(k, -1) < v:
                deps[k] = v
        for b in reads:
            if b.w is not None:
                add(*b.w)
        for b in writes:
            if b.w is not None:
                add(*b.w)
            for k, (v, e) in b.r.items():
                add(k, v, e)
        for k, v in deps.items():
            self._wait(eng, k, v)

    def _update(self, tok, reads, writes):
        k, v, e = tok
        for b in reads:
            b.r[k] = (v, e)
        for b in writes:
            b.w = tok
            b.r = {}

    def op(self, eng, fn, reads=(), writes=()):
        self._deps(eng, reads, writes, False)
        pos = len(self.prog[eng])
        self.prog[eng].append(["op", fn, eng, False])
        self.nops[eng] += 1
        self._update((eng, pos, eng), reads, writes)

    def dma(self, q, fn, reads=(), writes=()):
        n = self.dma_n[q]
        self.dma_n[q] += 1
        key = ("dma", q, n % DMA_RING[q])
        if self.dma_val[key] > 0:
            self._wait(q, key, self.dma_val[key])
        self._deps(q, reads, writes, True)
        self.dma_val[key] += 16
        tok = (key, self.dma_val[key], q)
        self.prog[q].append(["dma", fn, key, 16])
        self._update(tok, reads, writes)

    def _last_op(self, eng):
        for i in range(len(self.prog[eng]) - 1, -1, -1):
            if self.prog[eng][i][0] == "op":
                return i
        return None

    def barrier(self):
        for e in ENGS:
            for e2 in ENGS:
                if e2 != e:
                    lp = self._last_op(e2)
                    if lp is not None:
                        self._wait(e, e2, lp)
            for k, v in self.dma_val.items():
                if v > 0:
                    self._wait(e, k, v)

    def emit(self, sems):
        nc = self.nc
        value_at = {}
        for e in ENGS:
            c = 0
            va = {}
            for pos, item in enumerate(self.prog[e]):
                if item[0] == "op" and item[3]:
                    c += 1
                    va[pos] = c
            value_at[e] = va
        with nc.Block() as block:
            def run(engname):
                def body(eng):
                    for item in self.prog[engname]:
                        if item[0] == "w":
                            k, v = item[1], item[2]
                            eng.wait_ge(sems[k], value_at[k][v] if isinstance(k, str) else v)
                        elif item[0] == "dma":
                            item[1](eng).then_inc(sems[item[2]], 16)
                        elif item[3]:
                            item[1](eng).then_inc(sems[item[2]], 1)
                        else:
                            item[1](eng)
                return body
            block.tensor(run("pe"))
            block.scalar(run("act"))
            block.vector(run("dve"))
            block.gpsimd(run("pool"))
            block.sync(run("sp"))


class T:
    def __init__(self, t, nb=1):
        self.t = t
        self.b = Buf()
        self.bs = [Buf() for _ in range(nb)] if nb > 1 else [self.b]


def build_program(S, CTXL, NE, debug=False):
    NT = S // 128
    NC = CTXL // 128
    NKT = NT + NC
    TK = S + CTXL
    BLK = min(512, S)
    NB = S // BLK
    TPB = BLK // 128
    NKC = S // 128
    KG = min(8, NKC)
    NKG = NKC // KG
    QS = min(1024, S)
    NQ = S // QS
    NQT = QS // 128
    NQB = QS // BLK
    N2 = 2 * S
    NR = 4 * S + NE * RB
    NBLK = NR // RB
    NZ = NR // 256
    NST = RB // 128

    nc = bass.Bass("TRN2", target_bir_lowering=False)
    S_ = Sched(nc)

    def din(name, shape, dt=F32):
        return nc.dram_tensor(name, list(shape), dt, kind="ExternalInput").ap()

    def dscr(name, shape, dt=BF16):
        return nc.dram_tensor(name, list(shape), dt, kind="Internal").ap()

    x_d = din("x", [S, D]); ctx_d = din("ctx", [CTXL, D]); cc_d = din("cc", [128, 8, 2])
    adaw_d = din("ada_w", [D, 6 * D]); adab_row_d = din("ada_b_row", [1, 6 * D]); adab_fm_d = din("ada_b_fm", [128, 48])
    n1g_d = din("n1g", [128, 8]); n2g_d = din("n2g", [128, 8])
    win_d = din("w_in", [D, 5120]); bin_row_d = din("b_in_row", [1, 5120]); bin_fm_d = din("b_in_fm", [128, 40])
    qg_d = din("qg", [1, 64]); kg_d = din("kg", [1, 64]); lam4_d = din("lam4", [1, 256]); subg_d = din("subg", [128, 1])
    convw_d = din("convw", [128, 12, 3]); convb_d = din("convb", [128, 12])
    fw1_d = din("fw1", [33, 64]); fw2_d = din("fw2", [64, 64]); fw3_d = din("fw3", [64, 64]); fw4_d = din("fw4", [64, 1024])
    fvec_d = din("fvec", [64, 4])
    hb_d = din("hbias", [128, 4])
    wua_d = din("w_up_a", [512, D]); wub_d = din("w_up_b", [512, D]); wout_d = din("w_out", [D, D])
    rw_d = din("router_w", [D, 32]); rb_d = din("router_b", [1, 32])
    ew1_d = din("ew1", [NE, D, 2048]); eb1_d = din("eb1", [NE * 128, 16]); ew2_d = din("ew2", [NE, D, D]); eb2_d = din("eb2", [NE, D])
    ustr_d = din("ustrict", [128, 128], BF16); kp_d = din("kp", [128, 8]); bstart_d = din("bstart", [128, NBLK]); n2grow_d = din("n2g_row", [1, D])
    ident_d = din("ident", [128, 128], BF16)
    ropec_d = din("ropec", [S, 64]); ropes_d = din("ropes", [S, 64])
    zT_d = din("zT", [33, S]); trow_d = din("trow", [1, S]); drow_d = din("drow", [1, 512])
    Cf_d = din("Cf", [NKC, 128, NT, 128], BF16); Sf_d = din("Sf", [NKC, 128, NT, 128], BF16)
    Ci_d = din("Ci", [NB, 128, NKC, BLK], BF16); Si_d = din("nSi", [NB, 128, NKC, BLK], BF16)
    y_d = nc.dram_tensor("y", [S, D], F32, kind="ExternalOutput").ap()

    hTc_d = dscr("hTc_d", [128, 8, CTXL]); hT_d = dscr("hT_d", [128, 8, S])
    oaT_d = dscr("oaT_d", [128, 4, S]); obT_d = dscr("obT_d", [128, 4, S])
    x0c_d = dscr("x0c_d", [128, 4, S]); uT_d = dscr("uT_d", [128, 4, S])
    Y_d = dscr("Y_d", [128, 2, NKC, 512])
    H2tm_d = dscr("H2tm_d", [S, D]); Xs_d = dscr("Xs_d", [NR, D]); Ys_d = dscr("Ys_d", [NR, D], F32)
    W1b_d = dscr("W1b_d", [NE * 128, 8 * 2048]); W2b_d = dscr("W2b_d", [NE * 128, 8 * D])
    dbg = {}
    if debug:
        for n, shp in [("dbg_oaT", [128, 4, S]), ("dbg_obT", [128, 4, S]), ("dbg_hT", [128, 8, S]), ("dbg_G", [128, NT, 32])]:
            dbg[n] = nc.dram_tensor(n, shp, F32, kind="ExternalOutput").ap()
    B_hTc = Buf(); B_hT = [Buf() for _ in range(NB)]
    B_oaT = [Buf() for _ in range(NB)]; B_obT = [Buf() for _ in range(NB)]
    B_x0c = [Buf() for _ in range(4)]; B_uT = [Buf() for _ in range(4)]
    B_Y = [Buf() for _ in range(NKC)]; B_h2tm = [Buf() for _ in range(NT)]
    B_Xs = [Buf() for _ in range(NZ)]; B_Ys = [Buf() for _ in range(NBLK)]
    B_y = [Buf() for _ in range(NT)]

    w_in_v = win_d.rearrange("(k p) n -> p k n", p=128)

    def bl(xs):
        return [x.b if isinstance(x, T) else x for x in xs]

    def mm(out, lhsT, rhs, start, stop, r, w, tp=None):
        if tp is None:
            S_.op("pe", lambda e: e.matmul(out, lhsT, rhs, start=start, stop=stop), bl(r), bl(w))
        else:
            S_.op("pe", lambda e: e.matmul(out, lhsT, rhs, start=start, stop=stop, tile_position=tp), bl(r), bl(w))

    def tr(out, in_, ident, r, w):
        S_.op("pe", lambda e: e.transpose(out, in_, ident), bl(r), bl(w))

    def act(out, in_, func, r, w, **kw):
        S_.op("act", lambda e: e.activation(out=out, in_=in_, func=func, **kw), bl(r), bl(w))

    def tt(eng, out, in0, in1, op, r, w):
        S_.op(eng, lambda e: e.tensor_tensor(out, in0, in1, op), bl(r), bl(w))

    def ts(eng, out, in0, s1, s2, op0, op1, r, w):
        if op1 is None:
            S_.op(eng, lambda e: e.tensor_scalar(out, in0, s1, None, op0), bl(r), bl(w))
        else:
            S_.op(eng, lambda e: e.tensor_scalar(out, in0, s1, s2, op0, op1), bl(r), bl(w))

    def stt(eng, out, in0, sc, in1, op0, op1, r, w):
        S_.op(eng, lambda e: e.scalar_tensor_tensor(out, in0, sc, in1, op0, op1), bl(r), bl(w))

    def cp(eng, out, in_, r, w):
        S_.op(eng, lambda e: e.tensor_copy(out, in_), bl(r), bl(w))

    def recip(out, in_, r, w):
        S_.op("dve", lambda e: e.reciprocal(out, in_), bl(r), bl(w))

    def mset(eng, ap, val, w):
        S_.op(eng, lambda e: e.memset(ap, val), [], bl(w))

    def dma(q, out, in_, r, w):
        S_.dma(q, lambda e: e.dma_start(out=out, in_=in_), bl(r), bl(w))

    def vmax8(out, in_, r, w):
        S_.op("dve", lambda e: e.max(out, in_), bl(r), bl(w))

    def red(out, in_, r, w):
        S_.op("dve", lambda e: e.tensor_reduce(out, in_, AX.X, ALU.add), bl(r), bl(w))

    def gather(out, in_, idx_ap, r, w):
        S_.dma("pool", lambda e: e.indirect_dma_start(out=out, out_offset=None, in_=in_,
                                                      in_offset=bass.IndirectOffsetOnAxis(ap=idx_ap, axis=0), oob_is_err=False), bl(r), bl(w))

    def scatter(out, in_, idx_ap, r, w):
        S_.dma("pool", lambda e: e.indirect_dma_start(out=out, out_offset=bass.IndirectOffsetOnAxis(ap=idx_ap, axis=0), in_=in_,
                                                      in_offset=None, oob_is_err=False), bl(r), bl(w))

    with contextlib.ExitStack() as ges:
        def gsb(name, shape, dt=F32, nb=1):
            return T(ges.enter_context(nc.sbuf_tensor("s_" + name, list(shape), dt)), nb)
        psall = ges.enter_context(nc.psum_tensor("psall", [128, 4096], F32))
        PS = [T(psall[:, i * 512:(i + 1) * 512]) for i in range(8)]

        def psbf(i):
            return PS[i].t[:].bitcast(BF16)

        ident = gsb("ident", [128, 128], BF16); onesb = gsb("onesb", [128, 128], BF16); onesf = gsb("onesf", [128, 128])
        epst = gsb("epst", [128, 1])
        A1 = gsb("A1", [128, 8]); B1 = gsb("B1", [128, 8]); A1c = gsb("A1c", [128, 8]); B1c = gsb("B1c", [128, 8])
        A2 = gsb("A2", [128, 8]); B2 = gsb("B2", [128, 8])
        g1row = gsb("g1row", [128, D]); g2row = gsb("g2row", [128, D])
        neglam = gsb("neglam", [128, 1]); gsub = gsb("gsub", [128, 1])
        G = gsb("G", [128, NT, 32], F32, nb=NT)
        A2row = gsb("A2row", [128, D]); B2row = gsb("B2row", [128, D])
        LG = gsb("LG", [128, NT, 32], F32, nb=NT); MX = gsb("MX", [128, NT, 8], F32, nb=NT); POS = gsb("POS", [128, NT, 32], F32, nb=NT)
        IDX = gsb("IDX", [128, NT, 4], I32, nb=NT); W4 = gsb("W4", [128, NT, 4], F32, nb=NT)
        BIDX = gsb("BIDX", [128, NBLK, 2], I32)
        ustr = gsb("ustr", [128, 128], BF16); cumf = gsb("cumf", [128, 32]); cumb = gsb("cumb", [128, 32], BF16)

        dma("sp", ident.t[:], ident_d, [], [ident]); dma("sp", ustr.t[:], ustr_d, [], [ustr])
        mset("pool", cumf.t[:], 0.0, [cumf]); mset("pool", cumb.t[:], 0.0, [cumb])
        mset("pool", onesb.t[:], 1.0, [onesb]); mset("pool", onesf.t[:], 1.0, [onesf]); mset("pool", epst.t[:], EPS, [epst])

        with contextlib.ExitStack() as es:
            def sb(name, shape, dt=F32, nb=1):
                return T(es.enter_context(nc.sbuf_tensor("s_" + name, list(shape), dt)), nb)
            cc = sb("cc", [128, 8, 2]); scv = sb("scv", [128, 8, 2]); screp = sb("screp", [128, 8, 128])
            aw = [sb("aw%d" % i, [128, 8, 512]) for i in range(2)]
            adab_fm = sb("adab_fm", [128, 48]); adab_row = sb("adab_row", [1, 6 * D])
            modF = sb("modF", [128, 48, 2]); n1g = sb("n1g", [128, 8]); n2g = sb("n2g", [128, 8])
            lam4 = sb("lam4", [128, 256]); lt1 = sb("lt1", [128, 64]); lt2 = sb("lt2", [128, 64])
            ls1 = sb("ls1", [128, 1]); ls2 = sb("ls2", [128, 1]); subg = sb("subg", [128, 1])
            dma("sp", cc.t[:], cc_d, [], [cc]); dma("sp", adab_fm.t[:], adab_fm_d, [], [adab_fm])
            dma("sp", adab_row.t[:], adab_row_d, [], [adab_row])
            dma("sp", n1g.t[:], n1g_d, [], [n1g]); dma("sp", n2g.t[:], n2g_d, [], [n2g])
            dma("sp", lam4.t[:], lam4_d.partition_broadcast(128), [], [lam4]); dma("sp", subg.t[:], subg_d, [], [subg])
            act(scv.t[:], cc.t[:], AF.Silu, [cc], [scv])
            for k in range(8):
                cp("dve", screp.t[:, k, :], scv.t[:, k, 0:1].broadcast_to([128, 128]), [scv], [screp])
            adaw_v = adaw_d.rearrange("(k p) n -> p k n", p=128)
            pmod = PS[0]
            for g in range(12):
                a = aw[g % 2]
                dma("sp", a.t[:], adaw_v[:, :, g * 512:(g + 1) * 512], [], [a])
                for c in range(4):
                    ch = g * 4 + c
                    for k in range(8):
                        mm(pmod.t[:, 2 * ch:2 * ch + 2], a.t[:, k, c * 128:(c + 1) * 128], scv.t[:, k, :], k == 0, k == 7, [a, scv], [pmod])
                if g in (4, 5, 6, 7, 8, 9, 10, 11):
                    pr = PS[1 + (g % 2)]
                    for k in range(8):
                        mm(pr.t[:], screp.t[:, k, :], a.t[:, k, :], k == 0, False, [a, screp], [pr])
                    mm(pr.t[:], onesf.t[0:1, :], adab_row.t[0:1, g * 512:(g + 1) * 512], False, True, [onesf, adab_row], [pr])
                    dst = {2: g1row, 3: B2row, 4: A2row, 5: g2row}[g // 2]
                    half = g % 2
                    cp("dve", dst.t[:, half * 512:(half + 1) * 512], pr.t[:], [pr], [dst])
            tt("dve", modF.t[:], pmod.t[:, 0:96].rearrange("p (c j) -> p c j", j=2),
               adab_fm.t[:].unsqueeze(2).broadcast_to([128, 48, 2]), ALU.add, [pmod, adab_fm], [modF])
            stt("dve", A1.t[:], modF.t[:, 8:16, 0], 1.0, n1g.t[:], ALU.add, ALU.mult, [modF, n1g], [A1])
            stt("dve", A1c.t[:], modF.t[:, 8:16, 1], 1.0, n1g.t[:], ALU.add, ALU.mult, [modF, n1g], [A1c])
            stt("dve", A2.t[:], modF.t[:, 32:40, 0], 1.0, n2g.t[:], ALU.add, ALU.mult, [modF, n2g], [A2])
            cp("dve", B1.t[:], modF.t[:, 0:8, 0], [modF], [B1]); cp("dve", B1c.t[:], modF.t[:, 0:8, 1], [modF], [B1c])
            cp("dve", B2.t[:], modF.t[:, 24:32, 0], [modF], [B2])
            n2grow = sb("n2grow", [128, D])
            dma("sp", n2grow.t[:], n2grow_d.partition_broadcast(128), [], [n2grow])
            stt("dve", A2row.t[:], A2row.t[:], 1.0, n2grow.t[:], ALU.add, ALU.mult, [A2row, n2grow], [A2row])
            tt("dve", lt1.t[:], lam4.t[:, 0:64], lam4.t[:, 64:128], ALU.mult, [lam4], [lt1])
            tt("dve", lt2.t[:], lam4.t[:, 128:192], lam4.t[:, 192:256], ALU.mult, [lam4], [lt2])
            S_.op("dve", lambda e: e.tensor_reduce(ls1.t[:], lt1.t[:], AX.X, ALU.add), [lt1.b], [ls1.b])
            S_.op("dve", lambda e: e.tensor_reduce(ls2.t[:], lt2.t[:], AX.X, ALU.add), [lt2.b], [ls2.b])
            act(ls1.t[:], ls1.t[:], AF.Exp, [ls1], [ls1]); act(ls2.t[:], ls2.t[:], AF.Exp, [ls2], [ls2])
            tt("dve", neglam.t[:], ls2.t[:], ls1.t[:], ALU.subtract, [ls1, ls2], [neglam])
            ts("dve", neglam.t[:], neglam.t[:], -0.2, None, ALU.add, None, [neglam], [neglam])
            ts("dve", gsub.t[:], subg.t[:], 0.8, None, ALU.mult, None, [subg], [gsub])
            S_.barrier()

        def norm_transpose(es, tag):
            def sb(name, shape, dt=F32, nb=1):
                return T(es.enter_context(nc.sbuf_tensor("s_" + tag + name, list(shape), dt)), nb)
            st = dict(junk=sb("junk", [128, D], BF16), ss=[sb("ss%d" % i, [128, 1]) for i in range(2)],
                      xs=[sb("xs%d" % i, [128, D], BF16) for i in range(2)], n=0)
            return st

        def do_norm_transpose(st, xin, A, Bv, psi, dst_fn, dst_bufs, defer=False):
            i = st["n"]; st["n"] += 1
            ss = st["ss"][i % 2]; xs = st["xs"][i % 2]
            mset("pool", ss.t[:], 0.0, [ss])
            act(st["junk"].t[:], xin.t[:], AF.Square, [xin], [st["junk"], ss], accum_out=ss.t[:])
            act(ss.t[:], ss.t[:], AF.Sqrt, [ss, epst], [ss], scale=1.0 / D, bias=epst.t[:])
            recip(ss.t[:], ss.t[:], [ss], [ss])
            ts("dve", xs.t[:], xin.t[:], ss.t[:, 0:1], None, ALU.mult, None, [xin, ss], [xs])
            pb = psbf(psi)
            for k in range(8):
                tr(pb[:, k * 128:(k + 1) * 128], xs.t[:, k * 128:(k + 1) * 128], ident.t[:], [xs, ident], [PS[psi]])
            def evac():
                for k in range(8):
                    act(dst_fn(k), pb[:, k * 128:(k + 1) * 128], AF.Identity, [PS[psi], A, Bv], dst_bufs,
                        scale=A.t[:, k:k + 1], bias=Bv.t[:, k:k + 1])
            if defer:
                return ss, evac
            evac()
            return ss

        with contextlib.ExitStack() as es:
            def sb(name, shape, dt=F32, nb=1):
                return T(es.enter_context(nc.sbuf_tensor("s_" + name, list(shape), dt)), nb)
            st = norm_transpose(es, "p1")
            xin = [sb("p1xin%d" % i, [128, D]) for i in range(3)]
            hblk = [sb("p1hb%d" % i, [128, 8, BLK], BF16) for i in range(2)]
            n = 0
            pend_ev = None
            hb = hblk[0]
            for i in range(NC):
                xi = xin[n % 3]
                dma("sp", xi.t[:], ctx_d[i * 128:(i + 1) * 128, :], [], [xi])
                _, ev_ = do_norm_transpose(st, xi, A1c, B1c, n % 2, lambda k, i=i, hb=hb: hb.t[:, k, i * 128:(i + 1) * 128], [hb], defer=True)
                if pend_ev is not None:
                    pend_ev()
                pend_ev = ev_
                n += 1
            pend_ev(); pend_ev = None
            dma("sp", hTc_d, hblk[0].t[:, :, 0:CTXL], [hblk[0]], [B_hTc])
            for b in range(NB):
                hb = hblk[(b + 1) % 2]
                for s in range(TPB):
                    i = b * TPB + s
                    xi = xin[n % 3]
                    dma("sp", xi.t[:], x_d[i * 128:(i + 1) * 128, :], [], [xi])
                    _, ev_ = do_norm_transpose(st, xi, A1, B1, n % 2, lambda k, s=s, hb=hb: hb.t[:, k, s * 128:(s + 1) * 128], [hb], defer=True)
                    if pend_ev is not None:
                        pend_ev()
                    pend_ev = ev_
                    n += 1
                pend_ev(); pend_ev = None
                dma("sp", hT_d[:, :, b * BLK:(b + 1) * BLK], hb.t[:], [hb], [B_hT[b]])
            S_.barrier()

        with contextlib.ExitStack() as es2:
            def sb2(name, shape, dt=F32, nb=1):
                return T(es2.enter_context(nc.sbuf_tensor("s_" + name, list(shape), dt)), nb)
            QT = sb2("QT", [128, 4, S], BF16, nb=NT); KT = sb2("KT", [128, 4, TK], BF16, nb=NKT); V = sb2("V", [128, NKT, 512], BF16, nb=NKT)
            with contextlib.ExitStack() as es:
                def sb(name, shape, dt=F32, nb=1):
                    return T(es.enter_context(nc.sbuf_tensor("s_" + name, list(shape), dt)), nb)
                wqkv = sb("wqkv", [128, 8, 1536], BF16); brow = sb("brow", [1, 1536], BF16)
                gq = sb("gq", [128, 64]); gk = sb("gk", [128, 64])
                rcs = [sb("rc%d" % i, [128, 64]) for i in range(3)]; rss = [sb("rs%d" % i, [128, 64]) for i in range(3)]
                gtab = [[sb("gtab%d_%d" % (i, j), [128, 64]) for j in range(4)] for i in range(3)]
                hblk = [sb("p2hb%d" % i, [128, 8, BLK], BF16) for i in range(2)]
                sqt = [sb("sqt%d" % i, [128, 512]) for i in range(2)]
                ssq = [sb("ssq%d" % i, [128, 8]) for i in range(2)]
                qn = [sb("qn%d" % i, [128, 512]) for i in range(2)]
                qg2 = [sb("qg2%d" % i, [128, 512]) for i in range(2)]
                ru = [sb("ru%d" % i, [128, 512]) for i in range(2)]
                rw_ = [sb("rw%d" % i, [128, 512]) for i in range(2)]
                qr = [sb("qr%d" % i, [128, 512], BF16) for i in range(4)]
                for c in range(3):
                    dma("pool", wqkv.t[:, :, c * 512:(c + 1) * 512], w_in_v[:, :, c * 512:(c + 1) * 512], [], [wqkv])
                dma("pool", brow.t[:], bin_row_d[0:1, 0:1536], [], [brow])
                dma("sp", gq.t[:], qg_d.partition_broadcast(128), [], [gq]); dma("sp", gk.t[:], kg_d.partition_broadcast(128), [], [gk])
                cnt = {"n": 0}

                def qknorm(ps, gt, xt, dstT, dbuf, dcol, psT, pcol, rc=None, rs_=None):
                    i = cnt["n"]; cnt["n"] += 1
                    sq = sqt[i % 2]; sm = ssq[i % 2]; q1 = qn[i % 2]; q2 = qg2[i % 2]; u_ = ru[i % 2]; w_ = rw_[i % 2]; o_ = qr[i % 4]
                    def part_a():
                        act(sq.t[:], ps.t[:], AF.Square, [ps], [sq])
                        yield
                        S_.op("dve", lambda e: e.tensor_reduce(sm.t[:], sq.t[:].rearrange("p (g d) -> p g d", d=64), AX.X, ALU.add), [sq.b], [sm.b])
                        yield
                        act(sm.t[:], sm.t[:], AF.Sqrt, [sm, epst], [sm], scale=1.0 / 64, bias=epst.t[:])
                        yield
                        recip(sm.t[:], sm.t[:], [sm], [sm])
                        yield
                        tt("dve", q1.t[:].rearrange("p (g d) -> p g d", d=64), ps.t[:].rearrange("p (g d) -> p g d", d=64),
                           sm.t[:].unsqueeze(2).broadcast_to([128, 8, 64]), ALU.mult, [ps, sm], [q1])
                        yield
                        if xt is None:
                            tt("pool", o_.t[:].rearrange("p (g d) -> p g d", d=64), q1.t[:].rearrange("p (g d) -> p g d", d=64),
                               gt.t[:].unsqueeze(1).broadcast_to([128, 8, 64]), ALU.mult, [q1, gt], [o_])
                            yield
                        else:
                            tt("pool", u_.t[:].rearrange("p (g d) -> p g d", d=64), q1.t[:].rearrange("p (g d) -> p g d", d=64),
                               rc.t[:].unsqueeze(1).broadcast_to([128, 8, 64]), ALU.mult, [q1, rc], [u_])
                            yield
                            tt("dve", w_.t[:].rearrange("p (g d) -> p g d", d=64), q1.t[:].rearrange("p (g d) -> p g d", d=64),
                               rs_.t[:].unsqueeze(1).broadcast_to([128, 8, 64]), ALU.mult, [q1, rs_], [w_])
                            yield
                            u4 = u_.t[:].rearrange("p (a h f) -> p a h f", h=2, f=16)
                            w4 = w_.t[:].rearrange("p (a h f) -> p a h f", h=2, f=16)
                            o4 = o_.t[:].rearrange("p (a h f) -> p a h f", h=2, f=16)
                            tt("dve", o4[:, :, 0, :], u4[:, :, 0, :], w4[:, :, 1, :], ALU.add, [u_, w_], [o_])
                            yield
                            tt("dve", o4[:, :, 1, :], u4[:, :, 1, :], w4[:, :, 0, :], ALU.add, [u_, w_], [o_])
                            yield
                    def part_b():
                        pb = psbf(psT)
                        for h in range(4):
                            tr(pb[:, pcol + h * 128: pcol + (h + 1) * 128], o_.t[:, h * 128:(h + 1) * 128], ident.t[:], [o_, ident], [PS[psT]])
                        cp("dve", dstT.t[:, :, dcol:dcol + 128], pb[:, pcol:pcol + 512].rearrange("p (h t) -> p h t", t=128), [PS[psT]], [dbuf])
                    return part_a(), part_b

                tno = 0
                pend_b = []
                blocks = [("c", 0, NC)] + [("x", b, TPB) for b in range(NB)]
                for bi, (kind, b, ntl) in enumerate(blocks):
                    hb = hblk[bi % 2]
                    if kind == "c":
                        dma("sp", hb.t[:, :, 0:CTXL], hTc_d, [B_hTc], [hb])
                    else:
                        dma("sp", hb.t[:], hT_d[:, :, b * BLK:(b + 1) * BLK], [B_hT[b]], [hb])
                    for s in range(ntl):
                        kt = s if kind == "c" else NC + b * TPB + s
                        xt = None if kind == "c" else b * TPB + s
                        st3 = (tno % 2) * 3
                        psT = 6 + (tno % 2)
                        tno += 1
                        lh = lambda k: hb.t[:, k, s * 128:(s + 1) * 128]
                        for c in range(3):
                            if c == 0 and kind == "c":
                                continue
                            pp = PS[st3 + c]
                            for k in range(8):
                                mm(pp.t[:], lh(k), wqkv.t[:, k, c * 512:(c + 1) * 512], k == 0, False, [hb, wqkv], [pp])
                            mm(pp.t[:], onesb.t[0:1, :], brow.t[0:1, c * 512:(c + 1) * 512], False, True, [onesb, brow], [pp])
                        act(V.t[:, kt, :], PS[st3 + 2].t[:], AF.Copy, [PS[st3 + 2]], [V.bs[kt]])
                        rc = rs_ = None
                        if kind == "x":
                            rc = rcs[xt % 3]; rs_ = rss[xt % 3]
                            dma("sp", rc.t[:], ropec_d[xt * 128:(xt + 1) * 128, :], [], [rc])
                            dma("sp", rs_.t[:], ropes_d[xt * 128:(xt + 1) * 128, :], [], [rs_])
                            gt4 = gtab[xt % 3]
                            tt("pool", gt4[0].t[:], rc.t[:], gq.t[:], ALU.mult, [rc, gq], [gt4[0]]); tt("pool", gt4[1].t[:], rs_.t[:], gq.t[:], ALU.mult, [rs_, gq], [gt4[1]])
                            tt("pool", gt4[2].t[:], rc.t[:], gk.t[:], ALU.mult, [rc, gk], [gt4[2]]); tt("pool", gt4[3].t[:], rs_.t[:], gk.t[:], ALU.mult, [rs_, gk], [gt4[3]])
                            pairs = [qknorm(PS[st3 + 0], gq, xt, QT, QT.bs[xt], xt * 128, psT, 0, gt4[0], gt4[1]),
                                     qknorm(PS[st3 + 1], gk, xt, KT, KT.bs[kt], kt * 128, psT, 512, gt4[2], gt4[3])]
                        else:
                            pairs = [qknorm(PS[st3 + 1], gk, xt, KT, KT.bs[kt], kt * 128, psT, 512, None, None)]
                        gens = [p_[0] for p_ in pairs]
                        while gens:
                            gens = [g_ for g_ in gens if next(g_, "done") != "done"]
                        newb = [p_[1] for p_ in pairs]
                        for fb_ in pend_b:
                            fb_()
                        pend_b = newb
                for fb_ in pend_b:
                    fb_()
                S_.barrier()

            with contextlib.ExitStack() as es:
                def sb(name, shape, dt=F32, nb=1):
                    return T(es.enter_context(nc.sbuf_tensor("s_" + name, list(shape), dt)), nb)
                pt2 = [sb("pt2_%d" % i, [128, 2, 512], BF16) for i in range(3)]
                r0 = sb("r0", [128, BLK]); r1 = sb("r1", [128, BLK]); t0 = sb("t0", [128, BLK]); t1 = sb("t1", [128, BLK])
                dd = sb("dd", [128, BLK]); dsq = sb("dsq", [128, BLK]); rsd = sb("rsd", [128, BLK])
                oat = [sb("oat%d" % i, [128, BLK], BF16) for i in range(2)]
                o_acc = [PS[0], PS[1]]; s_acc = [PS[2], PS[3]]
                scp = [[PS[4], PS[5]], [PS[6], PS[7]]]
                zt = sb("zt", [128, 2 * D], BF16)
                mset("pool", zt.t[:], 0.0, [zt])
                Xs_z = Xs_d.rearrange("(c p r) d -> c p (r d)", p=128, r=2)
                for c_ in range(NZ):
                    dma("pool", Xs_z[c_], zt.t[:], [zt], [B_Xs[c_]])
                for e_ in range(NE):
                    w1src = ew1_d[e_].rearrange("(k p) n -> p k n", p=128)
                    w1dst = W1b_d[e_ * 128:(e_ + 1) * 128, :].rearrange("p (k n) -> p k n", k=8)
                    for k0 in (0, 4):
                        dma("pool", w1dst[:, k0:k0 + 4, :], w1src[:, k0:k0 + 4, :], [], [])
                    w2src = ew2_d[e_].rearrange("(k p) n -> p k n", p=128)
                    w2dst = W2b_d[e_ * 128:(e_ + 1) * 128, :].rearrange("p (k n) -> p k n", k=8)
                    dma("pool", w2dst, w2src, [], [])
                its = [(h, qb, kt) for h in range(4) for qb in range(NB) for kt in range(NKT)]

                def scores(n_):
                    h, qb, kt = its[n_]
                    sp_ = scp[n_ % 2]
                    for m in range(2):
                        mm(sp_[m].t[:, 0:BLK], KT.t[m * 64:(m + 1) * 64, h, kt * 128:(kt + 1) * 128],
                           QT.t[m * 64:(m + 1) * 64, h, qb * BLK:(qb + 1) * BLK], True, True,
                           [KT.bs[kt]] + [QT.bs[qb * TPB + j] for j in range(TPB)], [sp_[m]])
                o0s = sb("o0s", [128, BLK]); o1s = sb("o1s", [128, BLK]); ssb = sb("ssb", [128, BLK]); w32 = sb("w32", [128, 128])
                mset("dve", w32.t[:], 1.0 / 32, [w32])
                sbank = PS[2]; fbank = PS[3]

                def finalize_gen(h, qb):
                    recip(ssb.t[0:64, :], ssb.t[0:64, :], [ssb], [ssb])
                    mm(fbank.t[:, 0:BLK], w32.t[0:32, :], ssb.t[0:32, :], True, True, [w32, ssb], [fbank])
                    yield
                    tt("dve", t0.t[:], o0s.t[:], fbank.t[:, 0:BLK], ALU.mult, [o0s, fbank], [t0])
                    mm(fbank.t[:, 0:BLK], w32.t[32:64, :], ssb.t[32:64, :], True, True, [w32, ssb], [fbank])
                    yield
                    tt("dve", t1.t[:], o1s.t[:], fbank.t[:, 0:BLK], ALU.mult, [o1s, fbank], [t1])
                    stt("dve", dd.t[:], t1.t[:], neglam.t[:, 0:1], t0.t[:], ALU.mult, ALU.add, [t0, t1, neglam], [dd])
                    tt("dve", dsq.t[:], dd.t[:], dd.t[:], ALU.mult, [dd], [dsq])
                    yield
                    mm(fbank.t[:, 0:BLK], onesf.t[:], dsq.t[:], True, True, [onesf, dsq], [fbank])
                    yield
                    act(rsd.t[:], fbank.t[:, 0:BLK], AF.Sqrt, [fbank, epst], [rsd], scale=1.0 / 128, bias=epst.t[:])
                    recip(rsd.t[:], rsd.t[:], [rsd], [rsd])
                    oo = oat[(h * NB + qb) % 2]
                    stt("dve", oo.t[:], dd.t[:], gsub.t[:, 0:1], rsd.t[:], ALU.mult, ALU.mult, [dd, gsub, rsd], [oo])
                    dma("sp", oaT_d[:, h, qb * BLK:(qb + 1) * BLK], oo.t[:], [oo], [B_oaT[qb]])

                pending = []
                scores(0)
                for it in range(len(its)):
                    h, qb, kt = its[it]
                    if it + 1 < len(its):
                        scores(it + 1)
                    sp_ = scp[it % 2]
                    p2 = pt2[it % 3]
                    bank0 = 4 + 2 * (it % 2)
                    act(p2.t[:, :, 0:BLK], psall[:, bank0 * 512:(bank0 + 2) * 512].rearrange("p (m q) -> p m q", m=2)[:, :, 0:BLK], AF.Exp,
                        [sp_[0], sp_[1]], [p2], scale=0.125)
                    for m in range(2):
                        mm(o_acc[m].t[:, 0:BLK], V.t[:, kt, h * 128:(h + 1) * 128], p2.t[:, m, 0:BLK], kt == 0, kt == NKT - 1, [V.bs[kt], p2], [o_acc[m]])
                    for m in range(2):
                        mm(sbank.t[32 * m:32 * (m + 1), 0:BLK], onesb.t[:, 32 * m:32 * (m + 1)], p2.t[:, m, 0:BLK], kt == 0, kt == NKT - 1,
                           [onesb, p2], [sbank], tp=(0, 32 * m))
                    if pending and kt >= 1:
                        if next(pending[0], "done") == "done":
                            pending.pop(0)
                    if kt == NKT - 1:
                        for g_ in pending:
                            for _ in g_:
                                pass
                        pending = []
                        S_.op("act", lambda e: e.copy(o0s.t[:], o_acc[0].t[:, 0:BLK]), [o_acc[0].b], [o0s.b])
                        cp("dve", o1s.t[:], o_acc[1].t[:, 0:BLK], [o_acc[1]], [o1s])
                        cp("dve", ssb.t[0:64, :], sbank.t[0:64, 0:BLK], [sbank], [ssb])
                        pending.append(finalize_gen(h, qb))
                for g_ in pending:
                    for _ in g_:
                        pass
                S_.barrier()

        with contextlib.ExitStack() as esh:
            def sbh(name, shape, dt=F32, nb=1):
                return T(esh.enter_context(nc.sbuf_tensor("s_" + name, list(shape), dt)), nb)
            u_tm = sbh("u_tm", [128, NT, 512], BF16, nb=NT)
            with contextlib.ExitStack() as es:
                def sb(name, shape, dt=F32, nb=1):
                    return T(es.enter_context(nc.sbuf_tensor("s_" + name, list(shape), dt)), nb)
                hring = [sb("h0hb%d" % i, [128, 8, BLK], BF16) for i in range(2)]; whY = sb("whY", [128, 8, 1536], BF16)
                bhy = sb("bhy", [128, 40]); cw = sb("cw", [128, 12, 3]); cb = sb("cb", [128, 12])
                ppads = [[sb("ppad%d_%d" % (i, c), [128, S + 2], BF16) for c in range(3)] for i in range(2)]
                zf = sb("zf", [128, S])
                zX = sb("zX", [128, S], BF16); zA = sb("zA", [128, S], BF16); zB = sb("zB", [128, S], BF16); uTb = sb("uTb", [128, S], BF16)
                for c in range(3):
                    dma("pool", whY.t[:, :, c * 512:(c + 1) * 512], w_in_v[:, :, 1536 + c * 512:1536 + (c + 1) * 512], [], [whY])
                dma("sp", bhy.t[:], bin_fm_d, [], [bhy]); dma("sp", cw.t[:], convw_d, [], [cw]); dma("sp", cb.t[:], convb_d, [], [cb])
                for i in range(2):
                    for c in range(3):
                        mset("pool", ppads[i][c].t[:, 0:1], 0.0, [ppads[i][c]]); mset("pool", ppads[i][c].t[:, S + 1:S + 2], 0.0, [ppads[i][c]])
                zn = 0
                pend_tr = None
                for j in range(4):
                    pset = ppads[j % 2]
                    for b in range(NB):
                        hb = hring[b % 2]
                        dma("sp", hb.t[:], hT_d[:, :, b * BLK:(b + 1) * BLK], [B_hT[b]], [hb])
                        for c3 in range(3):
                            ch = c3 * 4 + j
                            pp = PS[zn % 4]; zn += 1
                            for k in range(8):
                                mm(pp.t[:, 0:BLK], whY.t[:, k, ch * 128:(ch + 1) * 128], hb.t[:, k, :], k == 0, k == 7, [whY, hb], [pp])
                            act(pset[c3].t[:, 1 + b * BLK:1 + (b + 1) * BLK], pp.t[:, 0:BLK], AF.Identity, [pp, bhy], [pset[c3]], bias=bhy.t[:, 12 + ch:13 + ch])
                    if pend_tr is not None:
                        pend_tr()
                        pend_tr = None
                    for c3, out in ((0, zX), (1, zA), (2, zB)):
                        ch = c3 * 4 + j
                        pp_ = pset[c3]
                        ts("dve", zf.t[:], pp_.t[:, 0:S], cw.t[:, ch, 0:1], cb.t[:, ch:ch + 1], ALU.mult, ALU.add, [pp_, cw, cb], [zf])
                        stt("dve", zf.t[:], pp_.t[:, 1:S + 1], cw.t[:, ch, 1:2], zf.t[:], ALU.mult, ALU.add, [pp_, cw, zf], [zf])
                        stt("dve", out.t[:], pp_.t[:, 2:S + 2], cw.t[:, ch, 2:3], zf.t[:], ALU.mult, ALU.add, [pp_, cw, zf], [out])
                    dma("sp", x0c_d[:, j, :], zX.t[:], [zX], [B_x0c[j]])
                    tt("pool", uTb.t[:], zA.t[:], zB.t[:], ALU.mult, [zA, zB], [uTb])
                    dma("sp", uT_d[:, j, :], uTb.t[:], [uTb], [B_uT[j]])
                    def do_tr(j=j):
                        for t0_ in range(0, NT, 8):
                            nt_ = min(8, NT - t0_)
                            psi = 4 + ((j * NT + t0_) // 8) % 2
                            pb = psbf(psi)
                            for t_ in range(nt_):
                                tr(pb[:, t_ * 128:(t_ + 1) * 128], uTb.t[:, (t0_ + t_) * 128:(t0_ + t_ + 1) * 128], ident.t[:], [uTb, ident], [PS[psi]])
                            cp("dve", u_tm.t[:, t0_:t0_ + nt_, j * 128:(j + 1) * 128], pb[:, 0:nt_ * 128].rearrange("p (t c) -> p t c", c=128),
                               [PS[psi]], [u_tm.bs[t] for t in range(t0_, t0_ + nt_)])
                    pend_tr = do_tr
                pend_tr()
                S_.barrier()

            with contextlib.ExitStack() as esk:
                ksum = T(esk.enter_context(nc.sbuf_tensor("s_ksum", [128, NT, 512], BF16)), NT)
                kdiff = T(esk.enter_context(nc.sbuf_tensor("s_kdiff", [128, NT, 512], BF16)), NT)
                with contextlib.ExitStack() as es:
                    def sb(name, shape, dt=F32, nb=1):
                        return T(es.enter_context(nc.sbuf_tensor("s_" + name, list(shape), dt)), nb)
                    zT = sb("zT", [33, S]); fw1 = sb("fw1", [33, 64]); fw2 = sb("fw2", [64, 64]); fw3 = sb("fw3", [64, 64]); fw4 = sb("fw4", [64, 1024])
                    fvec = sb("fvec", [64, 4]); trow = sb("trow", [1, S]); drow = sb("drow", [1, 512])
                    H3 = sb("H3", [64, S])
                    arg = sb("arg", [64, BLK]); ai = sb("ai", [64, BLK], I32); af = sb("af", [64, BLK]); hh = [sb("hh%d" % i, [64, BLK]) for i in range(2)]
                    dec = sb("dec", [128, 512]); kf = sb("kf", [128, 512]); kb = sb("kb", [128, 512])
                    for t_, d_ in [(zT, zT_d), (fw1, fw1_d), (fw2, fw2_d), (fw3, fw3_d), (fw4, fw4_d), (fvec, fvec_d), (trow, trow_d), (drow, drow_d)]:
                        dma("sp", t_.t[:], d_, [], [t_])

                    def sin_layer(ps, li, out_ap, out_t):
                        ts("dve", arg.t[:], ps.t[0:64, 0:BLK], fvec.t[:, li:li + 1], fvec.t[:, 3:4], ALU.add, ALU.mult, [ps, fvec], [arg])
                        ts("dve", arg.t[:], arg.t[:], 1.0 / (2 * PI), 16.0, ALU.mult, ALU.add, [arg], [arg])
                        cp("dve", ai.t[:], arg.t[:], [arg], [ai])
                        cp("dve", af.t[:], ai.t[:], [ai], [af])
                        tt("dve", arg.t[:], arg.t[:], af.t[:], ALU.subtract, [arg, af], [arg])
                        ts("dve", af.t[:], arg.t[:], 0.5, None, ALU.is_gt, None, [arg], [af])
                        tt("dve", arg.t[:], arg.t[:], af.t[:], ALU.subtract, [arg, af], [arg])
                        act(out_ap, arg.t[:], AF.Sin, [arg], [out_t], scale=2 * PI)

                    for b in range(NB):
                        sl = slice(b * BLK, (b + 1) * BLK)
                        mm(PS[0].t[0:64, 0:BLK], fw1.t[:], zT.t[:, sl], True, True, [fw1, zT], [PS[0]])
                        sin_layer(PS[0], 0, hh[0].t[:], hh[0])
                        mm(PS[1].t[0:64, 0:BLK], fw2.t[:], hh[0].t[:], True, True, [fw2, hh[0]], [PS[1]])
                        sin_layer(PS[1], 1, hh[1].t[:], hh[1])
                        mm(PS[2].t[0:64, 0:BLK], fw3.t[:], hh[1].t[:], True, True, [fw3, hh[1]], [PS[2]])
                        sin_layer(PS[2], 2, H3.t[:, sl], H3)
                    for lt in range(NT):
                        pf = PS[(lt % 2) * 3]; pb_ = PS[(lt % 2) * 3 + 1]; pd = PS[(lt % 2) * 3 + 2]
                        mm(pf.t[:], H3.t[:, lt * 128:(lt + 1) * 128], fw4.t[:, 0:512], True, True, [H3, fw4], [pf])
                        mm(pb_.t[:], H3.t[:, lt * 128:(lt + 1) * 128], fw4.t[:, 512:1024], True, True, [H3, fw4], [pb_])
                        mm(pd.t[:], trow.t[0:1, lt * 128:(lt + 1) * 128], drow.t[0:1, :], True, True, [trow, drow], [pd])
                        act(dec.t[:], pd.t[:], AF.Exp, [pd], [dec], scale=-1.0)
                        stt("dve", kf.t[:], pf.t[:], 2.0 / N2, dec.t[:], ALU.mult, ALU.mult, [pf, dec], [kf])
                        stt("dve", kb.t[:], pb_.t[:], 2.0 / N2, dec.t[:], ALU.mult, ALU.mult, [pb_, dec], [kb])
                        if lt == 0:
                            mset("dve", kb.t[0:1, :], 0.0, [kb])
                        tt("pool", ksum.t[:, lt, :], kf.t[:], kb.t[:], ALU.add, [kf, kb], [ksum.bs[lt]])
                        tt("pool", kdiff.t[:, lt, :], kb.t[:], kf.t[:], ALU.subtract, [kf, kb], [kdiff.bs[lt]])
                    S_.barrier()

                with contextlib.ExitStack() as es:
                    def sb(name, shape, dt=F32, nb=1):
                        return T(es.enter_context(nc.sbuf_tensor("s_" + name, list(shape), dt)), nb)
                    cf = [sb("cf%d" % i, [128, NT, 128], BF16) for i in range(2)]; sf = [sb("sf%d" % i, [128, NT, 128], BF16) for i in range(2)]
                    kre = sb("kre", [128, 512]); kim = sb("kim", [128, 512]); m1 = sb("m1", [128, 512]); m2 = sb("m2", [128, 512])
                    yt = [sb("yt%d" % i, [128, 2, 512], BF16) for i in range(2)]
                    for kc in range(NKC):
                        c_ = cf[kc % 2]; s_ = sf[kc % 2]
                        if kc == 0:
                            dma("sp", c_.t[:], Cf_d[0], [], [c_]); dma("sp", s_.t[:], Sf_d[0], [], [s_])
                        if kc + 1 < NKC:
                            cn_ = cf[(kc + 1) % 2]; sn_ = sf[(kc + 1) % 2]
                            dma("sp", cn_.t[:], Cf_d[kc + 1], [], [cn_]); dma("sp", sn_.t[:], Sf_d[kc + 1], [], [sn_])
                        pa = PS[(kc % 2) * 4: (kc % 2) * 4 + 4]
                        for t_ in range(NT):
                            st_, sp2 = (t_ == 0), (t_ == NT - 1)
                            mm(pa[0].t[:], c_.t[:, t_, :], u_tm.t[:, t_, :], st_, sp2, [c_, u_tm.bs[t_]], [pa[0]])
                            mm(pa[1].t[:], s_.t[:, t_, :], u_tm.t[:, t_, :], st_, sp2, [s_, u_tm.bs[t_]], [pa[1]])
                            mm(pa[2].t[:], c_.t[:, t_, :], ksum.t[:, t_, :], st_, sp2, [c_, ksum.bs[t_]], [pa[2]])
                            mm(pa[3].t[:], s_.t[:, t_, :], kdiff.t[:, t_, :], st_, sp2, [s_, kdiff.bs[t_]], [pa[3]])
                        y_ = yt[kc % 2]
                        act(kre.t[:], pa[2].t[:], AF.Copy, [pa[2]], [kre]); act(kim.t[:], pa[3].t[:], AF.Copy, [pa[3]], [kim])
                        tt("dve", m1.t[:], pa[0].t[:], kre.t[:], ALU.mult, [pa[0], kre], [m1])
                        tt("dve", m2.t[:], pa[1].t[:], kim.t[:], ALU.mult, [pa[1], kim], [m2])
                        tt("pool", y_.t[:, 0, :], m1.t[:], m2.t[:], ALU.add, [m1, m2], [y_])
                        tt("dve", m1.t[:], pa[0].t[:], kim.t[:], ALU.mult, [pa[0], kim], [m1])
                        tt("dve", m2.t[:], pa[1].t[:], kre.t[:], ALU.mult, [pa[1], kre], [m2])
                        tt("pool", y_.t[:, 1, :], m1.t[:], m2.t[:], ALU.subtract, [m1, m2], [y_])
                        dma("sp", Y_d[:, :, kc, :], y_.t[:], [y_], [B_Y[kc]])
                    S_.barrier()

        with contextlib.ExitStack() as es:
            def sb(name, shape, dt=F32, nb=1):
                return T(es.enter_context(nc.sbuf_tensor("s_" + name, list(shape), dt)), nb)
            Yall = sb("Yall", [128, 2, NKC, 512], BF16, nb=NKC)
            ci = [sb("ci%d" % i, [128, KG, BLK], BF16) for i in range(3)]; si = [sb("si%d" % i, [128, KG, BLK], BF16) for i in range(3)]
            x0b = [sb("x0b%d" % i, [128, 4, BLK], BF16) for i in range(2)]; uTk = [sb("uTk%d" % i, [128, 4, BLK], BF16) for i in range(2)]
            obb = [sb("obb%d" % i, [128, 4, BLK], BF16) for i in range(2)]
            tmp = [sb("h2tmp%d" % i, [128, BLK]) for i in range(2)]; hbv = sb("hbv", [128, 4])
            dma("sp", hbv.t[:], hb_d, [], [hbv])
            def load_y(kg_):
                for kc_ in range(kg_ * KG, (kg_ + 1) * KG):
                    dma("sp", Yall.t[:, :, kc_, :], Y_d[:, :, kc_, :], [B_Y[kc_]], [Yall.bs[kc_]])
            load_y(0)
            n = 0
            for tb in range(NB):
                acc = PS[(tb % 2) * 4:(tb % 2) * 4 + 4]
                xb = x0b[tb % 2]; ub = uTk[tb % 2]; ob = obb[tb % 2]
                dma("sp", xb.t[:], x0c_d[:, :, tb * BLK:(tb + 1) * BLK], B_x0c, [xb])
                dma("sp", ub.t[:], uT_d[:, :, tb * BLK:(tb + 1) * BLK], B_uT, [ub])
                for kg in range(NKG):
                    c_ = ci[n % 3]; s_ = si[n % 3]; n += 1
                    dma("sp", c_.t[:], Ci_d[tb, :, kg * KG:(kg + 1) * KG, :], [], [c_])
                    dma("sp", s_.t[:], Si_d[tb, :, kg * KG:(kg + 1) * KG, :], [], [s_])
                    if tb == 0 and kg + 1 < NKG:
                        load_y(kg + 1)
                    for kl in range(KG):
                        kc = kg * KG + kl
                        for c4 in range(4):
                            mm(acc[c4].t[:, 0:BLK], Yall.t[:, 0, kc, c4 * 128:(c4 + 1) * 128], c_.t[:, kl, :], kc == 0, False, [Yall.bs[kc], c_], [acc[c4]])
                            mm(acc[c4].t[:, 0:BLK], Yall.t[:, 1, kc, c4 * 128:(c4 + 1) * 128], s_.t[:, kl, :], False, kc == NKC - 1, [Yall.bs[kc], s_], [acc[c4]])
                for c4 in range(4):
                    tm = tmp[c4 % 2]
                    stt("dve", tm.t[:], ub.t[:, c4, :], hbv.t[:, c4:c4 + 1], acc[c4].t[:, 0:BLK], ALU.mult, ALU.add, [ub, hbv, acc[c4]], [tm])
                    tt("pool", ob.t[:, c4, :], tm.t[:], xb.t[:, c4, :], ALU.mult, [tm, xb], [ob])
                dma("sp", obT_d[:, :, tb * BLK:(tb + 1) * BLK], ob.t[:], [ob], [B_obT[tb]])
            S_.barrier()

        with contextlib.ExitStack() as es:
            def sb(name, shape, dt=F32, nb=1):
                return T(es.enter_context(nc.sbuf_tensor("s_" + name, list(shape), dt)), nb)
            wg = sb("wg", [128, 8, 2048], BF16); wua = sb("wua", [128, 4, D], BF16); wub = sb("wub", [128, 4, D], BF16)
            wout = sb("wout", [128, 8, D], BF16); rwt = sb("rwt", [128, 8, 32], BF16); rbt = sb("rbt", [1, 32], BF16)
            bgf = sb("bgf", [128, 40])
            hblk = [sb("mhb%d" % i, [128, 8, BLK], BF16) for i in range(2)]
            oab = [sb("moa%d" % i, [128, 4, BLK], BF16) for i in range(2)]; obk = [sb("mob%d" % i, [128, 4, BLK], BF16) for i in range(2)]
            gas = [sb("gas%d" % i, [128, BLK]) for i in range(2)]; gbs = [sb("gbs%d" % i, [128, BLK]) for i in range(2)]
            mm1 = [sb("mm1%d" % i, [128, BLK]) for i in range(1)]; mm2 = [sb("mm2%d" % i, [128, BLK]) for i in range(1)]
            mTs = [sb("mT%d" % i, [128, 8, BLK], BF16, nb=8) for i in range(2)]
            xin = [sb("mxin%d" % i, [128, D]) for i in range(2)]; xn = [sb("mxn%d" % i, [128, D]) for i in range(2)]
            tg = sb("mtg", [128, 512])
            h2b = [sb("h2b%d" % i, [128, 8, BLK], BF16) for i in range(2)]
            msk = sb("msk", [128, 32]); mskb = sb("mskb", [128, 32], BF16); nmx = sb("nmx", [128, 1])
            htmp = sb("htmp", [128, D]); h2tm = [sb("h2tm%d" % i, [128, D], BF16) for i in range(1)]
            ex = sb("ex", [128, 32]); sm_ = sb("sm", [128, 1])
            st = norm_transpose(es, "m")
            for c in range(4):
                dma("pool", wg.t[:, :, c * 512:(c + 1) * 512], w_in_v[:, :, 3072 + c * 512:3072 + (c + 1) * 512], [], [wg])
            dma("pool", wua.t[:], wua_d.rearrange("(k p) n -> p k n", p=128), [], [wua])
            dma("pool", wub.t[:], wub_d.rearrange("(k p) n -> p k n", p=128), [], [wub])
            for c in range(2):
                dma("pool", wout.t[:, :, c * 512:(c + 1) * 512], wout_d.rearrange("(k p) n -> p k n", p=128)[:, :, c * 512:(c + 1) * 512], [], [wout])
            dma("pool", rwt.t[:], rw_d.rearrange("(k p) n -> p k n", p=128), [], [rwt]); dma("pool", rbt.t[:], rb_d, [], [rbt])
            dma("sp", bgf.t[:], bin_fm_d, [], [bgf])
            tno = 0

            def gates(tb):
                hb = hblk[tb % 2]; oa = oab[tb % 2]; ob = obk[tb % 2]; mT = mTs[tb % 2]
                dma("sp", hb.t[:], hT_d[:, :, tb * BLK:(tb + 1) * BLK], [B_hT[tb]], [hb])
                dma("sp", oa.t[:], oaT_d[:, :, tb * BLK:(tb + 1) * BLK], [B_oaT[tb]], [oa])
                dma("sp", ob.t[:], obT_d[:, :, tb * BLK:(tb + 1) * BLK], [B_obT[tb]], [ob])
                for j in range(8):
                    pga, pgb, pA, pB = PS[0], PS[1], PS[2], PS[3]
                    for k in range(8):
                        mm(pga.t[:, 0:BLK], wg.t[:, k, j * 128:(j + 1) * 128], hb.t[:, k, :], k == 0, k == 7, [wg, hb], [pga])
                    for k in range(8):
                        mm(pgb.t[:, 0:BLK], wg.t[:, k, 1024 + j * 128:1024 + (j + 1) * 128], hb.t[:, k, :], k == 0, k == 7, [wg, hb], [pgb])
                    for k in range(4):
                        mm(pA.t[:, 0:BLK], wua.t[:, k, j * 128:(j + 1) * 128], oa.t[:, k, :], k == 0, k == 3, [wua, oa], [pA])
                    for k in range(4):
                        mm(pB.t[:, 0:BLK], wub.t[:, k, j * 128:(j + 1) * 128], ob.t[:, k, :], k == 0, k == 3, [wub, ob], [pB])
                    ga_ = gas[j % 2]; gb_ = gbs[j % 2]; a1 = mm1[0]; a2 = mm2[0]
                    act(ga_.t[:], pga.t[:, 0:BLK], AF.Sigmoid, [pga, bgf], [ga_], bias=bgf.t[:, 24 + j:25 + j])
                    act(gb_.t[:], pgb.t[:, 0:BLK], AF.Sigmoid, [pgb, bgf], [gb_], bias=bgf.t[:, 32 + j:33 + j])
                    tt("dve", a1.t[:], pA.t[:, 0:BLK], ga_.t[:], ALU.mult, [pA, ga_], [a1])
                    tt("dve", a2.t[:], pB.t[:, 0:BLK], gb_.t[:], ALU.mult, [pB, gb_], [a2])
                    tt("pool", mT.t[:, j, :], a1.t[:], a2.t[:], ALU.add, [a1, a2], [mT.bs[j]])

            def tiles(tb):
                nonlocal tno
                h2 = h2b[tb % 2]; mT = mTs[tb % 2]
                for s in range(TPB):
                    i = tb * TPB + s
                    xi = xin[tno % 2]; xo = xn[tno % 2]; tno += 1
                    dma("sp", xi.t[:], x_d[i * 128:(i + 1) * 128, :], [], [xi])
                    for half in range(2):
                        po = PS[4 + half]
                        for j in range(8):
                            mm(po.t[:], mT.t[:, j, s * 128:(s + 1) * 128], wout.t[:, j, half * 512:(half + 1) * 512], j == 0, j == 7, [mT.bs[j], wout], [po])
                        tt("dve", tg.t[:], po.t[:], g1row.t[:, half * 512:(half + 1) * 512], ALU.mult, [po, g1row], [tg])
                        tt("pool", xo.t[:, half * 512:(half + 1) * 512], tg.t[:], xi.t[:, half * 512:(half + 1) * 512], ALU.add, [tg, xi], [xo])
                    dma("sp", y_d[i * 128:(i + 1) * 128, :], xo.t[:], [xo], [B_y[i]])
                    ss_t = do_norm_transpose(st, xo, A2, B2, 6, lambda k, s=s, h2=h2: h2.t[:, k, s * 128:(s + 1) * 128], [h2])
                    hm = h2tm[0]
                    stt("dve", htmp.t[:], xo.t[:], ss_t.t[:, 0:1], A2row.t[:], ALU.mult, ALU.mult, [xo, ss_t, A2row], [htmp])
                    tt("pool", hm.t[:], htmp.t[:], B2row.t[:], ALU.add, [htmp, B2row], [hm])
                    dma("sp", H2tm_d[i * 128:(i + 1) * 128, :], hm.t[:], [hm], [B_h2tm[i]])
                    pr = PS[7]
                    for k in range(8):
                        mm(pr.t[:, 0:32], h2.t[:, k, s * 128:(s + 1) * 128], rwt.t[:, k, :], k == 0, False, [h2, rwt], [pr])
                    mm(pr.t[:, 0:32], onesb.t[0:1, :], rbt.t[0:1, :], False, True, [onesb, rbt], [pr])
                    lgi = LG.t[:, i, :]
                    act(lgi, pr.t[:, 0:32], AF.Copy, [pr], [LG.bs[i]])
                    vmax8(MX.t[:, i, :], lgi, [LG.bs[i]], [MX.bs[i]])
                    ts("dve", msk.t[:], lgi, MX.t[:, i, 3:4], None, ALU.is_ge, None, [LG.bs[i], MX.bs[i]], [msk])
                    cp("dve", mskb.t[:], msk.t[:], [msk], [mskb])
                    ts("dve", nmx.t[:], MX.t[:, i, 0:1], -1.0, None, ALU.mult, None, [MX.bs[i]], [nmx])
                    act(ex.t[:], lgi, AF.Exp, [LG.bs[i], nmx], [ex], bias=nmx.t[:, 0:1])
                    tt("dve", ex.t[:], ex.t[:], msk.t[:], ALU.mult, [ex, msk], [ex])
                    red(sm_.t[:], ex.t[:], [ex], [sm_])
                    recip(sm_.t[:], sm_.t[:], [sm_], [sm_])
                    ts("dve", G.t[:, i, :], ex.t[:], sm_.t[:, 0:1], None, ALU.mult, None, [ex, sm_], [G.bs[i]])
                    mm(pr.t[:, 32:64], ustr.t[:], mskb.t[:], True, False, [ustr, mskb], [pr])
                    mm(pr.t[:, 32:64], onesb.t[:], cumb.t[:], False, True, [onesb, cumb], [pr])
                    cp("dve", POS.t[:, i, :], pr.t[:, 32:64], [pr], [POS.bs[i]])
                    tt("dve", cumf.t[:], cumf.t[:], msk.t[:], ALU.add, [cumf, msk], [cumf])
                    cp("dve", cumb.t[:], cumf.t[:], [cumf], [cumb])

            gates(0)
            for tb in range(NB):
                if tb + 1 < NB:
                    gates(tb + 1)
                tiles(tb)
            S_.barrier()
        if debug:
            with contextlib.ExitStack() as es3:
                dt_ = T(es3.enter_context(nc.sbuf_tensor("s_dbgt", [128, 8, S], F32)))
                db_ = T(es3.enter_context(nc.sbuf_tensor("s_dbgb", [128, 8, S], BF16)))
                for nm, src, nch in [("dbg_oaT", oaT_d, 4), ("dbg_obT", obT_d, 4), ("dbg_hT", hT_d, 8)]:
                    dma("sp", db_.t[:, 0:nch, :], src, [], [db_])
                    cp("dve", dt_.t[:, 0:nch, :], db_.t[:, 0:nch, :], [db_], [dt_])
                    dma("sp", dbg[nm], dt_.t[:, 0:nch, :], [dt_], [Buf()])
                    S_.barrier()
                dma("sp", dbg["dbg_G"], G.t[:], G.bs, [Buf()])
                S_.barrier()

        with contextlib.ExitStack() as es:
            def sb(name, shape, dt=F32, nb=1):
                return T(es.enter_context(nc.sbuf_tensor("s_" + name, list(shape), dt)), nb)
            cnt = sb("cnt", [128, 32]); yv = sb("yv", [128, 32]); yi_ = sb("yi", [128, 32], I32); yf = sb("yf", [128, 32]); ygt = sb("ygt", [128, 32])
            padded = sb("padded", [128, 32]); ca = sb("csuma", [128, 32]); cb_ = sb("csumb", [128, 32]); pstart = sb("pstart", [128, 32])
            kp = sb("kp", [128, 8]); bstart = sb("bstart", [128, NBLK]); cmp_ = sb("cmp", [128, NBLK, 32]); be = sb("be", [128, NBLK])
            bf_ = sb("bf", [128, NBLK, 2])
            slotv = [sb("slotv%d" % i, [128, 32]) for i in range(2)]; oh4 = [sb("oh4%d" % i, [128, 4, 32]) for i in range(2)]
            pr4 = [sb("pr4%d" % i, [128, 4, 32]) for i in range(2)]; i4f = [sb("i4f%d" % i, [128, 4]) for i in range(2)]
            hrow = [sb("hrow%d" % i, [128, D], BF16) for i in range(3)]
            dma("sp", kp.t[:], kp_d, [], [kp]); dma("sp", bstart.t[:], bstart_d, [], [bstart])
            mm(PS[0].t[:, 0:32], onesb.t[:], cumb.t[:], True, True, [onesb, cumb], [PS[0]])
            cp("dve", cnt.t[:], PS[0].t[:, 0:32], [PS[0]], [cnt])
            ts("dve", yv.t[:], cnt.t[:], float(RB - 1), 1.0 / RB, ALU.add, ALU.mult, [cnt], [yv])
            cp("dve", yi_.t[:], yv.t[:], [yv], [yi_]); cp("dve", yf.t[:], yi_.t[:], [yi_], [yf])
            tt("dve", ygt.t[:], yf.t[:], yv.t[:], ALU.is_gt, [yf, yv], [ygt])
            tt("dve", yf.t[:], yf.t[:], ygt.t[:], ALU.subtract, [yf, ygt], [yf])
            ts("dve", padded.t[:], yf.t[:], float(RB), None, ALU.mult, None, [yf], [padded])
            cp("dve", ca.t[:], padded.t[:], [padded], [ca])
            src_, dst_ = ca, cb_
            for sh in (1, 2, 4, 8, 16):
                cp("dve", dst_.t[:, 0:sh], src_.t[:, 0:sh], [src_], [dst_])
                tt("dve", dst_.t[:, sh:32], src_.t[:, sh:32], src_.t[:, 0:32 - sh], ALU.add, [src_], [dst_])
                src_, dst_ = dst_, src_
            pend = src_
            tt("dve", pstart.t[:], pend.t[:], padded.t[:], ALU.subtract, [pend, padded], [pstart])
            tt("dve", cmp_.t[:], pend.t[:].unsqueeze(1).broadcast_to([128, NBLK, 32]), bstart.t[:].unsqueeze(2).broadcast_to([128, NBLK, 32]),
               ALU.is_le, [pend, bstart], [cmp_])
            red(be.t[:], cmp_.t[:], [cmp_], [be])
            ts("dve", be.t[:], be.t[:], float(NE - 1), None, ALU.min, None, [be], [be])
            stt("dve", bf_.t[:, :, 0], be.t[:], 128.0, kp.t[:, 0:1].broadcast_to([128, NBLK]), ALU.mult, ALU.add, [be, kp], [bf_])
            cp("dve", bf_.t[:, :, 1], be.t[:], [be], [bf_])
            cp("dve", BIDX.t[:], bf_.t[:], [bf_], [BIDX])
            for i in range(NT):
                sv = slotv[i % 2]; oh = oh4[i % 2]; p4 = pr4[i % 2]; f4 = i4f[i % 2]; hr = hrow[i % 3]
                tt("dve", sv.t[:], POS.t[:, i, :], pstart.t[:], ALU.add, [POS.bs[i], pstart], [sv])
                tt("dve", oh.t[:], LG.t[:, i, :].unsqueeze(1).broadcast_to([128, 4, 32]), MX.t[:, i, 0:4].unsqueeze(2).broadcast_to([128, 4, 32]),
                   ALU.is_equal, [LG.bs[i], MX.bs[i]], [oh])
                tt("dve", p4.t[:], oh.t[:], sv.t[:].unsqueeze(1).broadcast_to([128, 4, 32]), ALU.mult, [oh, sv], [p4])
                red(f4.t[:], p4.t[:], [p4], [f4])
                cp("dve", IDX.t[:, i, :], f4.t[:], [f4], [IDX.bs[i]])
                tt("dve", p4.t[:], oh.t[:], G.t[:, i, :].unsqueeze(1).broadcast_to([128, 4, 32]), ALU.mult, [oh, G.bs[i]], [p4])
                red(W4.t[:, i, :], p4.t[:], [p4], [W4.bs[i]])
                dma("sp", hr.t[:], H2tm_d[i * 128:(i + 1) * 128, :], [B_h2tm[i]], [hr])
                for j in range(4):
                    scatter(Xs_d, hr.t[:], IDX.t[:, i, j:j + 1], [hr, IDX.bs[i]], [])
            S_.barrier()

        with contextlib.ExitStack() as es:
            def sb(name, shape, dt=F32, nb=1):
                return T(es.enter_context(nc.sbuf_tensor("s_" + name, list(shape), dt)), nb)
            w1t = [sb("w1t%d" % i, [128, 8, 2048], BF16) for i in range(2)]; w2t = [sb("w2t%d" % i, [128, 8, D], BF16) for i in range(2)]
            b1t = [sb("b1t%d" % i, [128, 16]) for i in range(2)]; b2rep = [sb("b2rep%d" % i, [128, D], BF16) for i in range(2)]
            xsb = [sb("xsb%d" % i, [128, NST, D], BF16) for i in range(2)]; XsT = [sb("XsT%d" % i, [128, 8, RB], BF16) for i in range(2)]
            actT = [sb("actT%d" % i, [128, 8, RB], BF16, nb=8) for i in range(2)]
            gcl = [sb("gcl%d" % i, [128, RB]) for i in range(2)]; sg = [sb("sg%d" % i, [128, RB]) for i in range(2)]
            ucl = [sb("ucl%d" % i, [128, RB]) for i in range(2)]
            ysb = [sb("ysb%d" % i, [128, D]) for i in range(2)]
            ew1f = ew1_d.rearrange("e d n -> (e d) n"); ew2f = ew2_d.rearrange("e d n -> (e d) n")
            en = 0; yn = 0
            for b in range(NBLK):
                w1 = w1t[b % 2]; w2 = w2t[b % 2]; b1 = b1t[b % 2]; b2 = b2rep[b % 2]; xs_ = xsb[b % 2]; xT = XsT[b % 2]; aT = actT[b % 2]
                gather(w1.t[:].rearrange("p k n -> p (k n)"), W1b_d, BIDX.t[:, b, 0:1], [BIDX], [w1])
                gather(b1.t[:], eb1_d, BIDX.t[:, b, 0:1], [BIDX], [b1])
                gather(w2.t[:].rearrange("p k n -> p (k n)"), W2b_d, BIDX.t[:, b, 0:1], [BIDX], [w2])
                gather(b2.t[:], eb2_d, BIDX.t[:, b, 1:2], [BIDX], [b2])
                ts("dve", b1.t[:, 8:16], b1.t[:, 8:16], 1.0, None, ALU.add, None, [b1], [b1])
                if b == 0:
                    dma("sp", xs_.t[:], Xs_d[0:RB, :].rearrange("(s p) d -> p s d", p=128), [], [xs_])
                if b + 1 < NBLK:
                    xn_ = xsb[(b + 1) % 2]
                    dma("sp", xn_.t[:], Xs_d[(b + 1) * RB:(b + 2) * RB, :].rearrange("(s p) d -> p s d", p=128), [], [xn_])
                for s2 in range(NST):
                    pb = psbf(6 + s2 % 2)
                    for k in range(8):
                        tr(pb[:, k * 128:(k + 1) * 128], xs_.t[:, s2, k * 128:(k + 1) * 128], ident.t[:], [xs_, ident], [PS[6 + s2 % 2]])
                    S_.op("act", (lambda o, i_: lambda e: e.copy(o, i_))(xT.t[:, :, s2 * 128:(s2 + 1) * 128], pb[:, :].rearrange("p (k t) -> p k t", t=128)),
                          [PS[6 + s2 % 2].b], [xT.b])
                for Fi in range(8):
                    pg = PS[(en % 2) * 2]; pu = PS[(en % 2) * 2 + 1]
                    gc = gcl[en % 2]; sgt = sg[en % 2]; uc = ucl[en % 2]; gs_ = sgt; en += 1
                    for k in range(8):
                        mm(pg.t[:, 0:RB], w1.t[:, k, Fi * 128:(Fi + 1) * 128], xT.t[:, k, :], k == 0, k == 7, [w1, xT], [pg])
                    for k in range(8):
                        mm(pu.t[:, 0:RB], w1.t[:, k, 1024 + Fi * 128:1024 + (Fi + 1) * 128], xT.t[:, k, :], k == 0, k == 7, [w1, xT], [pu])
                    ts("dve", gc.t[:], pg.t[:, 0:RB], b1.t[:, Fi:Fi + 1], 7.0, ALU.add, ALU.min, [pg, b1], [gc])
                    act(sgt.t[:], gc.t[:], AF.Sigmoid, [gc], [sgt], scale=1.702)
                    ts("dve", uc.t[:], pu.t[:, 0:RB], b1.t[:, 8 + Fi:9 + Fi], 8.0, ALU.add, ALU.min, [pu, b1], [uc])
                    tt("dve", gs_.t[:], gc.t[:], sgt.t[:], ALU.mult, [gc, sgt], [gs_])
                    stt("dve", aT.t[:, Fi, :], uc.t[:], -6.0, gs_.t[:], ALU.max, ALU.mult, [gs_, uc], [aT.bs[Fi]])
                for s2 in range(NST):
                    yt_ = ysb[yn % 2]; yn += 1
                    for half in range(2):
                        py = PS[4 + half]
                        for k in range(8):
                            mm(py.t[:], aT.t[:, k, s2 * 128:(s2 + 1) * 128], w2.t[:, k, half * 512:(half + 1) * 512], k == 0, k == 7, [aT.bs[k], w2], [py])
                        tt("dve", yt_.t[:, half * 512:(half + 1) * 512], py.t[:], b2.t[:, half * 512:(half + 1) * 512], ALU.add, [py, b2], [yt_])
                    dma("sp", Ys_d[b * RB + s2 * 128:b * RB + (s2 + 1) * 128, :], yt_.t[:], [yt_], [B_Ys[b]])
            S_.barrier()

        with contextlib.ExitStack() as es:
            def sb(name, shape, dt=F32, nb=1):
                return T(es.enter_context(nc.sbuf_tensor("s_" + name, list(shape), dt)), nb)
            gat = [[sb("gat%d_%d" % (i, j), [128, D]) for j in range(4)] for i in range(2)]
            xq = [sb("cxq%d" % i, [128, D]) for i in range(2)]; acc_ = [sb("cacc%d" % i, [128, D]) for i in range(2)]
            for i in range(NT):
                g4 = gat[i % 2]; xq_ = xq[i % 2]; ac = acc_[i % 2]
                for j in range(4):
                    gather(g4[j].t[:], Ys_d, IDX.t[:, i, j:j + 1], [IDX.bs[i]] + B_Ys, [g4[j]])
                if i == 0:
                    dma("sp", xq_.t[:], y_d[0:128, :], [B_y[0]], [xq_])
                if i + 1 < NT:
                    xqn = xq[(i + 1) % 2]
                    dma("sp", xqn.t[:], y_d[(i + 1) * 128:(i + 2) * 128, :], [B_y[i + 1]], [xqn])
                ts("dve", ac.t[:], g4[0].t[:], W4.t[:, i, 0:1], None, ALU.mult, None, [g4[0], W4.bs[i]], [ac])
                for j in range(1, 4):
                    stt("dve", ac.t[:], g4[j].t[:], W4.t[:, i, j:j + 1], ac.t[:], ALU.mult, ALU.add, [g4[j], W4.bs[i], ac], [ac])
                tt("dve", ac.t[:], ac.t[:], g2row.t[:], ALU.mult, [ac, g2row], [ac])
                tt("dve", ac.t[:], ac.t[:], xq_.t[:], ALU.add, [ac, xq_], [ac])
                dma("sp", y_d[i * 128:(i + 1) * 128, :], ac.t[:], [ac], [B_y[i]])
            S_.barrier()

        sems = {k: ges.enter_context(nc.semaphore("s%d" % i)) for i, k in enumerate(S_.semkeys)}
        S_.emit(sems)
    return nc


_CONST_CACHE = {}


def make_constants(S):
    if S in _CONST_CACHE:
        return _CONST_CACHE[S]
    bf = ml_dtypes.bfloat16
    NT = S // 128; NKC = S // 128; BLK = min(512, S); NB = S // BLK
    N2 = 2 * S
    n = np.arange(S, dtype=np.int64)[:, None]; k = np.arange(S, dtype=np.int64)[None, :]
    ph = ((2 * k + 1) * n) % (2 * N2)
    ang = ph.astype(np.float64) * (np.pi / N2)
    C = np.cos(ang); Sm = np.sin(ang)
    del ang, ph
    def fwd(M):
        return np.ascontiguousarray(M.reshape(NT, 128, NKC, 128).transpose(2, 1, 0, 3)).astype(bf)
    def inv(M):
        return np.ascontiguousarray(M.reshape(NB, BLK, NKC, 128).transpose(0, 3, 2, 1)).astype(bf)
    consts = {"Cf": fwd(C), "Sf": fwd(Sm), "Ci": inv(C), "nSi": inv(-Sm)}
    del C, Sm
    consts["ident"] = np.eye(128, dtype=np.float32).astype(bf)
    GRID_W = 64; RF = 16
    rows = S // GRID_W
    row = np.repeat(np.arange(rows), GRID_W); col = np.tile(np.arange(GRID_W), rows)
    pos = np.stack([row, col], -1).astype(np.float32)
    freqs = (np.float32(10000.0) ** (-np.arange(RF, dtype=np.float32) / np.float32(RF))).astype(np.float32)
    ang = (pos[:, :, None] * freqs).astype(np.float32)
    cos = np.cos(ang).astype(np.float32); sin = np.sin(ang).astype(np.float32)
    ropec = np.stack([cos, cos], 2).reshape(S, 64)
    ropes = np.stack([sin, -sin], 2).reshape(S, 64)
    consts["ropec"] = np.ascontiguousarray(ropec, dtype=np.float32); consts["ropes"] = np.ascontiguousarray(ropes, dtype=np.float32)
    t = np.linspace(0.0, 1.0, S, dtype=np.float32)[:, None]
    w = (np.float32(2.0 * math.pi) * np.arange(S, dtype=np.float32)[:, None] / np.float32(S)).astype(np.float32)
    f = np.linspace(1e-4, 15, 16, dtype=np.float32)
    z = np.concatenate([t, np.cos(f * w), -np.sin(f * w)], -1).astype(np.float32)
    consts["zT"] = np.ascontiguousarray(z.T)
    consts["trow"] = np.ascontiguousarray(t.T)
    deltas = np.abs(np.linspace(math.log(1e-2) / 1.5, math.log(1e-2) / 0.3, 512, dtype=np.float32))
    consts["drow"] = deltas.reshape(1, 512).astype(np.float32)
    _CONST_CACHE[S] = consts
    return consts


def fm(v, nchunk):
    return np.ascontiguousarray(np.asarray(v, np.float32).reshape(nchunk, 128).T)


def make_in_maps(inp, S, CTXL, NE, B):
    consts = make_constants(S)
    f32 = lambda a: np.ascontiguousarray(np.asarray(a, np.float32))
    shared = dict(consts)
    shared["ada_w"] = f32(inp["ada_w"][0]); shared["ada_b_row"] = f32(inp["ada_b"][0]).reshape(1, -1); shared["ada_b_fm"] = fm(inp["ada_b"][0], 48)
    shared["n1g"] = fm(inp["norm1_g"][0], 8); shared["n2g"] = fm(inp["norm2_g"][0], 8)
    shared["w_in"] = f32(inp["w_in"][0]); shared["b_in_row"] = f32(inp["b_in"][0]).reshape(1, -1); shared["b_in_fm"] = fm(inp["b_in"][0], 40)
    shared["qg"] = f32(inp["q_norm_g"][0]).reshape(1, 64); shared["kg"] = f32(inp["k_norm_g"][0]).reshape(1, 64)
    shared["lam4"] = np.concatenate([f32(inp[n][0]) for n in ("lambda_q1", "lambda_k1", "lambda_q2", "lambda_k2")]).reshape(1, 256)
    shared["subg"] = f32(inp["subln_g"][0]).reshape(128, 1)
    cw = f32(inp["conv_w"][0])
    shared["convw"] = np.ascontiguousarray(cw.reshape(3, 12, 128).transpose(2, 1, 0)); shared["convb"] = fm(inp["conv_b"][0], 12)
    shared["fw1"] = f32(inp["filt_w1"][0]); shared["fw2"] = f32(inp["filt_w2"][0]); shared["fw3"] = f32(inp["filt_w3"][0]); shared["fw4"] = f32(inp["filt_w4"][0])
    shared["fvec"] = np.ascontiguousarray(np.stack([f32(inp["filt_b1"][0]), f32(inp["filt_b2"][0]), f32(inp["filt_b3"][0]), f32(inp["filt_freq"][0])], -1))
    shared["hbias"] = fm(inp["hyena_bias"][0], 4)
    shared["w_up_a"] = f32(inp["w_up_a"][0]); shared["w_up_b"] = f32(inp["w_up_b"][0]); shared["w_out"] = f32(inp["w_out"][0])
    shared["router_w"] = f32(inp["router_w"][0]); shared["router_b"] = f32(inp["router_b"][0]).reshape(1, 32)
    shared["ew1"] = f32(inp["exp_w1"][0]); shared["ew2"] = f32(inp["exp_w2"][0])
    shared["eb1"] = np.ascontiguousarray(f32(inp["exp_b1"][0]).reshape(NE, 16, 128).transpose(0, 2, 1).reshape(NE * 128, 16))
    shared["eb2"] = f32(inp["exp_b2"][0]).reshape(NE, D)
    shared["n2g_row"] = f32(inp["norm2_g"][0]).reshape(1, D)
    NBLK = (4 * S + NE * RB) // RB
    shared["ustrict"] = np.triu(np.ones((128, 128), np.float32), 1).astype(ml_dtypes.bfloat16)
    shared["kp"] = np.ascontiguousarray((np.arange(8)[None, :] * 128 + np.arange(128)[:, None]).astype(np.float32))
    shared["bstart"] = np.ascontiguousarray(np.broadcast_to((np.arange(NBLK) * RB).astype(np.float32)[None, :], (128, NBLK)))
    maps = []
    for b in range(B):
        m = dict(shared)
        m["x"] = f32(inp["x"][b]); m["ctx"] = f32(inp["ctx"][b])
        m["cc"] = np.ascontiguousarray(np.stack([f32(inp["c"][b]), f32(inp["c_ctx"])], -1).reshape(8, 128, 2).transpose(1, 0, 2))
        maps.append(m)
    return maps


_PROG_CACHE = {}


def kernel(**inputs):
    x = np.asarray(inputs["x"])
    B, S, _ = x.shape
    CTXL = np.asarray(inputs["ctx"]).shape[1]
    NE = np.asarray(inputs["exp_w1"]).shape[1]
    key = (S, CTXL, NE)
    if key not in _PROG_CACHE:
        _PROG_CACHE[key] = build_program(S, CTXL, NE)
    nc = _PROG_CACHE[key]
    maps = make_in_maps(inputs, S, CTXL, NE, B)
    res = run_bass_kernel_spmd(nc, maps, core_ids=list(range(B)))
    return np.stack([np.asarray(r["y"], dtype=np.float32) for r in res.results], 0)
```

```python
import contextlib
import math
import numpy as np
import ml_dtypes
import concourse.bass as bass
import concourse.mybir as mybir
from concourse.bass_utils import run_bass_kernel_spmd

F32 = mybir.dt.float32
BF16 = mybir.dt.bfloat16
I32 = mybir.dt.int32
AF = mybir.ActivationFunctionType
ALU = mybir.AluOpType
AX = mybir.AxisListType
D = 1024
EPS = 1e-6
RB = 256
PI = math.pi


class Buf:
    __slots__ = ("w", "r")

    def __init__(self):
        self.w = None
        self.r = {}


ENGS = ("pe", "act", "dve", "pool", "sp")
DMA_RING = {"sp": 8, "pool": 8}


class Sched:
    def __init__(self, nc, same_engine_sync=True):
        self.nc = nc
        self.prog = {e: [] for e in ENGS}
        self.nops = {e: 0 for e in ENGS}
        self.waited = {e: {} for e in ENGS}
        self.same = same_engine_sync
        self.dma_n = {q: 0 for q in DMA_RING}
        self.dma_val = {}
        self.semkeys = list(ENGS)
        for q, n in DMA_RING.items():
            for i in range(n):
                self.semkeys.append(("dma", q, i))
                self.dma_val[("dma", q, i)] = 0

    def _wait(self, eng, semkey, val):
        if self.waited[eng].get(semkey, -1) >= val:
            return
        self.waited[eng][semkey] = val
        if isinstance(semkey, str):
            self.prog[semkey][val][3] = True
        self.prog[eng].append(["w", semkey, val])

    def _deps(self, eng, reads, writes, is_dma):
        deps = {}

        def add(k, v, e):
            if (not is_dma) and e == eng and k == eng:
                if eng == "pe" or not self.same:
                    return
            if deps.get(k, -1) < v:
                deps[k] = v
        for b in reads:
            if b.w is not None:
                add(*b.w)
        for b in writes:
            if b.w is not None:
                add(*b.w)
            for k, (v, e) in b.r.items():
                add(k, v, e)
        for k, v in deps.items():
            self._wait(eng, k, v)

    def _update(self, tok, reads, writes):
        k, v, e = tok
        for b in reads:
            b.r[k] = (v, e)
        for b in writes:
            b.w = tok
            b.r = {}

    def op(self, eng, fn, reads=(), writes=()):
        self._deps(eng, reads, writes, False)
        pos = len(self.prog[eng])
        self.prog[eng].append(["op", fn, eng, False])
        self.nops[eng] += 1
        self._update((eng, pos, eng), reads, writes)

    def dma(self, q, fn, reads=(), writes=()):
        n = self.dma_n[q]
        self.dma_n[q] += 1
        key = ("dma", q, n % DMA_RING[q])
        if self.dma_val[key] > 0:
            self._wait(q, key, self.dma_val[key])
        self._deps(q, reads, writes, True)
        self.dma_val[key] += 16
        tok = (key, self.dma_val[key], q)
        self.prog[q].append(["dma", fn, key, 16])
        self._update(tok, reads, writes)

    def _last_op(self, eng):
        for i in range(len(self.prog[eng]) - 1, -1, -1):
            if self.prog[eng][i][0] == "op":
                return i
        return None

    def barrier(self):
        for e in ENGS:
            for e2 in ENGS:
                if e2 != e:
                    lp = self._last_op(e2)
                    if lp is not None:
                        self._wait(e, e2, lp)
            for k, v in self.dma_val.items():
                if v > 0:
                    self._wait(e, k, v)

    def emit(self, sems):
        nc = self.nc
        value_at = {}
        for e in ENGS:
            c = 0
            va = {}
            for pos, item in enumerate(self.prog[e]):
                if item[0] == "op" and item[3]:
                    c += 1
                    va[pos] = c
            value_at[e] = va
        with nc.Block() as block:
            def run(engname):
                def body(eng):
                    for item in self.prog[engname]:
                        if item[0] == "w":
                            k, v = item[1], item[2]
                            eng.wait_ge(sems[k], value_at[k][v] if isinstance(k, str) else v)
                        elif item[0] == "dma":
                            item[1](eng).then_inc(sems[item[2]], 16)
                        elif item[3]:
                            item[1](eng).then_inc(sems[item[2]], 1)
                        else:
                            item[1](eng)
                return body
            block.tensor(run("pe"))
            block.scalar(run("act"))
            block.vector(run("dve"))
            block.gpsimd(run("pool"))
            block.sync(run("sp"))


class T:
    def __init__(self, t, nb=1):
        self.t = t
        self.b = Buf()
        self.bs = [Buf() for _ in range(nb)] if nb > 1 else [self.b]


def build_program(S, CTXL, NE, debug=False):
    NT = S // 128
    NC = CTXL // 128
    NKT = NT + NC
    TK = S + CTXL
    BLK = min(512, S)
    NB = S // BLK
    TPB = BLK // 128
    NKC = S // 128
    KG = min(8, NKC)
    NKG = NKC // KG
    QS = min(1024, S)
    NQ = S // QS
    NQT = QS // 128
    NQB = QS // BLK
    N2 = 2 * S
    NR = 4 * S + NE * RB
    NBLK = NR // RB
    NZ = NR // 256
    NST = RB // 128

    nc = bass.Bass("TRN2", target_bir_lowering=False)
    S_ = Sched(nc)

    def din(name, shape, dt=F32):
        return nc.dram_tensor(name, list(shape), dt, kind="ExternalInput").ap()

    def dscr(name, shape, dt=BF16):
        return nc.dram_tensor(name, list(shape), dt, kind="Internal").ap()

    x_d = din("x", [S, D]); ctx_d = din("ctx", [CTXL, D]); cc_d = din("cc", [128, 8, 2])
    adaw_d = din("ada_w", [D, 6 * D]); adab_row_d = din("ada_b_row", [1, 6 * D]); adab_fm_d = din("ada_b_fm", [128, 48])
    n1g_d = din("n1g", [128, 8]); n2g_d = din("n2g", [128, 8])
    win_d = din("w_in", [D, 5120]); bin_row_d = din("b_in_row", [1, 5120]); bin_fm_d = din("b_in_fm", [128, 40])
    qg_d = din("qg", [1, 64]); kg_d = din("kg", [1, 64]); lam4_d = din("lam4", [1, 256]); subg_d = din("subg", [128, 1])
    convw_d = din("convw", [128, 12, 3]); convb_d = din("convb", [128, 12])
    fw1_d = din("fw1", [33, 64]); fw2_d = din("fw2", [64, 64]); fw3_d = din("fw3", [64, 64]); fw4_d = din("fw4", [64, 1024])
    fvec_d = din("fvec", [64, 4])
    hb_d = din("hbias", [128, 4])
    wua_d = din("w_up_a", [512, D]); wub_d = din("w_up_b", [512, D]); wout_d = din("w_out", [D, D])
    rw_d = din("router_w", [D, 32]); rb_d = din("router_b", [1, 32])
    ew1_d = din("ew1", [NE, D, 2048]); eb1_d = din("eb1", [NE * 128, 16]); ew2_d = din("ew2", [NE, D, D]); eb2_d = din("eb2", [NE, D])
    ustr_d = din("ustrict", [128, 128], BF16); kp_d = din("kp", [128, 8]); bstart_d = din("bstart", [128, NBLK]); n2grow_d = din("n2g_row", [1, D])
    ident_d = din("ident", [128, 128], BF16)
    ropec_d = din("ropec", [S, 64]); ropes_d = din("ropes", [S, 64])
    zT_d = din("zT", [33, S]); trow_d = din("trow", [1, S]); drow_d = din("drow", [1, 512])
    Cf_d = din("Cf", [NKC, 128, NT, 128], BF16); Sf_d = din("Sf", [NKC, 128, NT, 128], BF16)
    Ci_d = din("Ci", [NB, 128, NKC, BLK], BF16); Si_d = din("nSi", [NB, 128, NKC, BLK], BF16)
    y_d = nc.dram_tensor("y", [S, D], F32, kind="ExternalOutput").ap()

    hTc_d = dscr("hTc_d", [128, 8, CTXL]); hT_d = dscr("hT_d", [128, 8, S])
    oaT_d = dscr("oaT_d", [128, 4, S]); obT_d = dscr("obT_d", [128, 4, S])
    x0c_d = dscr("x0c_d", [128, 4, S]); uT_d = dscr("uT_d", [128, 4, S])
    Y_d = dscr("Y_d", [128, 2, NKC, 512])
    H2tm_d = dscr("H2tm_d", [S, D]); Xs_d = dscr("Xs_d", [NR, D]); Ys_d = dscr("Ys_d", [NR, D], F32)
    W1b_d = dscr("W1b_d", [NE * 128, 8 * 2048]); W2b_d = dscr("W2b_d", [NE * 128, 8 * D])
    dbg = {}
    if debug:
        for n, shp in [("dbg_oaT", [128, 4, S]), ("dbg_obT", [128, 4, S]), ("dbg_hT", [128, 8, S]), ("dbg_G", [128, NT, 32])]:
            dbg[n] = nc.dram_tensor(n, shp, F32, kind="ExternalOutput").ap()
    B_hTc = Buf(); B_hT = [Buf() for _ in range(NB)]
    B_oaT = [Buf() for _ in range(NB)]; B_obT = [Buf() for _ in range(NB)]
    B_x0c = [Buf() for _ in range(4)]; B_uT = [Buf() for _ in range(4)]
    B_Y = [Buf() for _ in range(NKC)]; B_h2tm = [Buf() for _ in range(NT)]
    B_Xs = [Buf() for _ in range(NZ)]; B_Ys = [Buf() for _ in range(NBLK)]
    B_y = [Buf() for _ in range(NT)]

    w_in_v = win_d.rearrange("(k p) n -> p k n", p=128)

    def bl(xs):
        return [x.b if isinstance(x, T) else x for x in xs]

    def mm(out, lhsT, rhs, start, stop, r, w, tp=None):
        if tp is None:
            S_.op("pe", lambda e: e.matmul(out, lhsT, rhs, start=start, stop=stop), bl(r), bl(w))
        else:
            S_.op("pe", lambda e: e.matmul(out, lhsT, rhs, start=start, stop=stop, tile_position=tp), bl(r), bl(w))

    def tr(out, in_, ident, r, w):
        S_.op("pe", lambda e: e.transpose(out, in_, ident), bl(r), bl(w))

    def act(out, in_, func, r, w, **kw):
        S_.op("act", lambda e: e.activation(out=out, in_=in_, func=func, **kw), bl(r), bl(w))

    def tt(eng, out, in0, in1, op, r, w):
        S_.op(eng, lambda e: e.tensor_tensor(out, in0, in1, op), bl(r), bl(w))

    def ts(eng, out, in0, s1, s2, op0, op1, r, w):
        if op1 is None:
            S_.op(eng, lambda e: e.tensor_scalar(out, in0, s1, None, op0), bl(r), bl(w))
        else:
            S_.op(eng, lambda e: e.tensor_scalar(out, in0, s1, s2, op0, op1), bl(r), bl(w))

    def stt(eng, out, in0, sc, in1, op0, op1, r, w):
        S_.op(eng, lambda e: e.scalar_tensor_tensor(out, in0, sc, in1, op0, op1), bl(r), bl(w))

    def cp(eng, out, in_, r, w):
        S_.op(eng, lambda e: e.tensor_copy(out, in_), bl(r), bl(w))

    def recip(out, in_, r, w):
        S_.op("dve", lambda e: e.reciprocal(out, in_), bl(r), bl(w))

    def mset(eng, ap, val, w):
        S_.op(eng, lambda e: e.memset(ap, val), [], bl(w))

    def dma(q, out, in_, r, w):
        S_.dma(q, lambda e: e.dma_start(out=out, in_=in_), bl(r), bl(w))

    def vmax8(out, in_, r, w):
        S_.op("dve", lambda e: e.max(out, in_), bl(r), bl(w))

    def red(out, in_, r, w):
        S_.op("dve", lambda e: e.tensor_reduce(out, in_, AX.X, ALU.add), bl(r), bl(w))

    def gather(out, in_, idx_ap, r, w):
        S_.dma("pool", lambda e: e.indirect_dma_start(out=out, out_offset=None, in_=in_,
                                                      in_offset=bass.IndirectOffsetOnAxis(ap=idx_ap, axis=0), oob_is_err=False), bl(r), bl(w))

    def scatter(out, in_, idx_ap, r, w):
        S_.dma("pool", lambda e: e.indirect_dma_start(out=out, out_offset=bass.IndirectOffsetOnAxis(ap=idx_ap, axis=0), in_=in_,
                                                      in_offset=None, oob_is_err=False), bl(r), bl(w))

    with contextlib.ExitStack() as ges:
        def gsb(name, shape, dt=F32, nb=1):
            return T(ges.enter_context(nc.sbuf_tensor("s_" + name, list(shape), dt)), nb)
        psall = ges.enter_context(nc.psum_tensor("psall", [128, 4096], F32))
        PS = [T(psall[:, i * 512:(i + 1) * 512]) for i in range(8)]

        def psbf(i):
            return PS[i].t[:].bitcast(BF16)

        ident = gsb("ident", [128, 128], BF16); onesb = gsb("onesb", [128, 128], BF16); onesf = gsb("onesf", [128, 128])
        epst = gsb("epst", [128, 1])
        A1 = gsb("A1", [128, 8]); B1 = gsb("B1", [128, 8]); A1c = gsb("A1c", [128, 8]); B1c = gsb("B1c", [128, 8])
        A2 = gsb("A2", [128, 8]); B2 = gsb("B2", [128, 8])
        g1row = gsb("g1row", [128, D]); g2row = gsb("g2row", [128, D])
        neglam = gsb("neglam", [128, 1]); gsub = gsb("gsub", [128, 1])
        G = gsb("G", [128, NT, 32], F32, nb=NT)
        A2row = gsb("A2row", [128, D]); B2row = gsb("B2row", [128, D])
        LG = gsb("LG", [128, NT, 32], F32, nb=NT); MX = gsb("MX", [128, NT, 8], F32, nb=NT); POS = gsb("POS", [128, NT, 32], F32, nb=NT)
        IDX = gsb("IDX", [128, NT, 4], I32, nb=NT); W4 = gsb("W4", [128, NT, 4], F32, nb=NT)
        BIDX = gsb("BIDX", [128, NBLK, 2], I32)
        ustr = gsb("ustr", [128, 128], BF16); cumf = gsb("cumf", [128, 32]); cumb = gsb("cumb", [128, 32], BF16)

        dma("sp", ident.t[:], ident_d, [], [ident]); dma("sp", ustr.t[:], ustr_d, [], [ustr])
        mset("pool", cumf.t[:], 0.0, [cumf]); mset("pool", cumb.t[:], 0.0, [cumb])
        mset("pool", onesb.t[:], 1.0, [onesb]); mset("pool", onesf.t[:], 1.0, [onesf]); mset("pool", epst.t[:], EPS, [epst])

        with contextlib.ExitStack() as es:
            def sb(name, shape, dt=F32, nb=1):
                return T(es.enter_context(nc.sbuf_tensor("s_" + name, list(shape), dt)), nb)
            cc = sb("cc", [128, 8, 2]); scv = sb("scv", [128, 8, 2]); screp = sb("screp", [128, 8, 128])
            aw = [sb("aw%d" % i, [128, 8, 512]) for i in range(2)]
            adab_fm = sb("adab_fm", [128, 48]); adab_row = sb("adab_row", [1, 6 * D])
            modF = sb("modF", [128, 48, 2]); n1g = sb("n1g", [128, 8]); n2g = sb("n2g", [128, 8])
            lam4 = sb("lam4", [128, 256]); lt1 = sb("lt1", [128, 64]); lt2 = sb("lt2", [128, 64])
            ls1 = sb("ls1", [128, 1]); ls2 = sb("ls2", [128, 1]); subg = sb("subg", [128, 1])
            dma("sp", cc.t[:], cc_d, [], [cc]); dma("sp", adab_fm.t[:], adab_fm_d, [], [adab_fm])
            dma("sp", adab_row.t[:], adab_row_d, [], [adab_row])
            dma("sp", n1g.t[:], n1g_d, [], [n1g]); dma("sp", n2g.t[:], n2g_d, [], [n2g])
            dma("sp", lam4.t[:], lam4_d.partition_broadcast(128), [], [lam4]); dma("sp", subg.t[:], subg_d, [], [subg])
            act(scv.t[:], cc.t[:], AF.Silu, [cc], [scv])
            for k in range(8):
                cp("dve", screp.t[:, k, :], scv.t[:, k, 0:1].broadcast_to([128, 128]), [scv], [screp])
            adaw_v = adaw_d.rearrange("(k p) n -> p k n", p=128)
            pmod = PS[0]
            for g in range(12):
                a = aw[g % 2]
                dma("sp", a.t[:], adaw_v[:, :, g * 512:(g + 1) * 512], [], [a])
                for c in range(4):
                    ch = g * 4 + c
                    for k in range(8):
                        mm(pmod.t[:, 2 * ch:2 * ch + 2], a.t[:, k, c * 128:(c + 1) * 128], scv.t[:, k, :], k == 0, k == 7, [a, scv], [pmod])
                if g in (4, 5, 6, 7, 8, 9, 10, 11):
                    pr = PS[1 + (g % 2)]
                    for k in range(8):
                        mm(pr.t[:], screp.t[:, k, :], a.t[:, k, :], k == 0, False, [a, screp], [pr])
                    mm(pr.t[:], onesf.t[0:1, :], adab_row.t[0:1, g * 512:(g + 1) * 512], False, True, [onesf, adab_row], [pr])
                    dst = {2: g1row, 3: B2row, 4: A2row, 5: g2row}[g // 2]
                    half = g % 2
                    cp("dve", dst.t[:, half * 512:(half + 1) * 512], pr.t[:], [pr], [dst])
            tt("dve", modF.t[:], pmod.t[:, 0:96].rearrange("p (c j) -> p c j", j=2),
               adab_fm.t[:].unsqueeze(2).broadcast_to([128, 48, 2]), ALU.add, [pmod, adab_fm], [modF])
            stt("dve", A1.t[:], modF.t[:, 8:16, 0], 1.0, n1g.t[:], ALU.add, ALU.mult, [modF, n1g], [A1])
            stt("dve", A1c.t[:], modF.t[:, 8:16, 1], 1.0, n1g.t[:], ALU.add, ALU.mult, [modF, n1g], [A1c])
            stt("dve", A2.t[:], modF.t[:, 32:40, 0], 1.0, n2g.t[:], ALU.add, ALU.mult, [modF, n2g], [A2])
            cp("dve", B1.t[:], modF.t[:, 0:8, 0], [modF], [B1]); cp("dve", B1c.t[:], modF.t[:, 0:8, 1], [modF], [B1c])
            cp("dve", B2.t[:], modF.t[:, 24:32, 0], [modF], [B2])
            n2grow = sb("n2grow", [128, D])
            dma("sp", n2grow.t[:], n2grow_d.partition_broadcast(128), [], [n2grow])
            stt("dve", A2row.t[:], A2row.t[:], 1.0, n2grow.t[:], ALU.add, ALU.mult, [A2row, n2grow], [A2row])
            tt("dve", lt1.t[:], lam4.t[:, 0:64], lam4.t[:, 64:128], ALU.mult, [lam4], [lt1])
            tt("dve", lt2.t[:], lam4.t[:, 128:192], lam4.t[:, 192:256], ALU.mult, [lam4], [lt2])
            S_.op("dve", lambda e: e.tensor_reduce(ls1.t[:], lt1.t[:], AX.X, ALU.add), [lt1.b], [ls1.b])
            S_.op("dve", lambda e: e.tensor_reduce(ls2.t[:], lt2.t[:], AX.X, ALU.add), [lt2.b], [ls2.b])
            act(ls1.t[:], ls1.t[:], AF.Exp, [ls1], [ls1]); act(ls2.t[:], ls2.t[:], AF.Exp, [ls2], [ls2])
            tt("dve", neglam.t[:], ls2.t[:], ls1.t[:], ALU.subtract, [ls1, ls2], [neglam])
            ts("dve", neglam.t[:], neglam.t[:], -0.2, None, ALU.add, None, [neglam], [neglam])
            ts("dve", gsub.t[:], subg.t[:], 0.8, None, ALU.mult, None, [subg], [gsub])
            S_.barrier()

        def norm_transpose(es, tag):
            def sb(name, shape, dt=F32, nb=1):
                return T(es.enter_context(nc.sbuf_tensor("s_" + tag + name, list(shape), dt)), nb)
            st = dict(junk=sb("junk", [128, D], BF16), ss=[sb("ss%d" % i, [128, 1]) for i in range(2)],
                      xs=[sb("xs%d" % i, [128, D], BF16) for i in range(2)], n=0)
            return st

        def do_norm_transpose(st, xin, A, Bv, psi, dst_fn, dst_bufs, defer=False):
            i = st["n"]; st["n"] += 1
            ss = st["ss"][i % 2]; xs = st["xs"][i % 2]
            mset("pool", ss.t[:], 0.0, [ss])
            act(st["junk"].t[:], xin.t[:], AF.Square, [xin], [st["junk"], ss], accum_out=ss.t[:])
            act(ss.t[:], ss.t[:], AF.Sqrt, [ss, epst], [ss], scale=1.0 / D, bias=epst.t[:])
            recip(ss.t[:], ss.t[:], [ss], [ss])
            ts("dve", xs.t[:], xin.t[:], ss.t[:, 0:1], None, ALU.mult, None, [xin, ss], [xs])
            pb = psbf(psi)
            for k in range(8):
                tr(pb[:, k * 128:(k + 1) * 128], xs.t[:, k * 128:(k + 1) * 128], ident.t[:], [xs, ident], [PS[psi]])
            def evac():
                for k in range(8):
                    act(dst_fn(k), pb[:, k * 128:(k + 1) * 128], AF.Identity, [PS[psi], A, Bv], dst_bufs,
                        scale=A.t[:, k:k + 1], bias=Bv.t[:, k:k + 1])
            if defer:
                return ss, evac
            evac()
            return ss

        with contextlib.ExitStack() as es:
            def sb(name, shape, dt=F32, nb=1):
                return T(es.enter_context(nc.sbuf_tensor("s_" + name, list(shape), dt)), nb)
            st = norm_transpose(es, "p1")
            xin = [sb("p1xin%d" % i, [128, D]) for i in range(3)]
            hblk = [sb("p1hb%d" % i, [128, 8, BLK], BF16) for i in range(2)]
            n = 0
            pend_ev = None
            hb = hblk[0]
            for i in range(NC):
                xi = xin[n % 3]
                dma("sp", xi.t[:], ctx_d[i * 128:(i + 1) * 128, :], [], [xi])
                _, ev_ = do_norm_transpose(st, xi, A1c, B1c, n % 2, lambda k, i=i, hb=hb: hb.t[:, k, i * 128:(i + 1) * 128], [hb], defer=True)
                if pend_ev is not None:
                    pend_ev()
                pend_ev = ev_
                n += 1
            pend_ev(); pend_ev = None
            dma("sp", hTc_d, hblk[0].t[:, :, 0:CTXL], [hblk[0]], [B_hTc])
            for b in range(NB):
                hb = hblk[(b + 1) % 2]
                for s in range(TPB):
                    i = b * TPB + s
                    xi = xin[n % 3]
                    dma("sp", xi.t[:], x_d[i * 128:(i + 1) * 128, :], [], [xi])
                    _, ev_ = do_norm_transpose(st, xi, A1, B1, n % 2, lambda k, s=s, hb=hb: hb.t[:, k, s * 128:(s + 1) * 128], [hb], defer=True)
                    if pend_ev is not None:
                        pend_ev()
                    pend_ev = ev_
                    n += 1
                pend_ev(); pend_ev = None
                dma("sp", hT_d[:, :, b * BLK:(b + 1) * BLK], hb.t[:], [hb], [B_hT[b]])
            S_.barrier()

        with contextlib.ExitStack() as es2:
            def sb2(name, shape, dt=F32, nb=1):
                return T(es2.enter_context(nc.sbuf_tensor("s_" + name, list(shape), dt)), nb)
            QT = sb2("QT", [128, 4, S], BF16, nb=NT); KT = sb2("KT", [128, 4, TK], BF16, nb=NKT); V = sb2("V", [128, NKT, 512], BF16, nb=NKT)
            with contextlib.ExitStack() as es:
                def sb(name, shape, dt=F32, nb=1):
                    return T(es.enter_context(nc.sbuf_tensor("s_" + name, list(shape), dt)), nb)
                wqkv = sb("wqkv", [128, 8, 1536], BF16); brow = sb("brow", [1, 1536], BF16)
                gq = sb("gq", [128, 64]); gk = sb("gk", [128, 64])
                rcs = [sb("rc%d" % i, [128, 64]) for i in range(3)]; rss = [sb("rs%d" % i, [128, 64]) for i in range(3)]
                gtab = [[sb("gtab%d_%d" % (i, j), [128, 64]) for j in range(4)] for i in range(3)]
                hblk = [sb("p2hb%d" % i, [128, 8, BLK], BF16) for i in range(2)]
                sqt = [sb("sqt%d" % i, [128, 512]) for i in range(2)]
                ssq = [sb("ssq%d" % i, [128, 8]) for i in range(2)]
                qn = [sb("qn%d" % i, [128, 512]) for i in range(2)]
                qg2 = [sb("qg2%d" % i, [128, 512]) for i in range(2)]
                ru = [sb("ru%d" % i, [128, 512]) for i in range(2)]
                rw_ = [sb("rw%d" % i, [128, 512]) for i in range(2)]
                qr = [sb("qr%d" % i, [128, 512], BF16) for i in range(4)]
                for c in range(3):
                    dma("pool", wqkv.t[:, :, c * 512:(c + 1) * 512], w_in_v[:, :, c * 512:(c + 1) * 512], [], [wqkv])
                dma("pool", brow.t[:], bin_row_d[0:1, 0:1536], [], [brow])
                dma("sp", gq.t[:], qg_d.partition_broadcast(128), [], [gq]); dma("sp", gk.t[:], kg_d.partition_broadcast(128), [], [gk])
                cnt = {"n": 0}

                def qknorm(ps, gt, xt, dstT, dbuf, dcol, psT, pcol, rc=None, rs_=None):
                    i = cnt["n"]; cnt["n"] += 1
                    sq = sqt[i % 2]; sm = ssq[i % 2]; q1 = qn[i % 2]; q2 = qg2[i % 2]; u_ = ru[i % 2]; w_ = rw_[i % 2]; o_ = qr[i % 4]
                    def part_a():
                        act(sq.t[:], ps.t[:], AF.Square, [ps], [sq])
                        yield
                        S_.op("dve", lambda e: e.tensor_reduce(sm.t[:], sq.t[:].rearrange("p (g d) -> p g d", d=64), AX.X, ALU.add), [sq.b], [sm.b])
                        yield
                        act(sm.t[:], sm.t[:], AF.Sqrt, [sm, epst], [sm], scale=1.0 / 64, bias=epst.t[:])
                        yield
                        recip(sm.t[:], sm.t[:], [sm], [sm])
                        yield
                        tt("dve", q1.t[:].rearrange("p (g d) -> p g d", d=64), ps.t[:].rearrange("p (g d) -> p g d", d=64),
                           sm.t[:].unsqueeze(2).broadcast_to([128, 8, 64]), ALU.mult, [ps, sm], [q1])
                        yield
                        if xt is None:
                            tt("pool", o_.t[:].rearrange("p (g d) -> p g d", d=64), q1.t[:].rearrange("p (g d) -> p g d", d=64),
                               gt.t[:].unsqueeze(1).broadcast_to([128, 8, 64]), ALU.mult, [q1, gt], [o_])
                            yield
                        else:
                            tt("pool", u_.t[:].rearrange("p (g d) -> p g d", d=64), q1.t[:].rearrange("p (g d) -> p g d", d=64),
                               rc.t[:].unsqueeze(1).broadcast_to([128, 8, 64]), ALU.mult, [q1, rc], [u_])
                            yield
                            tt("dve", w_.t[:].rearrange("p (g d) -> p g d", d=64), q1.t[:].rearrange("p (g d) -> p g d", d=64),
                               rs_.t[:].unsqueeze(1).broadcast_to([128, 8, 64]), ALU.mult, [q1, rs_], [w_])
                            yield
                            u4 = u_.t[:].rearrange("p (a h f) -> p a h f", h=2, f=16)
                            w4 = w_.t[:].rearrange("p (a h f) -> p a h f", h=2, f=16)
                            o4 = o_.t[:].rearrange("p (a h f) -> p a h f", h=2, f=16)
                            tt("dve", o4[:, :, 0, :], u4[:, :, 0, :], w4[:, :, 1, :], ALU.add, [u_, w_], [o_])
                            yield
                            tt("dve", o4[:, :, 1, :], u4[:, :, 1, :], w4[:, :, 0, :], ALU.add, [u_, w_], [o_])
                            yield
                    def part_b():
                        pb = psbf(psT)
                        for h in range(4):
                            tr(pb[:, pcol + h * 128: pcol + (h + 1) * 128], o_.t[:, h * 128:(h + 1) * 128], ident.t[:], [o_, ident], [PS[psT]])
                        cp("dve", dstT.t[:, :, dcol:dcol + 128], pb[:, pcol:pcol + 512].rearrange("p (h t) -> p h t", t=128), [PS[psT]], [dbuf])
                    return part_a(), part_b

                tno = 0
                pend_b = []
                blocks = [("c", 0, NC)] + [("x", b, TPB) for b in range(NB)]
                for bi, (kind, b, ntl) in enumerate(blocks):
                    hb = hblk[bi % 2]
                    if kind == "c":
                        dma("sp", hb.t[:, :, 0:CTXL], hTc_d, [B_hTc], [hb])
                    else:
                        dma("sp", hb.t[:], hT_d[:, :, b * BLK:(b + 1) * BLK], [B_hT[b]], [hb])
                    for s in range(ntl):
                        kt = s if kind == "c" else NC + b * TPB + s
                        xt = None if kind == "c" else b * TPB + s
                        st3 = (tno % 2) * 3
                        psT = 6 + (tno % 2)
                        tno += 1
                        lh = lambda k: hb.t[:, k, s * 128:(s + 1) * 128]
                        for c in range(3):
                            if c == 0 and kind == "c":
                                continue
                            pp = PS[st3 + c]
                            for k in range(8):
                                mm(pp.t[:], lh(k), wqkv.t[:, k, c * 512:(c + 1) * 512], k == 0, False, [hb, wqkv], [pp])
                            mm(pp.t[:], onesb.t[0:1, :], brow.t[0:1, c * 512:(c + 1) * 512], False, True, [onesb, brow], [pp])
                        act(V.t[:, kt, :], PS[st3 + 2].t[:], AF.Copy, [PS[st3 + 2]], [V.bs[kt]])
                        rc = rs_ = None
                        if kind == "x":
                            rc = rcs[xt % 3]; rs_ = rss[xt % 3]
                            dma("sp", rc.t[:], ropec_d[xt * 128:(xt + 1) * 128, :], [], [rc])
                            dma("sp", rs_.t[:], ropes_d[xt * 128:(xt + 1) * 128, :], [], [rs_])
                            gt4 = gtab[xt % 3]
                            tt("pool", gt4[0].t[:], rc.t[:], gq.t[:], ALU.mult, [rc, gq], [gt4[0]]); tt("pool", gt4[1].t[:], rs_.t[:], gq.t[:], ALU.mult, [rs_, gq], [gt4[1]])
                            tt("pool", gt4[2].t[:], rc.t[:], gk.t[:], ALU.mult, [rc, gk], [gt4[2]]); tt("pool", gt4[3].t[:], rs_.t[:], gk.t[:], ALU.mult, [rs_, gk], [gt4[3]])
                            pairs = [qknorm(PS[st3 + 0], gq, xt, QT, QT.bs[xt], xt * 128, psT, 0, gt4[0], gt4[1]),
                                     qknorm(PS[st3 + 1], gk, xt, KT, KT.bs[kt], kt * 128, psT, 512, gt4[2], gt4[3])]
                        else:
                            pairs = [qknorm(PS[st3 + 1], gk, xt, KT, KT.bs[kt], kt * 128, psT, 512, None, None)]
                        gens = [p_[0] for p_ in pairs]
                        while gens:
                            gens = [g_ for g_ in gens if next(g_, "done") != "done"]
                        newb = [p_[1] for p_ in pairs]
                        for fb_ in pend_b:
                            fb_()
                        pend_b = newb
                for fb_ in pend_b:
                    fb_()
                S_.barrier()

            with contextlib.ExitStack() as es:
                def sb(name, shape, dt=F32, nb=1):
                    return T(es.enter_context(nc.sbuf_tensor("s_" + name, list(shape), dt)), nb)
                pt2 = [sb("pt2_%d" % i, [128, 2, 512], BF16) for i in range(3)]
                r0 = sb("r0", [128, BLK]); r1 = sb("r1", [128, BLK]); t0 = sb("t0", [128, BLK]); t1 = sb("t1", [128, BLK])
                dd = sb("dd", [128, BLK]); dsq = sb("dsq", [128, BLK]); rsd = sb("rsd", [128, BLK])
                oat = [sb("oat%d" % i, [128, BLK], BF16) for i in range(2)]
                o_acc = [PS[0], PS[1]]; s_acc = [PS[2], PS[3]]
                scp = [[PS[4], PS[5]], [PS[6], PS[7]]]
                zt = sb("zt", [128, 2 * D], BF16)
                mset("pool", zt.t[:], 0.0, [zt])
                Xs_z = Xs_d.rearrange("(c p r) d -> c p (r d)", p=128, r=2)
                for c_ in range(NZ):
                    dma("pool", Xs_z[c_], zt.t[:], [zt], [B_Xs[c_]])
                for e_ in range(NE):
                    w1src = ew1_d[e_].rearrange("(k p) n -> p k n", p=128)
                    w1dst = W1b_d[e_ * 128:(e_ + 1) * 128, :].rearrange("p (k n) -> p k n", k=8)
                    for k0 in (0, 4):
                        dma("pool", w1dst[:, k0:k0 + 4, :], w1src[:, k0:k0 + 4, :], [], [])
                    w2src = ew2_d[e_].rearrange("(k p) n -> p k n", p=128)
                    w2dst = W2b_d[e_ * 128:(e_ + 1) * 128, :].rearrange("p (k n) -> p k n", k=8)
                    dma("pool", w2dst, w2src, [], [])
                its = [(h, qb, kt) for h in range(4) for qb in range(NB) for kt in range(NKT)]

                def scores(n_):
                    h, qb, kt = its[n_]
                    sp_ = scp[n_ % 2]
                    for m in range(2):
                        mm(sp_[m].t[:, 0:BLK], KT.t[m * 64:(m + 1) * 64, h, kt * 128:(kt + 1) * 128],
                           QT.t[m * 64:(m + 1) * 64, h, qb * BLK:(qb + 1) * BLK], True, True,
                           [KT.bs[kt]] + [QT.bs[qb * TPB + j] for j in range(TPB)], [sp_[m]])
                o0s = sb("o0s", [128, BLK]); o1s = sb("o1s", [128, BLK]); ssb = sb("ssb", [128, BLK]); w32 = sb("w32", [128, 128])
                mset("dve", w32.t[:], 1.0 / 32, [w32])
                sbank = PS[2]; fbank = PS[3]

                def finalize_gen(h, qb):
                    recip(ssb.t[0:64, :], ssb.t[0:64, :], [ssb], [ssb])
                    mm(fbank.t[:, 0:BLK], w32.t[0:32, :], ssb.t[0:32, :], True, True, [w32, ssb], [fbank])
                    yield
                    tt("dve", t0.t[:], o0s.t[:], fbank.t[:, 0:BLK], ALU.mult, [o0s, fbank], [t0])
                    mm(fbank.t[:, 0:BLK], w32.t[32:64, :], ssb.t[32:64, :], True, True, [w32, ssb], [fbank])
                    yield
                    tt("dve", t1.t[:], o1s.t[:], fbank.t[:, 0:BLK], ALU.mult, [o1s, fbank], [t1])
                    stt("dve", dd.t[:], t1.t[:], neglam.t[:, 0:1], t0.t[:], ALU.mult, ALU.add, [t0, t1, neglam], [dd])
                    tt("dve", dsq.t[:], dd.t[:], dd.t[:], ALU.mult, [dd], [dsq])
                    yield
                    mm(fbank.t[:, 0:BLK], onesf.t[:], dsq.t[:], True, True, [onesf, dsq], [fbank])
                    yield
                    act(rsd.t[:], fbank.t[:, 0:BLK], AF.Sqrt, [fbank, epst], [rsd], scale=1.0 / 128, bias=epst.t[:])
                    recip(rsd.t[:], rsd.t[:], [rsd], [rsd])
                    oo = oat[(h * NB + qb) % 2]
                    stt("dve", oo.t[:], dd.t[:], gsub.t[:, 0:1], rsd.t[:], ALU.mult, ALU.mult, [dd, gsub, rsd], [oo])
                    dma("sp", oaT_d[:, h, qb * BLK:(qb + 1) * BLK], oo.t[:], [oo], [B_oaT[qb]])

                pending = []
                scores(0)
                for it in range(len(its)):
                    h, qb, kt = its[it]
                    if it + 1 < len(its):
                        scores(it + 1)
                    sp_ = scp[it % 2]
                    p2 = pt2[it % 3]
                    bank0 = 4 + 2 * (it % 2)
                    act(p2.t[:, :, 0:BLK], psall[:, bank0 * 512:(bank0 + 2) * 512].rearrange("p (m q) -> p m q", m=2)[:, :, 0:BLK], AF.Exp,
                        [sp_[0], sp_[1]], [p2], scale=0.125)
                    for m in range(2):
                        mm(o_acc[m].t[:, 0:BLK], V.t[:, kt, h * 128:(h + 1) * 128], p2.t[:, m, 0:BLK], kt == 0, kt == NKT - 1, [V.bs[kt], p2], [o_acc[m]])
                    for m in range(2):
                        mm(sbank.t[32 * m:32 * (m + 1), 0:BLK], onesb.t[:, 32 * m:32 * (m + 1)], p2.t[:, m, 0:BLK], kt == 0, kt == NKT - 1,
                           [onesb, p2], [sbank], tp=(0, 32 * m))
                    if pending and kt >= 1:
                        if next(pending[0], "done") == "done":
                            pending.pop(0)
                    if kt == NKT - 1:
                        for g_ in pending:
                            for _ in g_:
                                pass
                        pending = []
                        S_.op("act", lambda e: e.copy(o0s.t[:], o_acc[0].t[:, 0:BLK]), [o_acc[0].b], [o0s.b])
                        cp("dve", o1s.t[:], o_acc[1].t[:, 0:BLK], [o_acc[1]], [o1s])
                        cp("dve", ssb.t[0:64, :], sbank.t[0:64, 0:BLK], [sbank], [ssb])
                        pending.append(finalize_gen(h, qb))
                for g_ in pending:
                    for _ in g_:
                        pass
                S_.barrier()

        with contextlib.ExitStack() as esh:
            def sbh(name, shape, dt=F32, nb=1):
                return T(esh.enter_context(nc.sbuf_tensor("s_" + name, list(shape), dt)), nb)
            u_tm = sbh("u_tm", [128, NT, 512], BF16, nb=NT)
            with contextlib.ExitStack() as es:
                def sb(name, shape, dt=F32, nb=1):
                    return T(es.enter_context(nc.sbuf_tensor("s_" + name, list(shape), dt)), nb)
                hring = [sb("h0hb%d" % i, [128, 8, BLK], BF16) for i in range(2)]; whY = sb("whY", [128, 8, 1536], BF16)
                bhy = sb("bhy", [128, 40]); cw = sb("cw", [128, 12, 3]); cb = sb("cb", [128, 12])
                ppads = [[sb("ppad%d_%d" % (i, c), [128, S + 2], BF16) for c in range(3)] for i in range(2)]
                zf = sb("zf", [128, S])
                zX = sb("zX", [128, S], BF16); zA = sb("zA", [128, S], BF16); zB = sb("zB", [128, S], BF16); uTb = sb("uTb", [128, S], BF16)
                for c in range(3):
                    dma("pool", whY.t[:, :, c * 512:(c + 1) * 512], w_in_v[:, :, 1536 + c * 512:1536 + (c + 1) * 512], [], [whY])
                dma("sp", bhy.t[:], bin_fm_d, [], [bhy]); dma("sp", cw.t[:], convw_d, [], [cw]); dma("sp", cb.t[:], convb_d, [], [cb])
                for i in range(2):
                    for c in range(3):
                        mset("pool", ppads[i][c].t[:, 0:1], 0.0, [ppads[i][c]]); mset("pool", ppads[i][c].t[:, S + 1:S + 2], 0.0, [ppads[i][c]])
                zn = 0
                pend_tr = None
                for j in range(4):
                    pset = ppads[j % 2]
                    for b in range(NB):
                        hb = hring[b % 2]
                        dma("sp", hb.t[:], hT_d[:, :, b * BLK:(b + 1) * BLK], [B_hT[b]], [hb])
                        for c3 in range(3):
                            ch = c3 * 4 + j
                            pp = PS[zn % 4]; zn += 1
                            for k in range(8):
                                mm(pp.t[:, 0:BLK], whY.t[:, k, ch * 128:(ch + 1) * 128], hb.t[:, k, :], k == 0, k == 7, [whY, hb], [pp])
                            act(pset[c3].t[:, 1 + b * BLK:1 + (b + 1) * BLK], pp.t[:, 0:BLK], AF.Identity, [pp, bhy], [pset[c3]], bias=bhy.t[:, 12 + ch:13 + ch])
                    if pend_tr is not None:
                        pend_tr()
                        pend_tr = None
                    for c3, out in ((0, zX), (1, zA), (2, zB)):
                        ch = c3 * 4 + j
                        pp_ = pset[c3]
                        ts("dve", zf.t[:], pp_.t[:, 0:S], cw.t[:, ch, 0:1], cb.t[:, ch:ch + 1], ALU.mult, ALU.add, [pp_, cw, cb], [zf])
                        stt("dve", zf.t[:], pp_.t[:, 1:S + 1], cw.t[:, ch, 1:2], zf.t[:], ALU.mult, ALU.add, [pp_, cw, zf], [zf])
                        stt("dve", out.t[:], pp_.t[:, 2:S + 2], cw.t[:, ch, 2:3], zf.t[:], ALU.mult, ALU.add, [pp_, cw, zf], [out])
                    dma("sp", x0c_d[:, j, :], zX.t[:], [zX], [B_x0c[j]])
                    tt("pool", uTb.t[:], zA.t[:], zB.t[:], ALU.mult, [zA, zB], [uTb])
                    dma("sp", uT_d[:, j, :], uTb.t[:], [uTb], [B_uT[j]])
                    def do_tr(j=j):
                        for t0_ in range(0, NT, 8):
                            nt_ = min(8, NT - t0_)
                            psi = 4 + ((j * NT + t0_) // 8) % 2
                            pb = psbf(psi)
                            for t_ in range(nt_):
                                tr(pb[:, t_ * 128:(t_ + 1) * 128], uTb.t[:, (t0_ + t_) * 128:(t0_ + t_ + 1) * 128], ident.t[:], [uTb, ident], [PS[psi]])
                            cp("dve", u_tm.t[:, t0_:t0_ + nt_, j * 128:(j + 1) * 128], pb[:, 0:nt_ * 128].rearrange("p (t c) -> p t c", c=128),
                               [PS[psi]], [u_tm.bs[t] for t in range(t0_, t0_ + nt_)])
                    pend_tr = do_tr
                pend_tr()
                S_.barrier()

            with contextlib.ExitStack() as esk:
                ksum = T(esk.enter_context(nc.sbuf_tensor("s_ksum", [128, NT, 512], BF16)), NT)
                kdiff = T(esk.enter_context(nc.sbuf_tensor("s_kdiff", [128, NT, 512], BF16)), NT)
                with contextlib.ExitStack() as es:
                    def sb(name, shape, dt=F32, nb=1):
                        return T(es.enter_context(nc.sbuf_tensor("s_" + name, list(shape), dt)), nb)
                    zT = sb("zT", [33, S]); fw1 = sb("fw1", [33, 64]); fw2 = sb("fw2", [64, 64]); fw3 = sb("fw3", [64, 64]); fw4 = sb("fw4", [64, 1024])
                    fvec = sb("fvec", [64, 4]); trow = sb("trow", [1, S]); drow = sb("drow", [1, 512])
                    H3 = sb("H3", [64, S])
                    arg = sb("arg", [64, BLK]); ai = sb("ai", [64, BLK], I32); af = sb("af", [64, BLK]); hh = [sb("hh%d" % i, [64, BLK]) for i in range(2)]
                    dec = sb("dec", [128, 512]); kf = sb("kf", [128, 512]); kb = sb("kb", [128, 512])
                    for t_, d_ in [(zT, zT_d), (fw1, fw1_d), (fw2, fw2_d), (fw3, fw3_d), (fw4, fw4_d), (fvec, fvec_d), (trow, trow_d), (drow, drow_d)]:
                        dma("sp", t_.t[:], d_, [], [t_])

                    def sin_layer(ps, li, out_ap, out_t):
                        ts("dve", arg.t[:], ps.t[0:64, 0:BLK], fvec.t[:, li:li + 1], fvec.t[:, 3:4], ALU.add, ALU.mult, [ps, fvec], [arg])
                        ts("dve", arg.t[:], arg.t[:], 1.0 / (2 * PI), 16.0, ALU.mult, ALU.add, [arg], [arg])
                        cp("dve", ai.t[:], arg.t[:], [arg], [ai])
                        cp("dve", af.t[:], ai.t[:], [ai], [af])
                        tt("dve", arg.t[:], arg.t[:], af.t[:], ALU.subtract, [arg, af], [arg])
                        ts("dve", af.t[:], arg.t[:], 0.5, None, ALU.is_gt, None, [arg], [af])
                        tt("dve", arg.t[:], arg.t[:], af.t[:], ALU.subtract, [arg, af], [arg])
                        act(out_ap, arg.t[:], AF.Sin, [arg], [out_t], scale=2 * PI)

                    for b in range(NB):
                        sl = slice(b * BLK, (b + 1) * BLK)
                        mm(PS[0].t[0:64, 0:BLK], fw1.t[:], zT.t[:, sl], True, True, [fw1, zT], [PS[0]])
                        sin_layer(PS[0], 0, hh[0].t[:], hh[0])
                        mm(PS[1].t[0:64, 0:BLK], fw2.t[:], hh[0].t[:], True, True, [fw2, hh[0]], [PS[1]])
                        sin_layer(PS[1], 1, hh[1].t[:], hh[1])
                        mm(PS[2].t[0:64, 0:BLK], fw3.t[:], hh[1].t[:], True, True, [fw3, hh[1]], [PS[2]])
                        sin_layer(PS[2], 2, H3.t[:, sl], H3)
                    for lt in range(NT):
                        pf = PS[(lt % 2) * 3]; pb_ = PS[(lt % 2) * 3 + 1]; pd = PS[(lt % 2) * 3 + 2]
                        mm(pf.t[:], H3.t[:, lt * 128:(lt + 1) * 128], fw4.t[:, 0:512], True, True, [H3, fw4], [pf])
                        mm(pb_.t[:], H3.t[:, lt * 128:(lt + 1) * 128], fw4.t[:, 512:1024], True, True, [H3, fw4], [pb_])
                        mm(pd.t[:], trow.t[0:1, lt * 128:(lt + 1) * 128], drow.t[0:1, :], True, True, [trow, drow], [pd])
                        act(dec.t[:], pd.t[:], AF.Exp, [pd], [dec], scale=-1.0)
                        stt("dve", kf.t[:], pf.t[:], 2.0 / N2, dec.t[:], ALU.mult, ALU.mult, [pf, dec], [kf])
                        stt("dve", kb.t[:], pb_.t[:], 2.0 / N2, dec.t[:], ALU.mult, ALU.mult, [pb_, dec], [kb])
                        if lt == 0:
                            mset("dve", kb.t[0:1, :], 0.0, [kb])
                        tt("pool", ksum.t[:, lt, :], kf.t[:], kb.t[:], ALU.add, [kf, kb], [ksum.bs[lt]])
                        tt("pool", kdiff.t[:, lt, :], kb.t[:], kf.t[:], ALU.subtract, [kf, kb], [kdiff.bs[lt]])
                    S_.barrier()

                with contextlib.ExitStack() as es:
                    def sb(name, shape, dt=F32, nb=1):
                        return T(es.enter_context(nc.sbuf_tensor("s_" + name, list(shape), dt)), nb)
                    cf = [sb("cf%d" % i, [128, NT, 128], BF16) for i in range(2)]; sf = [sb("sf%d" % i, [128, NT, 128], BF16) for i in range(2)]
                    kre = sb("kre", [128, 512]); kim = sb("kim", [128, 512]); m1 = sb("m1", [128, 512]); m2 = sb("m2", [128, 512])
                    yt = [sb("yt%d" % i, [128, 2, 512], BF16) for i in range(2)]
                    for kc in range(NKC):
                        c_ = cf[kc % 2]; s_ = sf[kc % 2]
                        if kc == 0:
                            dma("sp", c_.t[:], Cf_d[0], [], [c_]); dma("sp", s_.t[:], Sf_d[0], [], [s_])
                        if kc + 1 < NKC:
                            cn_ = cf[(kc + 1) % 2]; sn_ = sf[(kc + 1) % 2]
                            dma("sp", cn_.t[:], Cf_d[kc + 1], [], [cn_]); dma("sp", sn_.t[:], Sf_d[kc + 1], [], [sn_])
                        pa = PS[(kc % 2) * 4: (kc % 2) * 4 + 4]
                        for t_ in range(NT):
                            st_, sp2 = (t_ == 0), (t_ == NT - 1)
                            mm(pa[0].t[:], c_.t[:, t_, :], u_tm.t[:, t_, :], st_, sp2, [c_, u_tm.bs[t_]], [pa[0]])
                            mm(pa[1].t[:], s_.t[:, t_, :], u_tm.t[:, t_, :], st_, sp2, [s_, u_tm.bs[t_]], [pa[1]])
                            mm(pa[2].t[:], c_.t[:, t_, :], ksum.t[:, t_, :], st_, sp2, [c_, ksum.bs[t_]], [pa[2]])
                            mm(pa[3].t[:], s_.t[:, t_, :], kdiff.t[:, t_, :], st_, sp2, [s_, kdiff.bs[t_]], [pa[3]])
                        y_ = yt[kc % 2]
                        act(kre.t[:], pa[2].t[:], AF.Copy, [pa[2]], [kre]); act(kim.t[:], pa[3].t[:], AF.Copy, [pa[3]], [kim])
                        tt("dve", m1.t[:], pa[0].t[:], kre.t[:], ALU.mult, [pa[0], kre], [m1])
                        tt("dve", m2.t[:], pa[1].t[:], kim.t[:], ALU.mult, [pa[1], kim], [m2])
                        tt("pool", y_.t[:, 0, :], m1.t[:], m2.t[:], ALU.add, [m1, m2], [y_])
                        tt("dve", m1.t[:], pa[0].t[:], kim.t[:], ALU.mult, [pa[0], kim], [m1])
                        tt("dve", m2.t[:], pa[1].t[:], kre.t[:], ALU.mult, [pa[1], kre], [m2])
                        tt("pool", y_.t[:, 1, :], m1.t[:], m2.t[:], ALU.subtract, [m1, m2], [y_])
                        dma("sp", Y_d[:, :, kc, :], y_.t[:], [y_], [B_Y[kc]])
                    S_.barrier()

        with contextlib.ExitStack() as es:
            def sb(name, shape, dt=F32, nb=1):
                return T(es.enter_context(nc.sbuf_tensor("s_" + name, list(shape), dt)), nb)
            Yall = sb("Yall", [128, 2, NKC, 512], BF16, nb=NKC)
            ci = [sb("ci%d" % i, [128, KG, BLK], BF16) for i in range(3)]; si = [sb("si%d" % i, [128, KG, BLK], BF16) for i in range(3)]
            x0b = [sb("x0b%d" % i, [128, 4, BLK], BF16) for i in range(2)]; uTk = [sb("uTk%d" % i, [128, 4, BLK], BF16) for i in range(2)]
            obb = [sb("obb%d" % i, [128, 4, BLK], BF16) for i in range(2)]
            tmp = [sb("h2tmp%d" % i, [128, BLK]) for i in range(2)]; hbv = sb("hbv", [128, 4])
            dma("sp", hbv.t[:], hb_d, [], [hbv])
            def load_y(kg_):
                for kc_ in range(kg_ * KG, (kg_ + 1) * KG):
                    dma("sp", Yall.t[:, :, kc_, :], Y_d[:, :, kc_, :], [B_Y[kc_]], [Yall.bs[kc_]])
            load_y(0)
            n = 0
            for tb in range(NB):
                acc = PS[(tb % 2) * 4:(tb % 2) * 4 + 4]
                xb = x0b[tb % 2]; ub = uTk[tb % 2]; ob = obb[tb % 2]
                dma("sp", xb.t[:], x0c_d[:, :, tb * BLK:(tb + 1) * BLK], B_x0c, [xb])
                dma("sp", ub.t[:], uT_d[:, :, tb * BLK:(tb + 1) * BLK], B_uT, [ub])
                for kg in range(NKG):
                    c_ = ci[n % 3]; s_ = si[n % 3]; n += 1
                    dma("sp", c_.t[:], Ci_d[tb, :, kg * KG:(kg + 1) * KG, :], [], [c_])
                    dma("sp", s_.t[:], Si_d[tb, :, kg * KG:(kg + 1) * KG, :], [], [s_])
                    if tb == 0 and kg + 1 < NKG:
                        load_y(kg + 1)
                    for kl in range(KG):
                        kc = kg * KG + kl
                        for c4 in range(4):
                            mm(acc[c4].t[:, 0:BLK], Yall.t[:, 0, kc, c4 * 128:(c4 + 1) * 128], c_.t[:, kl, :], kc == 0, False, [Yall.bs[kc], c_], [acc[c4]])
                            mm(acc[c4].t[:, 0:BLK], Yall.t[:, 1, kc, c4 * 128:(c4 + 1) * 128], s_.t[:, kl, :], False, kc == NKC - 1, [Yall.bs[kc], s_], [acc[c4]])
                for c4 in range(4):
                    tm = tmp[c4 % 2]
                    stt("dve", tm.t[:], ub.t[:, c4, :], hbv.t[:, c4:c4 + 1], acc[c4].t[:, 0:BLK], ALU.mult, ALU.add, [ub, hbv, acc[c4]], [tm])
                    tt("pool", ob.t[:, c4, :], tm.t[:], xb.t[:, c4, :], ALU.mult, [tm, xb], [ob])
                dma("sp", obT_d[:, :, tb * BLK:(tb + 1) * BLK], ob.t[:], [ob], [B_obT[tb]])
            S_.barrier()

        with contextlib.ExitStack() as es:
            def sb(name, shape, dt=F32, nb=1):
                return T(es.enter_context(nc.sbuf_tensor("s_" + name, list(shape), dt)), nb)
            wg = sb("wg", [128, 8, 2048], BF16); wua = sb("wua", [128, 4, D], BF16); wub = sb("wub", [128, 4, D], BF16)
            wout = sb("wout", [128, 8, D], BF16); rwt = sb("rwt", [128, 8, 32], BF16); rbt = sb("rbt", [1, 32], BF16)
            bgf = sb("bgf", [128, 40])
            hblk = [sb("mhb%d" % i, [128, 8, BLK], BF16) for i in range(2)]
            oab = [sb("moa%d" % i, [128, 4, BLK], BF16) for i in range(2)]; obk = [sb("mob%d" % i, [128, 4, BLK], BF16) for i in range(2)]
            gas = [sb("gas%d" % i, [128, BLK]) for i in range(2)]; gbs = [sb("gbs%d" % i, [128, BLK]) for i in range(2)]
            mm1 = [sb("mm1%d" % i, [128, BLK]) for i in range(1)]; mm2 = [sb("mm2%d" % i, [128, BLK]) for i in range(1)]
            mTs = [sb("mT%d" % i, [128, 8, BLK], BF16, nb=8) for i in range(2)]
            xin = [sb("mxin%d" % i, [128, D]) for i in range(2)]; xn = [sb("mxn%d" % i, [128, D]) for i in range(2)]
            tg = sb("mtg", [128, 512])
            h2b = [sb("h2b%d" % i, [128, 8, BLK], BF16) for i in range(2)]
            msks = [sb("msk%d" % i, [128, 32]) for i in range(2)]; mskbs = [sb("mskb%d" % i, [128, 32], BF16) for i in range(2)]; nmx = sb("nmx", [128, 1])
            htmp = sb("htmp", [128, D]); h2tm = [sb("h2tm%d" % i, [128, D], BF16) for i in range(1)]
            ex = sb("ex", [128, 32]); sm_ = sb("sm", [128, 1])
            st = norm_transpose(es, "m")
            for c in range(4):
                dma("pool", wg.t[:, :, c * 512:(c + 1) * 512], w_in_v[:, :, 3072 + c * 512:3072 + (c + 1) * 512], [], [wg])
            dma("pool", wua.t[:], wua_d.rearrange("(k p) n -> p k n", p=128), [], [wua])
            dma("pool", wub.t[:], wub_d.rearrange("(k p) n -> p k n", p=128), [], [wub])
            for c in range(2):
                dma("pool", wout.t[:, :, c * 512:(c + 1) * 512], wout_d.rearrange("(k p) n -> p k n", p=128)[:, :, c * 512:(c + 1) * 512], [], [wout])
            dma("pool", rwt.t[:], rw_d.rearrange("(k p) n -> p k n", p=128), [], [rwt]); dma("pool", rbt.t[:], rb_d, [], [rbt])
            dma("sp", bgf.t[:], bin_fm_d, [], [bgf])
            tno = 0

            def gates(tb):
                hb = hblk[tb % 2]; oa = oab[tb % 2]; ob = obk[tb % 2]; mT = mTs[tb % 2]
                dma("sp", hb.t[:], hT_d[:, :, tb * BLK:(tb + 1) * BLK], [B_hT[tb]], [hb])
                dma("sp", oa.t[:], oaT_d[:, :, tb * BLK:(tb + 1) * BLK], [B_oaT[tb]], [oa])
                dma("sp", ob.t[:], obT_d[:, :, tb * BLK:(tb + 1) * BLK], [B_obT[tb]], [ob])
                for j in range(8):
                    pga, pgb, pA, pB = PS[0], PS[1], PS[2], PS[3]
                    for k in range(8):
                        mm(pga.t[:, 0:BLK], wg.t[:, k, j * 128:(j + 1) * 128], hb.t[:, k, :], k == 0, k == 7, [wg, hb], [pga])
                    for k in range(8):
                        mm(pgb.t[:, 0:BLK], wg.t[:, k, 1024 + j * 128:1024 + (j + 1) * 128], hb.t[:, k, :], k == 0, k == 7, [wg, hb], [pgb])
                    for k in range(4):
                        mm(pA.t[:, 0:BLK], wua.t[:, k, j * 128:(j + 1) * 128], oa.t[:, k, :], k == 0, k == 3, [wua, oa], [pA])
                    for k in range(4):
                        mm(pB.t[:, 0:BLK], wub.t[:, k, j * 128:(j + 1) * 128], ob.t[:, k, :], k == 0, k == 3, [wub, ob], [pB])
                    ga_ = gas[j % 2]; gb_ = gbs[j % 2]; a1 = mm1[0]; a2 = mm2[0]
                    act(ga_.t[:], pga.t[:, 0:BLK], AF.Sigmoid, [pga, bgf], [ga_], bias=bgf.t[:, 24 + j:25 + j])
                    act(gb_.t[:], pgb.t[:, 0:BLK], AF.Sigmoid, [pgb, bgf], [gb_], bias=bgf.t[:, 32 + j:33 + j])
                    tt("dve", a1.t[:], pA.t[:, 0:BLK], ga_.t[:], ALU.mult, [pA, ga_], [a1])
                    tt("dve", a2.t[:], pB.t[:, 0:BLK], gb_.t[:], ALU.mult, [pB, gb_], [a2])
                    tt("pool", mT.t[:, j, :], a1.t[:], a2.t[:], ALU.add, [a1, a2], [mT.bs[j]])

            def tile_chain(tb, s):
                nonlocal tno
                h2 = h2b[tb % 2]; mT = mTs[tb % 2]
                if True:
                    i = tb * TPB + s
                    msk = msks[s % 2]; mskb = mskbs[s % 2]
                    xi = xin[tno % 2]; xo = xn[tno % 2]; tno += 1
                    dma("sp", xi.t[:], x_d[i * 128:(i + 1) * 128, :], [], [xi])
                    for half in range(2):
                        po = PS[4 + half]
                        for j in range(8):
                            mm(po.t[:], mT.t[:, j, s * 128:(s + 1) * 128], wout.t[:, j, half * 512:(half + 1) * 512], j == 0, j == 7, [mT.bs[j], wout], [po])
                        tt("dve", tg.t[:], po.t[:], g1row.t[:, half * 512:(half + 1) * 512], ALU.mult, [po, g1row], [tg])
                        tt("pool", xo.t[:, half * 512:(half + 1) * 512], tg.t[:], xi.t[:, half * 512:(half + 1) * 512], ALU.add, [tg, xi], [xo])
                    dma("sp", y_d[i * 128:(i + 1) * 128, :], xo.t[:], [xo], [B_y[i]])
                    yield
                    ss_t = do_norm_transpose(st, xo, A2, B2, 6, lambda k, s=s, h2=h2: h2.t[:, k, s * 128:(s + 1) * 128], [h2])
                    hm = h2tm[0]
                    stt("dve", htmp.t[:], xo.t[:], ss_t.t[:, 0:1], A2row.t[:], ALU.mult, ALU.mult, [xo, ss_t, A2row], [htmp])
                    tt("pool", hm.t[:], htmp.t[:], B2row.t[:], ALU.add, [htmp, B2row], [hm])
                    dma("sp", H2tm_d[i * 128:(i + 1) * 128, :], hm.t[:], [hm], [B_h2tm[i]])
                    yield
                    pr = PS[7]
                    for k in range(8):
                        mm(pr.t[:, 0:32], h2.t[:, k, s * 128:(s + 1) * 128], rwt.t[:, k, :], k == 0, False, [h2, rwt], [pr])
                    mm(pr.t[:, 0:32], onesb.t[0:1, :], rbt.t[0:1, :], False, True, [onesb, rbt], [pr])
                    lgi = LG.t[:, i, :]
                    act(lgi, pr.t[:, 0:32], AF.Copy, [pr], [LG.bs[i]])
                    vmax8(MX.t[:, i, :], lgi, [LG.bs[i]], [MX.bs[i]])
                    ts("dve", msk.t[:], lgi, MX.t[:, i, 3:4], None, ALU.is_ge, None, [LG.bs[i], MX.bs[i]], [msk])
                    cp("dve", mskb.t[:], msk.t[:], [msk], [mskb])
                    ts("dve", nmx.t[:], MX.t[:, i, 0:1], -1.0, None, ALU.mult, None, [MX.bs[i]], [nmx])
                    act(ex.t[:], lgi, AF.Exp, [LG.bs[i], nmx], [ex], bias=nmx.t[:, 0:1])
                    tt("dve", ex.t[:], ex.t[:], msk.t[:], ALU.mult, [ex, msk], [ex])
                    red(sm_.t[:], ex.t[:], [ex], [sm_])
                    recip(sm_.t[:], sm_.t[:], [sm_], [sm_])
                    ts("dve", G.t[:, i, :], ex.t[:], sm_.t[:, 0:1], None, ALU.mult, None, [ex, sm_], [G.bs[i]])
                    yield
                    mm(pr.t[:, 32:64], ustr.t[:], mskb.t[:], True, False, [ustr, mskb], [pr])
                    mm(pr.t[:, 32:64], onesb.t[:], cumb.t[:], False, True, [onesb, cumb], [pr])
                    cp("dve", POS.t[:, i, :], pr.t[:, 32:64], [pr], [POS.bs[i]])
                    tt("dve", cumf.t[:], cumf.t[:], msk.t[:], ALU.add, [cumf, msk], [cumf])
                    cp("dve", cumb.t[:], cumf.t[:], [cumf], [cumb])


            def tiles(tb):
                for s0 in range(0, TPB, 2):
                    gens = [tile_chain(tb, s_) for s_ in range(s0, min(TPB, s0 + 2))]
                    while gens:
                        gens = [g_ for g_ in gens if next(g_, "done") != "done"]

            gates(0)
            for tb in range(NB):
                if tb + 1 < NB:
                    gates(tb + 1)
                tiles(tb)
            S_.barrier()
        if debug:
            with contextlib.ExitStack() as es3:
                dt_ = T(es3.enter_context(nc.sbuf_tensor("s_dbgt", [128, 8, S], F32)))
                db_ = T(es3.enter_context(nc.sbuf_tensor("s_dbgb", [128, 8, S], BF16)))
                for nm, src, nch in [("dbg_oaT", oaT_d, 4), ("dbg_obT", obT_d, 4), ("dbg_hT", hT_d, 8)]:
                    dma("sp", db_.t[:, 0:nch, :], src, [], [db_])
                    cp("dve", dt_.t[:, 0:nch, :], db_.t[:, 0:nch, :], [db_], [dt_])
                    dma("sp", dbg[nm], dt_.t[:, 0:nch, :], [dt_], [Buf()])
                    S_.barrier()
                dma("sp", dbg["dbg_G"], G.t[:], G.bs, [Buf()])
                S_.barrier()

        with contextlib.ExitStack() as es:
            def sb(name, shape, dt=F32, nb=1):
                return T(es.enter_context(nc.sbuf_tensor("s_" + name, list(shape), dt)), nb)
            cnt = sb("cnt", [128, 32]); yv = sb("yv", [128, 32]); yi_ = sb("yi", [128, 32], I32); yf = sb("yf", [128, 32]); ygt = sb("ygt", [128, 32])
            padded = sb("padded", [128, 32]); ca = sb("csuma", [128, 32]); cb_ = sb("csumb", [128, 32]); pstart = sb("pstart", [128, 32])
            kp = sb("kp", [128, 8]); bstart = sb("bstart", [128, NBLK]); cmp_ = sb("cmp", [128, NBLK, 32]); be = sb("be", [128, NBLK])
            bf_ = sb("bf", [128, NBLK, 2])
            slotv = [sb("slotv%d" % i, [128, 32]) for i in range(2)]; oh4 = [sb("oh4%d" % i, [128, 4, 32]) for i in range(2)]
            pr4 = [sb("pr4%d" % i, [128, 4, 32]) for i in range(2)]; i4f = [sb("i4f%d" % i, [128, 4]) for i in range(2)]
            hrow = [sb("hrow%d" % i, [128, D], BF16) for i in range(3)]
            dma("sp", kp.t[:], kp_d, [], [kp]); dma("sp", bstart.t[:], bstart_d, [], [bstart])
            mm(PS[0].t[:, 0:32], onesb.t[:], cumb.t[:], True, True, [onesb, cumb], [PS[0]])
            cp("dve", cnt.t[:], PS[0].t[:, 0:32], [PS[0]], [cnt])
            ts("dve", yv.t[:], cnt.t[:], float(RB - 1), 1.0 / RB, ALU.add, ALU.mult, [cnt], [yv])
            cp("dve", yi_.t[:], yv.t[:], [yv], [yi_]); cp("dve", yf.t[:], yi_.t[:], [yi_], [yf])
            tt("dve", ygt.t[:], yf.t[:], yv.t[:], ALU.is_gt, [yf, yv], [ygt])
            tt("dve", yf.t[:], yf.t[:], ygt.t[:], ALU.subtract, [yf, ygt], [yf])
            ts("dve", padded.t[:], yf.t[:], float(RB), None, ALU.mult, None, [yf], [padded])
            cp("dve", ca.t[:], padded.t[:], [padded], [ca])
            src_, dst_ = ca, cb_
            for sh in (1, 2, 4, 8, 16):
                cp("dve", dst_.t[:, 0:sh], src_.t[:, 0:sh], [src_], [dst_])
                tt("dve", dst_.t[:, sh:32], src_.t[:, sh:32], src_.t[:, 0:32 - sh], ALU.add, [src_], [dst_])
                src_, dst_ = dst_, src_
            pend = src_
            tt("dve", pstart.t[:], pend.t[:], padded.t[:], ALU.subtract, [pend, padded], [pstart])
            tt("dve", cmp_.t[:], pend.t[:].unsqueeze(1).broadcast_to([128, NBLK, 32]), bstart.t[:].unsqueeze(2).broadcast_to([128, NBLK, 32]),
               ALU.is_le, [pend, bstart], [cmp_])
            red(be.t[:], cmp_.t[:], [cmp_], [be])
            ts("dve", be.t[:], be.t[:], float(NE - 1), None, ALU.min, None, [be], [be])
            stt("dve", bf_.t[:, :, 0], be.t[:], 128.0, kp.t[:, 0:1].broadcast_to([128, NBLK]), ALU.mult, ALU.add, [be, kp], [bf_])
            cp("dve", bf_.t[:, :, 1], be.t[:], [be], [bf_])
            cp("dve", BIDX.t[:], bf_.t[:], [bf_], [BIDX])
            for i in range(NT):
                sv = slotv[i % 2]; oh = oh4[i % 2]; p4 = pr4[i % 2]; f4 = i4f[i % 2]; hr = hrow[i % 3]
                tt("dve", sv.t[:], POS.t[:, i, :], pstart.t[:], ALU.add, [POS.bs[i], pstart], [sv])
                tt("dve", oh.t[:], LG.t[:, i, :].unsqueeze(1).broadcast_to([128, 4, 32]), MX.t[:, i, 0:4].unsqueeze(2).broadcast_to([128, 4, 32]),
                   ALU.is_equal, [LG.bs[i], MX.bs[i]], [oh])
                tt("dve", p4.t[:], oh.t[:], sv.t[:].unsqueeze(1).broadcast_to([128, 4, 32]), ALU.mult, [oh, sv], [p4])
                red(f4.t[:], p4.t[:], [p4], [f4])
                cp("dve", IDX.t[:, i, :], f4.t[:], [f4], [IDX.bs[i]])
                tt("dve", p4.t[:], oh.t[:], G.t[:, i, :].unsqueeze(1).broadcast_to([128, 4, 32]), ALU.mult, [oh, G.bs[i]], [p4])
                red(W4.t[:, i, :], p4.t[:], [p4], [W4.bs[i]])
                dma("sp", hr.t[:], H2tm_d[i * 128:(i + 1) * 128, :], [B_h2tm[i]], [hr])
                for j in range(4):
                    scatter(Xs_d, hr.t[:], IDX.t[:, i, j:j + 1], [hr, IDX.bs[i]], [])
            S_.barrier()

        with contextlib.ExitStack() as es:
            def sb(name, shape, dt=F32, nb=1):
                return T(es.enter_context(nc.sbuf_tensor("s_" + name, list(shape), dt)), nb)
            w1t = [sb("w1t%d" % i, [128, 8, 2048], BF16) for i in range(2)]; w2t = [sb("w2t%d" % i, [128, 8, D], BF16) for i in range(2)]
            b1t = [sb("b1t%d" % i, [128, 16]) for i in range(2)]; b2rep = [sb("b2rep%d" % i, [128, D], BF16) for i in range(2)]
            xsb = [sb("xsb%d" % i, [128, NST, D], BF16) for i in range(2)]; XsT = [sb("XsT%d" % i, [128, 8, RB], BF16) for i in range(2)]
            actT = [sb("actT%d" % i, [128, 8, RB], BF16, nb=8) for i in range(2)]
            gcl = [sb("gcl%d" % i, [128, RB]) for i in range(2)]; sg = [sb("sg%d" % i, [128, RB]) for i in range(2)]
            ucl = [sb("ucl%d" % i, [128, RB]) for i in range(2)]
            ysb = [sb("ysb%d" % i, [128, D]) for i in range(2)]
            ew1f = ew1_d.rearrange("e d n -> (e d) n"); ew2f = ew2_d.rearrange("e d n -> (e d) n")
            en = 0; yn = 0
            for b in range(NBLK):
                w1 = w1t[b % 2]; w2 = w2t[b % 2]; b1 = b1t[b % 2]; b2 = b2rep[b % 2]; xs_ = xsb[b % 2]; xT = XsT[b % 2]; aT = actT[b % 2]
                gather(w1.t[:].rearrange("p k n -> p (k n)"), W1b_d, BIDX.t[:, b, 0:1], [BIDX], [w1])
                gather(b1.t[:], eb1_d, BIDX.t[:, b, 0:1], [BIDX], [b1])
                gather(w2.t[:].rearrange("p k n -> p (k n)"), W2b_d, BIDX.t[:, b, 0:1], [BIDX], [w2])
                gather(b2.t[:], eb2_d, BIDX.t[:, b, 1:2], [BIDX], [b2])
                ts("dve", b1.t[:, 8:16], b1.t[:, 8:16], 1.0, None, ALU.add, None, [b1], [b1])
                if b == 0:
                    dma("sp", xs_.t[:], Xs_d[0:RB, :].rearrange("(s p) d -> p s d", p=128), [], [xs_])
                if b + 1 < NBLK:
                    xn_ = xsb[(b + 1) % 2]
                    dma("sp", xn_.t[:], Xs_d[(b + 1) * RB:(b + 2) * RB, :].rearrange("(s p) d -> p s d", p=128), [], [xn_])
                for s2 in range(NST):
                    pb = psbf(6 + s2 % 2)
                    for k in range(8):
                        tr(pb[:, k * 128:(k + 1) * 128], xs_.t[:, s2, k * 128:(k + 1) * 128], ident.t[:], [xs_, ident], [PS[6 + s2 % 2]])
                    S_.op("act", (lambda o, i_: lambda e: e.copy(o, i_))(xT.t[:, :, s2 * 128:(s2 + 1) * 128], pb[:, :].rearrange("p (k t) -> p k t", t=128)),
                          [PS[6 + s2 % 2].b], [xT.b])
                for Fi in range(8):
                    pg = PS[(en % 2) * 2]; pu = PS[(en % 2) * 2 + 1]
                    gc = gcl[en % 2]; sgt = sg[en % 2]; uc = ucl[en % 2]; gs_ = sgt; en += 1
                    for k in range(8):
                        mm(pg.t[:, 0:RB], w1.t[:, k, Fi * 128:(Fi + 1) * 128], xT.t[:, k, :], k == 0, k == 7, [w1, xT], [pg])
                    for k in range(8):
                        mm(pu.t[:, 0:RB], w1.t[:, k, 1024 + Fi * 128:1024 + (Fi + 1) * 128], xT.t[:, k, :], k == 0, k == 7, [w1, xT], [pu])
                    ts("dve", gc.t[:], pg.t[:, 0:RB], b1.t[:, Fi:Fi + 1], 7.0, ALU.add, ALU.min, [pg, b1], [gc])
                    act(sgt.t[:], gc.t[:], AF.Sigmoid, [gc], [sgt], scale=1.702)
                    ts("dve", uc.t[:], pu.t[:, 0:RB], b1.t[:, 8 + Fi:9 + Fi], 8.0, ALU.add, ALU.min, [pu, b1], [uc])
                    tt("dve", gs_.t[:], gc.t[:], sgt.t[:], ALU.mult, [gc, sgt], [gs_])
                    stt("dve", aT.t[:, Fi, :], uc.t[:], -6.0, gs_.t[:], ALU.max, ALU.mult, [gs_, uc], [aT.bs[Fi]])
                for s2 in range(NST):
                    yt_ = ysb[yn % 2]; yn += 1
                    for half in range(2):
                        py = PS[4 + half]
                        for k in range(8):
                            mm(py.t[:], aT.t[:, k, s2 * 128:(s2 + 1) * 128], w2.t[:, k, half * 512:(half + 1) * 512], k == 0, k == 7, [aT.bs[k], w2], [py])
                        tt("dve", yt_.t[:, half * 512:(half + 1) * 512], py.t[:], b2.t[:, half * 512:(half + 1) * 512], ALU.add, [py, b2], [yt_])
                    dma("sp", Ys_d[b * RB + s2 * 128:b * RB + (s2 + 1) * 128, :], yt_.t[:], [yt_], [B_Ys[b]])
            S_.barrier()

        with contextlib.ExitStack() as es:
            def sb(name, shape, dt=F32, nb=1):
                return T(es.enter_context(nc.sbuf_tensor("s_" + name, list(shape), dt)), nb)
            gat = [[sb("gat%d_%d" % (i, j), [128, D]) for j in range(4)] for i in range(2)]
            xq = [sb("cxq%d" % i, [128, D]) for i in range(2)]; acc_ = [sb("cacc%d" % i, [128, D]) for i in range(2)]
            for i in range(NT):
                g4 = gat[i % 2]; xq_ = xq[i % 2]; ac = acc_[i % 2]
                for j in range(4):
                    gather(g4[j].t[:], Ys_d, IDX.t[:, i, j:j + 1], [IDX.bs[i]] + B_Ys, [g4[j]])
                if i == 0:
                    dma("sp", xq_.t[:], y_d[0:128, :], [B_y[0]], [xq_])
                if i + 1 < NT:
                    xqn = xq[(i + 1) % 2]
                    dma("sp", xqn.t[:], y_d[(i + 1) * 128:(i + 2) * 128, :], [B_y[i + 1]], [xqn])
                ts("dve", ac.t[:], g4[0].t[:], W4.t[:, i, 0:1], None, ALU.mult, None, [g4[0], W4.bs[i]], [ac])
                for j in range(1, 4):
                    stt("dve", ac.t[:], g4[j].t[:], W4.t[:, i, j:j + 1], ac.t[:], ALU.mult, ALU.add, [g4[j], W4.bs[i], ac], [ac])
                tt("dve", ac.t[:], ac.t[:], g2row.t[:], ALU.mult, [ac, g2row], [ac])
                tt("dve", ac.t[:], ac.t[:], xq_.t[:], ALU.add, [ac, xq_], [ac])
                dma("sp", y_d[i * 128:(i + 1) * 128, :], ac.t[:], [ac], [B_y[i]])
            S_.barrier()

        sems = {k: ges.enter_context(nc.semaphore("s%d" % i)) for i, k in enumerate(S_.semkeys)}
        S_.emit(sems)
    return nc


_CONST_CACHE = {}


def make_constants(S):
    if S in _CONST_CACHE:
        return _CONST_CACHE[S]
    bf = ml_dtypes.bfloat16
    NT = S // 128; NKC = S // 128; BLK = min(512, S); NB = S // BLK
    N2 = 2 * S
    n = np.arange(S, dtype=np.int64)[:, None]; k = np.arange(S, dtype=np.int64)[None, :]
    ph = ((2 * k + 1) * n) % (2 * N2)
    ang = ph.astype(np.float64) * (np.pi / N2)
    C = np.cos(ang); Sm = np.sin(ang)
    del ang, ph
    def fwd(M):
        return np.ascontiguousarray(M.reshape(NT, 128, NKC, 128).transpose(2, 1, 0, 3)).astype(bf)
    def inv(M):
        return np.ascontiguousarray(M.reshape(NB, BLK, NKC, 128).transpose(0, 3, 2, 1)).astype(bf)
    consts = {"Cf": fwd(C), "Sf": fwd(Sm), "Ci": inv(C), "nSi": inv(-Sm)}
    del C, Sm
    consts["ident"] = np.eye(128, dtype=np.float32).astype(bf)
    GRID_W = 64; RF = 16
    rows = S // GRID_W
    row = np.repeat(np.arange(rows), GRID_W); col = np.tile(np.arange(GRID_W), rows)
    pos = np.stack([row, col], -1).astype(np.float32)
    freqs = (np.float32(10000.0) ** (-np.arange(RF, dtype=np.float32) / np.float32(RF))).astype(np.float32)
    ang = (pos[:, :, None] * freqs).astype(np.float32)
    cos = np.cos(ang).astype(np.float32); sin = np.sin(ang).astype(np.float32)
    ropec = np.stack([cos, cos], 2).reshape(S, 64)
    ropes = np.stack([sin, -sin], 2).reshape(S, 64)
    consts["ropec"] = np.ascontiguousarray(ropec, dtype=np.float32); consts["ropes"] = np.ascontiguousarray(ropes, dtype=np.float32)
    t = np.linspace(0.0, 1.0, S, dtype=np.float32)[:, None]
    w = (np.float32(2.0 * math.pi) * np.arange(S, dtype=np.float32)[:, None] / np.float32(S)).astype(np.float32)
    f = np.linspace(1e-4, 15, 16, dtype=np.float32)
    z = np.concatenate([t, np.cos(f * w), -np.sin(f * w)], -1).astype(np.float32)
    consts["zT"] = np.ascontiguousarray(z.T)
    consts["trow"] = np.ascontiguousarray(t.T)
    deltas = np.abs(np.linspace(math.log(1e-2) / 1.5, math.log(1e-2) / 0.3, 512, dtype=np.float32))
    consts["drow"] = deltas.reshape(1, 512).astype(np.float32)
    _CONST_CACHE[S] = consts
    return consts


def fm(v, nchunk):
    return np.ascontiguousarray(np.asarray(v, np.float32).reshape(nchunk, 128).T)


def make_in_maps(inp, S, CTXL, NE, B):
    consts = make_constants(S)
    f32 = lambda a: np.ascontiguousarray(np.asarray(a, np.float32))
    shared = dict(consts)
    shared["ada_w"] = f32(inp["ada_w"][0]); shared["ada_b_row"] = f32(inp["ada_b"][0]).reshape(1, -1); shared["ada_b_fm"] = fm(inp["ada_b"][0], 48)
    shared["n1g"] = fm(inp["norm1_g"][0], 8); shared["n2g"] = fm(inp["norm2_g"][0], 8)
    shared["w_in"] = f32(inp["w_in"][0]); shared["b_in_row"] = f32(inp["b_in"][0]).reshape(1, -1); shared["b_in_fm"] = fm(inp["b_in"][0], 40)
    shared["qg"] = f32(inp["q_norm_g"][0]).reshape(1, 64); shared["kg"] = f32(inp["k_norm_g"][0]).reshape(1, 64)
    shared["lam4"] = np.concatenate([f32(inp[n][0]) for n in ("lambda_q1", "lambda_k1", "lambda_q2", "lambda_k2")]).reshape(1, 256)
    shared["subg"] = f32(inp["subln_g"][0]).reshape(128, 1)
    cw = f32(inp["conv_w"][0])
    shared["convw"] = np.ascontiguousarray(cw.reshape(3, 12, 128).transpose(2, 1, 0)); shared["convb"] = fm(inp["conv_b"][0], 12)
    shared["fw1"] = f32(inp["filt_w1"][0]); shared["fw2"] = f32(inp["filt_w2"][0]); shared["fw3"] = f32(inp["filt_w3"][0]); shared["fw4"] = f32(inp["filt_w4"][0])
    shared["fvec"] = np.ascontiguousarray(np.stack([f32(inp["filt_b1"][0]), f32(inp["filt_b2"][0]), f32(inp["filt_b3"][0]), f32(inp["filt_freq"][0])], -1))
    shared["hbias"] = fm(inp["hyena_bias"][0], 4)
    shared["w_up_a"] = f32(inp["w_up_a"][0]); shared["w_up_b"] = f32(inp["w_up_b"][0]); shared["w_out"] = f32(inp["w_out"][0])
    shared["router_w"] = f32(inp["router_w"][0]); shared["router_b"] = f32(inp["router_b"][0]).reshape(1, 32)
    shared["ew1"] = f32(inp["exp_w1"][0]); shared["ew2"] = f32(inp["exp_w2"][0])
    shared["eb1"] = np.ascontiguousarray(f32(inp["exp_b1"][0]).reshape(NE, 16, 128).transpose(0, 2, 1).reshape(NE * 128, 16))
    shared["eb2"] = f32(inp["exp_b2"][0]).reshape(NE, D)
    shared["n2g_row"] = f32(inp["norm2_g"][0]).reshape(1, D)
    NBLK = (4 * S + NE * RB) // RB
    shared["ustrict"] = np.triu(np.ones((128, 128), np.float32), 1).astype(ml_dtypes.bfloat16)
    shared["kp"] = np.ascontiguousarray((np.arange(8)[None, :] * 128 + np.arange(128)[:, None]).astype(np.float32))
    shared["bstart"] = np.ascontiguousarray(np.broadcast_to((np.arange(NBLK) * RB).astype(np.float32)[None, :], (128, NBLK)))
    maps = []
    for b in range(B):
        m = dict(shared)
        m["x"] = f32(inp["x"][b]); m["ctx"] = f32(inp["ctx"][b])
        m["cc"] = np.ascontiguousarray(np.stack([f32(inp["c"][b]), f32(inp["c_ctx"])], -1).reshape(8, 128, 2).transpose(1, 0, 2))
        maps.append(m)
    return maps


_PROG_CACHE = {}


def kernel(**inputs):
    x = np.asarray(inputs["x"])
    B, S, _ = x.shape
    CTXL = np.asarray(inputs["ctx"]).shape[1]
    NE = np.asarray(inputs["exp_w1"]).shape[1]
    key = (S, CTXL, NE)
    if key not in _PROG_CACHE:
        _PROG_CACHE[key] = build_program(S, CTXL, NE)
    nc = _PROG_CACHE[key]
    maps = make_in_maps(inputs, S, CTXL, NE, B)
    res = run_bass_kernel_spmd(nc, maps, core_ids=list(range(B)))
    return np.stack([np.asarray(r["y"], dtype=np.float32) for r in res.results], 0)
```

```python
import contextlib
import math
import numpy as np
import ml_dtypes
import concourse.bass as bass
import concourse.mybir as mybir
from concourse.bass_utils import run_bass_kernel_spmd

F32 = mybir.dt.float32
BF16 = mybir.dt.bfloat16
I32 = mybir.dt.int32
AF = mybir.ActivationFunctionType
ALU = mybir.AluOpType
AX = mybir.AxisListType
D = 1024
EPS = 1e-6
RB = 256
PI = math.pi


class Buf:
    __slots__ = ("w", "r")

    def __init__(self):
        self.w = None
        self.r = {}


ENGS = ("pe", "act", "dve", "pool", "sp")
DMA_RING = {"sp": 8, "pool": 8}


class Sched:
    def __init__(self, nc, same_engine_sync=True):
        self.nc = nc
        self.prog = {e: [] for e in ENGS}
        self.nops = {e: 0 for e in ENGS}
        self.waited = {e: {} for e in ENGS}
        self.same = same_engine_sync
        self.dma_n = {q: 0 for q in DMA_RING}
        self.dma_val = {}
        self.semkeys = list(ENGS)
        for q, n in DMA_RING.items():
            for i in range(n):
                self.semkeys.append(("dma", q, i))
                self.dma_val[("dma", q, i)] = 0

    def _wait(self, eng, semkey, val):
        if self.waited[eng].get(semkey, -1) >= val:
            return
        self.waited[eng][semkey] = val
        if isinstance(semkey, str):
            self.prog[semkey][val][3] = True
        self.prog[eng].append(["w", semkey, val])

    def _deps(self, eng, reads, writes, is_dma):
        deps = {}

        def add(k, v, e):
            if (not is_dma) and e == eng and k == eng:
                if eng == "pe" or not self.same:
                    return
            if deps.get(k, -1) < v:
                deps[k] = v
        for b in reads:
            if b.w is not None:
                add(*b.w)
        for b in writes:
            if b.w is not None:
                add(*b.w)
            for k, (v, e) in b.r.items():
                add(k, v, e)
        for k, v in deps.items():
            self._wait(eng, k, v)

    def _update(self, tok, reads, writes):
        k, v, e = tok
        for b in reads:
            b.r[k] = (v, e)
        for b in writes:
            b.w = tok
            b.r = {}

    def op(self, eng, fn, reads=(), writes=()):
        self._deps(eng, reads, writes, False)
        pos = len(self.prog[eng])
        self.prog[eng].append(["op", fn, eng, False])
        self.nops[eng] += 1
        self._update((eng, pos, eng), reads, writes)

    def dma(self, q, fn, reads=(), writes=()):
        n = self.dma_n[q]
        self.dma_n[q] += 1
        key = ("dma", q, n % DMA_RING[q])
        if self.dma_val[key] > 0:
            self._wait(q, key, self.dma_val[key])
        self._deps(q, reads, writes, True)
        self.dma_val[key] += 16
        tok = (key, self.dma_val[key], q)
        self.prog[q].append(["dma", fn, key, 16])
        self._update(tok, reads, writes)

    def _last_op(self, eng):
        for i in range(len(self.prog[eng]) - 1, -1, -1):
            if self.prog[eng][i][0] == "op":
                return i
        return None

    def barrier(self):
        for e in ENGS:
            for e2 in ENGS:
                if e2 != e:
                    lp = self._last_op(e2)
                    if lp is not None:
                        self._wait(e, e2, lp)
            for k, v in self.dma_val.items():
                if v > 0:
                    self._wait(e, k, v)

    def emit(self, sems):
        nc = self.nc
        value_at = {}
        for e in ENGS:
            c = 0
            va = {}
            for pos, item in enumerate(self.prog[e]):
                if item[0] == "op" and item[3]:
                    c += 1
                    va[pos] = c
            value_at[e] = va
        with nc.Block() as block:
            def run(engname):
                def body(eng):
                    for item in self.prog[engname]:
                        if item[0] == "w":
                            k, v = item[1], item[2]
                            eng.wait_ge(sems[k], value_at[k][v] if isinstance(k, str) else v)
                        elif item[0] == "dma":
                            item[1](eng).then_inc(sems[item[2]], 16)
                        elif item[3]:
                            item[1](eng).then_inc(sems[item[2]], 1)
                        else:
                            item[1](eng)
                return body
            block.tensor(run("pe"))
            block.scalar(run("act"))
            block.vector(run("dve"))
            block.gpsimd(run("pool"))
            block.sync(run("sp"))


class T:
    def __init__(self, t, nb=1):
        self.t = t
        self.b = Buf()
        self.bs = [Buf() for _ in range(nb)] if nb > 1 else [self.b]


def build_program(S, CTXL, NE, debug=False):
    NT = S // 128
    NC = CTXL // 128
    NKT = NT + NC
    TK = S + CTXL
    BLK = min(512, S)
    NB = S // BLK
    TPB = BLK // 128
    NKC = S // 128
    KG = min(8, NKC)
    NKG = NKC // KG
    QS = min(1024, S)
    NQ = S // QS
    NQT = QS // 128
    NQB = QS // BLK
    N2 = 2 * S
    NR = 4 * S + NE * RB
    NBLK = NR // RB
    NZ = NR // 256
    NST = RB // 128

    nc = bass.Bass("TRN2", target_bir_lowering=False)
    S_ = Sched(nc)

    def din(name, shape, dt=F32):
        return nc.dram_tensor(name, list(shape), dt, kind="ExternalInput").ap()

    def dscr(name, shape, dt=BF16):
        return nc.dram_tensor(name, list(shape), dt, kind="Internal").ap()

    x_d = din("x", [S, D]); ctx_d = din("ctx", [CTXL, D]); cc_d = din("cc", [128, 8, 2])
    adaw_d = din("ada_w", [D, 6 * D]); adab_row_d = din("ada_b_row", [1, 6 * D]); adab_fm_d = din("ada_b_fm", [128, 48])
    n1g_d = din("n1g", [128, 8]); n2g_d = din("n2g", [128, 8])
    win_d = din("w_in", [D, 5120]); bin_row_d = din("b_in_row", [1, 5120]); bin_fm_d = din("b_in_fm", [128, 40])
    qg_d = din("qg", [1, 64]); kg_d = din("kg", [1, 64]); lam4_d = din("lam4", [1, 256]); subg_d = din("subg", [128, 1])
    convw_d = din("convw", [128, 12, 3]); convb_d = din("convb", [128, 12])
    fw1_d = din("fw1", [33, 64]); fw2_d = din("fw2", [64, 64]); fw3_d = din("fw3", [64, 64]); fw4_d = din("fw4", [64, 1024])
    fvec_d = din("fvec", [64, 4])
    hb_d = din("hbias", [128, 4])
    wua_d = din("w_up_a", [512, D]); wub_d = din("w_up_b", [512, D]); wout_d = din("w_out", [D, D])
    rw_d = din("router_w", [D, 32]); rb_d = din("router_b", [1, 32])
    ew1_d = din("ew1", [NE, D, 2048]); eb1_d = din("eb1", [NE * 128, 16]); ew2_d = din("ew2", [NE, D, D]); eb2_d = din("eb2", [NE, D])
    ustr_d = din("ustrict", [128, 128], BF16); kp_d = din("kp", [128, 8]); bstart_d = din("bstart", [128, NBLK]); n2grow_d = din("n2g_row", [1, D])
    ident_d = din("ident", [128, 128], BF16)
    ropec_d = din("ropec", [S, 64]); ropes_d = din("ropes", [S, 64])
    zT_d = din("zT", [33, S]); trow_d = din("trow", [1, S]); drow_d = din("drow", [1, 512])
    Cf_d = din("Cf", [NKC, 128, NT, 128], BF16); Sf_d = din("Sf", [NKC, 128, NT, 128], BF16)
    Ci_d = din("Ci", [NB, 128, NKC, BLK], BF16); Si_d = din("nSi", [NB, 128, NKC, BLK], BF16)
    y_d = nc.dram_tensor("y", [S, D], F32, kind="ExternalOutput").ap()

    hTc_d = dscr("hTc_d", [128, 8, CTXL]); hT_d = dscr("hT_d", [128, 8, S])
    oaT_d = dscr("oaT_d", [128, 4, S]); obT_d = dscr("obT_d", [128, 4, S])
    x0c_d = dscr("x0c_d", [128, 4, S]); uT_d = dscr("uT_d", [128, 4, S])
    Y_d = dscr("Y_d", [128, 2, NKC, 512])
    H2tm_d = dscr("H2tm_d", [S, D]); Xs_d = dscr("Xs_d", [NR, D]); Ys_d = dscr("Ys_d", [NR, D], F32)
    W1b_d = dscr("W1b_d", [NE * 128, 8 * 2048]); W2b_d = dscr("W2b_d", [NE * 128, 8 * D])
    dbg = {}
    if debug:
        for n, shp in [("dbg_oaT", [128, 4, S]), ("dbg_obT", [128, 4, S]), ("dbg_hT", [128, 8, S]), ("dbg_G", [128, NT, 32])]:
            dbg[n] = nc.dram_tensor(n, shp, F32, kind="ExternalOutput").ap()
    B_hTc = Buf(); B_hT = [Buf() for _ in range(NB)]
    B_oaT = [Buf() for _ in range(NB)]; B_obT = [Buf() for _ in range(NB)]
    B_x0c = [Buf() for _ in range(4)]; B_uT = [Buf() for _ in range(4)]
    B_Y = [Buf() for _ in range(NKC)]; B_h2tm = [Buf() for _ in range(NT)]
    B_Xs = [Buf() for _ in range(NZ)]; B_Ys = [Buf() for _ in range(NBLK)]
    B_y = [Buf() for _ in range(NT)]

    w_in_v = win_d.rearrange("(k p) n -> p k n", p=128)

    def bl(xs):
        return [x.b if isinstance(x, T) else x for x in xs]

    def mm(out, lhsT, rhs, start, stop, r, w, tp=None):
        if tp is None:
            S_.op("pe", lambda e: e.matmul(out, lhsT, rhs, start=start, stop=stop), bl(r), bl(w))
        else:
            S_.op("pe", lambda e: e.matmul(out, lhsT, rhs, start=start, stop=stop, tile_position=tp), bl(r), bl(w))

    def tr(out, in_, ident, r, w):
        S_.op("pe", lambda e: e.transpose(out, in_, ident), bl(r), bl(w))

    def act(out, in_, func, r, w, **kw):
        S_.op("act", lambda e: e.activation(out=out, in_=in_, func=func, **kw), bl(r), bl(w))

    def tt(eng, out, in0, in1, op, r, w):
        S_.op(eng, lambda e: e.tensor_tensor(out, in0, in1, op), bl(r), bl(w))

    def ts(eng, out, in0, s1, s2, op0, op1, r, w):
        if op1 is None:
            S_.op(eng, lambda e: e.tensor_scalar(out, in0, s1, None, op0), bl(r), bl(w))
        else:
            S_.op(eng, lambda e: e.tensor_scalar(out, in0, s1, s2, op0, op1), bl(r), bl(w))

    def stt(eng, out, in0, sc, in1, op0, op1, r, w):
        S_.op(eng, lambda e: e.scalar_tensor_tensor(out, in0, sc, in1, op0, op1), bl(r), bl(w))

    def cp(eng, out, in_, r, w):
        S_.op(eng, lambda e: e.tensor_copy(out, in_), bl(r), bl(w))

    def recip(out, in_, r, w):
        S_.op("dve", lambda e: e.reciprocal(out, in_), bl(r), bl(w))

    def mset(eng, ap, val, w):
        S_.op(eng, lambda e: e.memset(ap, val), [], bl(w))

    def dma(q, out, in_, r, w):
        S_.dma(q, lambda e: e.dma_start(out=out, in_=in_), bl(r), bl(w))

    def vmax8(out, in_, r, w):
        S_.op("dve", lambda e: e.max(out, in_), bl(r), bl(w))

    def red(out, in_, r, w):
        S_.op("dve", lambda e: e.tensor_reduce(out, in_, AX.X, ALU.add), bl(r), bl(w))

    def gather(out, in_, idx_ap, r, w):
        S_.dma("pool", lambda e: e.indirect_dma_start(out=out, out_offset=None, in_=in_,
                                                      in_offset=bass.IndirectOffsetOnAxis(ap=idx_ap, axis=0), oob_is_err=False), bl(r), bl(w))

    def scatter(out, in_, idx_ap, r, w):
        S_.dma("pool", lambda e: e.indirect_dma_start(out=out, out_offset=bass.IndirectOffsetOnAxis(ap=idx_ap, axis=0), in_=in_,
                                                      in_offset=None, oob_is_err=False), bl(r), bl(w))

    with contextlib.ExitStack() as ges:
        def gsb(name, shape, dt=F32, nb=1):
            return T(ges.enter_context(nc.sbuf_tensor("s_" + name, list(shape), dt)), nb)
        psall = ges.enter_context(nc.psum_tensor("psall", [128, 4096], F32))
        PS = [T(psall[:, i * 512:(i + 1) * 512]) for i in range(8)]

        def psbf(i):
            return PS[i].t[:].bitcast(BF16)

        ident = gsb("ident", [128, 128], BF16); onesb = gsb("onesb", [128, 128], BF16); onesf = gsb("onesf", [128, 128])
        epst = gsb("epst", [128, 1])
        A1 = gsb("A1", [128, 8]); B1 = gsb("B1", [128, 8]); A1c = gsb("A1c", [128, 8]); B1c = gsb("B1c", [128, 8])
        A2 = gsb("A2", [128, 8]); B2 = gsb("B2", [128, 8])
        g1row = gsb("g1row", [128, D]); g2row = gsb("g2row", [128, D])
        neglam = gsb("neglam", [128, 1]); gsub = gsb("gsub", [128, 1])
        G = gsb("G", [128, NT, 32], F32, nb=NT)
        A2row = gsb("A2row", [128, D]); B2row = gsb("B2row", [128, D])
        LG = gsb("LG", [128, NT, 32], F32, nb=NT); MX = gsb("MX", [128, NT, 8], F32, nb=NT); POS = gsb("POS", [128, NT, 32], F32, nb=NT)
        IDX = gsb("IDX", [128, NT, 4], I32, nb=NT); W4 = gsb("W4", [128, NT, 4], F32, nb=NT)
        BIDX = gsb("BIDX", [128, NBLK, 2], I32)
        ustr = gsb("ustr", [128, 128], BF16); cumf = gsb("cumf", [128, 32]); cumb = gsb("cumb", [128, 32], BF16)

        dma("sp", ident.t[:], ident_d, [], [ident]); dma("sp", ustr.t[:], ustr_d, [], [ustr])
        mset("pool", cumf.t[:], 0.0, [cumf]); mset("pool", cumb.t[:], 0.0, [cumb])
        mset("pool", onesb.t[:], 1.0, [onesb]); mset("pool", onesf.t[:], 1.0, [onesf]); mset("pool", epst.t[:], EPS, [epst])

        with contextlib.ExitStack() as es:
            def sb(name, shape, dt=F32, nb=1):
                return T(es.enter_context(nc.sbuf_tensor("s_" + name, list(shape), dt)), nb)
            cc = sb("cc", [128, 8, 2]); scv = sb("scv", [128, 8, 2]); screp = sb("screp", [128, 8, 128])
            aw = [sb("aw%d" % i, [128, 8, 512]) for i in range(2)]
            adab_fm = sb("adab_fm", [128, 48]); adab_row = sb("adab_row", [1, 6 * D])
            modF = sb("modF", [128, 48, 2]); n1g = sb("n1g", [128, 8]); n2g = sb("n2g", [128, 8])
            lam4 = sb("lam4", [128, 256]); lt1 = sb("lt1", [128, 64]); lt2 = sb("lt2", [128, 64])
            ls1 = sb("ls1", [128, 1]); ls2 = sb("ls2", [128, 1]); subg = sb("subg", [128, 1])
            dma("sp", cc.t[:], cc_d, [], [cc]); dma("sp", adab_fm.t[:], adab_fm_d, [], [adab_fm])
            dma("sp", adab_row.t[:], adab_row_d, [], [adab_row])
            dma("sp", n1g.t[:], n1g_d, [], [n1g]); dma("sp", n2g.t[:], n2g_d, [], [n2g])
            dma("sp", lam4.t[:], lam4_d.partition_broadcast(128), [], [lam4]); dma("sp", subg.t[:], subg_d, [], [subg])
            act(scv.t[:], cc.t[:], AF.Silu, [cc], [scv])
            for k in range(8):
                cp("dve", screp.t[:, k, :], scv.t[:, k, 0:1].broadcast_to([128, 128]), [scv], [screp])
            adaw_v = adaw_d.rearrange("(k p) n -> p k n", p=128)
            pmod = PS[0]
            for g in range(12):
                a = aw[g % 2]
                dma("sp", a.t[:], adaw_v[:, :, g * 512:(g + 1) * 512], [], [a])
                for c in range(4):
                    ch = g * 4 + c
                    for k in range(8):
                        mm(pmod.t[:, 2 * ch:2 * ch + 2], a.t[:, k, c * 128:(c + 1) * 128], scv.t[:, k, :], k == 0, k == 7, [a, scv], [pmod])
                if g in (4, 5, 6, 7, 8, 9, 10, 11):
                    pr = PS[1 + (g % 2)]
                    for k in range(8):
                        mm(pr.t[:], screp.t[:, k, :], a.t[:, k, :], k == 0, False, [a, screp], [pr])
                    mm(pr.t[:], onesf.t[0:1, :], adab_row.t[0:1, g * 512:(g + 1) * 512], False, True, [onesf, adab_row], [pr])
                    dst = {2: g1row, 3: B2row, 4: A2row, 5: g2row}[g // 2]
                    half = g % 2
                    cp("dve", dst.t[:, half * 512:(half + 1) * 512], pr.t[:], [pr], [dst])
            tt("dve", modF.t[:], pmod.t[:, 0:96].rearrange("p (c j) -> p c j", j=2),
               adab_fm.t[:].unsqueeze(2).broadcast_to([128, 48, 2]), ALU.add, [pmod, adab_fm], [modF])
            stt("dve", A1.t[:], modF.t[:, 8:16, 0], 1.0, n1g.t[:], ALU.add, ALU.mult, [modF, n1g], [A1])
            stt("dve", A1c.t[:], modF.t[:, 8:16, 1], 1.0, n1g.t[:], ALU.add, ALU.mult, [modF, n1g], [A1c])
            stt("dve", A2.t[:], modF.t[:, 32:40, 0], 1.0, n2g.t[:], ALU.add, ALU.mult, [modF, n2g], [A2])
            cp("dve", B1.t[:], modF.t[:, 0:8, 0], [modF], [B1]); cp("dve", B1c.t[:], modF.t[:, 0:8, 1], [modF], [B1c])
            cp("dve", B2.t[:], modF.t[:, 24:32, 0], [modF], [B2])
            n2grow = sb("n2grow", [128, D])
            dma("sp", n2grow.t[:], n2grow_d.partition_broadcast(128), [], [n2grow])
            stt("dve", A2row.t[:], A2row.t[:], 1.0, n2grow.t[:], ALU.add, ALU.mult, [A2row, n2grow], [A2row])
            tt("dve", lt1.t[:], lam4.t[:, 0:64], lam4.t[:, 64:128], ALU.mult, [lam4], [lt1])
            tt("dve", lt2.t[:], lam4.t[:, 128:192], lam4.t[:, 192:256], ALU.mult, [lam4], [lt2])
            S_.op("dve", lambda e: e.tensor_reduce(ls1.t[:], lt1.t[:], AX.X, ALU.add), [lt1.b], [ls1.b])
            S_.op("dve", lambda e: e.tensor_reduce(ls2.t[:], lt2.t[:], AX.X, ALU.add), [lt2.b], [ls2.b])
            act(ls1.t[:], ls1.t[:], AF.Exp, [ls1], [ls1]); act(ls2.t[:], ls2.t[:], AF.Exp, [ls2], [ls2])
            tt("dve", neglam.t[:], ls2.t[:], ls1.t[:], ALU.subtract, [ls1, ls2], [neglam])
            ts("dve", neglam.t[:], neglam.t[:], -0.2, None, ALU.add, None, [neglam], [neglam])
            ts("dve", gsub.t[:], subg.t[:], 0.8, None, ALU.mult, None, [subg], [gsub])
            S_.barrier()

        def norm_transpose(es, tag):
            def sb(name, shape, dt=F32, nb=1):
                return T(es.enter_context(nc.sbuf_tensor("s_" + tag + name, list(shape), dt)), nb)
            st = dict(junk=sb("junk", [128, D], BF16), ss=[sb("ss%d" % i, [128, 1]) for i in range(2)],
                      xs=[sb("xs%d" % i, [128, D], BF16) for i in range(2)], n=0)
            return st

        def do_norm_transpose(st, xin, A, Bv, psi, dst_fn, dst_bufs, defer=False):
            i = st["n"]; st["n"] += 1
            ss = st["ss"][i % 2]; xs = st["xs"][i % 2]
            mset("pool", ss.t[:], 0.0, [ss])
            act(st["junk"].t[:], xin.t[:], AF.Square, [xin], [st["junk"], ss], accum_out=ss.t[:])
            act(ss.t[:], ss.t[:], AF.Sqrt, [ss, epst], [ss], scale=1.0 / D, bias=epst.t[:])
            recip(ss.t[:], ss.t[:], [ss], [ss])
            ts("dve", xs.t[:], xin.t[:], ss.t[:, 0:1], None, ALU.mult, None, [xin, ss], [xs])
            pb = psbf(psi)
            for k in range(8):
                tr(pb[:, k * 128:(k + 1) * 128], xs.t[:, k * 128:(k + 1) * 128], ident.t[:], [xs, ident], [PS[psi]])
            def evac():
                for k in range(8):
                    act(dst_fn(k), pb[:, k * 128:(k + 1) * 128], AF.Identity, [PS[psi], A, Bv], dst_bufs,
                        scale=A.t[:, k:k + 1], bias=Bv.t[:, k:k + 1])
            if defer:
                return ss, evac
            evac()
            return ss

        with contextlib.ExitStack() as es:
            def sb(name, shape, dt=F32, nb=1):
                return T(es.enter_context(nc.sbuf_tensor("s_" + name, list(shape), dt)), nb)
            st = norm_transpose(es, "p1")
            xin = [sb("p1xin%d" % i, [128, D]) for i in range(3)]
            hblk = [sb("p1hb%d" % i, [128, 8, BLK], BF16) for i in range(2)]
            n = 0
            pend_ev = None
            hb = hblk[0]
            for i in range(NC):
                xi = xin[n % 3]
                dma("sp", xi.t[:], ctx_d[i * 128:(i + 1) * 128, :], [], [xi])
                _, ev_ = do_norm_transpose(st, xi, A1c, B1c, n % 2, lambda k, i=i, hb=hb: hb.t[:, k, i * 128:(i + 1) * 128], [hb], defer=True)
                if pend_ev is not None:
                    pend_ev()
                pend_ev = ev_
                n += 1
            pend_ev(); pend_ev = None
            dma("sp", hTc_d, hblk[0].t[:, :, 0:CTXL], [hblk[0]], [B_hTc])
            for b in range(NB):
                hb = hblk[(b + 1) % 2]
                for s in range(TPB):
                    i = b * TPB + s
                    xi = xin[n % 3]
                    dma("sp", xi.t[:], x_d[i * 128:(i + 1) * 128, :], [], [xi])
                    _, ev_ = do_norm_transpose(st, xi, A1, B1, n % 2, lambda k, s=s, hb=hb: hb.t[:, k, s * 128:(s + 1) * 128], [hb], defer=True)
                    if pend_ev is not None:
                        pend_ev()
                    pend_ev = ev_
                    n += 1
                pend_ev(); pend_ev = None
                dma("sp", hT_d[:, :, b * BLK:(b + 1) * BLK], hb.t[:], [hb], [B_hT[b]])
            S_.barrier()

        with contextlib.ExitStack() as es2:
            def sb2(name, shape, dt=F32, nb=1):
                return T(es2.enter_context(nc.sbuf_tensor("s_" + name, list(shape), dt)), nb)
            QT = sb2("QT", [128, 4, S], BF16, nb=NT); KT = sb2("KT", [128, 4, TK], BF16, nb=NKT); V = sb2("V", [128, NKT, 512], BF16, nb=NKT)
            with contextlib.ExitStack() as es:
                def sb(name, shape, dt=F32, nb=1):
                    return T(es.enter_context(nc.sbuf_tensor("s_" + name, list(shape), dt)), nb)
                wqkv = sb("wqkv", [128, 8, 1536], BF16); brow = sb("brow", [1, 1536], BF16)
                gq = sb("gq", [128, 64]); gk = sb("gk", [128, 64])
                rcs = [sb("rc%d" % i, [128, 64]) for i in range(3)]; rss = [sb("rs%d" % i, [128, 64]) for i in range(3)]
                gtab = [[sb("gtab%d_%d" % (i, j), [128, 64]) for j in range(4)] for i in range(3)]
                hblk = [sb("p2hb%d" % i, [128, 8, BLK], BF16) for i in range(2)]
                sqt = [sb("sqt%d" % i, [128, 512]) for i in range(2)]
                ssq = [sb("ssq%d" % i, [128, 8]) for i in range(2)]
                qn = [sb("qn%d" % i, [128, 512]) for i in range(2)]
                qg2 = [sb("qg2%d" % i, [128, 512]) for i in range(2)]
                ru = [sb("ru%d" % i, [128, 512]) for i in range(2)]
                rw_ = [sb("rw%d" % i, [128, 512]) for i in range(2)]
                qr = [sb("qr%d" % i, [128, 512], BF16) for i in range(4)]
                for c in range(3):
                    dma("pool", wqkv.t[:, :, c * 512:(c + 1) * 512], w_in_v[:, :, c * 512:(c + 1) * 512], [], [wqkv])
                dma("pool", brow.t[:], bin_row_d[0:1, 0:1536], [], [brow])
                dma("sp", gq.t[:], qg_d.partition_broadcast(128), [], [gq]); dma("sp", gk.t[:], kg_d.partition_broadcast(128), [], [gk])
                cnt = {"n": 0}

                def qknorm(ps, gt, xt, dstT, dbuf, dcol, psT, pcol, rc=None, rs_=None):
                    i = cnt["n"]; cnt["n"] += 1
                    sq = sqt[i % 2]; sm = ssq[i % 2]; q1 = qn[i % 2]; q2 = qg2[i % 2]; u_ = ru[i % 2]; w_ = rw_[i % 2]; o_ = qr[i % 4]
                    def part_a():
                        act(sq.t[:], ps.t[:], AF.Square, [ps], [sq])
                        yield
                        S_.op("dve", lambda e: e.tensor_reduce(sm.t[:], sq.t[:].rearrange("p (g d) -> p g d", d=64), AX.X, ALU.add), [sq.b], [sm.b])
                        yield
                        act(sm.t[:], sm.t[:], AF.Sqrt, [sm, epst], [sm], scale=1.0 / 64, bias=epst.t[:])
                        yield
                        recip(sm.t[:], sm.t[:], [sm], [sm])
                        yield
                        tt("dve", q1.t[:].rearrange("p (g d) -> p g d", d=64), ps.t[:].rearrange("p (g d) -> p g d", d=64),
                           sm.t[:].unsqueeze(2).broadcast_to([128, 8, 64]), ALU.mult, [ps, sm], [q1])
                        yield
                        if xt is None:
                            tt("pool", o_.t[:].rearrange("p (g d) -> p g d", d=64), q1.t[:].rearrange("p (g d) -> p g d", d=64),
                               gt.t[:].unsqueeze(1).broadcast_to([128, 8, 64]), ALU.mult, [q1, gt], [o_])
                            yield
                        else:
                            tt("pool", u_.t[:].rearrange("p (g d) -> p g d", d=64), q1.t[:].rearrange("p (g d) -> p g d", d=64),
                               rc.t[:].unsqueeze(1).broadcast_to([128, 8, 64]), ALU.mult, [q1, rc], [u_])
                            yield
                            tt("dve", w_.t[:].rearrange("p (g d) -> p g d", d=64), q1.t[:].rearrange("p (g d) -> p g d", d=64),
                               rs_.t[:].unsqueeze(1).broadcast_to([128, 8, 64]), ALU.mult, [q1, rs_], [w_])
                            yield
                            u4 = u_.t[:].rearrange("p (a h f) -> p a h f", h=2, f=16)
                            w4 = w_.t[:].rearrange("p (a h f) -> p a h f", h=2, f=16)
                            o4 = o_.t[:].rearrange("p (a h f) -> p a h f", h=2, f=16)
                            tt("dve", o4[:, :, 0, :], u4[:, :, 0, :], w4[:, :, 1, :], ALU.add, [u_, w_], [o_])
                            yield
                            tt("dve", o4[:, :, 1, :], u4[:, :, 1, :], w4[:, :, 0, :], ALU.add, [u_, w_], [o_])
                            yield
                    def part_b():
                        pb = psbf(psT)
                        for h in range(4):
                            tr(pb[:, pcol + h * 128: pcol + (h + 1) * 128], o_.t[:, h * 128:(h + 1) * 128], ident.t[:], [o_, ident], [PS[psT]])
                        S_.op("act", (lambda o_ap, i_ap: lambda e: e.copy(o_ap, i_ap))(dstT.t[:, :, dcol:dcol + 128], pb[:, pcol:pcol + 512].rearrange("p (h t) -> p h t", t=128)),
                              bl([PS[psT]]), bl([dbuf]))
                    return part_a(), part_b

                tno = 0
                pend_b = []
                blocks = [("c", 0, NC)] + [("x", b, TPB) for b in range(NB)]
                for bi, (kind, b, ntl) in enumerate(blocks):
                    hb = hblk[bi % 2]
                    if kind == "c":
                        dma("sp", hb.t[:, :, 0:CTXL], hTc_d, [B_hTc], [hb])
                    else:
                        dma("sp", hb.t[:], hT_d[:, :, b * BLK:(b + 1) * BLK], [B_hT[b]], [hb])
                    for s in range(ntl):
                        kt = s if kind == "c" else NC + b * TPB + s
                        xt = None if kind == "c" else b * TPB + s
                        st3 = (tno % 2) * 3
                        psT = 6 + (tno % 2)
                        tno += 1
                        lh = lambda k: hb.t[:, k, s * 128:(s + 1) * 128]
                        for c in range(3):
                            if c == 0 and kind == "c":
                                continue
                            pp = PS[st3 + c]
                            for k in range(8):
                                mm(pp.t[:], lh(k), wqkv.t[:, k, c * 512:(c + 1) * 512], k == 0, False, [hb, wqkv], [pp])
                            mm(pp.t[:], onesb.t[0:1, :], brow.t[0:1, c * 512:(c + 1) * 512], False, True, [onesb, brow], [pp])
                        act(V.t[:, kt, :], PS[st3 + 2].t[:], AF.Copy, [PS[st3 + 2]], [V.bs[kt]])
                        rc = rs_ = None
                        if kind == "x":
                            rc = rcs[xt % 3]; rs_ = rss[xt % 3]
                            dma("sp", rc.t[:], ropec_d[xt * 128:(xt + 1) * 128, :], [], [rc])
                            dma("sp", rs_.t[:], ropes_d[xt * 128:(xt + 1) * 128, :], [], [rs_])
                            gt4 = gtab[xt % 3]
                            tt("pool", gt4[0].t[:], rc.t[:], gq.t[:], ALU.mult, [rc, gq], [gt4[0]]); tt("pool", gt4[1].t[:], rs_.t[:], gq.t[:], ALU.mult, [rs_, gq], [gt4[1]])
                            tt("pool", gt4[2].t[:], rc.t[:], gk.t[:], ALU.mult, [rc, gk], [gt4[2]]); tt("pool", gt4[3].t[:], rs_.t[:], gk.t[:], ALU.mult, [rs_, gk], [gt4[3]])
                            pairs = [qknorm(PS[st3 + 0], gq, xt, QT, QT.bs[xt], xt * 128, psT, 0, gt4[0], gt4[1]),
                                     qknorm(PS[st3 + 1], gk, xt, KT, KT.bs[kt], kt * 128, psT, 512, gt4[2], gt4[3])]
                        else:
                            pairs = [qknorm(PS[st3 + 1], gk, xt, KT, KT.bs[kt], kt * 128, psT, 512, None, None)]
                        gens = [p_[0] for p_ in pairs]
                        while gens:
                            gens = [g_ for g_ in gens if next(g_, "done") != "done"]
                        newb = [p_[1] for p_ in pairs]
                        for fb_ in pend_b:
                            fb_()
                        pend_b = newb
                for fb_ in pend_b:
                    fb_()
                S_.barrier()

            with contextlib.ExitStack() as es:
                def sb(name, shape, dt=F32, nb=1):
                    return T(es.enter_context(nc.sbuf_tensor("s_" + name, list(shape), dt)), nb)
                pt2 = [sb("pt2_%d" % i, [128, 2, 512], BF16) for i in range(3)]
                r0 = sb("r0", [128, BLK]); r1 = sb("r1", [128, BLK]); t0 = sb("t0", [128, BLK]); t1 = sb("t1", [128, BLK])
                dd = sb("dd", [128, BLK]); dsq = sb("dsq", [128, BLK]); rsd = sb("rsd", [128, BLK])
                oat = [sb("oat%d" % i, [128, BLK], BF16) for i in range(2)]
                o_acc = [PS[0], PS[1]]; s_acc = [PS[2], PS[3]]
                scp = [[PS[4], PS[5]], [PS[6], PS[7]]]
                zt = sb("zt", [128, 2 * D], BF16)
                mset("pool", zt.t[:], 0.0, [zt])
                Xs_z = Xs_d.rearrange("(c p r) d -> c p (r d)", p=128, r=2)
                for c_ in range(NZ):
                    dma("pool", Xs_z[c_], zt.t[:], [zt], [B_Xs[c_]])
                for e_ in range(NE):
                    w1src = ew1_d[e_].rearrange("(k p) n -> p k n", p=128)
                    w1dst = W1b_d[e_ * 128:(e_ + 1) * 128, :].rearrange("p (k n) -> p k n", k=8)
                    for k0 in (0, 4):
                        dma("pool", w1dst[:, k0:k0 + 4, :], w1src[:, k0:k0 + 4, :], [], [])
                    w2src = ew2_d[e_].rearrange("(k p) n -> p k n", p=128)
                    w2dst = W2b_d[e_ * 128:(e_ + 1) * 128, :].rearrange("p (k n) -> p k n", k=8)
                    dma("pool", w2dst, w2src, [], [])
                its = [(h, qb, kt) for h in range(4) for qb in range(NB) for kt in range(NKT)]

                def scores(n_):
                    h, qb, kt = its[n_]
                    sp_ = scp[n_ % 2]
                    for m in range(2):
                        mm(sp_[m].t[:, 0:BLK], KT.t[m * 64:(m + 1) * 64, h, kt * 128:(kt + 1) * 128],
                           QT.t[m * 64:(m + 1) * 64, h, qb * BLK:(qb + 1) * BLK], True, True,
                           [KT.bs[kt]] + [QT.bs[qb * TPB + j] for j in range(TPB)], [sp_[m]])
                o0s = sb("o0s", [128, BLK]); o1s = sb("o1s", [128, BLK]); ssb = sb("ssb", [128, BLK]); w32 = sb("w32", [128, 128])
                mset("dve", w32.t[:], 1.0 / 32, [w32])
                sbank = PS[2]; fbank = PS[3]

                def finalize_gen(h, qb):
                    recip(ssb.t[0:64, :], ssb.t[0:64, :], [ssb], [ssb])
                    mm(fbank.t[:, 0:BLK], w32.t[0:32, :], ssb.t[0:32, :], True, True, [w32, ssb], [fbank])
                    yield
                    tt("dve", t0.t[:], o0s.t[:], fbank.t[:, 0:BLK], ALU.mult, [o0s, fbank], [t0])
                    mm(fbank.t[:, 0:BLK], w32.t[32:64, :], ssb.t[32:64, :], True, True, [w32, ssb], [fbank])
                    yield
                    tt("dve", t1.t[:], o1s.t[:], fbank.t[:, 0:BLK], ALU.mult, [o1s, fbank], [t1])
                    stt("dve", dd.t[:], t1.t[:], neglam.t[:, 0:1], t0.t[:], ALU.mult, ALU.add, [t0, t1, neglam], [dd])
                    tt("dve", dsq.t[:], dd.t[:], dd.t[:], ALU.mult, [dd], [dsq])
                    yield
                    mm(fbank.t[:, 0:BLK], onesf.t[:], dsq.t[:], True, True, [onesf, dsq], [fbank])
                    yield
                    act(rsd.t[:], fbank.t[:, 0:BLK], AF.Sqrt, [fbank, epst], [rsd], scale=1.0 / 128, bias=epst.t[:])
                    recip(rsd.t[:], rsd.t[:], [rsd], [rsd])
                    oo = oat[(h * NB + qb) % 2]
                    stt("dve", oo.t[:], dd.t[:], gsub.t[:, 0:1], rsd.t[:], ALU.mult, ALU.mult, [dd, gsub, rsd], [oo])
                    dma("sp", oaT_d[:, h, qb * BLK:(qb + 1) * BLK], oo.t[:], [oo], [B_oaT[qb]])

                pending = []
                scores(0)
                for it in range(len(its)):
                    h, qb, kt = its[it]
                    if it + 1 < len(its):
                        scores(it + 1)
                    sp_ = scp[it % 2]
                    p2 = pt2[it % 3]
                    bank0 = 4 + 2 * (it % 2)
                    act(p2.t[:, :, 0:BLK], psall[:, bank0 * 512:(bank0 + 2) * 512].rearrange("p (m q) -> p m q", m=2)[:, :, 0:BLK], AF.Exp,
                        [sp_[0], sp_[1]], [p2], scale=0.125)
                    for m in range(2):
                        mm(o_acc[m].t[:, 0:BLK], V.t[:, kt, h * 128:(h + 1) * 128], p2.t[:, m, 0:BLK], kt == 0, kt == NKT - 1, [V.bs[kt], p2], [o_acc[m]])
                    for m in range(2):
                        mm(sbank.t[32 * m:32 * (m + 1), 0:BLK], onesb.t[:, 32 * m:32 * (m + 1)], p2.t[:, m, 0:BLK], kt == 0, kt == NKT - 1,
                           [onesb, p2], [sbank], tp=(0, 32 * m))
                    if pending and kt >= 1:
                        if next(pending[0], "done") == "done":
                            pending.pop(0)
                    if kt == NKT - 1:
                        for g_ in pending:
                            for _ in g_:
                                pass
                        pending = []
                        S_.op("act", lambda e: e.copy(o0s.t[:], o_acc[0].t[:, 0:BLK]), [o_acc[0].b], [o0s.b])
                        cp("dve", o1s.t[:], o_acc[1].t[:, 0:BLK], [o_acc[1]], [o1s])
                        cp("dve", ssb.t[0:64, :], sbank.t[0:64, 0:BLK], [sbank], [ssb])
                        pending.append(finalize_gen(h, qb))
                for g_ in pending:
                    for _ in g_:
                        pass
                S_.barrier()

        with contextlib.ExitStack() as esh:
            def sbh(name, shape, dt=F32, nb=1):
                return T(esh.enter_context(nc.sbuf_tensor("s_" + name, list(shape), dt)), nb)
            u_tm = sbh("u_tm", [128, NT, 512], BF16, nb=NT)
            with contextlib.ExitStack() as es:
                def sb(name, shape, dt=F32, nb=1):
                    return T(es.enter_context(nc.sbuf_tensor("s_" + name, list(shape), dt)), nb)
                hring = [sb("h0hb%d" % i, [128, 8, BLK], BF16) for i in range(2)]; whY = sb("whY", [128, 8, 1536], BF16)
                bhy = sb("bhy", [128, 40]); cw = sb("cw", [128, 12, 3]); cb = sb("cb", [128, 12])
                ppads = [[sb("ppad%d_%d" % (i, c), [128, S + 2], BF16) for c in range(3)] for i in range(2)]
                zf = sb("zf", [128, S])
                zX = sb("zX", [128, S], BF16); zA = sb("zA", [128, S], BF16); zB = sb("zB", [128, S], BF16); uTb = sb("uTb", [128, S], BF16)
                for c in range(3):
                    dma("pool", whY.t[:, :, c * 512:(c + 1) * 512], w_in_v[:, :, 1536 + c * 512:1536 + (c + 1) * 512], [], [whY])
                dma("sp", bhy.t[:], bin_fm_d, [], [bhy]); dma("sp", cw.t[:], convw_d, [], [cw]); dma("sp", cb.t[:], convb_d, [], [cb])
                for i in range(2):
                    for c in range(3):
                        mset("pool", ppads[i][c].t[:, 0:1], 0.0, [ppads[i][c]]); mset("pool", ppads[i][c].t[:, S + 1:S + 2], 0.0, [ppads[i][c]])
                zn = 0
                pend_tr = None
                for j in range(4):
                    pset = ppads[j % 2]
                    for b in range(NB):
                        hb = hring[b % 2]
                        dma("sp", hb.t[:], hT_d[:, :, b * BLK:(b + 1) * BLK], [B_hT[b]], [hb])
                        for c3 in range(3):
                            ch = c3 * 4 + j
                            pp = PS[zn % 4]; zn += 1
                            for k in range(8):
                                mm(pp.t[:, 0:BLK], whY.t[:, k, ch * 128:(ch + 1) * 128], hb.t[:, k, :], k == 0, k == 7, [whY, hb], [pp])
                            act(pset[c3].t[:, 1 + b * BLK:1 + (b + 1) * BLK], pp.t[:, 0:BLK], AF.Identity, [pp, bhy], [pset[c3]], bias=bhy.t[:, 12 + ch:13 + ch])
                    if pend_tr is not None:
                        pend_tr()
                        pend_tr = None
                    for c3, out in ((0, zX), (1, zA), (2, zB)):
                        ch = c3 * 4 + j
                        pp_ = pset[c3]
                        act(zf.t[:], pp_.t[:, 0:S], AF.Identity, [pp_, cw, cb], [zf], scale=cw.t[:, ch, 0:1], bias=cb.t[:, ch:ch + 1])
                        stt("dve", zf.t[:], pp_.t[:, 1:S + 1], cw.t[:, ch, 1:2], zf.t[:], ALU.mult, ALU.add, [pp_, cw, zf], [zf])
                        stt("dve", out.t[:], pp_.t[:, 2:S + 2], cw.t[:, ch, 2:3], zf.t[:], ALU.mult, ALU.add, [pp_, cw, zf], [out])
                    dma("sp", x0c_d[:, j, :], zX.t[:], [zX], [B_x0c[j]])
                    tt("pool", uTb.t[:], zA.t[:], zB.t[:], ALU.mult, [zA, zB], [uTb])
                    dma("sp", uT_d[:, j, :], uTb.t[:], [uTb], [B_uT[j]])
                    def do_tr(j=j):
                        for t0_ in range(0, NT, 8):
                            nt_ = min(8, NT - t0_)
                            psi = 4 + ((j * NT + t0_) // 8) % 2
                            pb = psbf(psi)
                            for t_ in range(nt_):
                                tr(pb[:, t_ * 128:(t_ + 1) * 128], uTb.t[:, (t0_ + t_) * 128:(t0_ + t_ + 1) * 128], ident.t[:], [uTb, ident], [PS[psi]])
                            S_.op("act", (lambda o_ap, i_ap: lambda e: e.copy(o_ap, i_ap))(u_tm.t[:, t0_:t0_ + nt_, j * 128:(j + 1) * 128],
                                                                                           pb[:, 0:nt_ * 128].rearrange("p (t c) -> p t c", c=128)),
                                  bl([PS[psi]]), bl([u_tm.bs[t] for t in range(t0_, t0_ + nt_)]))
                    pend_tr = do_tr
                pend_tr()
                S_.barrier()

            with contextlib.ExitStack() as esk:
                ksum = T(esk.enter_context(nc.sbuf_tensor("s_ksum", [128, NT, 512], BF16)), NT)
                kdiff = T(esk.enter_context(nc.sbuf_tensor("s_kdiff", [128, NT, 512], BF16)), NT)
                with contextlib.ExitStack() as es:
                    def sb(name, shape, dt=F32, nb=1):
                        return T(es.enter_context(nc.sbuf_tensor("s_" + name, list(shape), dt)), nb)
                    zT = sb("zT", [33, S]); fw1 = sb("fw1", [33, 64]); fw2 = sb("fw2", [64, 64]); fw3 = sb("fw3", [64, 64]); fw4 = sb("fw4", [64, 1024])
                    fvec = sb("fvec", [64, 4]); trow = sb("trow", [1, S]); drow = sb("drow", [1, 512])
                    H3 = sb("H3", [64, S])
                    arg = sb("arg", [64, BLK]); ai = sb("ai", [64, BLK], I32); af = sb("af", [64, BLK]); hh = [sb("hh%d" % i, [64, BLK]) for i in range(2)]
                    dec = sb("dec", [128, 512]); kf = sb("kf", [128, 512]); kb = sb("kb", [128, 512])
                    for t_, d_ in [(zT, zT_d), (fw1, fw1_d), (fw2, fw2_d), (fw3, fw3_d), (fw4, fw4_d), (fvec, fvec_d), (trow, trow_d), (drow, drow_d)]:
                        dma("sp", t_.t[:], d_, [], [t_])

                    def sin_layer(ps, li, out_ap, out_t):
                        ts("dve", arg.t[:], ps.t[0:64, 0:BLK], fvec.t[:, li:li + 1], fvec.t[:, 3:4], ALU.add, ALU.mult, [ps, fvec], [arg])
                        ts("dve", arg.t[:], arg.t[:], 1.0 / (2 * PI), 16.0, ALU.mult, ALU.add, [arg], [arg])
                        cp("dve", ai.t[:], arg.t[:], [arg], [ai])
                        cp("dve", af.t[:], ai.t[:], [ai], [af])
                        tt("dve", arg.t[:], arg.t[:], af.t[:], ALU.subtract, [arg, af], [arg])
                        ts("dve", af.t[:], arg.t[:], 0.5, None, ALU.is_gt, None, [arg], [af])
                        tt("dve", arg.t[:], arg.t[:], af.t[:], ALU.subtract, [arg, af], [arg])
                        act(out_ap, arg.t[:], AF.Sin, [arg], [out_t], scale=2 * PI)

                    for b in range(NB):
                        sl = slice(b * BLK, (b + 1) * BLK)
                        mm(PS[0].t[0:64, 0:BLK], fw1.t[:], zT.t[:, sl], True, True, [fw1, zT], [PS[0]])
                        sin_layer(PS[0], 0, hh[0].t[:], hh[0])
                        mm(PS[1].t[0:64, 0:BLK], fw2.t[:], hh[0].t[:], True, True, [fw2, hh[0]], [PS[1]])
                        sin_layer(PS[1], 1, hh[1].t[:], hh[1])
                        mm(PS[2].t[0:64, 0:BLK], fw3.t[:], hh[1].t[:], True, True, [fw3, hh[1]], [PS[2]])
                        sin_layer(PS[2], 2, H3.t[:, sl], H3)
                    for lt in range(NT):
                        pf = PS[(lt % 2) * 3]; pb_ = PS[(lt % 2) * 3 + 1]; pd = PS[(lt % 2) * 3 + 2]
                        mm(pf.t[:], H3.t[:, lt * 128:(lt + 1) * 128], fw4.t[:, 0:512], True, True, [H3, fw4], [pf])
                        mm(pb_.t[:], H3.t[:, lt * 128:(lt + 1) * 128], fw4.t[:, 512:1024], True, True, [H3, fw4], [pb_])
                        mm(pd.t[:], trow.t[0:1, lt * 128:(lt + 1) * 128], drow.t[0:1, :], True, True, [trow, drow], [pd])
                        act(dec.t[:], pd.t[:], AF.Exp, [pd], [dec], scale=-1.0)
                        stt("dve", kf.t[:], pf.t[:], 2.0 / N2, dec.t[:], ALU.mult, ALU.mult, [pf, dec], [kf])
                        stt("dve", kb.t[:], pb_.t[:], 2.0 / N2, dec.t[:], ALU.mult, ALU.mult, [pb_, dec], [kb])
                        if lt == 0:
                            mset("dve", kb.t[0:1, :], 0.0, [kb])
                        tt("pool", ksum.t[:, lt, :], kf.t[:], kb.t[:], ALU.add, [kf, kb], [ksum.bs[lt]])
                        tt("pool", kdiff.t[:, lt, :], kb.t[:], kf.t[:], ALU.subtract, [kf, kb], [kdiff.bs[lt]])
                    S_.barrier()

                with contextlib.ExitStack() as es:
                    def sb(name, shape, dt=F32, nb=1):
                        return T(es.enter_context(nc.sbuf_tensor("s_" + name, list(shape), dt)), nb)
                    cf = [sb("cf%d" % i, [128, NT, 128], BF16) for i in range(2)]; sf = [sb("sf%d" % i, [128, NT, 128], BF16) for i in range(2)]
                    kre = sb("kre", [128, 512]); kim = sb("kim", [128, 512]); m1 = sb("m1", [128, 512]); m2 = sb("m2", [128, 512])
                    yt = [sb("yt%d" % i, [128, 2, 512], BF16) for i in range(2)]
                    for kc in range(NKC):
                        c_ = cf[kc % 2]; s_ = sf[kc % 2]
                        if kc == 0:
                            dma("sp", c_.t[:], Cf_d[0], [], [c_]); dma("sp", s_.t[:], Sf_d[0], [], [s_])
                        if kc + 1 < NKC:
                            cn_ = cf[(kc + 1) % 2]; sn_ = sf[(kc + 1) % 2]
                            dma("sp", cn_.t[:], Cf_d[kc + 1], [], [cn_]); dma("sp", sn_.t[:], Sf_d[kc + 1], [], [sn_])
                        pa = PS[(kc % 2) * 4: (kc % 2) * 4 + 4]
                        for t_ in range(NT):
                            st_, sp2 = (t_ == 0), (t_ == NT - 1)
                            mm(pa[0].t[:], c_.t[:, t_, :], u_tm.t[:, t_, :], st_, sp2, [c_, u_tm.bs[t_]], [pa[0]])
                            mm(pa[1].t[:], s_.t[:, t_, :], u_tm.t[:, t_, :], st_, sp2, [s_, u_tm.bs[t_]], [pa[1]])
                            mm(pa[2].t[:], c_.t[:, t_, :], ksum.t[:, t_, :], st_, sp2, [c_, ksum.bs[t_]], [pa[2]])
                            mm(pa[3].t[:], s_.t[:, t_, :], kdiff.t[:, t_, :], st_, sp2, [s_, kdiff.bs[t_]], [pa[3]])
                        y_ = yt[kc % 2]
                        act(kre.t[:], pa[2].t[:], AF.Copy, [pa[2]], [kre]); act(kim.t[:], pa[3].t[:], AF.Copy, [pa[3]], [kim])
                        tt("dve", m1.t[:], pa[0].t[:], kre.t[:], ALU.mult, [pa[0], kre], [m1])
                        tt("dve", m2.t[:], pa[1].t[:], kim.t[:], ALU.mult, [pa[1], kim], [m2])
                        tt("pool", y_.t[:, 0, :], m1.t[:], m2.t[:], ALU.add, [m1, m2], [y_])
                        tt("dve", m1.t[:], pa[0].t[:], kim.t[:], ALU.mult, [pa[0], kim], [m1])
                        tt("dve", m2.t[:], pa[1].t[:], kre.t[:], ALU.mult, [pa[1], kre], [m2])
                        tt("pool", y_.t[:, 1, :], m1.t[:], m2.t[:], ALU.subtract, [m1, m2], [y_])
                        dma("sp", Y_d[:, :, kc, :], y_.t[:], [y_], [B_Y[kc]])
                    S_.barrier()

        with contextlib.ExitStack() as es:
            def sb(name, shape, dt=F32, nb=1):
                return T(es.enter_context(nc.sbuf_tensor("s_" + name, list(shape), dt)), nb)
            Yall = sb("Yall", [128, 2, NKC, 512], BF16, nb=NKC)
            ci = [sb("ci%d" % i, [128, KG, BLK], BF16) for i in range(3)]; si = [sb("si%d" % i, [128, KG, BLK], BF16) for i in range(3)]
            x0b = [sb("x0b%d" % i, [128, 4, BLK], BF16) for i in range(2)]; uTk = [sb("uTk%d" % i, [128, 4, BLK], BF16) for i in range(2)]
            obb = [sb("obb%d" % i, [128, 4, BLK], BF16) for i in range(2)]
            tmp = [sb("h2tmp%d" % i, [128, BLK]) for i in range(2)]; hbv = sb("hbv", [128, 4])
            dma("sp", hbv.t[:], hb_d, [], [hbv])
            def load_y(kg_):
                for kc_ in range(kg_ * KG, (kg_ + 1) * KG):
                    dma("sp", Yall.t[:, :, kc_, :], Y_d[:, :, kc_, :], [B_Y[kc_]], [Yall.bs[kc_]])
            load_y(0)
            n = 0
            for tb in range(NB):
                acc = PS[(tb % 2) * 4:(tb % 2) * 4 + 4]
                xb = x0b[tb % 2]; ub = uTk[tb % 2]; ob = obb[tb % 2]
                dma("sp", xb.t[:], x0c_d[:, :, tb * BLK:(tb + 1) * BLK], B_x0c, [xb])
                dma("sp", ub.t[:], uT_d[:, :, tb * BLK:(tb + 1) * BLK], B_uT, [ub])
                for kg in range(NKG):
                    c_ = ci[n % 3]; s_ = si[n % 3]; n += 1
                    dma("sp", c_.t[:], Ci_d[tb, :, kg * KG:(kg + 1) * KG, :], [], [c_])
                    dma("sp", s_.t[:], Si_d[tb, :, kg * KG:(kg + 1) * KG, :], [], [s_])
                    if tb == 0 and kg + 1 < NKG:
                        load_y(kg + 1)
                    for kl in range(KG):
                        kc = kg * KG + kl
                        for c4 in range(4):
                            mm(acc[c4].t[:, 0:BLK], Yall.t[:, 0, kc, c4 * 128:(c4 + 1) * 128], c_.t[:, kl, :], kc == 0, False, [Yall.bs[kc], c_], [acc[c4]])
                            mm(acc[c4].t[:, 0:BLK], Yall.t[:, 1, kc, c4 * 128:(c4 + 1) * 128], s_.t[:, kl, :], False, kc == NKC - 1, [Yall.bs[kc], s_], [acc[c4]])
                for c4 in range(4):
                    tm = tmp[c4 % 2]
                    stt("dve", tm.t[:], ub.t[:, c4, :], hbv.t[:, c4:c4 + 1], acc[c4].t[:, 0:BLK], ALU.mult, ALU.add, [ub, hbv, acc[c4]], [tm])
                    tt("pool", ob.t[:, c4, :], tm.t[:], xb.t[:, c4, :], ALU.mult, [tm, xb], [ob])
                dma("sp", obT_d[:, :, tb * BLK:(tb + 1) * BLK], ob.t[:], [ob], [B_obT[tb]])
            S_.barrier()

        with contextlib.ExitStack() as es:
            def sb(name, shape, dt=F32, nb=1):
                return T(es.enter_context(nc.sbuf_tensor("s_" + name, list(shape), dt)), nb)
            wg = sb("wg", [128, 8, 2048], BF16); wua = sb("wua", [128, 4, D], BF16); wub = sb("wub", [128, 4, D], BF16)
            wout = sb("wout", [128, 8, D], BF16); rwt = sb("rwt", [128, 8, 32], BF16); rbt = sb("rbt", [1, 32], BF16)
            bgf = sb("bgf", [128, 40])
            hblk = [sb("mhb%d" % i, [128, 8, BLK], BF16) for i in range(2)]
            oab = [sb("moa%d" % i, [128, 4, BLK], BF16) for i in range(2)]; obk = [sb("mob%d" % i, [128, 4, BLK], BF16) for i in range(2)]
            gas = [sb("gas%d" % i, [128, BLK]) for i in range(2)]; gbs = [sb("gbs%d" % i, [128, BLK]) for i in range(2)]
            mm1 = [sb("mm1%d" % i, [128, BLK]) for i in range(1)]; mm2 = [sb("mm2%d" % i, [128, BLK]) for i in range(1)]
            mTs = [sb("mT%d" % i, [128, 8, BLK], BF16, nb=8) for i in range(2)]
            xin = [sb("mxin%d" % i, [128, D]) for i in range(2)]; xn = [sb("mxn%d" % i, [128, D]) for i in range(2)]
            tg = sb("mtg", [128, 512])
            h2b = [sb("h2b%d" % i, [128, 8, BLK], BF16) for i in range(2)]
            msk = sb("msk", [128, 32]); mskb = sb("mskb", [128, 32], BF16); nmx = sb("nmx", [128, 1])
            htmp = sb("htmp", [128, D]); h2tm = [sb("h2tm%d" % i, [128, D], BF16) for i in range(1)]
            ex = sb("ex", [128, 32]); sm_ = sb("sm", [128, 1])
            st = norm_transpose(es, "m")
            for c in range(4):
                dma("pool", wg.t[:, :, c * 512:(c + 1) * 512], w_in_v[:, :, 3072 + c * 512:3072 + (c + 1) * 512], [], [wg])
            dma("pool", wua.t[:], wua_d.rearrange("(k p) n -> p k n", p=128), [], [wua])
            dma("pool", wub.t[:], wub_d.rearrange("(k p) n -> p k n", p=128), [], [wub])
            for c in range(2):
                dma("pool", wout.t[:, :, c * 512:(c + 1) * 512], wout_d.rearrange("(k p) n -> p k n", p=128)[:, :, c * 512:(c + 1) * 512], [], [wout])
            dma("pool", rwt.t[:], rw_d.rearrange("(k p) n -> p k n", p=128), [], [rwt]); dma("pool", rbt.t[:], rb_d, [], [rbt])
            dma("sp", bgf.t[:], bin_fm_d, [], [bgf])
            tno = 0

            def gates(tb):
                hb = hblk[tb % 2]; oa = oab[tb % 2]; ob = obk[tb % 2]; mT = mTs[tb % 2]
                dma("sp", hb.t[:], hT_d[:, :, tb * BLK:(tb + 1) * BLK], [B_hT[tb]], [hb])
                dma("sp", oa.t[:], oaT_d[:, :, tb * BLK:(tb + 1) * BLK], [B_oaT[tb]], [oa])
                dma("sp", ob.t[:], obT_d[:, :, tb * BLK:(tb + 1) * BLK], [B_obT[tb]], [ob])
                for j in range(8):
                    pga, pgb, pA, pB = PS[0], PS[1], PS[2], PS[3]
                    for k in range(8):
                        mm(pga.t[:, 0:BLK], wg.t[:, k, j * 128:(j + 1) * 128], hb.t[:, k, :], k == 0, k == 7, [wg, hb], [pga])
                    for k in range(8):
                        mm(pgb.t[:, 0:BLK], wg.t[:, k, 1024 + j * 128:1024 + (j + 1) * 128], hb.t[:, k, :], k == 0, k == 7, [wg, hb], [pgb])
                    for k in range(4):
                        mm(pA.t[:, 0:BLK], wua.t[:, k, j * 128:(j + 1) * 128], oa.t[:, k, :], k == 0, k == 3, [wua, oa], [pA])
                    for k in range(4):
                        mm(pB.t[:, 0:BLK], wub.t[:, k, j * 128:(j + 1) * 128], ob.t[:, k, :], k == 0, k == 3, [wub, ob], [pB])
                    ga_ = gas[j % 2]; gb_ = gbs[j % 2]; a1 = mm1[0]; a2 = mm2[0]
                    act(ga_.t[:], pga.t[:, 0:BLK], AF.Sigmoid, [pga, bgf], [ga_], bias=bgf.t[:, 24 + j:25 + j])
                    act(gb_.t[:], pgb.t[:, 0:BLK], AF.Sigmoid, [pgb, bgf], [gb_], bias=bgf.t[:, 32 + j:33 + j])
                    tt("dve", a1.t[:], pA.t[:, 0:BLK], ga_.t[:], ALU.mult, [pA, ga_], [a1])
                    tt("dve", a2.t[:], pB.t[:, 0:BLK], gb_.t[:], ALU.mult, [pB, gb_], [a2])
                    tt("pool", mT.t[:, j, :], a1.t[:], a2.t[:], ALU.add, [a1, a2], [mT.bs[j]])

            def tiles(tb):
                nonlocal tno
                h2 = h2b[tb % 2]; mT = mTs[tb % 2]
                for s in range(TPB):
                    i = tb * TPB + s
                    xi = xin[tno % 2]; xo = xn[tno % 2]; tno += 1
                    dma("sp", xi.t[:], x_d[i * 128:(i + 1) * 128, :], [], [xi])
                    for half in range(2):
                        po = PS[4 + half]
                        for j in range(8):
                            mm(po.t[:], mT.t[:, j, s * 128:(s + 1) * 128], wout.t[:, j, half * 512:(half + 1) * 512], j == 0, j == 7, [mT.bs[j], wout], [po])
                        tt("dve", tg.t[:], po.t[:], g1row.t[:, half * 512:(half + 1) * 512], ALU.mult, [po, g1row], [tg])
                        tt("pool", xo.t[:, half * 512:(half + 1) * 512], tg.t[:], xi.t[:, half * 512:(half + 1) * 512], ALU.add, [tg, xi], [xo])
                    dma("sp", y_d[i * 128:(i + 1) * 128, :], xo.t[:], [xo], [B_y[i]])
                    ss_t = do_norm_transpose(st, xo, A2, B2, 6, lambda k, s=s, h2=h2: h2.t[:, k, s * 128:(s + 1) * 128], [h2])
                    hm = h2tm[0]
                    stt("dve", htmp.t[:], xo.t[:], ss_t.t[:, 0:1], A2row.t[:], ALU.mult, ALU.mult, [xo, ss_t, A2row], [htmp])
                    tt("pool", hm.t[:], htmp.t[:], B2row.t[:], ALU.add, [htmp, B2row], [hm])
                    dma("sp", H2tm_d[i * 128:(i + 1) * 128, :], hm.t[:], [hm], [B_h2tm[i]])
                    pr = PS[7]
                    for k in range(8):
                        mm(pr.t[:, 0:32], h2.t[:, k, s * 128:(s + 1) * 128], rwt.t[:, k, :], k == 0, False, [h2, rwt], [pr])
                    mm(pr.t[:, 0:32], onesb.t[0:1, :], rbt.t[0:1, :], False, True, [onesb, rbt], [pr])
                    lgi = LG.t[:, i, :]
                    act(lgi, pr.t[:, 0:32], AF.Copy, [pr], [LG.bs[i]])
                    vmax8(MX.t[:, i, :], lgi, [LG.bs[i]], [MX.bs[i]])
                    ts("dve", msk.t[:], lgi, MX.t[:, i, 3:4], None, ALU.is_ge, None, [LG.bs[i], MX.bs[i]], [msk])
                    cp("dve", mskb.t[:], msk.t[:], [msk], [mskb])
                    ts("dve", nmx.t[:], MX.t[:, i, 0:1], -1.0, None, ALU.mult, None, [MX.bs[i]], [nmx])
                    act(ex.t[:], lgi, AF.Exp, [LG.bs[i], nmx], [ex], bias=nmx.t[:, 0:1])
                    tt("dve", ex.t[:], ex.t[:], msk.t[:], ALU.mult, [ex, msk], [ex])
                    red(sm_.t[:], ex.t[:], [ex], [sm_])
                    recip(sm_.t[:], sm_.t[:], [sm_], [sm_])
                    ts("dve", G.t[:, i, :], ex.t[:], sm_.t[:, 0:1], None, ALU.mult, None, [ex, sm_], [G.bs[i]])
                    mm(pr.t[:, 32:64], ustr.t[:], mskb.t[:], True, False, [ustr, mskb], [pr])
                    mm(pr.t[:, 32:64], onesb.t[:], cumb.t[:], False, True, [onesb, cumb], [pr])
                    cp("dve", POS.t[:, i, :], pr.t[:, 32:64], [pr], [POS.bs[i]])
                    tt("dve", cumf.t[:], cumf.t[:], msk.t[:], ALU.add, [cumf, msk], [cumf])
                    cp("dve", cumb.t[:], cumf.t[:], [cumf], [cumb])

            gates(0)
            for tb in range(NB):
                if tb + 1 < NB:
                    gates(tb + 1)
                tiles(tb)
            S_.barrier()
        if debug:
            with contextlib.ExitStack() as es3:
                dt_ = T(es3.enter_context(nc.sbuf_tensor("s_dbgt", [128, 8, S], F32)))
                db_ = T(es3.enter_context(nc.sbuf_tensor("s_dbgb", [128, 8, S], BF16)))
                for nm, src, nch in [("dbg_oaT", oaT_d, 4), ("dbg_obT", obT_d, 4), ("dbg_hT", hT_d, 8)]:
                    dma("sp", db_.t[:, 0:nch, :], src, [], [db_])
                    cp("dve", dt_.t[:, 0:nch, :], db_.t[:, 0:nch, :], [db_], [dt_])
                    dma("sp", dbg[nm], dt_.t[:, 0:nch, :], [dt_], [Buf()])
                    S_.barrier()
                dma("sp", dbg["dbg_G"], G.t[:], G.bs, [Buf()])
                S_.barrier()

        with contextlib.ExitStack() as es:
            def sb(name, shape, dt=F32, nb=1):
                return T(es.enter_context(nc.sbuf_tensor("s_" + name, list(shape), dt)), nb)
            cnt = sb("cnt", [128, 32]); yv = sb("yv", [128, 32]); yi_ = sb("yi", [128, 32], I32); yf = sb("yf", [128, 32]); ygt = sb("ygt", [128, 32])
            padded = sb("padded", [128, 32]); ca = sb("csuma", [128, 32]); cb_ = sb("csumb", [128, 32]); pstart = sb("pstart", [128, 32])
            kp = sb("kp", [128, 8]); bstart = sb("bstart", [128, NBLK]); cmp_ = sb("cmp", [128, NBLK, 32]); be = sb("be", [128, NBLK])
            bf_ = sb("bf", [128, NBLK, 2])
            slotv = [sb("slotv%d" % i, [128, 32]) for i in range(2)]; oh4 = [sb("oh4%d" % i, [128, 4, 32]) for i in range(2)]
            pr4 = [sb("pr4%d" % i, [128, 4, 32]) for i in range(2)]; i4f = [sb("i4f%d" % i, [128, 4]) for i in range(2)]
            hrow = [sb("hrow%d" % i, [128, D], BF16) for i in range(3)]
            dma("sp", kp.t[:], kp_d, [], [kp]); dma("sp", bstart.t[:], bstart_d, [], [bstart])
            mm(PS[0].t[:, 0:32], onesb.t[:], cumb.t[:], True, True, [onesb, cumb], [PS[0]])
            cp("dve", cnt.t[:], PS[0].t[:, 0:32], [PS[0]], [cnt])
            ts("dve", yv.t[:], cnt.t[:], float(RB - 1), 1.0 / RB, ALU.add, ALU.mult, [cnt], [yv])
            cp("dve", yi_.t[:], yv.t[:], [yv], [yi_]); cp("dve", yf.t[:], yi_.t[:], [yi_], [yf])
            tt("dve", ygt.t[:], yf.t[:], yv.t[:], ALU.is_gt, [yf, yv], [ygt])
            tt("dve", yf.t[:], yf.t[:], ygt.t[:], ALU.subtract, [yf, ygt], [yf])
            ts("dve", padded.t[:], yf.t[:], float(RB), None, ALU.mult, None, [yf], [padded])
            cp("dve", ca.t[:], padded.t[:], [padded], [ca])
            src_, dst_ = ca, cb_
            for sh in (1, 2, 4, 8, 16):
                cp("dve", dst_.t[:, 0:sh], src_.t[:, 0:sh], [src_], [dst_])
                tt("dve", dst_.t[:, sh:32], src_.t[:, sh:32], src_.t[:, 0:32 - sh], ALU.add, [src_], [dst_])
                src_, dst_ = dst_, src_
            pend = src_
            tt("dve", pstart.t[:], pend.t[:], padded.t[:], ALU.subtract, [pend, padded], [pstart])
            tt("dve", cmp_.t[:], pend.t[:].unsqueeze(1).broadcast_to([128, NBLK, 32]), bstart.t[:].unsqueeze(2).broadcast_to([128, NBLK, 32]),
               ALU.is_le, [pend, bstart], [cmp_])
            red(be.t[:], cmp_.t[:], [cmp_], [be])
            ts("dve", be.t[:], be.t[:], float(NE - 1), None, ALU.min, None, [be], [be])
            stt("dve", bf_.t[:, :, 0], be.t[:], 128.0, kp.t[:, 0:1].broadcast_to([128, NBLK]), ALU.mult, ALU.add, [be, kp], [bf_])
            cp("dve", bf_.t[:, :, 1], be.t[:], [be], [bf_])
            cp("dve", BIDX.t[:], bf_.t[:], [bf_], [BIDX])
            for i in range(NT):
                sv = slotv[i % 2]; oh = oh4[i % 2]; p4 = pr4[i % 2]; f4 = i4f[i % 2]; hr = hrow[i % 3]
                tt("dve", sv.t[:], POS.t[:, i, :], pstart.t[:], ALU.add, [POS.bs[i], pstart], [sv])
                tt("dve", oh.t[:], LG.t[:, i, :].unsqueeze(1).broadcast_to([128, 4, 32]), MX.t[:, i, 0:4].unsqueeze(2).broadcast_to([128, 4, 32]),
                   ALU.is_equal, [LG.bs[i], MX.bs[i]], [oh])
                tt("dve", p4.t[:], oh.t[:], sv.t[:].unsqueeze(1).broadcast_to([128, 4, 32]), ALU.mult, [oh, sv], [p4])
                red(f4.t[:], p4.t[:], [p4], [f4])
                cp("dve", IDX.t[:, i, :], f4.t[:], [f4], [IDX.bs[i]])
                tt("dve", p4.t[:], oh.t[:], G.t[:, i, :].unsqueeze(1).broadcast_to([128, 4, 32]), ALU.mult, [oh, G.bs[i]], [p4])
                red(W4.t[:, i, :], p4.t[:], [p4], [W4.bs[i]])
                dma("sp", hr.t[:], H2tm_d[i * 128:(i + 1) * 128, :], [B_h2tm[i]], [hr])
                for j in range(4):
                    scatter(Xs_d, hr.t[:], IDX.t[:, i, j:j + 1], [hr, IDX.bs[i]], [])
            S_.barrier()

        with contextlib.ExitStack() as es:
            def sb(name, shape, dt=F32, nb=1):
                return T(es.enter_context(nc.sbuf_tensor("s_" + name, list(shape), dt)), nb)
            w1t = [sb("w1t%d" % i, [128, 8, 2048], BF16) for i in range(2)]; w2t = [sb("w2t%d" % i, [128, 8, D], BF16) for i in range(2)]
            b1t = [sb("b1t%d" % i, [128, 16]) for i in range(2)]; b2rep = [sb("b2rep%d" % i, [128, D], BF16) for i in range(2)]
            xsb = [sb("xsb%d" % i, [128, NST, D], BF16) for i in range(2)]; XsT = [sb("XsT%d" % i, [128, 8, RB], BF16) for i in range(2)]
            actT = [sb("actT%d" % i, [128, 8, RB], BF16, nb=8) for i in range(2)]
            gcl = [sb("gcl%d" % i, [128, RB]) for i in range(2)]; sg = [sb("sg%d" % i, [128, RB]) for i in range(2)]
            ucl = [sb("ucl%d" % i, [128, RB]) for i in range(2)]
            ysb = [sb("ysb%d" % i, [128, D]) for i in range(2)]
            ew1f = ew1_d.rearrange("e d n -> (e d) n"); ew2f = ew2_d.rearrange("e d n -> (e d) n")
            en = 0; yn = 0
            for b in range(NBLK):
                w1 = w1t[b % 2]; w2 = w2t[b % 2]; b1 = b1t[b % 2]; b2 = b2rep[b % 2]; xs_ = xsb[b % 2]; xT = XsT[b % 2]; aT = actT[b % 2]
                gather(w1.t[:].rearrange("p k n -> p (k n)"), W1b_d, BIDX.t[:, b, 0:1], [BIDX], [w1])
                gather(b1.t[:], eb1_d, BIDX.t[:, b, 0:1], [BIDX], [b1])
                gather(w2.t[:].rearrange("p k n -> p (k n)"), W2b_d, BIDX.t[:, b, 0:1], [BIDX], [w2])
                gather(b2.t[:], eb2_d, BIDX.t[:, b, 1:2], [BIDX], [b2])
                ts("dve", b1.t[:, 8:16], b1.t[:, 8:16], 1.0, None, ALU.add, None, [b1], [b1])
                if b == 0:
                    dma("sp", xs_.t[:], Xs_d[0:RB, :].rearrange("(s p) d -> p s d", p=128), [], [xs_])
                if b + 1 < NBLK:
                    xn_ = xsb[(b + 1) % 2]
                    dma("sp", xn_.t[:], Xs_d[(b + 1) * RB:(b + 2) * RB, :].rearrange("(s p) d -> p s d", p=128), [], [xn_])
                for s2 in range(NST):
                    pb = psbf(6 + s2 % 2)
                    for k in range(8):
                        tr(pb[:, k * 128:(k + 1) * 128], xs_.t[:, s2, k * 128:(k + 1) * 128], ident.t[:], [xs_, ident], [PS[6 + s2 % 2]])
                    S_.op("act", (lambda o, i_: lambda e: e.copy(o, i_))(xT.t[:, :, s2 * 128:(s2 + 1) * 128], pb[:, :].rearrange("p (k t) -> p k t", t=128)),
                          [PS[6 + s2 % 2].b], [xT.b])
                for Fi in range(8):
                    pg = PS[(en % 2) * 2]; pu = PS[(en % 2) * 2 + 1]
                    gc = gcl[en % 2]; sgt = sg[en % 2]; uc = ucl[en % 2]; gs_ = sgt; en += 1
                    for k in range(8):
                        mm(pg.t[:, 0:RB], w1.t[:, k, Fi * 128:(Fi + 1) * 128], xT.t[:, k, :], k == 0, k == 7, [w1, xT], [pg])
                    for k in range(8):
                        mm(pu.t[:, 0:RB], w1.t[:, k, 1024 + Fi * 128:1024 + (Fi + 1) * 128], xT.t[:, k, :], k == 0, k == 7, [w1, xT], [pu])
                    ts("dve", gc.t[:], pg.t[:, 0:RB], b1.t[:, Fi:Fi + 1], 7.0, ALU.add, ALU.min, [pg, b1], [gc])
                    act(sgt.t[:], gc.t[:], AF.Sigmoid, [gc], [sgt], scale=1.702)
                    ts("dve", uc.t[:], pu.t[:, 0:RB], b1.t[:, 8 + Fi:9 + Fi], 8.0, ALU.add, ALU.min, [pu, b1], [uc])
                    tt("dve", gs_.t[:], gc.t[:], sgt.t[:], ALU.mult, [gc, sgt], [gs_])
                    stt("dve", aT.t[:, Fi, :], uc.t[:], -6.0, gs_.t[:], ALU.max, ALU.mult, [gs_, uc], [aT.bs[Fi]])
                for s2 in range(NST):
                    yt_ = ysb[yn % 2]; yn += 1
                    for half in range(2):
                        py = PS[4 + half]
                        for k in range(8):
                            mm(py.t[:], aT.t[:, k, s2 * 128:(s2 + 1) * 128], w2.t[:, k, half * 512:(half + 1) * 512], k == 0, k == 7, [aT.bs[k], w2], [py])
                        tt("dve", yt_.t[:, half * 512:(half + 1) * 512], py.t[:], b2.t[:, half * 512:(half + 1) * 512], ALU.add, [py, b2], [yt_])
                    dma("sp", Ys_d[b * RB + s2 * 128:b * RB + (s2 + 1) * 128, :], yt_.t[:], [yt_], [B_Ys[b]])
            S_.barrier()

        with contextlib.ExitStack() as es:
            def sb(name, shape, dt=F32, nb=1):
                return T(es.enter_context(nc.sbuf_tensor("s_" + name, list(shape), dt)), nb)
            gat = [[sb("gat%d_%d" % (i, j), [128, D]) for j in range(4)] for i in range(2)]
            xq = [sb("cxq%d" % i, [128, D]) for i in range(2)]; acc_ = [sb("cacc%d" % i, [128, D]) for i in range(2)]
            for i in range(NT):
                g4 = gat[i % 2]; xq_ = xq[i % 2]; ac = acc_[i % 2]
                for j in range(4):
                    gather(g4[j].t[:], Ys_d, IDX.t[:, i, j:j + 1], [IDX.bs[i]] + B_Ys, [g4[j]])
                if i == 0:
                    dma("sp", xq_.t[:], y_d[0:128, :], [B_y[0]], [xq_])
                if i + 1 < NT:
                    xqn = xq[(i + 1) % 2]
                    dma("sp", xqn.t[:], y_d[(i + 1) * 128:(i + 2) * 128, :], [B_y[i + 1]], [xqn])
                act(ac.t[:], g4[0].t[:], AF.Copy, [g4[0], W4.bs[i]], [ac], scale=W4.t[:, i, 0:1])
                for j in range(1, 4):
                    stt("dve", ac.t[:], g4[j].t[:], W4.t[:, i, j:j + 1], ac.t[:], ALU.mult, ALU.add, [g4[j], W4.bs[i], ac], [ac])
                tt("dve", ac.t[:], ac.t[:], g2row.t[:], ALU.mult, [ac, g2row], [ac])
                tt("dve", ac.t[:], ac.t[:], xq_.t[:], ALU.add, [ac, xq_], [ac])
                dma("sp", y_d[i * 128:(i + 1) * 128, :], ac.t[:], [ac], [B_y[i]])
            S_.barrier()

        sems = {k: ges.enter_context(nc.semaphore("s%d" % i)) for i, k in enumerate(S_.semkeys)}
        S_.emit(sems)
    return nc


_CONST_CACHE = {}


def make_constants(S):
    if S in _CONST_CACHE:
        return _CONST_CACHE[S]
    bf = ml_dtypes.bfloat16
    NT = S // 128; NKC = S // 128; BLK = min(512, S); NB = S // BLK
    N2 = 2 * S
    n = np.arange(S, dtype=np.int64)[:, None]; k = np.arange(S, dtype=np.int64)[None, :]
    ph = ((2 * k + 1) * n) % (2 * N2)
    ang = ph.astype(np.float64) * (np.pi / N2)
    C = np.cos(ang); Sm = np.sin(ang)
    del ang, ph
    def fwd(M):
        return np.ascontiguousarray(M.reshape(NT, 128, NKC, 128).transpose(2, 1, 0, 3)).astype(bf)
    def inv(M):
        return np.ascontiguousarray(M.reshape(NB, BLK, NKC, 128).transpose(0, 3, 2, 1)).astype(bf)
    consts = {"Cf": fwd(C), "Sf": fwd(Sm), "Ci": inv(C), "nSi": inv(-Sm)}
    del C, Sm
    consts["ident"] = np.eye(128, dtype=np.float32).astype(bf)
    GRID_W = 64; RF = 16
    rows = S // GRID_W
    row = np.repeat(np.arange(rows), GRID_W); col = np.tile(np.arange(GRID_W), rows)
    pos = np.stack([row, col], -1).astype(np.float32)
    freqs = (np.float32(10000.0) ** (-np.arange(RF, dtype=np.float32) / np.float32(RF))).astype(np.float32)
    ang = (pos[:, :, None] * freqs).astype(np.float32)
    cos = np.cos(ang).astype(np.float32); sin = np.sin(ang).astype(np.float32)
    ropec = np.stack([cos, cos], 2).reshape(S, 64)
    ropes = np.stack([sin, -sin], 2).reshape(S, 64)
    consts["ropec"] = np.ascontiguousarray(ropec, dtype=np.float32); consts["ropes"] = np.ascontiguousarray(ropes, dtype=np.float32)
    t = np.linspace(0.0, 1.0, S, dtype=np.float32)[:, None]
    w = (np.float32(2.0 * math.pi) * np.arange(S, dtype=np.float32)[:, None] / np.float32(S)).astype(np.float32)
    f = np.linspace(1e-4, 15, 16, dtype=np.float32)
    z = np.concatenate([t, np.cos(f * w), -np.sin(f * w)], -1).astype(np.float32)
    consts["zT"] = np.ascontiguousarray(z.T)
    consts["trow"] = np.ascontiguousarray(t.T)
    deltas = np.abs(np.linspace(math.log(1e-2) / 1.5, math.log(1e-2) / 0.3, 512, dtype=np.float32))
    consts["drow"] = deltas.reshape(1, 512).astype(np.float32)
    _CONST_CACHE[S] = consts
    return consts


def fm(v, nchunk):
    return np.ascontiguousarray(np.asarray(v, np.float32).reshape(nchunk, 128).T)


def make_in_maps(inp, S, CTXL, NE, B):
    consts = make_constants(S)
    f32 = lambda a: np.ascontiguousarray(np.asarray(a, np.float32))
    shared = dict(consts)
    shared["ada_w"] = f32(inp["ada_w"][0]); shared["ada_b_row"] = f32(inp["ada_b"][0]).reshape(1, -1); shared["ada_b_fm"] = fm(inp["ada_b"][0], 48)
    shared["n1g"] = fm(inp["norm1_g"][0], 8); shared["n2g"] = fm(inp["norm2_g"][0], 8)
    shared["w_in"] = f32(inp["w_in"][0]); shared["b_in_row"] = f32(inp["b_in"][0]).reshape(1, -1); shared["b_in_fm"] = fm(inp["b_in"][0], 40)
    shared["qg"] = f32(inp["q_norm_g"][0]).reshape(1, 64); shared["kg"] = f32(inp["k_norm_g"][0]).reshape(1, 64)
    shared["lam4"] = np.concatenate([f32(inp[n][0]) for n in ("lambda_q1", "lambda_k1", "lambda_q2", "lambda_k2")]).reshape(1, 256)
    shared["subg"] = f32(inp["subln_g"][0]).reshape(128, 1)
    cw = f32(inp["conv_w"][0])
    shared["convw"] = np.ascontiguousarray(cw.reshape(3, 12, 128).transpose(2, 1, 0)); shared["convb"] = fm(inp["conv_b"][0], 12)
    shared["fw1"] = f32(inp["filt_w1"][0]); shared["fw2"] = f32(inp["filt_w2"][0]); shared["fw3"] = f32(inp["filt_w3"][0]); shared["fw4"] = f32(inp["filt_w4"][0])
    shared["fvec"] = np.ascontiguousarray(np.stack([f32(inp["filt_b1"][0]), f32(inp["filt_b2"][0]), f32(inp["filt_b3"][0]), f32(inp["filt_freq"][0])], -1))
    shared["hbias"] = fm(inp["hyena_bias"][0], 4)
    shared["w_up_a"] = f32(inp["w_up_a"][0]); shared["w_up_b"] = f32(inp["w_up_b"][0]); shared["w_out"] = f32(inp["w_out"][0])
    shared["router_w"] = f32(inp["router_w"][0]); shared["router_b"] = f32(inp["router_b"][0]).reshape(1, 32)
    shared["ew1"] = f32(inp["exp_w1"][0]); shared["ew2"] = f32(inp["exp_w2"][0])
    shared["eb1"] = np.ascontiguousarray(f32(inp["exp_b1"][0]).reshape(NE, 16, 128).transpose(0, 2, 1).reshape(NE * 128, 16))
    shared["eb2"] = f32(inp["exp_b2"][0]).reshape(NE, D)
    shared["n2g_row"] = f32(inp["norm2_g"][0]).reshape(1, D)
    NBLK = (4 * S + NE * RB) // RB
    shared["ustrict"] = np.triu(np.ones((128, 128), np.float32), 1).astype(ml_dtypes.bfloat16)
    shared["kp"] = np.ascontiguousarray((np.arange(8)[None, :] * 128 + np.arange(128)[:, None]).astype(np.float32))
    shared["bstart"] = np.ascontiguousarray(np.broadcast_to((np.arange(NBLK) * RB).astype(np.float32)[None, :], (128, NBLK)))
    maps = []
    for b in range(B):
        m = dict(shared)
        m["x"] = f32(inp["x"][b]); m["ctx"] = f32(inp["ctx"][b])
        m["cc"] = np.ascontiguousarray(np.stack([f32(inp["c"][b]), f32(inp["c_ctx"])], -1).reshape(8, 128, 2).transpose(1, 0, 2))
        maps.append(m)
    return maps


_PROG_CACHE = {}


def kernel(**inputs):
    x = np.asarray(inputs["x"])
    B, S, _ = x.shape
    CTXL = np.asarray(inputs["ctx"]).shape[1]
    NE = np.asarray(inputs["exp_w1"]).shape[1]
    key = (S, CTXL, NE)
    if key not in _PROG_CACHE:
        _PROG_CACHE[key] = build_program(S, CTXL, NE)
    nc = _PROG_CACHE[key]
    maps = make_in_maps(inputs, S, CTXL, NE, B)
    res = run_bass_kernel_spmd(nc, maps, core_ids=list(range(B)))
    return np.stack([np.asarray(r["y"], dtype=np.float32) for r in res.results], 0)
```
